# Optimizing a Trainium2 kernel written in Bass

```python
import jax, jax.numpy as jnp
from jax import lax
import numpy as np

D_MODEL = 1024
BATCH = 16
SEQ = 2048
DEPTH = 2

N_MIXERS = 2
N_LAYERS_A = (DEPTH + 1) // 2
N_LAYERS_B = DEPTH // 2
EPS = 1e-6
N_MOD = 6

MLA_HEADS = 16
Q_LORA = 512
KV_LORA = 256
QK_NOPE = 64
QK_ROPE = 32
V_HEAD = 64
ROPE_THETA = 10000.0
Q_BLOCK = 128
MASK_VALUE = -1e30

GDN_HEADS = 8
GDN_DK = 128
GDN_DV = 128
CONV_K = 4
CHUNK = 64

D_FF = 2816
N_EXPERTS = 8
TOP_K = 2
D_FF_EXPERT = 1408

MLA_IN = Q_LORA + KV_LORA + QK_ROPE
GDN_QKV = 2 * GDN_HEADS * GDN_DK + GDN_HEADS * GDN_DV
GDN_IN = GDN_QKV + GDN_HEADS * GDN_DV + 2 * GDN_HEADS

kernel_name = 'hybrid_mla_gdn_moe_adaln'


def rms_norm(x, g):
    xf = x.astype(jnp.float32)
    y = xf * lax.rsqrt(jnp.mean(xf * xf, axis=-1, keepdims=True) + EPS)
    return (y * g.astype(jnp.float32)).astype(x.dtype)


def l2_norm(x):
    xf = x.astype(jnp.float32)
    return xf * lax.rsqrt(jnp.sum(xf * xf, axis=-1, keepdims=True) + EPS)


def apply_rope(x, pos):
    half = QK_ROPE // 2
    inv_freq = ROPE_THETA ** (-jnp.arange(half, dtype=jnp.float32) / half)
    ang = pos.astype(jnp.float32)[:, :, None, None] * inv_freq
    cos, sin = jnp.cos(ang), jnp.sin(ang)
    xf = x.astype(jnp.float32)
    x1, x2 = xf[..., :half], xf[..., half:]
    out = jnp.concatenate([x1 * cos - x2 * sin, x2 * cos + x1 * sin], axis=-1)
    return out.astype(x.dtype)


def mla_mixer(h, pos, w_in, q_norm, w_qb, kv_norm, w_kvb, w_out):
    B, S, _ = h.shape
    H = MLA_HEADS
    proj = h @ w_in
    q_lat, kv_lat, k_rope = jnp.split(proj, [Q_LORA, Q_LORA + KV_LORA], axis=-1)
    q = (rms_norm(q_lat, q_norm) @ w_qb).reshape(B, S, H, QK_NOPE + QK_ROPE)
    q_nope = q[..., :QK_NOPE]
    q_rope = apply_rope(q[..., QK_NOPE:], pos)
    kv = (rms_norm(kv_lat, kv_norm) @ w_kvb).reshape(B, S, H, QK_NOPE + V_HEAD)
    k_nope, v = kv[..., :QK_NOPE], kv[..., QK_NOPE:]
    k_rope = apply_rope(k_rope[:, :, None, :], pos)[:, :, 0, :]
    scale = (QK_NOPE + QK_ROPE) ** -0.5
    n_blk = S // Q_BLOCK
    qn_b = q_nope.reshape(B, n_blk, Q_BLOCK, H, QK_NOPE).transpose(1, 0, 2, 3, 4)
    qr_b = q_rope.reshape(B, n_blk, Q_BLOCK, H, QK_ROPE).transpose(1, 0, 2, 3, 4)
    key_idx = jnp.arange(S)

    def attend(args):
        blk, qn, qr = args
        s = (jnp.einsum('bqhd,bkhd->bhqk', qn, k_nope)
             + jnp.einsum('bqhr,bkr->bhqk', qr, k_rope)).astype(jnp.float32) * scale
        q_idx = blk * Q_BLOCK + jnp.arange(Q_BLOCK)
        causal = key_idx[None, :] <= q_idx[:, None]
        s = jnp.where(causal, s, MASK_VALUE)
        p = jax.nn.softmax(s, axis=-1).astype(v.dtype)
        return jnp.einsum('bhqk,bkhd->bqhd', p, v)

    o = lax.map(attend, (jnp.arange(n_blk), qn_b, qr_b))
    o = o.transpose(1, 0, 2, 3, 4).reshape(B, S, H * V_HEAD)
    return o @ w_out


def causal_conv(x, w):
    C = x.shape[-1]
    return lax.conv_general_dilated(
        x, w[:, None, :].astype(x.dtype), window_strides=(1,),
        padding=[(CONV_K - 1, 0)], dimension_numbers=('NWC', 'WIO', 'NWC'),
        feature_group_count=C)


def chunk_gated_delta(q, k, v, g, beta):
    B, S, H, DK = q.shape
    DV = v.shape[-1]
    N = S // CHUNK

    def to_chunks(t):
        return t.astype(jnp.float32).reshape(B, N, CHUNK, H, -1).transpose(0, 3, 1, 2, 4)

    q, k, v = to_chunks(q), to_chunks(k), to_chunks(v)
    beta = beta.astype(jnp.float32).reshape(B, N, CHUNK, H).transpose(0, 3, 1, 2)
    g = g.astype(jnp.float32).reshape(B, N, CHUNK, H).transpose(0, 3, 1, 2)
    gc = jnp.cumsum(g, axis=-1)
    causal = jnp.tril(jnp.ones((CHUNK, CHUNK), dtype=bool))
    strict = jnp.tril(jnp.ones((CHUNK, CHUNK), dtype=bool), -1)
    diff = gc[..., :, None] - gc[..., None, :]
    decay = jnp.where(causal, jnp.exp(jnp.where(causal, diff, 0.0)), 0.0)
    k_beta = k * beta[..., None]
    a_mat = jnp.where(strict, jnp.einsum('bhnid,bhnjd->bhnij', k_beta, k) * decay, 0.0)
    t_mat = a_mat + jnp.eye(CHUNK, dtype=jnp.float32)
    rhs = jnp.concatenate([v * beta[..., None], k_beta * jnp.exp(gc)[..., None]], axis=-1)
    sol = lax.linalg.triangular_solve(t_mat, rhs, left_side=True, lower=True,
                                      unit_diagonal=True)
    u, w = sol[..., :DV], sol[..., DV:]
    attn_intra = jnp.where(causal, jnp.einsum('bhnid,bhnjd->bhnij', q, k) * decay, 0.0)
    q_dec = q * jnp.exp(gc)[..., None]
    g_last = gc[..., -1]
    k_state = k * jnp.exp(g_last[..., None] - gc)[..., None]
    chunk_decay = jnp.exp(g_last)

    def step(state, xs):
        u_c, w_c, qd_c, at_c, ks_c, dec_c = xs
        v_new = u_c - w_c @ state
        o_c = qd_c @ state + at_c @ v_new
        state = state * dec_c[..., None, None] + jnp.einsum('bhcd,bhce->bhde', ks_c, v_new)
        return state, o_c

    xs = (jnp.moveaxis(u, 2, 0), jnp.moveaxis(w, 2, 0), jnp.moveaxis(q_dec, 2, 0),
          jnp.moveaxis(attn_intra, 2, 0), jnp.moveaxis(k_state, 2, 0),
          jnp.moveaxis(chunk_decay, 2, 0))
    s0 = jnp.zeros((B, H, DK, DV), jnp.float32)
    _, o = lax.scan(step, s0, xs)
    return o.transpose(1, 0, 3, 2, 4).reshape(B, S, H, DV)


def gdn_mixer(h, w_in, conv_w, a_log, dt_bias, out_norm, w_out):
    B, S, _ = h.shape
    H = GDN_HEADS
    proj = h @ w_in
    qkv, z, b, a = jnp.split(proj, [GDN_QKV, GDN_QKV + H * GDN_DV, GDN_QKV + H * GDN_DV + H],
                             axis=-1)
    qkv = jax.nn.silu(causal_conv(qkv, conv_w))
    q, k, v = jnp.split(qkv, [H * GDN_DK, 2 * H * GDN_DK], axis=-1)
    q = l2_norm(q.reshape(B, S, H, GDN_DK)) * (GDN_DK ** -0.5)
    k = l2_norm(k.reshape(B, S, H, GDN_DK))
    v = v.reshape(B, S, H, GDN_DV)
    beta = jax.nn.sigmoid(b.astype(jnp.float32))
    g = -jnp.exp(a_log.astype(jnp.float32)) * jax.nn.softplus(
        a.astype(jnp.float32) + dt_bias.astype(jnp.float32))
    o = chunk_gated_delta(q, k, v, g, beta).astype(h.dtype)
    o = rms_norm(o, out_norm) * jax.nn.silu(z.reshape(B, S, H, GDN_DV))
    return o.reshape(B, S, H * GDN_DV) @ w_out


def swiglu_ffn(h, w_gate_up, w_down):
    gt, up = jnp.split(h @ w_gate_up, 2, axis=-1)
    return (jax.nn.silu(gt) * up) @ w_down


def moe_ffn(h, w_router, w_gate_up, w_down):
    B, S, D = h.shape
    t = h.reshape(B * S, D)
    logits = (t @ w_router).astype(jnp.float32)
    top_val, top_idx = lax.top_k(logits, TOP_K)
    top_w = jax.nn.softmax(top_val, axis=-1)
    combine = jnp.sum(jax.nn.one_hot(top_idx, N_EXPERTS, dtype=jnp.float32)
                      * top_w[..., None], axis=1)
    out = jnp.zeros((B * S, D), jnp.float32)
    for e in range(N_EXPERTS):
        y_e = swiglu_ffn(t, w_gate_up[e], w_down[e])
        out = out + combine[:, e:e + 1] * y_e
    return out.reshape(B, S, D).astype(h.dtype)


def setup_inputs(seed: int = 0) -> dict:
    key = jax.random.key(seed)
    ks = iter(jax.random.split(key, 40))
    f32 = jnp.float32

    def nrm(shape, fan_in, scale=1.0):
        return jax.random.normal(next(ks), shape, f32) * (scale * fan_in ** -0.5)

    def gain(shape):
        return 1.0 + 0.05 * jax.random.normal(next(ks), shape, f32)

    D = D_MODEL
    A, Bn = N_LAYERS_A, N_LAYERS_B
    x = jax.random.normal(next(ks), (BATCH, SEQ, D), f32)
    c = jax.random.normal(next(ks), (BATCH, D), f32)
    positions = jnp.broadcast_to(jnp.arange(SEQ, dtype=jnp.int32)[None, :], (BATCH, SEQ))
    dt = jnp.exp(jax.random.uniform(next(ks), (Bn, GDN_HEADS), f32,
                                    minval=jnp.log(0.001), maxval=jnp.log(0.1)))
    return {
        'x': x,
        'c': c,
        'positions': positions,
        'ada_w': nrm((DEPTH, D, N_MOD * D), D, 0.5),
        'ada_b': 0.02 * jax.random.normal(next(ks), (DEPTH, N_MOD * D), f32),
        'norm_mix': gain((DEPTH, D)),
        'norm_ffn': gain((DEPTH, D)),
        'mla_w_in': nrm((A, D, MLA_IN), D),
        'mla_q_norm': gain((A, Q_LORA)),
        'mla_w_qb': nrm((A, Q_LORA, MLA_HEADS * (QK_NOPE + QK_ROPE)), Q_LORA),
        'mla_kv_norm': gain((A, KV_LORA)),
        'mla_w_kvb': nrm((A, KV_LORA, MLA_HEADS * (QK_NOPE + V_HEAD)), KV_LORA),
        'mla_w_out': nrm((A, MLA_HEADS * V_HEAD, D), MLA_HEADS * V_HEAD),
        'ffn_w_gate_up': nrm((A, D, 2 * D_FF), D),
        'ffn_w_down': nrm((A, D_FF, D), D_FF),
        'gdn_w_in': nrm((Bn, D, GDN_IN), D),
        'gdn_conv_w': nrm((Bn, CONV_K, GDN_QKV), CONV_K),
        'gdn_a_log': jnp.log(jax.random.uniform(next(ks), (Bn, GDN_HEADS), f32,
                                                minval=1.0, maxval=16.0)),
        'gdn_dt_bias': dt + jnp.log(-jnp.expm1(-dt)),
        'gdn_out_norm': gain((Bn, GDN_DV)),
        'gdn_w_out': nrm((Bn, GDN_HEADS * GDN_DV, D), GDN_HEADS * GDN_DV),
        'moe_w_router': nrm((Bn, D, N_EXPERTS), D),
        'moe_w_gate_up': nrm((Bn, N_EXPERTS, D, 2 * D_FF_EXPERT), D),
        'moe_w_down': nrm((Bn, N_EXPERTS, D_FF_EXPERT, D), D_FF_EXPERT),
        'final_norm': gain((D,)),
    }


def reference(x, c, positions, ada_w, ada_b, norm_mix, norm_ffn,
              mla_w_in, mla_q_norm, mla_w_qb, mla_kv_norm, mla_w_kvb, mla_w_out,
              ffn_w_gate_up, ffn_w_down,
              gdn_w_in, gdn_conv_w, gdn_a_log, gdn_dt_bias, gdn_out_norm, gdn_w_out,
              moe_w_router, moe_w_gate_up, moe_w_down, final_norm):
    cond = jax.nn.silu(c)
    for i in range(DEPTH):
        j = i // N_MIXERS
        mod = cond @ ada_w[i] + ada_b[i]
        sh1, sc1, g1, sh2, sc2, g2 = [m[:, None, :] for m in jnp.split(mod, N_MOD, axis=-1)]
        h = rms_norm(x, norm_mix[i]) * (1.0 + sc1) + sh1
        if i % N_MIXERS == 0:
            y = mla_mixer(h, positions, mla_w_in[j], mla_q_norm[j], mla_w_qb[j],
                          mla_kv_norm[j], mla_w_kvb[j], mla_w_out[j])
        else:
            y = gdn_mixer(h, gdn_w_in[j], gdn_conv_w[j], gdn_a_log[j], gdn_dt_bias[j],
                          gdn_out_norm[j], gdn_w_out[j])
        x = x + g1 * y
        h = rms_norm(x, norm_ffn[i]) * (1.0 + sc2) + sh2
        if i % 2 == 0:
            y = swiglu_ffn(h, ffn_w_gate_up[j], ffn_w_down[j])
        else:
            y = moe_ffn(h, moe_w_router[j], moe_w_gate_up[j], moe_w_down[j])
        x = x + g2 * y
    return rms_norm(x, final_norm)
```

```python
import math
import numpy as np
import concourse.bass as bass
import concourse.mybir as mybir
from concourse.bass_utils import run_bass_kernel_spmd

F32 = mybir.dt.float32
BF16 = mybir.dt.bfloat16
I32 = mybir.dt.int32
AF = mybir.ActivationFunctionType
ALU = mybir.AluOpType
AX = mybir.AxisListType

NCORES = 8
SEQ = 2048
D = 1024
NT = SEQ // 128
NB = SEQ // 512
EPS = 1e-6
GEN = 30000


class Prog:
    ENG = ("pe", "act", "dve", "pool", "sp")

    def __init__(self, nc):
        self.nc = nc
        self.q = {e: [] for e in self.ENG}
        self.cnt = {e: 0 for e in self.ENG}
        self.res = {}
        self.seen = {e: {} for e in self.ENG}
        self.dma_cnt = {}
        self.semkeys = set()
        self.off = 16512
        self.ntile = 0
        self.cap = nc.SBUF_PARTITION_SIZE_BYTES

    def tile(self, name, shape, dt):
        nbytes = int(np.prod(shape[1:])) * (2 if dt == BF16 else 4)
        self.off = (self.off + 63) // 64 * 64
        assert self.off + nbytes <= self.cap, (name, self.off, nbytes)
        self.ntile += 1
        t = self.nc.alloc_sbuf_tensor_at(f"{name}_{self.ntile}", list(shape), dt, offset=self.off)
        self.off += nbytes
        return t

    def mark(self):
        return self.off

    def release(self, m):
        self.barrier()
        self.off = m

    def _key(self, r):
        return r if isinstance(r, (str, tuple)) else r.name

    def _st(self, r):
        r = self._key(r)
        s = self.res.get(r)
        if s is None:
            s = self.res[r] = {"w": None, "r": []}
        return s

    def _deps(self, eng, reads, writes):
        deps = {}

        def add(d):
            if d is None:
                return
            k, v = d
            if eng == "pe" and isinstance(k, tuple) and k[0] == "pe":
                return
            if deps.get(k, 0) < v:
                deps[k] = v
        for r in reads:
            add(self._st(r)["w"])
        for r in writes:
            s = self._st(r)
            add(s["w"])
            for d in s["r"]:
                add(d)
        out = []
        seen = self.seen[eng]
        for k, v in deps.items():
            if seen.get(k, 0) < v:
                seen[k] = v
                out.append((k, v))
        return out

    def _commit(self, token, reads, writes):
        for r in reads:
            lst = self._st(r)["r"]
            lst[:] = [d for d in lst if d[0] != token[0]]
            lst.append(token)
        for r in writes:
            s = self._st(r)
            s["w"] = token
            s["r"] = []

    limit = None

    def op(self, eng, fn, reads=(), writes=()):
        if self.limit is not None:
            if self.limit <= 0:
                return None
            self.limit -= 1
        waits = self._deps(eng, reads, writes)
        if self.limit == 0:
            print("LAST OP", eng, "waits", waits, "cnt", self.cnt, "reads", [self._key(r) for r in reads], "writes", [self._key(r) for r in writes])
        self.cnt[eng] += 1
        n = self.cnt[eng]
        key = (eng, (n - 1) // GEN)
        self.semkeys.add(key)
        token = (key, (n - 1) % GEN + 1)
        self.q[eng].append((waits, fn, (key, 1)))
        self._commit(token, reads, writes)
        return token

    def dma(self, queue, fn, reads=(), writes=(), key=None):
        if key is None:
            r0 = writes[0] if writes else reads[0]
            key = ("dma", self._key(r0))
        self.semkeys.add(key)
        waits = self._deps(queue, reads, writes)
        v = self.dma_cnt.get(key, 0) + 16
        self.dma_cnt[key] = v
        token = (key, v)
        self.q[queue].append((waits, fn, (key, 16)))
        self._commit(token, reads, writes)
        return token

    def barrier(self):
        toks = []
        for e in self.ENG:
            n = self.cnt[e]
            if n:
                toks.append(((e, (n - 1) // GEN), (n - 1) % GEN + 1))
        toks += list(self.dma_cnt.items())
        for e in self.ENG:
            self.wait(e, toks)

    def wait(self, eng, tokens):
        waits = []
        for k, v in tokens:
            if eng == "pe" and isinstance(k, tuple) and k[0] == "pe":
                continue
            if self.seen[eng].get(k, 0) < v:
                self.seen[eng][k] = v
                waits.append((k, v))
        if waits:
            self.q[eng].append((waits, None, None))

    def emit(self):
        import contextlib
        nc = self.nc
        with contextlib.ExitStack() as es:
            sems = {}
            for i, k in enumerate(sorted(self.semkeys, key=str)):
                sems[k] = es.enter_context(nc.semaphore(f"s{i}"))
            block = es.enter_context(nc.Block())
            q = self.q

            def run(engname):
                def body(e):
                    for waits, fn, inc in q[engname]:
                        for k, v in waits:
                            e.wait_ge(sems[k], v)
                        if fn is not None:
                            fn(e).then_inc(sems[inc[0]], inc[1])
                return body
            block.tensor(run("pe"))
            block.scalar(run("act"))
            block.vector(run("dve"))
            block.gpsimd(run("pool"))
            block.sync(run("sp"))


class Builder:
    def __init__(self, nseq, stages):
        self.nseq = nseq
        self.stages = stages
        nc = self.nc = bass.Bass("TRN2", target_bir_lowering=False)
        self.p = Prog(nc)
        self.dram = {}
        self.rr = 0

    def din(self, name, shape, dt=F32):
        a = self.nc.dram_tensor(name, list(shape), dt, kind="ExternalInput").ap()
        self.dram[name] = a
        return a

    def setup(self):
        p, nc, ns = self.p, self.nc, self.nseq
        d = self.din
        d("x", [ns, SEQ, D]); d("cT", [ns, 128, 8]); d("pos", [ns, 128, SEQ], I32)
        d("ada", [2, 1025, 6 * D])
        d("gmix", [2, 128, 8]); d("gffn", [2, 128, 8]); d("gfin", [128, D])
        d("ffn_gu", [2, 11, 128, 8 * 256]); d("ffn_dn", [2, 1408, D])
        d("moe_gu", [8, 11, 128, 8 * 256]); d("moe_dn", [8, 1408, D]); d("moe_r", [128, 8, 8])
        d("mla_in", [128, 8, 896]); d("mla_qn", [128, 4]); d("mla_kvn", [128, 2])
        d("mla_qb", [128, 4, 2048]); d("mla_kb", [128, 2, 1024]); d("mla_vb", [128, 2, 1024])
        d("mla_out", [D, D]); d("ropec", [128, 2])
        d("gdn_in", [8, 128, 8, 512]); d("gdn_ba", [128, 8, 16]); d("gdn_conv", [128, 8, 3, 4])
        d("gdn_hp", [128, 24]); d("gdn_on", [128, 128]); d("gdn_out", [D, D])
        self.y = nc.dram_tensor("y", [ns, SEQ, D], F32, kind="ExternalOutput").ap()

        self.X = p.tile("X", [128, NT, D], F32)
        self.HT = p.tile("HT", [128, 8, SEQ], BF16)
        self.identb = p.tile("identb", [128, 128], BF16)
        self.identf = p.tile("identf", [128, 128], F32)
        self.onesb = p.tile("onesb", [128, 128], BF16)
        self.onesf = p.tile("onesf", [128, 128], F32)
        self.epsc = p.tile("epsc", [128, 1], F32)
        self.MODC = p.tile("MODC", [128, 2, 48, ns], F32)
        self.G = p.tile("G", [128, D], F32)
        self.Acol = p.tile("Acol", [128, 8], F32)
        self.gmix = p.tile("gmix", [128, 2, 8], F32)
        self.gffn = p.tile("gffn", [128, 2, 8], F32)
        self.GF = p.tile("GF", [128, D], F32)
        self.ssq = p.tile("ssq", [128, NT], F32)
        self.rstd = p.tile("rstd", [128, NT], F32)
        self.COMB = p.tile("COMB", [128, NT, 8], F32)
        self.PS = [nc.alloc_psum_tensor(f"ps{i}", [128, 512], F32) for i in range(8)]

        p.op("pool", lambda e: e.memset(self.identf[:], 1.0), writes=[self.identf])
        p.op("pool", lambda e: e.affine_select(out=self.identf[:], in_=self.identf[:], pattern=[[-1, 128]],
                                               compare_op=ALU.is_equal, fill=0.0, base=0, channel_multiplier=1),
             reads=[self.identf], writes=[self.identf])
        p.op("dve", lambda e: e.tensor_copy(self.identb[:], self.identf[:]), reads=[self.identf], writes=[self.identb])
        p.op("pool", lambda e: e.memset(self.onesb[:], 1.0), writes=[self.onesb])
        p.op("pool", lambda e: e.memset(self.onesf[:], 1.0), writes=[self.onesf])
        p.op("pool", lambda e: e.memset(self.epsc[:], EPS), writes=[self.epsc])
        p.dma("sp", lambda e: e.dma_start(out=self.gmix[:], in_=self.dram["gmix"].rearrange("l p k -> p l k")), writes=[self.gmix])
        p.dma("sp", lambda e: e.dma_start(out=self.gffn[:], in_=self.dram["gffn"].rearrange("l p k -> p l k")), writes=[self.gffn])
        p.dma("sp", lambda e: e.dma_start(out=self.GF[:], in_=self.dram["gfin"]), writes=[self.GF])

    def prologue_mod(self):
        p, ns = self.p, self.nseq
        m = p.mark()
        cT = p.tile("cTs", [128, ns, 8], F32)
        condb = p.tile("condb", [128, 8, ns], BF16)
        for s in range(ns):
            p.dma("sp", lambda e, s=s: e.dma_start(out=cT[:, s, :], in_=self.dram["cT"][s]), writes=[cT], key=("dma", "cT", s))
        for s in range(ns):
            p.op("act", lambda e, s=s: e.activation(out=condb[:, :, s], in_=cT[:, s, :], func=AF.Silu), reads=[cT], writes=[condb])
        W = [p.tile(f"adaw{i}", [128, 8, 512], BF16) for i in range(2)]
        Bv = [p.tile(f"adab{i}", [1, 512], BF16) for i in range(2)]
        ada = self.dram["ada"]
        it = 0
        for l in range(2):
            ps = self.PS[l]
            psv = ps[:, 0:48 * ns].rearrange("p (b s) -> p b s", s=ns)
            for ch in range(12):
                w, bv = W[it % 2], Bv[it % 2]
                it += 1
                cols = slice(ch * 512, (ch + 1) * 512)
                p.dma("pool", lambda e, w=w, l=l, cols=cols: e.dma_start(
                    out=w[:], in_=ada[l, 0:1024, cols].rearrange("(k p) n -> p k n", p=128)), writes=[w])
                p.dma("pool", lambda e, bv=bv, l=l, cols=cols: e.dma_start(out=bv[:], in_=ada[l, 1024:1025, cols]), writes=[bv])
                for qd in range(4):
                    blk = ch * 4 + qd
                    for k in range(8):
                        p.op("pe", lambda e, w=w, k=k, qd=qd, blk=blk, psv=psv: e.matmul(
                            psv[:, blk, :], lhsT=w[:, k, qd * 128:(qd + 1) * 128], rhs=condb[:, k, :], start=(k == 0), stop=False),
                            reads=[w, condb], writes=[ps])
                    p.op("pe", lambda e, bv=bv, qd=qd, blk=blk, psv=psv: e.matmul(
                        psv[:, blk, :], lhsT=bv[0:1, qd * 128:(qd + 1) * 128], rhs=self.onesb[0:1, 0:ns], start=False, stop=True),
                        reads=[bv, self.onesb], writes=[ps])
            p.op("act", lambda e, l=l, psv=psv: e.copy(self.MODC[:, l, :, :], psv), reads=[ps], writes=[self.MODC])
        p.release(m)

    def modc(self, l, s, j, k=None):
        if k is None:
            return self.MODC[:, l, j * 8:(j + 1) * 8, s]
        return self.MODC[:, l, j * 8 + k, s:s + 1]

    def make_gate(self, l, s, j):
        p = self.p
        m = p.mark()
        dg = [p.tile(f"dg{i}", [128, 128], F32) for i in range(2)]
        for k in range(8):
            t = dg[k % 2]
            ps = self.PS[k // 4]
            p.op("dve", lambda e, t=t, k=k: e.tensor_scalar(out=t[:], in0=self.identf[:], scalar1=self.modc(l, s, j, k), scalar2=None, op0=ALU.mult),
                 reads=[self.identf, self.MODC], writes=[t])
            p.op("pe", lambda e, t=t, k=k, ps=ps: e.matmul(ps[:, (k % 4) * 128:(k % 4 + 1) * 128], lhsT=self.onesf[:], rhs=t[:], start=True, stop=True),
                 reads=[t, self.onesf], writes=[ps])
        for h in range(2):
            p.op("act", lambda e, h=h: e.copy(self.G[:, h * 512:(h + 1) * 512], self.PS[h][:]), reads=[self.PS[h]], writes=[self.G])
        p.release(m)

    def load_x(self, s):
        p = self.p
        for t in range(NT):
            q = "sp" if t % 2 == 0 else "act"
            p.dma(q, lambda e, t=t: e.dma_start(out=self.X[:, t, :], in_=self.dram["x"][s, t * 128:(t + 1) * 128, :]),
                  writes=[("X", t)])

    def final_out(self, s):
        p = self.p
        m = p.mark()
        ob = [p.tile(f"ob{i}", [128, D], F32) for i in range(2)]
        junk = p.tile("junkf", [128, D], BF16)
        toks = []
        for t in range(NT):
            if t == 0 and getattr(self, "skip0", False):
                continue
            self.rms_stats(t, junk)
            o = ob[t % 2]
            p.op("dve", lambda e, t=t, o=o: e.scalar_tensor_tensor(out=o[:], in0=self.X[:, t, :], scalar=self.rstd[:, t:t + 1], in1=self.GF[:],
                                                                  op0=ALU.mult, op1=ALU.mult),
                 reads=[("X", t), ("rstd", t), self.GF], writes=[o])
            toks.append(p.dma("sp", lambda e, t=t, o=o: e.dma_start(out=self.y[s, t * 128:(t + 1) * 128, :], in_=o[:]), reads=[o]))
        p.release(m)
        return toks

    def rms_stats(self, t, junk):
        p = self.p
        p.op("pool", lambda e: e.memset(self.ssq[:, t:t + 1], 0.0), writes=[("ssq", t)])
        p.op("act", lambda e: e.activation(out=junk[:], in_=self.X[:, t, :], func=AF.Square, accum_out=self.ssq[:, t:t + 1]),
             reads=[("X", t)], writes=[junk, ("ssq", t)])
        p.op("act", lambda e: e.activation(out=self.ssq[:, t:t + 1], in_=self.ssq[:, t:t + 1], func=AF.Sqrt, bias=self.epsc[:], scale=1.0 / D),
             reads=[("ssq", t), self.epsc], writes=[("ssq", t)])
        p.op("dve", lambda e: e.reciprocal(out=self.rstd[:, t:t + 1], in_=self.ssq[:, t:t + 1]), reads=[("ssq", t)], writes=[("rstd", t)])

    def norm_phase(self, l, s, which, router=False):
        p = self.p
        m = p.mark()
        jsh, jsc = (0, 1) if which == 0 else (3, 4)
        gcol = (self.gmix if which == 0 else self.gffn)[:, l, :]
        p.op("dve", lambda e: e.tensor_scalar(out=self.Acol[:], in0=self.modc(l, s, jsc), scalar1=1.0, scalar2=None, op0=ALU.add),
             reads=[self.MODC], writes=[self.Acol])
        p.op("dve", lambda e: e.tensor_tensor(out=self.Acol[:], in0=self.Acol[:], in1=gcol, op=ALU.mult),
             reads=[self.Acol, self.gmix, self.gffn], writes=[self.Acol])
        junk = p.tile("junk", [128, D], BF16)
        xn = [p.tile(f"xn{i}", [128, D], BF16) for i in range(2)]
        if router:
            xnf = p.tile("xnf", [128, D], F32)
            hTf = p.tile("hTf", [128, 8, 128], F32)
            wr = p.tile("wr", [128, 8, 8], F32)
            lg = p.tile("lg", [128, 8], F32)
            mx = p.tile("mx", [128, 8], F32)
            sc = p.tile("rsc", [128, 4], F32)
            c1 = p.tile("c1", [128, 8], F32)
            p.dma("sp", lambda e: e.dma_start(out=wr[:], in_=self.dram["moe_r"]), writes=[wr])
        psT = [self.PS[i].bitcast(BF16) for i in range(4)]
        for blk in range(NB):
            for tt in range(4):
                t = blk * 4 + tt
                self.rms_stats(t, junk)
                x_ = xn[t % 2]
                p.op("dve", lambda e, t=t, x_=x_: e.tensor_scalar(out=x_[:], in0=self.X[:, t, :], scalar1=self.rstd[:, t:t + 1], scalar2=None, op0=ALU.mult),
                     reads=[("X", t), ("rstd", t)], writes=[x_])
                for k in range(8):
                    p.op("pe", lambda e, k=k, tt=tt, x_=x_: e.transpose(psT[k // 2][:, (k % 2) * 512 + tt * 128:(k % 2) * 512 + (tt + 1) * 128],
                                                                       x_[:, k * 128:(k + 1) * 128], self.identb[:]),
                         reads=[x_, self.identb], writes=[self.PS[k // 2]])
                if router:
                    p.op("dve", lambda e, t=t: e.tensor_scalar(out=xnf[:], in0=self.X[:, t, :], scalar1=self.rstd[:, t:t + 1], scalar2=None, op0=ALU.mult),
                         reads=[("X", t), ("rstd", t)], writes=[xnf])
                    for k in range(8):
                        ps = self.PS[4 + k // 4]
                        p.op("pe", lambda e, k=k, ps=ps: e.transpose(ps[:, (k % 4) * 128:(k % 4 + 1) * 128], xnf[:, k * 128:(k + 1) * 128], self.identf[:]),
                             reads=[xnf, self.identf], writes=[ps])
                    for k in range(8):
                        ps = self.PS[4 + k // 4]
                        p.op("act", lambda e, k=k, ps=ps: e.activation(out=hTf[:, k, :], in_=ps[:, (k % 4) * 128:(k % 4 + 1) * 128], func=AF.Identity,
                                                                      bias=self.modc(l, s, jsh, k), scale=self.Acol[:, k:k + 1]),
                             reads=[ps, self.Acol, self.MODC], writes=[hTf])
                    for k in range(8):
                        p.op("pe", lambda e, k=k: e.matmul(self.PS[6][:, 0:8], lhsT=hTf[:, k, :], rhs=wr[:, k, :], start=(k == 0), stop=(k == 7)),
                             reads=[hTf, wr], writes=[self.PS[6]])
                    p.op("act", lambda e: e.copy(lg[:], self.PS[6][:, 0:8]), reads=[self.PS[6]], writes=[lg])
                    p.op("dve", lambda e: e.max(out=mx[:], in_=lg[:]), reads=[lg], writes=[mx])
                    p.op("dve", lambda e: e.tensor_tensor(out=sc[:, 0:1], in0=mx[:, 1:2], in1=mx[:, 0:1], op=ALU.subtract), reads=[mx], writes=[sc])
                    p.op("act", lambda e: e.activation(out=sc[:, 0:1], in_=sc[:, 0:1], func=AF.Exp), reads=[sc], writes=[sc])
                    p.op("dve", lambda e: e.tensor_scalar(out=sc[:, 1:2], in0=sc[:, 0:1], scalar1=1.0, scalar2=None, op0=ALU.add), reads=[sc], writes=[sc])
                    p.op("dve", lambda e: e.reciprocal(out=sc[:, 1:2], in_=sc[:, 1:2]), reads=[sc], writes=[sc])
                    p.op("dve", lambda e: e.tensor_tensor(out=sc[:, 2:3], in0=sc[:, 0:1], in1=sc[:, 1:2], op=ALU.mult), reads=[sc], writes=[sc])
                    p.op("dve", lambda e: e.tensor_scalar(out=c1[:], in0=lg[:], scalar1=mx[:, 0:1], scalar2=sc[:, 1:2], op0=ALU.is_equal, op1=ALU.mult),
                         reads=[lg, mx, sc], writes=[c1])
                    p.op("dve", lambda e, t=t: e.tensor_scalar(out=self.COMB[:, t, :], in0=lg[:], scalar1=mx[:, 1:2], scalar2=sc[:, 2:3], op0=ALU.is_equal, op1=ALU.mult),
                         reads=[lg, mx, sc], writes=[("COMB", t)])
                    p.op("dve", lambda e, t=t: e.tensor_tensor(out=self.COMB[:, t, :], in0=self.COMB[:, t, :], in1=c1[:], op=ALU.add),
                         reads=[c1, ("COMB", t)], writes=[("COMB", t)])
            for k in range(8):
                src = psT[k // 2][:, (k % 2) * 512:(k % 2 + 1) * 512]
                dst = self.HT[:, k, blk * 512:(blk + 1) * 512]
                if k % 2 == 0:
                    p.op("act", lambda e, k=k, src=src, dst=dst: e.activation(out=dst, in_=src, func=AF.Identity, bias=self.modc(l, s, jsh, k),
                                                                            scale=self.Acol[:, k:k + 1]),
                         reads=[self.PS[k // 2], self.Acol, self.MODC], writes=[("HT", blk)])
                else:
                    p.op("dve", lambda e, k=k, src=src, dst=dst: e.tensor_scalar(out=dst, in0=src, scalar1=self.Acol[:, k:k + 1], scalar2=self.modc(l, s, jsh, k),
                                                                               op0=ALU.mult, op1=ALU.add),
                         reads=[self.PS[k // 2], self.Acol, self.MODC], writes=[("HT", blk)])
        p.release(m)

    def ffn_phase(self, experts):
        p = self.p
        m = p.mark()
        ACT_ = p.tile("actT", [128, 11, SEQ], BF16)
        WGU = [p.tile(f"wgu{i}", [128, 8, 256], BF16) for i in range(3)]
        WD = p.tile("wd", [128, 11, D], BF16)
        SG = [p.tile(f"sg{i}", [128, 512], BF16) for i in range(2)]
        HTr = [("HT", b) for b in range(NB)]
        it = 0
        for (gu, dn, ci) in experts:
            p.dma("pool", lambda e, dn=dn: e.dma_start(out=WD[:], in_=dn.rearrange("(c p) n -> p c n", p=128)), writes=[WD])
            for c in range(11):
                w = WGU[c % 3]
                p.dma("pool", lambda e, w=w, c=c, gu=gu: e.dma_start(out=w[:].rearrange("p k n -> p (k n)"), in_=gu[c]), writes=[w])
                for blk in range(NB):
                    psg, psu = self.PS[it % 2], self.PS[2 + it % 2]
                    sg = SG[it % 2]
                    it += 1
                    for k in range(8):
                        p.op("pe", lambda e, w=w, k=k, blk=blk, psg=psg: e.matmul(psg[:], lhsT=w[:, k, 0:128], rhs=self.HT[:, k, blk * 512:(blk + 1) * 512],
                                                                                  start=(k == 0), stop=(k == 7)), reads=[w, HTr[blk]], writes=[psg])
                    for k in range(8):
                        p.op("pe", lambda e, w=w, k=k, blk=blk, psu=psu: e.matmul(psu[:], lhsT=w[:, k, 128:256], rhs=self.HT[:, k, blk * 512:(blk + 1) * 512],
                                                                                  start=(k == 0), stop=(k == 7)), reads=[w, HTr[blk]], writes=[psu])
                    p.op("act", lambda e, sg=sg, psg=psg: e.activation(out=sg[:], in_=psg[:], func=AF.Silu), reads=[psg], writes=[sg])
                    p.op("dve", lambda e, sg=sg, psu=psu, c=c, blk=blk: e.tensor_tensor(out=ACT_[:, c, blk * 512:(blk + 1) * 512], in0=psu[:], in1=sg[:], op=ALU.mult),
                         reads=[sg, psu], writes=[("actT", c)])
                if c == 5:
                    for cc in range(11):
                        p.op("pool", lambda e, cc=cc: e.tensor_tensor(out=WD[:, cc, :], in0=WD[:, cc, :], in1=self.G[:], op=ALU.mult),
                             reads=[WD, self.G], writes=[WD])
            for t in range(NT):
                for hf in range(2):
                    ps = self.PS[4 + (2 * t + hf) % 4]
                    for c in range(11):
                        p.op("pe", lambda e, c=c, t=t, hf=hf, ps=ps: e.matmul(ps[:], lhsT=ACT_[:, c, t * 128:(t + 1) * 128], rhs=WD[:, c, hf * 512:(hf + 1) * 512],
                                                                              start=(c == 0), stop=(c == 10)), reads=[("actT", c), WD], writes=[ps])
                    xs = self.X[:, t, hf * 512:(hf + 1) * 512]
                    if ci is None:
                        p.op("dve", lambda e, xs=xs, ps=ps: e.tensor_tensor(out=xs, in0=ps[:], in1=xs, op=ALU.add), reads=[ps, ("X", t)], writes=[("X", t)])
                    else:
                        p.op("dve", lambda e, xs=xs, ps=ps, t=t, ci=ci: e.scalar_tensor_tensor(out=xs, in0=ps[:], scalar=self.COMB[:, t, ci:ci + 1], in1=xs,
                                                                                              op0=ALU.mult, op1=ALU.add),
                             reads=[ps, ("X", t), ("COMB", t)], writes=[("X", t)])
        p.release(m)


    def mla_phase(self, s):
        p = self.p
        PS = self.PS
        m0 = p.mark()
        QNT = p.tile("QNT", [128, 4, SEQ], BF16)
        KVNT = p.tile("KVNT", [128, 2, SEQ], BF16)
        ROPE = p.tile("ROPE", [128, SEQ], F32)
        KT = p.tile("KT", [128, SEQ], BF16)
        QT = p.tile("QT", [128, SEQ], BF16)
        VA = p.tile("VA", [128, NT, 128], BF16)
        PT = [p.tile(f"PT{i}", [128, 512], BF16) for i in range(4)]
        TRI = p.tile("TRI", [128, 128], BF16)
        tA = p.tile("tA", [128, 512], F32)
        tB = p.tile("tB", [128, 512], F32)
        rec = p.tile("rec", [128, 512], F32)
        qnc = p.tile("qnc", [128, 4], F32)
        kvnc = p.tile("kvnc", [128, 2], F32)
        ropec = p.tile("ropec", [128, 2], F32)
        st = p.tile("mst", [128, 4], F32)
        p.dma("sp", lambda e: e.dma_start(out=qnc[:], in_=self.dram["mla_qn"]), writes=[qnc])
        p.dma("sp", lambda e: e.dma_start(out=kvnc[:], in_=self.dram["mla_kvn"]), writes=[kvnc])
        p.dma("sp", lambda e: e.dma_start(out=ropec[:], in_=self.dram["ropec"]), writes=[ropec])
        p.op("pool", lambda e: e.memset(TRI[:], 1.0), writes=[TRI])
        p.op("pool", lambda e: e.affine_select(out=TRI[:], in_=TRI[:], pattern=[[1, 128]], compare_op=ALU.is_ge, fill=0.0, base=0, channel_multiplier=-1),
             reads=[TRI], writes=[TRI])
        p.op("pool", lambda e: e.memset(VA[:, :, 64:128], 1.0), writes=[("VA", 0), ("VA", 1)])
        m1 = p.mark()
        WIN = p.tile("WIN", [128, 8, 896], BF16)
        p.dma("pool", lambda e: e.dma_start(out=WIN[:], in_=self.dram["mla_in"]), writes=[WIN])
        posi = p.tile("posi", [128, 512], I32)
        u = p.tile("ropu", [128, 512], F32)
        ki = p.tile("ropk", [128, 512], I32)
        kf = p.tile("ropkf", [128, 512], F32)
        qtok = [p.tile(f"qtok{i}", [128, 512], BF16) for i in range(2)]
        kvtok = [p.tile(f"kvtok{i}", [128, 256], BF16) for i in range(2)]
        junk = p.tile("mjunk", [128, 512], BF16)
        for blk in range(NB):
            cs = slice(blk * 512, (blk + 1) * 512)
            p.dma("sp", lambda e, cs=cs: e.dma_start(out=posi[:], in_=self.dram["pos"][s, :, cs]), writes=[posi])
            p.op("dve", lambda e: e.tensor_copy(u[:], posi[:]), reads=[posi], writes=[u])
            p.op("dve", lambda e: e.tensor_scalar(out=u[:], in0=u[:], scalar1=ropec[:, 0:1], scalar2=ropec[:, 1:2], op0=ALU.mult, op1=ALU.add),
                 reads=[u, ropec], writes=[u])
            p.op("dve", lambda e: e.tensor_copy(ki[:], u[:]), reads=[u], writes=[ki])
            p.op("dve", lambda e: e.tensor_copy(kf[:], ki[:]), reads=[ki], writes=[kf])
            p.op("dve", lambda e: e.tensor_tensor(out=u[:], in0=u[:], in1=kf[:], op=ALU.subtract), reads=[u, kf], writes=[u])
            p.op("act", lambda e, cs=cs: e.activation(out=ROPE[:, cs], in_=u[:], func=AF.Sin, scale=2.0 * math.pi * (1.0 - 1e-6)),
                 reads=[u], writes=[("ROPE", blk)])
        psTb = PS[4].bitcast(BF16)
        for t in range(NT):
            ts_ = slice(t * 128, (t + 1) * 128)
            blk = t // 4
            pa, pb = PS[(t % 2) * 2], PS[(t % 2) * 2 + 1]
            for k in range(8):
                p.op("pe", lambda e, k=k, ts_=ts_, pa=pa: e.matmul(pa[:], lhsT=self.HT[:, k, ts_], rhs=WIN[:, k, 0:512], start=(k == 0), stop=(k == 7)),
                     reads=[("HT", blk), WIN], writes=[pa])
            for k in range(8):
                p.op("pe", lambda e, k=k, ts_=ts_, pb=pb: e.matmul(pb[:, 0:256], lhsT=self.HT[:, k, ts_], rhs=WIN[:, k, 512:768], start=(k == 0), stop=(k == 7)),
                     reads=[("HT", blk), WIN], writes=[pb])
            p.op("pool", lambda e: e.memset(st[:, 0:2], 0.0), writes=[st])
            p.op("act", lambda e, pa=pa: e.activation(out=junk[:], in_=pa[:], func=AF.Square, accum_out=st[:, 0:1]), reads=[pa], writes=[junk, st])
            p.op("act", lambda e, pb=pb: e.activation(out=junk[:, 0:256], in_=pb[:, 0:256], func=AF.Square, accum_out=st[:, 1:2]), reads=[pb], writes=[junk, st])
            p.op("act", lambda e: e.activation(out=st[:, 0:1], in_=st[:, 0:1], func=AF.Sqrt, bias=self.epsc[:], scale=1.0 / 512), reads=[st, self.epsc], writes=[st])
            p.op("act", lambda e: e.activation(out=st[:, 1:2], in_=st[:, 1:2], func=AF.Sqrt, bias=self.epsc[:], scale=1.0 / 256), reads=[st, self.epsc], writes=[st])
            p.op("dve", lambda e: e.reciprocal(out=st[:, 2:4], in_=st[:, 0:2]), reads=[st], writes=[st])
            qt_, kvt_ = qtok[t % 2], kvtok[t % 2]
            p.op("dve", lambda e, pa=pa, qt_=qt_: e.tensor_scalar(out=qt_[:], in0=pa[:], scalar1=st[:, 2:3], scalar2=None, op0=ALU.mult), reads=[pa, st], writes=[qt_])
            p.op("act", lambda e, pb=pb, kvt_=kvt_: e.activation(out=kvt_[:], in_=pb[:, 0:256], func=AF.Copy, scale=st[:, 3:4]), reads=[pb, st], writes=[kvt_])
            for c in range(4):
                p.op("pe", lambda e, c=c, qt_=qt_: e.transpose(psTb[:, c * 128:(c + 1) * 128], qt_[:, c * 128:(c + 1) * 128], self.identb[:]),
                     reads=[qt_, self.identb], writes=[PS[4]])
            for c in range(2):
                p.op("pe", lambda e, c=c, kvt_=kvt_: e.transpose(psTb[:, 512 + c * 128:512 + (c + 1) * 128], kvt_[:, c * 128:(c + 1) * 128], self.identb[:]),
                     reads=[kvt_, self.identb], writes=[PS[4]])
            for c in range(4):
                if c % 2 == 0:
                    p.op("act", lambda e, c=c, ts_=ts_: e.activation(out=QNT[:, c, ts_], in_=psTb[:, c * 128:(c + 1) * 128], func=AF.Copy, scale=qnc[:, c:c + 1]),
                         reads=[PS[4], qnc], writes=[("QNT", blk)])
                else:
                    p.op("dve", lambda e, c=c, ts_=ts_: e.tensor_scalar(out=QNT[:, c, ts_], in0=psTb[:, c * 128:(c + 1) * 128], scalar1=qnc[:, c:c + 1], scalar2=None, op0=ALU.mult),
                         reads=[PS[4], qnc], writes=[("QNT", blk)])
            for c in range(2):
                if c % 2 == 0:
                    p.op("act", lambda e, c=c, ts_=ts_: e.activation(out=KVNT[:, c, ts_], in_=psTb[:, 512 + c * 128:512 + (c + 1) * 128], func=AF.Copy, scale=kvnc[:, c:c + 1]),
                         reads=[PS[4], kvnc], writes=[("KVNT", blk)])
                else:
                    p.op("dve", lambda e, c=c, ts_=ts_: e.tensor_scalar(out=KVNT[:, c, ts_], in0=psTb[:, 512 + c * 128:512 + (c + 1) * 128], scalar1=kvnc[:, c:c + 1], scalar2=None, op0=ALU.mult),
                         reads=[PS[4], kvnc], writes=[("KVNT", blk)])
        for blk in range(NB):
            cs = slice(blk * 512, (blk + 1) * 512)
            ps = PS[5 + blk % 2]
            for k in range(8):
                p.op("pe", lambda e, k=k, cs=cs, ps=ps: e.matmul(ps[:], lhsT=WIN[:, k, 768:896], rhs=self.HT[:, k, cs], start=(k == 0), stop=(k == 7)),
                     reads=[("HT", blk), WIN], writes=[ps])
            p.op("dve", lambda e, cs=cs, ps=ps: e.tensor_tensor(out=tA[64:96, :], in0=ps[64:96, :], in1=ROPE[64:96, cs], op=ALU.mult), reads=[ps, ("ROPE", blk)], writes=[tA])
            p.op("dve", lambda e, cs=cs, ps=ps: e.tensor_tensor(out=tB[64:96, :], in0=ps[96:128, :], in1=ROPE[96:128, cs], op=ALU.mult), reads=[ps, ("ROPE", blk)], writes=[tB])
            p.op("pool", lambda e, cs=cs: e.tensor_tensor(out=KT[64:96, cs], in0=tA[64:96, :], in1=tB[64:96, :], op=ALU.add), reads=[tA, tB], writes=[("KTr", blk)])
        p.release(m1)
        m2 = p.mark()
        WQB = p.tile("WQB", [128, 4, 2048], BF16)
        WKB = p.tile("WKB", [128, 2, 1024], BF16)
        WVB = p.tile("WVB", [128, 2, 1024], BF16)
        p.dma("pool", lambda e: e.dma_start(out=WQB[:], in_=self.dram["mla_qb"]), writes=[WQB])
        p.dma("pool", lambda e: e.dma_start(out=WKB[:], in_=self.dram["mla_kb"]), writes=[WKB])
        p.dma("pool", lambda e: e.dma_start(out=WVB[:], in_=self.dram["mla_vb"]), writes=[WVB])
        OT = self.HT
        scale = 96.0 ** -0.5
        si = 0
        oi = 0
        pi_ = 0
        for h in range(16):
            for blk in range(NB):
                cs = slice(blk * 512, (blk + 1) * 512)
                ps = PS[blk % 2]
                for c in range(4):
                    p.op("pe", lambda e, c=c, cs=cs, ps=ps, h=h: e.matmul(ps[:], lhsT=WQB[:, c, h * 128:(h + 1) * 128], rhs=QNT[:, c, cs], start=(c == 0), stop=(c == 3)),
                         reads=[WQB, ("QNT", blk)], writes=[ps])
                p.op("act", lambda e, cs=cs, ps=ps: e.copy(QT[0:64, cs], ps[0:64, :]), reads=[ps], writes=[("QT", blk)])
                p.op("dve", lambda e, cs=cs, ps=ps: e.tensor_tensor(out=tA[64:96, :], in0=ps[64:96, :], in1=ROPE[64:96, cs], op=ALU.mult), reads=[ps, ("ROPE", blk)], writes=[tA])
                p.op("dve", lambda e, cs=cs, ps=ps: e.tensor_tensor(out=tB[64:96, :], in0=ps[96:128, :], in1=ROPE[96:128, cs], op=ALU.mult), reads=[ps, ("ROPE", blk)], writes=[tB])
                p.op("pool", lambda e, cs=cs: e.tensor_tensor(out=QT[64:96, cs], in0=tA[64:96, :], in1=tB[64:96, :], op=ALU.add), reads=[tA, tB], writes=[("QT", blk)])
            for blk in range(NB):
                cs = slice(blk * 512, (blk + 1) * 512)
                ps = PS[blk % 2]
                for c in range(2):
                    p.op("pe", lambda e, c=c, cs=cs, ps=ps, h=h: e.matmul(ps[0:64, :], lhsT=WKB[:, c, h * 64:(h + 1) * 64], rhs=KVNT[:, c, cs], start=(c == 0), stop=(c == 1)),
                         reads=[WKB, ("KVNT", blk)], writes=[ps])
                p.op("act", lambda e, cs=cs, ps=ps: e.copy(KT[0:64, cs], ps[0:64, :]), reads=[ps], writes=[("KTn", blk)])
            for half in range(2):
                ps = PS[half]
                psv = ps[:].rearrange("p (a b) -> p a b", b=64)
                for kk in range(8):
                    kt = half * 8 + kk
                    for c in range(2):
                        p.op("pe", lambda e, c=c, kt=kt, kk=kk, psv=psv, h=h: e.matmul(psv[:, kk, :], lhsT=KVNT[:, c, kt * 128:(kt + 1) * 128], rhs=WVB[:, c, h * 64:(h + 1) * 64],
                                                                                   start=(c == 0), stop=(c == 1)), reads=[WVB, ("KVNT", kt // 4)], writes=[ps])
                p.op("dve", lambda e, half=half, psv=psv: e.tensor_copy(VA[:, half * 8:(half + 1) * 8, 0:64], psv), reads=[ps], writes=[("VA", half)])
            for qb in range(NB):
                pso = PS[5 + oi % 2]
                oi += 1
                last = 4 * qb + 3
                for kt in range(last + 1):
                    col0 = max(0, kt - 4 * qb) * 128
                    pss = PS[2 + si % 3]
                    si += 1
                    pt = PT[pi_ % 4]
                    pi_ += 1
                    p.op("pe", lambda e, kt=kt, col0=col0, pss=pss, qb=qb: e.matmul(pss[:, col0:512], lhsT=KT[0:96, kt * 128:(kt + 1) * 128],
                                                                                   rhs=QT[0:96, qb * 512 + col0:(qb + 1) * 512], start=True, stop=True),
                         reads=[("KTn", kt // 4), ("KTr", kt // 4), ("QT", qb)], writes=[pss])
                    p.op("act", lambda e, col0=col0, pss=pss, pt=pt: e.activation(out=pt[:, col0:512], in_=pss[:, col0:512], func=AF.Exp, scale=scale),
                         reads=[pss], writes=[pt])
                    if kt >= 4 * qb:
                        dc = (kt - 4 * qb) * 128
                        p.op("pool", lambda e, dc=dc, pt=pt: e.tensor_tensor(out=pt[:, dc:dc + 128], in0=pt[:, dc:dc + 128], in1=TRI[:], op=ALU.mult),
                             reads=[pt, TRI], writes=[pt])
                    p.op("pe", lambda e, kt=kt, col0=col0, pso=pso, pt=pt: e.matmul(pso[:, col0:512], lhsT=VA[:, kt, :], rhs=pt[:, col0:512], start=(kt == 0), stop=(kt == last)),
                         reads=[("VA", kt // 8), pt], writes=[pso])
                p.op("dve", lambda e, pso=pso: e.reciprocal(out=rec[64:128, :], in_=pso[64:128, :]), reads=[pso], writes=[rec])
                r0 = (h % 2) * 64
                p.op("dve", lambda e, pso=pso, r0=r0, h=h, qb=qb: e.tensor_tensor(out=OT[r0:r0 + 64, h // 2, qb * 512:(qb + 1) * 512], in0=pso[0:64, :], in1=rec[64:128, :], op=ALU.mult),
                     reads=[pso, rec], writes=[("OT", qb)])
        p.release(m2)
        WOUT = p.tile("WOUT", [128, 8, D], BF16)
        p.dma("pool", lambda e: e.dma_start(out=WOUT[:], in_=self.dram["mla_out"].rearrange("(c p) n -> p c n", p=128)), writes=[WOUT])
        for c in range(8):
            p.op("pool", lambda e, c=c: e.tensor_tensor(out=WOUT[:, c, :], in0=WOUT[:, c, :], in1=self.G[:], op=ALU.mult), reads=[WOUT, self.G], writes=[WOUT])
        self.out_proj(OT, WOUT)
        p.release(m0)

    def out_proj(self, OT, WOUT):
        p = self.p
        for t in range(NT):
            for hf in range(2):
                ps = self.PS[(2 * t + hf) % 4]
                for c in range(8):
                    p.op("pe", lambda e, c=c, t=t, hf=hf, ps=ps: e.matmul(ps[:], lhsT=OT[:, c, t * 128:(t + 1) * 128], rhs=WOUT[:, c, hf * 512:(hf + 1) * 512],
                                                                          start=(c == 0), stop=(c == 7)), reads=[("OT", t // 4), WOUT], writes=[ps])
                xs = self.X[:, t, hf * 512:(hf + 1) * 512]
                p.op("dve", lambda e, xs=xs, ps=ps: e.tensor_tensor(out=xs, in0=ps[:], in1=xs, op=ALU.add), reads=[ps, ("X", t)], writes=[("X", t)])


    def mm(self, out, lhsT, rhs, start, stop, reads, writes):
        self.p.op("pe", lambda e: e.matmul(out, lhsT=lhsT, rhs=rhs, start=start, stop=stop), reads=reads, writes=writes)

    def tr(self, out, in_, ident, reads, writes):
        self.p.op("pe", lambda e: e.transpose(out, in_, ident), reads=list(reads) + [ident], writes=writes)

    def gdn_phase(self, s):
        p = self.p
        PS = self.PS
        PSb = [b.bitcast(BF16) for b in PS]
        HTr = [("HT", b) for b in range(NB)]
        m0 = p.mark()
        NEG = -1.0e30
        LTRI = p.tile("LTRI", [128, 128], F32)
        BDm = p.tile("BDm", [128, 128], F32)
        SEL = [p.tile(f"SEL{i}", [128, 128], F32) for i in range(2)]
        MNi = p.tile("MNi", [128, 128], F32)
        MNs = p.tile("MNs", [128, 128], F32)
        hp = p.tile("hp", [128, 24], F32)
        nA = p.tile("nA", [128, 8], F32)
        GON = p.tile("GON", [128, 128], F32)
        WBA = p.tile("WBA", [128, 8, 16], BF16)
        CW = p.tile("CW", [128, 8, 3, 4], F32)
        pl = lambda fn, r=(), w=(): p.op("pool", fn, reads=r, writes=w)
        pl(lambda e: e.memset(LTRI[:], 1.0), w=[LTRI])
        pl(lambda e: e.affine_select(out=LTRI[:], in_=LTRI[:], pattern=[[1, 128]], compare_op=ALU.is_ge, fill=0.0, base=0, channel_multiplier=-1), r=[LTRI], w=[LTRI])
        pl(lambda e: e.memset(LTRI[0:64, 64:128], 0.0), r=[LTRI], w=[LTRI])
        pl(lambda e: e.memset(BDm[:], 0.0), w=[BDm])
        pl(lambda e: e.memset(BDm[0:64, 0:64], 1.0), r=[BDm], w=[BDm])
        pl(lambda e: e.memset(BDm[64:128, 64:128], 1.0), r=[BDm], w=[BDm])
        for i in range(2):
            pl(lambda e, i=i: e.memset(SEL[i][:], 0.0), w=[SEL[i]])
            pl(lambda e, i=i: e.memset(SEL[i][64 * i:64 * i + 1, :], 1.0), r=[SEL[i]], w=[SEL[i]])
        pl(lambda e: e.memset(MNi[:], 0.0), w=[MNi])
        pl(lambda e: e.affine_select(out=MNi[:], in_=MNi[:], pattern=[[1, 128]], compare_op=ALU.is_ge, fill=NEG, base=0, channel_multiplier=-1), r=[MNi], w=[MNi])
        pl(lambda e: e.memset(MNi[0:64, 64:128], NEG), r=[MNi], w=[MNi])
        pl(lambda e: e.memset(MNs[:], 0.0), w=[MNs])
        pl(lambda e: e.affine_select(out=MNs[:], in_=MNs[:], pattern=[[1, 128]], compare_op=ALU.is_gt, fill=NEG, base=0, channel_multiplier=-1), r=[MNs], w=[MNs])
        pl(lambda e: e.memset(MNs[0:64, 64:128], NEG), r=[MNs], w=[MNs])
        p.dma("sp", lambda e: e.dma_start(out=hp[:], in_=self.dram["gdn_hp"]), writes=[hp])
        p.dma("sp", lambda e: e.dma_start(out=GON[:], in_=self.dram["gdn_on"]), writes=[GON])
        p.dma("sp", lambda e: e.dma_start(out=CW[:], in_=self.dram["gdn_conv"]), writes=[CW])
        p.dma("pool", lambda e: e.dma_start(out=WBA[:], in_=self.dram["gdn_ba"]), writes=[WBA])
        p.op("act", lambda e: e.activation(out=nA[:], in_=hp[:, 0:8], func=AF.Exp), reads=[hp], writes=[nA])
        p.op("dve", lambda e: e.tensor_scalar(out=nA[:], in0=nA[:], scalar1=-1.0, scalar2=None, op0=ALU.mult), reads=[nA], writes=[nA])
        BETA = p.tile("BETA", [128, NT, 8], F32)
        GAMS = p.tile("GAMS", [128, NT, 8], F32)
        EGL = p.tile("EGL", [128, NT, 8], F32)
        BG = p.tile("BG", [128, NT, 8], F32)
        DECB = p.tile("DECB", [128, NT, 2, 8], F32)
        GC3 = p.tile("GC3", [128, NT, 3, 8], F32)
        m1 = p.mark()
        ba = p.tile("ba", [128, 16], F32)
        xs = p.tile("gxs", [128, 8], F32)
        ax = p.tile("gax", [128, 8], F32)
        gg = p.tile("ggg", [128, 8], F32)
        lnb = p.tile("lnb", [128, 8], F32)
        gcl = p.tile("gcl", [128, 2, 8], F32)
        dk_s = 128.0 ** -0.5
        import os
        pre = int(os.environ.get("GDN_PRE", "99"))
        if "GDN_OPS" in os.environ:
            p.limit = int(os.environ["GDN_OPS"])
        for t in range(NT if pre >= 2 else pre):
            ts_ = slice(t * 128, (t + 1) * 128)
            ps = PS[t % 2]
            for k in range(8):
                self.mm(ps[:, 0:16], self.HT[:, k, ts_], WBA[:, k, :], k == 0, k == 7, [HTr[t // 4], WBA], [ps])
            p.op("act", lambda e, ps=ps: e.copy(ba[:], ps[:, 0:16]), reads=[ps], writes=[ba])
            p.op("act", lambda e, t=t: e.activation(out=BETA[:, t, :], in_=ba[:, 0:8], func=AF.Sigmoid), reads=[ba], writes=[BETA])
            p.op("act", lambda e, t=t: e.activation(out=lnb[:], in_=BETA[:, t, :], func=AF.Ln), reads=[BETA], writes=[lnb])
            p.op("dve", lambda e: e.tensor_tensor(out=xs[:], in0=ba[:, 8:16], in1=hp[:, 8:16], op=ALU.add), reads=[ba, hp], writes=[xs])
            p.op("act", lambda e: e.activation(out=ax[:], in_=xs[:], func=AF.Abs), reads=[xs], writes=[ax])
            p.op("act", lambda e: e.activation(out=ax[:], in_=ax[:], func=AF.Exp, scale=-1.0), reads=[ax], writes=[ax])
            p.op("act", lambda e: e.activation(out=ax[:], in_=ax[:], func=AF.Ln, bias=self.onesf[:, 0:1], scale=1.0), reads=[ax, self.onesf], writes=[ax])
            p.op("dve", lambda e: e.scalar_tensor_tensor(out=gg[:], in0=xs[:], scalar=0.0, in1=ax[:], op0=ALU.max, op1=ALU.add), reads=[xs, ax], writes=[gg])
            p.op("dve", lambda e: e.tensor_tensor(out=gg[:], in0=gg[:], in1=nA[:], op=ALU.mult), reads=[gg, nA], writes=[gg])
            self.mm(ps[:, 16:24], LTRI[:], gg[:], True, True, [LTRI, gg], [ps])
            self.mm(ps[:, 24:32], BDm[:], gg[:], True, True, [BDm, gg], [ps])
            p.op("act", lambda e, ps=ps: e.copy(gcl[:].rearrange("p a b -> p (a b)"), ps[:, 16:32]), reads=[ps], writes=[gcl])
            p.op("act", lambda e, t=t: e.activation(out=BG[:, t, :], in_=gcl[:, 0, :], func=AF.Exp), reads=[gcl], writes=[BG])
            p.op("dve", lambda e, t=t: e.tensor_scalar(out=GAMS[:, t, :], in0=BG[:, t, :], scalar1=dk_s, scalar2=None, op0=ALU.mult), reads=[BG], writes=[GAMS])
            p.op("dve", lambda e, t=t: e.tensor_tensor(out=BG[:, t, :], in0=BG[:, t, :], in1=BETA[:, t, :], op=ALU.mult), reads=[BG, BETA], writes=[BG])
            p.op("dve", lambda e, t=t: e.tensor_tensor(out=EGL[:, t, :], in0=gcl[:, 1, :], in1=gcl[:, 0, :], op=ALU.subtract), reads=[gcl], writes=[EGL])
            p.op("act", lambda e, t=t: e.activation(out=EGL[:, t, :], in_=EGL[:, t, :], func=AF.Exp), reads=[EGL], writes=[EGL])
            p.op("dve", lambda e, t=t: e.tensor_copy(GC3[:, t, 0, :], gcl[:, 0, :]), reads=[gcl], writes=[GC3])
            p.op("dve", lambda e, t=t: e.tensor_scalar(out=GC3[:, t, 1, :], in0=gcl[:, 0, :], scalar1=-1.0, scalar2=None, op0=ALU.mult), reads=[gcl, GC3], writes=[GC3])
            p.op("dve", lambda e, t=t: e.tensor_tensor(out=GC3[:, t, 2, :], in0=gcl[:, 0, :], in1=lnb[:], op=ALU.add), reads=[gcl, lnb, GC3], writes=[GC3])
            for hf in range(2):
                self.mm(ps[:, 32 + hf * 8:40 + hf * 8], SEL[hf][:], gcl[:, 1, :], True, True, [SEL[hf], gcl], [ps])
            p.op("act", lambda e, t=t, ps=ps: e.activation(out=DECB[:, t, :, :].rearrange("p a b -> p (a b)"), in_=ps[:, 32:48], func=AF.Exp), reads=[ps], writes=[DECB])
        p.limit = None
        p.release(m1)
        NPAR = int(os.environ.get("GDN_NPAR", "1"))
        HB = []
        for sl in range(NPAR):
            b = {}
            b["WH"] = p.tile(f"WH{sl}", [128, 8, 512], BF16)
            b["WO"] = p.tile(f"WO{sl}", [128, D], BF16)
            b["DG"] = p.tile(f"DG{sl}", [128, 12, 128], BF16)
            b["S"] = p.tile(f"S{sl}", [128, 128], F32)
            b["Sb"] = p.tile(f"Sb{sl}", [128, 128], BF16)
            b["XC"] = p.tile(f"XC{sl}", [128, 3, 515], BF16)
            for nm, shp, dt in (("ZS", [128, 128], F32), ("GZ", [128, 128], F32), ("QKV", [128, 3, 128], F32), ("SQ", [128, 2, 128], F32),
                                ("st", [128, 8], F32), ("TOK", [128, 6, 128], BF16), ("DGC", [128, 2, 128], F32), ("KQT", [128, 3, 128], BF16), ("DT2", [128, 2, 128], F32),
                                ("M2", [128, 2, 128], F32), ("ATb", [128, 128], BF16), ("YA0", [128, 2, 128], F32), ("YA1", [128, 2, 128], F32),
                                ("P0", [128, 128], F32), ("P1", [128, 128], F32), ("TTb", [128, 128], BF16), ("U", [128, 128], F32),
                                ("WTb", [128, 128], BF16), ("VNb", [128, 128], BF16), ("OT", [128, 128], F32), ("OG", [128, 128], BF16),
                                ("OGT", [128, 128], BF16), ("junk", [128, 128], BF16)):
                b[nm] = p.tile(f"{nm}{sl}", shp, dt)
            b["bank"] = [PS[4 * sl + i] for i in range(4)]
            b["bankb"] = [PSb[4 * sl + i] for i in range(4)]
            HB.append(b)

        def head_gen(h, b):
            sl = HB.index(b)
            WH, WO, DG, S, Sb, XC = b["WH"], b["WO"], b["DG"], b["S"], b["Sb"], b["XC"]
            bk, bkb = b["bank"], b["bankb"]
            R = lambda i, nm: (f"ps{sl}_{i}")
            r_zc = [R(0, "zc")]
            r_tr, r_d, r_og = [R(1, "tr")], [R(1, "d")], [R(1, "og")]
            r_kk, r_a, r_p = [R(2, "kk")], [R(2, "a")], [R(2, "p")]
            r_ya, r_u, r_wt = [R(3, "ya")], [R(3, "u")], [R(3, "wt")]
            allb = [r_zc, r_tr, r_kk, r_ya]
            p.dma("pool", lambda e: e.dma_start(out=WH[:].rearrange("p k n -> p (k n)"), in_=self.dram["gdn_in"][h].rearrange("p k n -> p (k n)")), writes=[WH])
            p.dma("pool", lambda e: e.dma_start(out=WO[:], in_=self.dram["gdn_out"][h * 128:(h + 1) * 128, :]), writes=[WO])
            p.op("pool", lambda e: e.tensor_tensor(out=WO[:], in0=WO[:], in1=self.G[:], op=ALU.mult), reads=[WO, self.G], writes=[WO])
            for part in range(3):
                for j in range(4):
                    p.op("dve", lambda e, part=part, j=j: e.tensor_scalar(out=DG[:, part * 4 + j, :], in0=self.identb[:], scalar1=CW[:, h, part, j:j + 1], scalar2=None, op0=ALU.mult),
                         reads=[self.identb, CW], writes=[DG])
            p.op("pool", lambda e: e.memset(S[:], 0.0), writes=[S])
            p.op("pool", lambda e: e.memset(Sb[:], 0.0), writes=[Sb])
            p.op("pool", lambda e: e.memset(XC[:, :, 0:3], 0.0), writes=[XC])
            yield
            for blk in range(NB):
                cs = slice(blk * 512, (blk + 1) * 512)
                for part in range(3):
                    ps = bk[part]
                    for k in range(8):
                        self.mm(ps[:], WH[:, k, part * 128:(part + 1) * 128], self.HT[:, k, cs], k == 0, k == 7, [WH, HTr[blk]], allb[part])
                    eng = ("act", "dve", "act")[part]
                    if eng == "act":
                        p.op("act", lambda e, part=part, ps=ps: e.copy(XC[:, part, 3:515], ps[:]), reads=allb[part], writes=[XC])
                    else:
                        p.op("dve", lambda e, part=part, ps=ps: e.tensor_copy(XC[:, part, 3:515], ps[:]), reads=allb[part], writes=[XC])
                yield
                for tt in range(4):
                    t = blk * 4 + tt
                    ts_ = slice(t * 128, (t + 1) * 128)
                    ZS, GZ, QKV, SQ, st, TOK, KQT, DT2, M2, ATb = (b[n] for n in ("ZS", "GZ", "QKV", "SQ", "st", "TOK", "KQT", "DT2", "M2", "ATb"))
                    YA, Pb, TTb, U, WTb, VNb, OTk, OG, OGT = [b["YA0"], b["YA1"]], [b["P0"], b["P1"]], b["TTb"], b["U"], b["WTb"], b["VNb"], b["OT"], b["OG"], b["OGT"]
                    for k in range(8):
                        self.mm(bk[0][:, 0:128], self.HT[:, k, ts_], WH[:, k, 384:512], k == 0, k == 7, [WH, HTr[blk]], r_zc)
                    for part in range(3):
                        for j in range(4):
                            self.mm(bk[0][:, 128 + part * 128:256 + part * 128], XC[:, part, tt * 128 + j:tt * 128 + j + 128], DG[:, part * 4 + j, :], j == 0, j == 3, [XC, DG], r_zc)
                    p.op("act", lambda e: e.activation(out=ZS[:], in_=bk[0][:, 0:128], func=AF.Silu), reads=r_zc, writes=[ZS])
                    p.op("act", lambda e: e.activation(out=QKV[:].rearrange("p a b -> p (a b)"), in_=bk[0][:, 128:512], func=AF.Silu), reads=r_zc, writes=[QKV])
                    p.op("pool", lambda e: e.tensor_tensor(out=GZ[:], in0=ZS[:], in1=GON[:], op=ALU.mult), reads=[ZS, GON], writes=[GZ])
                    yield
                    if "GDN_OPS2" in os.environ and h == 0 and t == 0:
                        p.limit = int(os.environ["GDN_OPS2"])
                    p.op("dve", lambda e: e.tensor_tensor(out=SQ[:], in0=QKV[:, 0:2, :], in1=QKV[:, 0:2, :], op=ALU.mult), reads=[QKV], writes=[SQ])
                    p.op("dve", lambda e: e.tensor_reduce(out=st[:, 0:2], in_=SQ[:], axis=AX.X, op=ALU.add), reads=[SQ], writes=[st])
                    p.op("act", lambda e: e.activation(out=st[:, 0:2], in_=st[:, 0:2], func=AF.Sqrt, bias=self.epsc[:], scale=1.0), reads=[st, self.epsc], writes=[st])
                    p.op("dve", lambda e: e.reciprocal(out=st[:, 2:4], in_=st[:, 0:2]), reads=[st], writes=[st])
                    p.op("act", lambda e: e.activation(out=TOK[:, 0, :], in_=QKV[:, 1, :], func=AF.Copy, scale=st[:, 3:4]), reads=[QKV, st], writes=[(TOK.name, 0)])
                    p.op("dve", lambda e: e.tensor_scalar(out=TOK[:, 1, :], in0=QKV[:, 0, :], scalar1=st[:, 2:3], scalar2=dk_s, op0=ALU.mult, op1=ALU.mult), reads=[QKV, st], writes=[(TOK.name, 0)])
                    p.op("dve", lambda e, t=t: e.tensor_scalar(out=TOK[:, 2, :], in0=QKV[:, 0, :], scalar1=st[:, 2:3], scalar2=GAMS[:, t, h:h + 1], op0=ALU.mult, op1=ALU.mult), reads=[QKV, st, GAMS], writes=[(TOK.name, 0)])
                    p.op("dve", lambda e, t=t: e.tensor_scalar(out=TOK[:, 3, :], in0=QKV[:, 1, :], scalar1=st[:, 3:4], scalar2=EGL[:, t, h:h + 1], op0=ALU.mult, op1=ALU.mult), reads=[QKV, st, EGL], writes=[(TOK.name, 1)])
                    p.op("dve", lambda e, t=t: e.tensor_scalar(out=TOK[:, 4, :], in0=QKV[:, 1, :], scalar1=st[:, 3:4], scalar2=BG[:, t, h:h + 1], op0=ALU.mult, op1=ALU.mult), reads=[QKV, st, BG], writes=[(TOK.name, 1)])
                    p.op("act", lambda e, t=t: e.activation(out=TOK[:, 5, :], in_=QKV[:, 2, :], func=AF.Copy, scale=BETA[:, t, h:h + 1]), reads=[QKV, BETA], writes=[(TOK.name, 1)])
                    for i in range(3):
                        self.tr(bkb[1][:, i * 128:(i + 1) * 128], TOK[:, i, :], self.identb[:], [(TOK.name, 0)], r_tr)
                    p.op("act", lambda e: e.copy(KQT[:].rearrange("p a b -> p (a b)"), bkb[1][:, 0:384]), reads=r_tr, writes=[KQT])
                    d0 = 256
                    DGC = b["DGC"]
                    p.op("act", lambda e, t=t: e.activation(out=DGC[:, 0, :], in_=self.identf[:], func=AF.Copy, scale=GC3[:, t, 0, h:h + 1]), reads=[self.identf, GC3], writes=[DGC])
                    p.op("act", lambda e, t=t: e.activation(out=DGC[:, 1, :], in_=self.identf[:], func=AF.Copy, scale=GC3[:, t, 2, h:h + 1]), reads=[self.identf, GC3], writes=[DGC])
                    self.mm(bk[1][:, d0:d0 + 128], self.onesf[:], DGC[:, 0, :], True, False, [self.onesf, DGC], r_d)
                    self.mm(bk[1][:, d0:d0 + 128], self.identf[:], MNi[:], False, True, [self.identf, MNi], r_d)
                    self.mm(bk[1][:, d0 + 128:d0 + 256], self.onesf[:], DGC[:, 1, :], True, False, [self.onesf, DGC], r_d)
                    self.mm(bk[1][:, d0 + 128:d0 + 256], self.identf[:], MNs[:], False, True, [self.identf, MNs], r_d)
                    p.op("act", lambda e, t=t: e.activation(out=DT2[:].rearrange("p a b -> p (a b)"), in_=bk[1][:, d0:d0 + 256], func=AF.Exp, bias=GC3[:, t, 1, h:h + 1], scale=1.0),
                         reads=r_d + [GC3], writes=[DT2])
                    yield
                    self.mm(bk[2][:, 0:128], KQT[:, 0, :], KQT[:, 1, :], True, True, [KQT], r_kk)
                    self.mm(bk[2][:, 128:256], KQT[:, 0, :], KQT[:, 0, :], True, True, [KQT], r_kk)
                    p.op("dve", lambda e: e.tensor_tensor(out=M2[:].rearrange("p a b -> p (a b)"), in0=bk[2][:, 0:256], in1=DT2[:].rearrange("p a b -> p (a b)"), op=ALU.mult),
                         reads=r_kk + [DT2], writes=[M2])
                    p.op("act", lambda e: e.copy(ATb[:], M2[:, 0, :]), reads=[M2], writes=[ATb])
                    Y = M2[:, 1, :]
                    self.tr(bk[2][:, 256:384], Y, self.identf[:], [M2], r_a)
                    p.op("act", lambda e: e.copy(YA[0][:, 1, :], bk[2][:, 256:384]), reads=r_a, writes=[(YA[0].name, 1)])
                    p.op("pool", lambda e: e.tensor_copy(YA[0][:, 0, :], Y), reads=[M2], writes=[(YA[0].name, 0)])
                    p.op("pool", lambda e: e.tensor_tensor(out=Pb[0][:], in0=self.identf[:], in1=Y, op=ALU.subtract), reads=[M2, self.identf], writes=[Pb[0]])
                    yield
                    for it in range(5):
                        cur, nxt = YA[it % 2], YA[(it + 1) % 2]
                        pc, pn = Pb[it % 2], Pb[(it + 1) % 2]
                        rc = [(cur.name, 0), (cur.name, 1)]
                        if it < 4:
                            self.mm(bk[3][:, 0:128], cur[:, 1, :], cur[:, 0, :], True, True, rc, r_ya)
                        self.mm(bk[3][:, 128:256], cur[:, 0, :], cur[:, 1, :], True, True, rc, r_ya)
                        if it < 4:
                            p.op("act", lambda e, nxt=nxt: e.copy(nxt[:].rearrange("p a b -> p (a b)"), bk[3][:, 0:256]), reads=r_ya, writes=[(nxt.name, 0), (nxt.name, 1)])
                        else:
                            p.op("act", lambda e, nxt=nxt: e.copy(nxt[:, 1, :], bk[3][:, 128:256]), reads=r_ya, writes=[(nxt.name, 1)])
                        self.mm(bk[2][:, 384:512], self.identf[:], pc[:], True, False, [self.identf, pc], r_p)
                        self.mm(bk[2][:, 384:512], nxt[:, 1, :], pc[:], False, True, [(nxt.name, 1), pc], r_p)
                        if it < 4:
                            p.op("dve", lambda e, pn=pn: e.tensor_copy(pn[:], bk[2][:, 384:512]), reads=r_p, writes=[pn])
                        else:
                            p.op("dve", lambda e: e.tensor_copy(TTb[:], bk[2][:, 384:512]), reads=r_p, writes=[TTb])
                        yield
                    if "GDN_OPS3" in os.environ and h == 1 and t == 0:
                        p.limit = int(os.environ["GDN_OPS3"])
                    self.mm(bk[3][:, 256:384], TTb[:], TOK[:, 5, :], True, True, [TTb, (TOK.name, 1)], r_u)
                    self.mm(bk[0][:, 0:128], TOK[:, 4, :], TTb[:], True, True, [TTb, (TOK.name, 1)], r_zc)
                    p.op("act", lambda e: e.copy(U[:], bk[3][:, 256:384]), reads=r_u, writes=[U])
                    p.op("dve", lambda e: e.tensor_copy(WTb[:], bk[0][:, 0:128]), reads=r_zc, writes=[WTb])
                    yield
                    for hf in range(2):
                        r0 = hf * 64
                        rr = slice(r0, r0 + 64)
                        if "GDN_OPS4" in os.environ and h == int(os.environ.get("GDN_H", "0")) and t == 0 and hf == 0:
                            p.limit = int(os.environ["GDN_OPS4"])
                        if os.environ.get("GDN_M128") == "1":
                            self.mm(bk[2][:, 0:128], WTb[:, :], Sb[:], True, True, [WTb, Sb], r_kk)
                        elif os.environ.get("GDN_M128") == "2":
                            self.mm(bk[2][0:64, 0:128], KQT[:, 2, rr], Sb[:], True, True, [KQT, Sb], r_kk)
                        elif os.environ.get("GDN_M128") == "4":
                            self.mm(bk[0][:, 0:128], WTb[:, :], Sb[:], True, True, [WTb, Sb], r_zc)
                        elif os.environ.get("GDN_M128") == "5":
                            self.mm(bk[2][:, 384:512], WTb[:, :], Sb[:], True, True, [WTb, Sb], r_p)
                        elif os.environ.get("GDN_M128") == "3":
                            self.mm(bk[2][0:64, 0:128], WTb[:, rr], KQT[:, 2, :], True, True, [KQT, WTb], r_kk)
                        else:
                            self.mm(bk[2][0:64, 0:128], WTb[:, rr], Sb[:], True, True, [WTb, Sb], r_kk)
                        p.op("dve", lambda e, rr=rr: e.scalar_tensor_tensor(out=VNb[rr, :], in0=bk[2][0:64, 0:128], scalar=-1.0, in1=U[rr, :], op0=ALU.mult, op1=ALU.add), reads=[U] + r_kk, writes=[VNb])
                        self.mm(bk[2][0:64, 128:256], KQT[:, 2, rr], Sb[:], True, False, [KQT, Sb], r_kk)
                        self.mm(bk[2][0:64, 128:256], ATb[rr, rr], VNb[rr, :], False, True, [ATb, VNb], r_kk)
                        p.op("act", lambda e, rr=rr: e.copy(OTk[rr, :], bk[2][0:64, 128:256]), reads=r_kk, writes=[OTk])
                        self.mm(bk[2][:, 256:384], TOK[rr, 3, :], VNb[rr, :], True, True, [(TOK.name, 1), VNb], r_a)
                        p.op("act", lambda e, t=t, hf=hf: e.activation(out=S[:], in_=S[:], func=AF.Copy, scale=DECB[:, t, hf, h:h + 1]), reads=[S, DECB], writes=[S])
                        p.op("dve", lambda e: e.tensor_tensor(out=S[:], in0=bk[2][:, 256:384], in1=S[:], op=ALU.add), reads=[S] + r_a, writes=[S])
                        p.op("act", lambda e: e.copy(Sb[:], S[:]), reads=[S], writes=[Sb])
                        yield
                    p.op("pool", lambda e: e.memset(st[:, 4:5], 0.0), reads=[st], writes=[st])
                    p.op("act", lambda e: e.activation(out=b["junk"][:], in_=OTk[:], func=AF.Square, accum_out=st[:, 4:5]), reads=[OTk, st], writes=[b["junk"], st])
                    p.op("act", lambda e: e.activation(out=st[:, 4:5], in_=st[:, 4:5], func=AF.Sqrt, bias=self.epsc[:], scale=1.0 / 128), reads=[st, self.epsc], writes=[st])
                    p.op("dve", lambda e: e.reciprocal(out=st[:, 5:6], in_=st[:, 4:5]), reads=[st], writes=[st])
                    p.op("dve", lambda e: e.scalar_tensor_tensor(out=OG[:], in0=OTk[:], scalar=st[:, 5:6], in1=GZ[:], op0=ALU.mult, op1=ALU.mult), reads=[OTk, st, GZ], writes=[OG])
                    self.tr(bkb[1][:, 384:512], OG[:], self.identb[:], [OG], r_og)
                    p.op("act", lambda e: e.copy(OGT[:], bkb[1][:, 384:512]), reads=r_og, writes=[OGT])
                    for hf in range(2):
                        self.mm(bk[0][:], OGT[:], WO[:, hf * 512:(hf + 1) * 512], True, True, [OGT, WO], r_zc)
                        xs_ = self.X[:, t, hf * 512:(hf + 1) * 512]
                        p.op("dve", lambda e, xs_=xs_: e.tensor_tensor(out=xs_, in0=bk[0][:], in1=xs_, op=ALU.add), reads=r_zc + [("X", t)], writes=[("X", t)])
                    yield
                p.op("dve", lambda e: e.tensor_copy(XC[:, :, 0:3], XC[:, :, 512:515]), reads=[XC], writes=[XC])
                yield

        import os
        steps = int(os.environ.get("GDN_STEPS", "1000000000"))
        for h0 in range(0, 8, NPAR):
            gens = [head_gen(h0 + i, HB[i]) for i in range(NPAR)]
            alive = list(gens)
            while alive and steps > 0:
                for g_ in list(alive):
                    steps -= 1
                    if steps <= 0:
                        break
                    try:
                        next(g_)
                    except StopIteration:
                        alive.remove(g_)
        if "GDN_DUMP" in os.environ:
            nm = os.environ["GDN_DUMP"]
            src = HB[0][nm]
            self.dump = p.tile("dump", [128, 512], F32)
            n = int(np.prod(src.shape[1:]))
            v = src[:] if len(src.shape) == 2 else src[:].rearrange("p a b -> p (a b)")
            p.op("pool", lambda e: e.memset(self.dump[:], 0.0), writes=[self.dump])
            p.op("dve", lambda e: e.tensor_copy(self.dump[:, 0:min(n, 512)], v[:, 0:min(n, 512)]), reads=[src, self.dump] + [(src.name, i) for i in range(2)], writes=[self.dump])
            p.dma("sp", lambda e: e.dma_start(out=self.y[s, 0:128, 0:512], in_=self.dump[:]), reads=[self.dump], key="dump")
            p.barrier()
            self.skip0 = True
        p.release(m0)

    def build(self):
        p = self.p
        st = self.stages
        self.setup()
        self.prologue_mod()
        final = []
        for s in range(self.nseq):
            self.load_x(s)
            for l in range(2):
                if f"mix{l}" in st:
                    self.norm_phase(l, s, 0)
                    self.make_gate(l, s, 2)
                    (self.mla_phase if l == 0 else self.gdn_phase)(s)
                if f"ffn{l}" in st:
                    self.norm_phase(l, s, 1, router=(l == 1))
                    self.make_gate(l, s, 5)
                    if l == 0:
                        ex = [(self.dram["ffn_gu"][j], self.dram["ffn_dn"][j], None) for j in range(2)]
                    else:
                        ex = [(self.dram["moe_gu"][j], self.dram["moe_dn"][j], j) for j in range(8)]
                    self.ffn_phase(ex)
            final += self.final_out(s)
        p.wait("sp", final)
        p.emit()
        return self.nc


def _cols(v, nk):
    return np.ascontiguousarray(np.asarray(v).reshape(nk, 128).T)


def _gu_layout(w, ff):
    nch = ff // 128
    g = w[:, :ff].reshape(8, 128, nch, 128)
    u = w[:, ff:].reshape(8, 128, nch, 128)
    gu = np.concatenate([g, u], axis=-1)
    return np.ascontiguousarray(gu.transpose(2, 1, 0, 3)).reshape(nch, 128, 8 * 256)


def _rope_consts():
    c = np.zeros((128, 2), np.float64)
    inv = 10000.0 ** (-np.arange(16, dtype=np.float64) / 16.0)
    for j in range(32):
        c[64 + j, 0] = inv[j % 16] / (2 * math.pi); c[64 + j, 1] = 0.25
        c[96 + j, 0] = inv[j % 16] / (2 * math.pi); c[96 + j, 1] = 0.5 if j < 16 else 0.0
    return c.astype(np.float32)


ROPEC = _rope_consts()


def prep_shared(inp):
    f = lambda k: np.asarray(inp[k], dtype=np.float32)
    sh = {}
    sh["ada"] = np.ascontiguousarray(np.concatenate([f("ada_w"), f("ada_b")[:, None, :]], axis=1))
    sh["gmix"] = np.stack([_cols(f("norm_mix")[l], 8) for l in range(2)])
    sh["gffn"] = np.stack([_cols(f("norm_ffn")[l], 8) for l in range(2)])
    sh["gfin"] = np.ascontiguousarray(np.broadcast_to(f("final_norm")[None, :], (128, D)))
    wgu = f("ffn_w_gate_up")[0]
    halves = []
    for j in range(2):
        sub = np.concatenate([wgu[:, j * 1408:(j + 1) * 1408], wgu[:, 2816 + j * 1408:2816 + (j + 1) * 1408]], axis=1)
        halves.append(_gu_layout(sub, 1408))
    sh["ffn_gu"] = np.stack(halves)
    sh["ffn_dn"] = np.ascontiguousarray(f("ffn_w_down")[0].reshape(2, 1408, D))
    sh["moe_gu"] = np.stack([_gu_layout(f("moe_w_gate_up")[0, e], 1408) for e in range(8)])
    sh["moe_dn"] = np.ascontiguousarray(f("moe_w_down")[0])
    sh["moe_r"] = np.ascontiguousarray(f("moe_w_router")[0].reshape(8, 128, 8).transpose(1, 0, 2))

    perm = np.concatenate([np.arange(16, 32), np.arange(0, 16)])
    win = f("mla_w_in")[0]
    kr = win[:, 768:800]
    win2 = np.concatenate([win[:, :768], win[:, 0:64], kr, kr[:, perm]], axis=1)
    sh["mla_in"] = np.ascontiguousarray(win2.reshape(8, 128, 896).transpose(1, 0, 2))
    sh["mla_qn"] = _cols(f("mla_q_norm")[0], 4)
    sh["mla_kvn"] = _cols(f("mla_kv_norm")[0], 2)
    wqb = f("mla_w_qb")[0].reshape(512, 16, 96)
    wqb2 = np.concatenate([wqb[:, :, :64], wqb[:, :, 64:96], wqb[:, :, 64:96][:, :, perm]], axis=2).reshape(512, 2048)
    sh["mla_qb"] = np.ascontiguousarray(wqb2.reshape(4, 128, 2048).transpose(1, 0, 2))
    wkv = f("mla_w_kvb")[0].reshape(256, 16, 128)
    sh["mla_kb"] = np.ascontiguousarray(wkv[:, :, :64].reshape(2, 128, 1024).transpose(1, 0, 2))
    sh["mla_vb"] = np.ascontiguousarray(wkv[:, :, 64:].reshape(2, 128, 1024).transpose(1, 0, 2))
    sh["mla_out"] = np.ascontiguousarray(f("mla_w_out")[0])
    sh["ropec"] = ROPEC
    gw = f("gdn_w_in")[0]
    heads = []
    for h in range(8):
        cols = np.concatenate([np.arange(h * 128, (h + 1) * 128) + off for off in (0, 1024, 2048, 3072)])
        heads.append(gw[:, cols].reshape(8, 128, 512).transpose(1, 0, 2))
    sh["gdn_in"] = np.ascontiguousarray(np.stack(heads))
    sh["gdn_ba"] = np.ascontiguousarray(gw[:, 4096:4112].reshape(8, 128, 16).transpose(1, 0, 2))
    cw = f("gdn_conv_w")[0]
    sh["gdn_conv"] = np.ascontiguousarray(cw.reshape(4, 3, 8, 128).transpose(3, 2, 1, 0))
    hpv = np.zeros((128, 24), np.float32)
    hpv[:, 0:8] = f("gdn_a_log")[0][None, :]
    hpv[:, 8:16] = f("gdn_dt_bias")[0][None, :]
    sh["gdn_hp"] = hpv
    sh["gdn_on"] = np.ascontiguousarray(np.broadcast_to(f("gdn_out_norm")[0][None, :], (128, 128)))
    sh["gdn_out"] = np.ascontiguousarray(f("gdn_w_out")[0])
    return sh


def prep_core(inp, b0, ns):
    x = np.ascontiguousarray(np.asarray(inp["x"], np.float32)[b0:b0 + ns])
    c = np.asarray(inp["c"], np.float32)[b0:b0 + ns]
    cT = np.stack([_cols(c[i], 8) for i in range(ns)])
    pos = np.asarray(inp["positions"]).astype(np.int32)[b0:b0 + ns]
    posr = np.ascontiguousarray(np.broadcast_to(pos[:, None, :], (ns, 128, SEQ)))
    return {"x": x, "cT": cT, "pos": posr}


_CACHE = {}


def run(inp, batches, ns, stages):
    key = (ns, tuple(sorted(stages)))
    if key not in _CACHE:
        _CACHE[key] = Builder(ns, stages).build()
    nc = _CACHE[key]
    sh = prep_shared(inp)
    maps = []
    for b0 in batches:
        mm = dict(sh)
        mm.update(prep_core(inp, b0, ns))
        maps.append(mm)
    res = run_bass_kernel_spmd(nc, maps, core_ids=list(range(len(batches))))
    return [r["y"] for r in res.results]


ALL = ("mix0", "ffn0", "mix1", "ffn1")


def kernel(**inputs):
    ys = run(inputs, [2 * i for i in range(NCORES)], 2, ALL)
    return np.concatenate(ys, axis=0).astype(np.float32)
```

```python
import math
import numpy as np
import concourse.bass as bass
import concourse.mybir as mybir
from concourse.bass_utils import run_bass_kernel_spmd

F32 = mybir.dt.float32
BF16 = mybir.dt.bfloat16
I32 = mybir.dt.int32
AF = mybir.ActivationFunctionType
ALU = mybir.AluOpType
AX = mybir.AxisListType

NCORES = 8
SEQ = 2048
D = 1024
NT = SEQ // 128
NB = SEQ // 512
EPS = 1e-6
GEN = 30000


class Prog:
    ENG = ("pe", "act", "dve", "pool", "sp")

    def __init__(self, nc):
        self.nc = nc
        self.q = {e: [] for e in self.ENG}
        self.cnt = {e: 0 for e in self.ENG}
        self.res = {}
        self.seen = {e: {} for e in self.ENG}
        self.dma_cnt = {}
        self.semkeys = set()
        self.off = 16512
        self.ntile = 0
        self.cap = nc.SBUF_PARTITION_SIZE_BYTES

    def tile(self, name, shape, dt):
        nbytes = int(np.prod(shape[1:])) * (2 if dt == BF16 else 4)
        self.off = (self.off + 63) // 64 * 64
        assert self.off + nbytes <= self.cap, (name, self.off, nbytes)
        self.ntile += 1
        t = self.nc.alloc_sbuf_tensor_at(f"{name}_{self.ntile}", list(shape), dt, offset=self.off)
        self.off += nbytes
        return t

    def mark(self):
        return self.off

    def release(self, m):
        self.barrier()
        self.off = m

    def _key(self, r):
        return r if isinstance(r, (str, tuple)) else r.name

    def _st(self, r):
        r = self._key(r)
        s = self.res.get(r)
        if s is None:
            s = self.res[r] = {"w": None, "r": []}
        return s

    def _deps(self, eng, reads, writes):
        deps = {}

        def add(d):
            if d is None:
                return
            k, v = d
            if eng == "pe" and isinstance(k, tuple) and k[0] == "pe":
                return
            if deps.get(k, 0) < v:
                deps[k] = v
        for r in reads:
            add(self._st(r)["w"])
        for r in writes:
            s = self._st(r)
            add(s["w"])
            for d in s["r"]:
                add(d)
        out = []
        seen = self.seen[eng]
        for k, v in deps.items():
            if seen.get(k, 0) < v:
                seen[k] = v
                out.append((k, v))
        return out

    def _commit(self, token, reads, writes):
        for r in reads:
            lst = self._st(r)["r"]
            lst[:] = [d for d in lst if d[0] != token[0]]
            lst.append(token)
        for r in writes:
            s = self._st(r)
            s["w"] = token
            s["r"] = []

    limit = None

    def op(self, eng, fn, reads=(), writes=()):
        if self.limit is not None:
            if self.limit <= 0:
                return None
            self.limit -= 1
        waits = self._deps(eng, reads, writes)
        if self.limit == 0:
            print("LAST OP", eng, "waits", waits, "cnt", self.cnt, "reads", [self._key(r) for r in reads], "writes", [self._key(r) for r in writes])
        self.cnt[eng] += 1
        n = self.cnt[eng]
        key = (eng, (n - 1) // GEN)
        self.semkeys.add(key)
        token = (key, (n - 1) % GEN + 1)
        self.q[eng].append((waits, fn, (key, 1)))
        self._commit(token, reads, writes)
        return token

    def dma(self, queue, fn, reads=(), writes=(), key=None):
        if key is None:
            r0 = writes[0] if writes else reads[0]
            key = ("dma", self._key(r0))
        self.semkeys.add(key)
        waits = self._deps(queue, reads, writes)
        v = self.dma_cnt.get(key, 0) + 16
        self.dma_cnt[key] = v
        token = (key, v)
        self.q[queue].append((waits, fn, (key, 16)))
        self._commit(token, reads, writes)
        return token

    def barrier(self):
        toks = []
        for e in self.ENG:
            n = self.cnt[e]
            if n:
                toks.append(((e, (n - 1) // GEN), (n - 1) % GEN + 1))
        toks += list(self.dma_cnt.items())
        for e in self.ENG:
            self.wait(e, toks)

    def wait(self, eng, tokens):
        waits = []
        for k, v in tokens:
            if eng == "pe" and isinstance(k, tuple) and k[0] == "pe":
                continue
            if self.seen[eng].get(k, 0) < v:
                self.seen[eng][k] = v
                waits.append((k, v))
        if waits:
            self.q[eng].append((waits, None, None))

    def emit(self):
        import contextlib
        nc = self.nc
        with contextlib.ExitStack() as es:
            sems = {}
            for i, k in enumerate(sorted(self.semkeys, key=str)):
                sems[k] = es.enter_context(nc.semaphore(f"s{i}"))
            block = es.enter_context(nc.Block())
            q = self.q

            def run(engname):
                def body(e):
                    for waits, fn, inc in q[engname]:
                        for k, v in waits:
                            e.wait_ge(sems[k], v)
                        if fn is not None:
                            fn(e).then_inc(sems[inc[0]], inc[1])
                return body
            block.tensor(run("pe"))
            block.scalar(run("act"))
            block.vector(run("dve"))
            block.gpsimd(run("pool"))
            block.sync(run("sp"))


class Builder:
    def __init__(self, nseq, stages):
        self.nseq = nseq
        self.stages = stages
        nc = self.nc = bass.Bass("TRN2", target_bir_lowering=False)
        self.p = Prog(nc)
        self.dram = {}
        self.rr = 0

    def din(self, name, shape, dt=F32):
        a = self.nc.dram_tensor(name, list(shape), dt, kind="ExternalInput").ap()
        self.dram[name] = a
        return a

    def setup(self):
        p, nc, ns = self.p, self.nc, self.nseq
        d = self.din
        d("x", [ns, SEQ, D]); d("cT", [ns, 128, 8]); d("pos", [ns, 128, SEQ], I32)
        d("ada", [2, 1025, 6 * D])
        d("gmix", [2, 128, 8]); d("gffn", [2, 128, 8]); d("gfin", [128, D])
        d("ffn_gu", [2, 11, 128, 8 * 256]); d("ffn_dn", [2, 1408, D])
        d("moe_gu", [8, 11, 128, 8 * 256]); d("moe_dn", [8, 1408, D]); d("moe_r", [128, 8, 8])
        d("mla_in", [128, 8, 896]); d("mla_qn", [128, 4]); d("mla_kvn", [128, 2])
        d("mla_qb", [128, 4, 2048]); d("mla_kb", [128, 2, 1024]); d("mla_vb", [128, 2, 1024])
        d("mla_out", [D, D]); d("ropec", [128, 2])
        d("gdn_in", [8, 128, 8, 512]); d("gdn_ba", [128, 8, 16]); d("gdn_conv", [128, 8, 3, 4])
        d("gdn_hp", [128, 24]); d("gdn_on", [128, 128]); d("gdn_out", [D, D])
        self.y = nc.dram_tensor("y", [ns, SEQ, D], F32, kind="ExternalOutput").ap()

        self.X = p.tile("X", [128, NT, D], F32)
        self.HT = p.tile("HT", [128, 8, SEQ], BF16)
        self.identb = p.tile("identb", [128, 128], BF16)
        self.identf = p.tile("identf", [128, 128], F32)
        self.onesb = p.tile("onesb", [128, 128], BF16)
        self.onesf = p.tile("onesf", [128, 128], F32)
        self.epsc = p.tile("epsc", [128, 1], F32)
        self.MODC = p.tile("MODC", [128, 2, 48, ns], F32)
        self.G = p.tile("G", [128, D], F32)
        self.Acol = p.tile("Acol", [128, 8], F32)
        self.gmix = p.tile("gmix", [128, 2, 8], F32)
        self.gffn = p.tile("gffn", [128, 2, 8], F32)
        self.GF = p.tile("GF", [128, D], F32)
        self.ssq = p.tile("ssq", [128, NT], F32)
        self.rstd = p.tile("rstd", [128, NT], F32)
        self.COMB = p.tile("COMB", [128, NT, 8], F32)
        self.PS = [nc.alloc_psum_tensor(f"ps{i}", [128, 512], F32) for i in range(8)]

        p.op("pool", lambda e: e.memset(self.identf[:], 1.0), writes=[self.identf])
        p.op("pool", lambda e: e.affine_select(out=self.identf[:], in_=self.identf[:], pattern=[[-1, 128]],
                                               compare_op=ALU.is_equal, fill=0.0, base=0, channel_multiplier=1),
             reads=[self.identf], writes=[self.identf])
        p.op("dve", lambda e: e.tensor_copy(self.identb[:], self.identf[:]), reads=[self.identf], writes=[self.identb])
        p.op("pool", lambda e: e.memset(self.onesb[:], 1.0), writes=[self.onesb])
        p.op("pool", lambda e: e.memset(self.onesf[:], 1.0), writes=[self.onesf])
        p.op("pool", lambda e: e.memset(self.epsc[:], EPS), writes=[self.epsc])
        p.dma("sp", lambda e: e.dma_start(out=self.gmix[:], in_=self.dram["gmix"].rearrange("l p k -> p l k")), writes=[self.gmix])
        p.dma("sp", lambda e: e.dma_start(out=self.gffn[:], in_=self.dram["gffn"].rearrange("l p k -> p l k")), writes=[self.gffn])
        p.dma("sp", lambda e: e.dma_start(out=self.GF[:], in_=self.dram["gfin"]), writes=[self.GF])

    def prologue_mod(self):
        p, ns = self.p, self.nseq
        m = p.mark()
        cT = p.tile("cTs", [128, ns, 8], F32)
        condb = p.tile("condb", [128, 8, ns], BF16)
        for s in range(ns):
            p.dma("sp", lambda e, s=s: e.dma_start(out=cT[:, s, :], in_=self.dram["cT"][s]), writes=[cT], key=("dma", "cT", s))
        for s in range(ns):
            p.op("act", lambda e, s=s: e.activation(out=condb[:, :, s], in_=cT[:, s, :], func=AF.Silu), reads=[cT], writes=[condb])
        W = [p.tile(f"adaw{i}", [128, 8, 512], BF16) for i in range(2)]
        Bv = [p.tile(f"adab{i}", [1, 512], BF16) for i in range(2)]
        ada = self.dram["ada"]
        it = 0
        for l in range(2):
            ps = self.PS[l]
            psv = ps[:, 0:48 * ns].rearrange("p (b s) -> p b s", s=ns)
            for ch in range(12):
                w, bv = W[it % 2], Bv[it % 2]
                it += 1
                cols = slice(ch * 512, (ch + 1) * 512)
                p.dma("pool", lambda e, w=w, l=l, cols=cols: e.dma_start(
                    out=w[:], in_=ada[l, 0:1024, cols].rearrange("(k p) n -> p k n", p=128)), writes=[w])
                p.dma("pool", lambda e, bv=bv, l=l, cols=cols: e.dma_start(out=bv[:], in_=ada[l, 1024:1025, cols]), writes=[bv])
                for qd in range(4):
                    blk = ch * 4 + qd
                    for k in range(8):
                        p.op("pe", lambda e, w=w, k=k, qd=qd, blk=blk, psv=psv: e.matmul(
                            psv[:, blk, :], lhsT=w[:, k, qd * 128:(qd + 1) * 128], rhs=condb[:, k, :], start=(k == 0), stop=False),
                            reads=[w, condb], writes=[ps])
                    p.op("pe", lambda e, bv=bv, qd=qd, blk=blk, psv=psv: e.matmul(
                        psv[:, blk, :], lhsT=bv[0:1, qd * 128:(qd + 1) * 128], rhs=self.onesb[0:1, 0:ns], start=False, stop=True),
                        reads=[bv, self.onesb], writes=[ps])
            p.op("act", lambda e, l=l, psv=psv: e.copy(self.MODC[:, l, :, :], psv), reads=[ps], writes=[self.MODC])
        p.release(m)

    def modc(self, l, s, j, k=None):
        if k is None:
            return self.MODC[:, l, j * 8:(j + 1) * 8, s]
        return self.MODC[:, l, j * 8 + k, s:s + 1]

    def make_gate(self, l, s, j):
        p = self.p
        m = p.mark()
        dg = [p.tile(f"dg{i}", [128, 128], F32) for i in range(2)]
        for k in range(8):
            t = dg[k % 2]
            ps = self.PS[k // 4]
            p.op("dve", lambda e, t=t, k=k: e.tensor_scalar(out=t[:], in0=self.identf[:], scalar1=self.modc(l, s, j, k), scalar2=None, op0=ALU.mult),
                 reads=[self.identf, self.MODC], writes=[t])
            p.op("pe", lambda e, t=t, k=k, ps=ps: e.matmul(ps[:, (k % 4) * 128:(k % 4 + 1) * 128], lhsT=self.onesf[:], rhs=t[:], start=True, stop=True),
                 reads=[t, self.onesf], writes=[ps])
        for h in range(2):
            p.op("act", lambda e, h=h: e.copy(self.G[:, h * 512:(h + 1) * 512], self.PS[h][:]), reads=[self.PS[h]], writes=[self.G])
        p.release(m)

    def load_x(self, s):
        p = self.p
        for t in range(NT):
            q = "sp" if t % 2 == 0 else "act"
            p.dma(q, lambda e, t=t: e.dma_start(out=self.X[:, t, :], in_=self.dram["x"][s, t * 128:(t + 1) * 128, :]),
                  writes=[("X", t)])

    def final_out(self, s):
        p = self.p
        m = p.mark()
        ob = [p.tile(f"ob{i}", [128, D], F32) for i in range(2)]
        junk = p.tile("junkf", [128, D], BF16)
        toks = []
        for t in range(NT):
            if t == 0 and getattr(self, "skip0", False):
                continue
            self.rms_stats(t, junk)
            o = ob[t % 2]
            p.op("dve", lambda e, t=t, o=o: e.scalar_tensor_tensor(out=o[:], in0=self.X[:, t, :], scalar=self.rstd[:, t:t + 1], in1=self.GF[:],
                                                                  op0=ALU.mult, op1=ALU.mult),
                 reads=[("X", t), ("rstd", t), self.GF], writes=[o])
            toks.append(p.dma("sp", lambda e, t=t, o=o: e.dma_start(out=self.y[s, t * 128:(t + 1) * 128, :], in_=o[:]), reads=[o]))
        p.release(m)
        return toks

    def rms_stats(self, t, junk):
        p = self.p
        p.op("pool", lambda e: e.memset(self.ssq[:, t:t + 1], 0.0), writes=[("ssq", t)])
        p.op("act", lambda e: e.activation(out=junk[:], in_=self.X[:, t, :], func=AF.Square, accum_out=self.ssq[:, t:t + 1]),
             reads=[("X", t)], writes=[junk, ("ssq", t)])
        p.op("act", lambda e: e.activation(out=self.ssq[:, t:t + 1], in_=self.ssq[:, t:t + 1], func=AF.Sqrt, bias=self.epsc[:], scale=1.0 / D),
             reads=[("ssq", t), self.epsc], writes=[("ssq", t)])
        p.op("dve", lambda e: e.reciprocal(out=self.rstd[:, t:t + 1], in_=self.ssq[:, t:t + 1]), reads=[("ssq", t)], writes=[("rstd", t)])

    def norm_phase(self, l, s, which, router=False):
        p = self.p
        m = p.mark()
        jsh, jsc = (0, 1) if which == 0 else (3, 4)
        gcol = (self.gmix if which == 0 else self.gffn)[:, l, :]
        p.op("dve", lambda e: e.tensor_scalar(out=self.Acol[:], in0=self.modc(l, s, jsc), scalar1=1.0, scalar2=None, op0=ALU.add),
             reads=[self.MODC], writes=[self.Acol])
        p.op("dve", lambda e: e.tensor_tensor(out=self.Acol[:], in0=self.Acol[:], in1=gcol, op=ALU.mult),
             reads=[self.Acol, self.gmix, self.gffn], writes=[self.Acol])
        junk = p.tile("junk", [128, D], BF16)
        xn = [p.tile(f"xn{i}", [128, D], BF16) for i in range(2)]
        if router:
            xnf = p.tile("xnf", [128, D], F32)
            hTf = p.tile("hTf", [128, 8, 128], F32)
            wr = p.tile("wr", [128, 8, 8], F32)
            lg = p.tile("lg", [128, 8], F32)
            mx = p.tile("mx", [128, 8], F32)
            sc = p.tile("rsc", [128, 4], F32)
            c1 = p.tile("c1", [128, 8], F32)
            p.dma("sp", lambda e: e.dma_start(out=wr[:], in_=self.dram["moe_r"]), writes=[wr])
        psT = [self.PS[i].bitcast(BF16) for i in range(4)]
        for blk in range(NB):
            for tt in range(4):
                t = blk * 4 + tt
                self.rms_stats(t, junk)
                x_ = xn[t % 2]
                p.op("dve", lambda e, t=t, x_=x_: e.tensor_scalar(out=x_[:], in0=self.X[:, t, :], scalar1=self.rstd[:, t:t + 1], scalar2=None, op0=ALU.mult),
                     reads=[("X", t), ("rstd", t)], writes=[x_])
                for k in range(8):
                    p.op("pe", lambda e, k=k, tt=tt, x_=x_: e.transpose(psT[k // 2][:, (k % 2) * 512 + tt * 128:(k % 2) * 512 + (tt + 1) * 128],
                                                                       x_[:, k * 128:(k + 1) * 128], self.identb[:]),
                         reads=[x_, self.identb], writes=[self.PS[k // 2]])
                if router:
                    p.op("dve", lambda e, t=t: e.tensor_scalar(out=xnf[:], in0=self.X[:, t, :], scalar1=self.rstd[:, t:t + 1], scalar2=None, op0=ALU.mult),
                         reads=[("X", t), ("rstd", t)], writes=[xnf])
                    for k in range(8):
                        ps = self.PS[4 + k // 4]
                        p.op("pe", lambda e, k=k, ps=ps: e.transpose(ps[:, (k % 4) * 128:(k % 4 + 1) * 128], xnf[:, k * 128:(k + 1) * 128], self.identf[:]),
                             reads=[xnf, self.identf], writes=[ps])
                    for k in range(8):
                        ps = self.PS[4 + k // 4]
                        p.op("act", lambda e, k=k, ps=ps: e.activation(out=hTf[:, k, :], in_=ps[:, (k % 4) * 128:(k % 4 + 1) * 128], func=AF.Identity,
                                                                      bias=self.modc(l, s, jsh, k), scale=self.Acol[:, k:k + 1]),
                             reads=[ps, self.Acol, self.MODC], writes=[hTf])
                    for k in range(8):
                        p.op("pe", lambda e, k=k: e.matmul(self.PS[6][:, 0:8], lhsT=hTf[:, k, :], rhs=wr[:, k, :], start=(k == 0), stop=(k == 7)),
                             reads=[hTf, wr], writes=[self.PS[6]])
                    p.op("act", lambda e: e.copy(lg[:], self.PS[6][:, 0:8]), reads=[self.PS[6]], writes=[lg])
                    p.op("dve", lambda e: e.max(out=mx[:], in_=lg[:]), reads=[lg], writes=[mx])
                    p.op("dve", lambda e: e.tensor_tensor(out=sc[:, 0:1], in0=mx[:, 1:2], in1=mx[:, 0:1], op=ALU.subtract), reads=[mx], writes=[sc])
                    p.op("act", lambda e: e.activation(out=sc[:, 0:1], in_=sc[:, 0:1], func=AF.Exp), reads=[sc], writes=[sc])
                    p.op("dve", lambda e: e.tensor_scalar(out=sc[:, 1:2], in0=sc[:, 0:1], scalar1=1.0, scalar2=None, op0=ALU.add), reads=[sc], writes=[sc])
                    p.op("dve", lambda e: e.reciprocal(out=sc[:, 1:2], in_=sc[:, 1:2]), reads=[sc], writes=[sc])
                    p.op("dve", lambda e: e.tensor_tensor(out=sc[:, 2:3], in0=sc[:, 0:1], in1=sc[:, 1:2], op=ALU.mult), reads=[sc], writes=[sc])
                    p.op("dve", lambda e: e.tensor_scalar(out=c1[:], in0=lg[:], scalar1=mx[:, 0:1], scalar2=sc[:, 1:2], op0=ALU.is_equal, op1=ALU.mult),
                         reads=[lg, mx, sc], writes=[c1])
                    p.op("dve", lambda e, t=t: e.tensor_scalar(out=self.COMB[:, t, :], in0=lg[:], scalar1=mx[:, 1:2], scalar2=sc[:, 2:3], op0=ALU.is_equal, op1=ALU.mult),
                         reads=[lg, mx, sc], writes=[("COMB", t)])
                    p.op("dve", lambda e, t=t: e.tensor_tensor(out=self.COMB[:, t, :], in0=self.COMB[:, t, :], in1=c1[:], op=ALU.add),
                         reads=[c1, ("COMB", t)], writes=[("COMB", t)])
            for k in range(8):
                src = psT[k // 2][:, (k % 2) * 512:(k % 2 + 1) * 512]
                dst = self.HT[:, k, blk * 512:(blk + 1) * 512]
                if k % 2 == 0:
                    p.op("act", lambda e, k=k, src=src, dst=dst: e.activation(out=dst, in_=src, func=AF.Identity, bias=self.modc(l, s, jsh, k),
                                                                            scale=self.Acol[:, k:k + 1]),
                         reads=[self.PS[k // 2], self.Acol, self.MODC], writes=[("HT", blk)])
                else:
                    p.op("dve", lambda e, k=k, src=src, dst=dst: e.tensor_scalar(out=dst, in0=src, scalar1=self.Acol[:, k:k + 1], scalar2=self.modc(l, s, jsh, k),
                                                                               op0=ALU.mult, op1=ALU.add),
                         reads=[self.PS[k // 2], self.Acol, self.MODC], writes=[("HT", blk)])
        p.release(m)

    def ffn_phase(self, experts):
        p = self.p
        m = p.mark()
        ACT_ = p.tile("actT", [128, 11, SEQ], BF16)
        WGU = [p.tile(f"wgu{i}", [128, 8, 256], BF16) for i in range(3)]
        WD = p.tile("wd", [128, 11, D], BF16)
        SG = [p.tile(f"sg{i}", [128, 512], BF16) for i in range(2)]
        HTr = [("HT", b) for b in range(NB)]
        it = 0
        for (gu, dn, ci) in experts:
            p.dma("pool", lambda e, dn=dn: e.dma_start(out=WD[:], in_=dn.rearrange("(c p) n -> p c n", p=128)), writes=[WD])
            for c in range(11):
                w = WGU[c % 3]
                p.dma("pool", lambda e, w=w, c=c, gu=gu: e.dma_start(out=w[:].rearrange("p k n -> p (k n)"), in_=gu[c]), writes=[w])
                for blk in range(NB):
                    psg, psu = self.PS[it % 2], self.PS[2 + it % 2]
                    sg = SG[it % 2]
                    it += 1
                    for k in range(8):
                        p.op("pe", lambda e, w=w, k=k, blk=blk, psg=psg: e.matmul(psg[:], lhsT=w[:, k, 0:128], rhs=self.HT[:, k, blk * 512:(blk + 1) * 512],
                                                                                  start=(k == 0), stop=(k == 7)), reads=[w, HTr[blk]], writes=[psg])
                    for k in range(8):
                        p.op("pe", lambda e, w=w, k=k, blk=blk, psu=psu: e.matmul(psu[:], lhsT=w[:, k, 128:256], rhs=self.HT[:, k, blk * 512:(blk + 1) * 512],
                                                                                  start=(k == 0), stop=(k == 7)), reads=[w, HTr[blk]], writes=[psu])
                    p.op("act", lambda e, sg=sg, psg=psg: e.activation(out=sg[:], in_=psg[:], func=AF.Silu), reads=[psg], writes=[sg])
                    p.op("dve", lambda e, sg=sg, psu=psu, c=c, blk=blk: e.tensor_tensor(out=ACT_[:, c, blk * 512:(blk + 1) * 512], in0=psu[:], in1=sg[:], op=ALU.mult),
                         reads=[sg, psu], writes=[("actT", c)])
                if c == 5:
                    for cc in range(11):
                        p.op("pool", lambda e, cc=cc: e.tensor_tensor(out=WD[:, cc, :], in0=WD[:, cc, :], in1=self.G[:], op=ALU.mult),
                             reads=[WD, self.G], writes=[WD])
            for t in range(NT):
                for hf in range(2):
                    ps = self.PS[4 + (2 * t + hf) % 4]
                    for c in range(11):
                        p.op("pe", lambda e, c=c, t=t, hf=hf, ps=ps: e.matmul(ps[:], lhsT=ACT_[:, c, t * 128:(t + 1) * 128], rhs=WD[:, c, hf * 512:(hf + 1) * 512],
                                                                              start=(c == 0), stop=(c == 10)), reads=[("actT", c), WD], writes=[ps])
                    xs = self.X[:, t, hf * 512:(hf + 1) * 512]
                    if ci is None:
                        p.op("dve", lambda e, xs=xs, ps=ps: e.tensor_tensor(out=xs, in0=ps[:], in1=xs, op=ALU.add), reads=[ps, ("X", t)], writes=[("X", t)])
                    else:
                        p.op("dve", lambda e, xs=xs, ps=ps, t=t, ci=ci: e.scalar_tensor_tensor(out=xs, in0=ps[:], scalar=self.COMB[:, t, ci:ci + 1], in1=xs,
                                                                                              op0=ALU.mult, op1=ALU.add),
                             reads=[ps, ("X", t), ("COMB", t)], writes=[("X", t)])
        p.release(m)


    def mla_phase(self, s):
        p = self.p
        PS = self.PS
        m0 = p.mark()
        QNT = p.tile("QNT", [128, 4, SEQ], BF16)
        KVNT = p.tile("KVNT", [128, 2, SEQ], BF16)
        ROPE = p.tile("ROPE", [128, SEQ], F32)
        KT = p.tile("KT", [128, SEQ], BF16)
        QT = p.tile("QT", [128, SEQ], BF16)
        VA = p.tile("VA", [128, NT, 128], BF16)
        PT = [p.tile(f"PT{i}", [128, 512], BF16) for i in range(4)]
        TRI = p.tile("TRI", [128, 128], BF16)
        tA = p.tile("tA", [128, 512], F32)
        tB = p.tile("tB", [128, 512], F32)
        rec = p.tile("rec", [128, 512], F32)
        qnc = p.tile("qnc", [128, 4], F32)
        kvnc = p.tile("kvnc", [128, 2], F32)
        ropec = p.tile("ropec", [128, 2], F32)
        st = p.tile("mst", [128, 4], F32)
        p.dma("sp", lambda e: e.dma_start(out=qnc[:], in_=self.dram["mla_qn"]), writes=[qnc])
        p.dma("sp", lambda e: e.dma_start(out=kvnc[:], in_=self.dram["mla_kvn"]), writes=[kvnc])
        p.dma("sp", lambda e: e.dma_start(out=ropec[:], in_=self.dram["ropec"]), writes=[ropec])
        p.op("pool", lambda e: e.memset(TRI[:], 1.0), writes=[TRI])
        p.op("pool", lambda e: e.affine_select(out=TRI[:], in_=TRI[:], pattern=[[1, 128]], compare_op=ALU.is_ge, fill=0.0, base=0, channel_multiplier=-1),
             reads=[TRI], writes=[TRI])
        p.op("pool", lambda e: e.memset(VA[:, :, 64:128], 1.0), writes=[("VA", 0), ("VA", 1)])
        m1 = p.mark()
        WIN = p.tile("WIN", [128, 8, 896], BF16)
        p.dma("pool", lambda e: e.dma_start(out=WIN[:], in_=self.dram["mla_in"]), writes=[WIN])
        posi = p.tile("posi", [128, 512], I32)
        u = p.tile("ropu", [128, 512], F32)
        ki = p.tile("ropk", [128, 512], I32)
        kf = p.tile("ropkf", [128, 512], F32)
        qtok = [p.tile(f"qtok{i}", [128, 512], BF16) for i in range(2)]
        kvtok = [p.tile(f"kvtok{i}", [128, 256], BF16) for i in range(2)]
        junk = p.tile("mjunk", [128, 512], BF16)
        for blk in range(NB):
            cs = slice(blk * 512, (blk + 1) * 512)
            p.dma("sp", lambda e, cs=cs: e.dma_start(out=posi[:], in_=self.dram["pos"][s, :, cs]), writes=[posi])
            p.op("dve", lambda e: e.tensor_copy(u[:], posi[:]), reads=[posi], writes=[u])
            p.op("dve", lambda e: e.tensor_scalar(out=u[:], in0=u[:], scalar1=ropec[:, 0:1], scalar2=ropec[:, 1:2], op0=ALU.mult, op1=ALU.add),
                 reads=[u, ropec], writes=[u])
            p.op("dve", lambda e: e.tensor_copy(ki[:], u[:]), reads=[u], writes=[ki])
            p.op("dve", lambda e: e.tensor_copy(kf[:], ki[:]), reads=[ki], writes=[kf])
            p.op("dve", lambda e: e.tensor_tensor(out=u[:], in0=u[:], in1=kf[:], op=ALU.subtract), reads=[u, kf], writes=[u])
            p.op("act", lambda e, cs=cs: e.activation(out=ROPE[:, cs], in_=u[:], func=AF.Sin, scale=2.0 * math.pi * (1.0 - 1e-6)),
                 reads=[u], writes=[("ROPE", blk)])
        psTb = PS[4].bitcast(BF16)
        for t in range(NT):
            ts_ = slice(t * 128, (t + 1) * 128)
            blk = t // 4
            pa, pb = PS[(t % 2) * 2], PS[(t % 2) * 2 + 1]
            for k in range(8):
                p.op("pe", lambda e, k=k, ts_=ts_, pa=pa: e.matmul(pa[:], lhsT=self.HT[:, k, ts_], rhs=WIN[:, k, 0:512], start=(k == 0), stop=(k == 7)),
                     reads=[("HT", blk), WIN], writes=[pa])
            for k in range(8):
                p.op("pe", lambda e, k=k, ts_=ts_, pb=pb: e.matmul(pb[:, 0:256], lhsT=self.HT[:, k, ts_], rhs=WIN[:, k, 512:768], start=(k == 0), stop=(k == 7)),
                     reads=[("HT", blk), WIN], writes=[pb])
            p.op("pool", lambda e: e.memset(st[:, 0:2], 0.0), writes=[st])
            p.op("act", lambda e, pa=pa: e.activation(out=junk[:], in_=pa[:], func=AF.Square, accum_out=st[:, 0:1]), reads=[pa], writes=[junk, st])
            p.op("act", lambda e, pb=pb: e.activation(out=junk[:, 0:256], in_=pb[:, 0:256], func=AF.Square, accum_out=st[:, 1:2]), reads=[pb], writes=[junk, st])
            p.op("act", lambda e: e.activation(out=st[:, 0:1], in_=st[:, 0:1], func=AF.Sqrt, bias=self.epsc[:], scale=1.0 / 512), reads=[st, self.epsc], writes=[st])
            p.op("act", lambda e: e.activation(out=st[:, 1:2], in_=st[:, 1:2], func=AF.Sqrt, bias=self.epsc[:], scale=1.0 / 256), reads=[st, self.epsc], writes=[st])
            p.op("dve", lambda e: e.reciprocal(out=st[:, 2:4], in_=st[:, 0:2]), reads=[st], writes=[st])
            qt_, kvt_ = qtok[t % 2], kvtok[t % 2]
            p.op("dve", lambda e, pa=pa, qt_=qt_: e.tensor_scalar(out=qt_[:], in0=pa[:], scalar1=st[:, 2:3], scalar2=None, op0=ALU.mult), reads=[pa, st], writes=[qt_])
            p.op("act", lambda e, pb=pb, kvt_=kvt_: e.activation(out=kvt_[:], in_=pb[:, 0:256], func=AF.Copy, scale=st[:, 3:4]), reads=[pb, st], writes=[kvt_])
            for c in range(4):
                p.op("pe", lambda e, c=c, qt_=qt_: e.transpose(psTb[:, c * 128:(c + 1) * 128], qt_[:, c * 128:(c + 1) * 128], self.identb[:]),
                     reads=[qt_, self.identb], writes=[PS[4]])
            for c in range(2):
                p.op("pe", lambda e, c=c, kvt_=kvt_: e.transpose(psTb[:, 512 + c * 128:512 + (c + 1) * 128], kvt_[:, c * 128:(c + 1) * 128], self.identb[:]),
                     reads=[kvt_, self.identb], writes=[PS[4]])
            for c in range(4):
                if c % 2 == 0:
                    p.op("act", lambda e, c=c, ts_=ts_: e.activation(out=QNT[:, c, ts_], in_=psTb[:, c * 128:(c + 1) * 128], func=AF.Copy, scale=qnc[:, c:c + 1]),
                         reads=[PS[4], qnc], writes=[("QNT", blk)])
                else:
                    p.op("dve", lambda e, c=c, ts_=ts_: e.tensor_scalar(out=QNT[:, c, ts_], in0=psTb[:, c * 128:(c + 1) * 128], scalar1=qnc[:, c:c + 1], scalar2=None, op0=ALU.mult),
                         reads=[PS[4], qnc], writes=[("QNT", blk)])
            for c in range(2):
                if c % 2 == 0:
                    p.op("act", lambda e, c=c, ts_=ts_: e.activation(out=KVNT[:, c, ts_], in_=psTb[:, 512 + c * 128:512 + (c + 1) * 128], func=AF.Copy, scale=kvnc[:, c:c + 1]),
                         reads=[PS[4], kvnc], writes=[("KVNT", blk)])
                else:
                    p.op("dve", lambda e, c=c, ts_=ts_: e.tensor_scalar(out=KVNT[:, c, ts_], in0=psTb[:, 512 + c * 128:512 + (c + 1) * 128], scalar1=kvnc[:, c:c + 1], scalar2=None, op0=ALU.mult),
                         reads=[PS[4], kvnc], writes=[("KVNT", blk)])
        for blk in range(NB):
            cs = slice(blk * 512, (blk + 1) * 512)
            ps = PS[5 + blk % 2]
            for k in range(8):
                p.op("pe", lambda e, k=k, cs=cs, ps=ps: e.matmul(ps[:], lhsT=WIN[:, k, 768:896], rhs=self.HT[:, k, cs], start=(k == 0), stop=(k == 7)),
                     reads=[("HT", blk), WIN], writes=[ps])
            p.op("dve", lambda e, cs=cs, ps=ps: e.tensor_tensor(out=tA[64:96, :], in0=ps[64:96, :], in1=ROPE[64:96, cs], op=ALU.mult), reads=[ps, ("ROPE", blk)], writes=[tA])
            p.op("dve", lambda e, cs=cs, ps=ps: e.tensor_tensor(out=tB[64:96, :], in0=ps[96:128, :], in1=ROPE[96:128, cs], op=ALU.mult), reads=[ps, ("ROPE", blk)], writes=[tB])
            p.op("pool", lambda e, cs=cs: e.tensor_tensor(out=KT[64:96, cs], in0=tA[64:96, :], in1=tB[64:96, :], op=ALU.add), reads=[tA, tB], writes=[("KTr", blk)])
        p.release(m1)
        m2 = p.mark()
        WQB = p.tile("WQB", [128, 4, 2048], BF16)
        WKB = p.tile("WKB", [128, 2, 1024], BF16)
        WVB = p.tile("WVB", [128, 2, 1024], BF16)
        p.dma("pool", lambda e: e.dma_start(out=WQB[:], in_=self.dram["mla_qb"]), writes=[WQB])
        p.dma("pool", lambda e: e.dma_start(out=WKB[:], in_=self.dram["mla_kb"]), writes=[WKB])
        p.dma("pool", lambda e: e.dma_start(out=WVB[:], in_=self.dram["mla_vb"]), writes=[WVB])
        OT = self.HT
        scale = 96.0 ** -0.5
        si = 0
        oi = 0
        pi_ = 0
        for h in range(16):
            for blk in range(NB):
                cs = slice(blk * 512, (blk + 1) * 512)
                ps = PS[blk % 2]
                for c in range(4):
                    p.op("pe", lambda e, c=c, cs=cs, ps=ps, h=h: e.matmul(ps[:], lhsT=WQB[:, c, h * 128:(h + 1) * 128], rhs=QNT[:, c, cs], start=(c == 0), stop=(c == 3)),
                         reads=[WQB, ("QNT", blk)], writes=[ps])
                p.op("act", lambda e, cs=cs, ps=ps: e.copy(QT[0:64, cs], ps[0:64, :]), reads=[ps], writes=[("QT", blk)])
                p.op("dve", lambda e, cs=cs, ps=ps: e.tensor_tensor(out=tA[64:96, :], in0=ps[64:96, :], in1=ROPE[64:96, cs], op=ALU.mult), reads=[ps, ("ROPE", blk)], writes=[tA])
                p.op("dve", lambda e, cs=cs, ps=ps: e.tensor_tensor(out=tB[64:96, :], in0=ps[96:128, :], in1=ROPE[96:128, cs], op=ALU.mult), reads=[ps, ("ROPE", blk)], writes=[tB])
                p.op("pool", lambda e, cs=cs: e.tensor_tensor(out=QT[64:96, cs], in0=tA[64:96, :], in1=tB[64:96, :], op=ALU.add), reads=[tA, tB], writes=[("QT", blk)])
            for blk in range(NB):
                cs = slice(blk * 512, (blk + 1) * 512)
                ps = PS[blk % 2]
                for c in range(2):
                    p.op("pe", lambda e, c=c, cs=cs, ps=ps, h=h: e.matmul(ps[0:64, :], lhsT=WKB[:, c, h * 64:(h + 1) * 64], rhs=KVNT[:, c, cs], start=(c == 0), stop=(c == 1)),
                         reads=[WKB, ("KVNT", blk)], writes=[ps])
                p.op("act", lambda e, cs=cs, ps=ps: e.copy(KT[0:64, cs], ps[0:64, :]), reads=[ps], writes=[("KTn", blk)])
            for half in range(2):
                ps = PS[half]
                psv = ps[:].rearrange("p (a b) -> p a b", b=64)
                for kk in range(8):
                    kt = half * 8 + kk
                    for c in range(2):
                        p.op("pe", lambda e, c=c, kt=kt, kk=kk, psv=psv, h=h: e.matmul(psv[:, kk, :], lhsT=KVNT[:, c, kt * 128:(kt + 1) * 128], rhs=WVB[:, c, h * 64:(h + 1) * 64],
                                                                                   start=(c == 0), stop=(c == 1)), reads=[WVB, ("KVNT", kt // 4)], writes=[ps])
                p.op("dve", lambda e, half=half, psv=psv: e.tensor_copy(VA[:, half * 8:(half + 1) * 8, 0:64], psv), reads=[ps], writes=[("VA", half)])
            for qb in range(NB):
                pso = PS[5 + oi % 2]
                oi += 1
                last = 4 * qb + 3
                for kt in range(last + 1):
                    col0 = max(0, kt - 4 * qb) * 128
                    pss = PS[2 + si % 3]
                    si += 1
                    pt = PT[pi_ % 4]
                    pi_ += 1
                    p.op("pe", lambda e, kt=kt, col0=col0, pss=pss, qb=qb: e.matmul(pss[:, col0:512], lhsT=KT[0:96, kt * 128:(kt + 1) * 128],
                                                                                   rhs=QT[0:96, qb * 512 + col0:(qb + 1) * 512], start=True, stop=True),
                         reads=[("KTn", kt // 4), ("KTr", kt // 4), ("QT", qb)], writes=[pss])
                    p.op("act", lambda e, col0=col0, pss=pss, pt=pt: e.activation(out=pt[:, col0:512], in_=pss[:, col0:512], func=AF.Exp, scale=scale),
                         reads=[pss], writes=[pt])
                    if kt >= 4 * qb:
                        dc = (kt - 4 * qb) * 128
                        p.op("pool", lambda e, dc=dc, pt=pt: e.tensor_tensor(out=pt[:, dc:dc + 128], in0=pt[:, dc:dc + 128], in1=TRI[:], op=ALU.mult),
                             reads=[pt, TRI], writes=[pt])
                    p.op("pe", lambda e, kt=kt, col0=col0, pso=pso, pt=pt: e.matmul(pso[:, col0:512], lhsT=VA[:, kt, :], rhs=pt[:, col0:512], start=(kt == 0), stop=(kt == last)),
                         reads=[("VA", kt // 8), pt], writes=[pso])
                p.op("dve", lambda e, pso=pso: e.reciprocal(out=rec[64:128, :], in_=pso[64:128, :]), reads=[pso], writes=[rec])
                r0 = (h % 2) * 64
                p.op("dve", lambda e, pso=pso, r0=r0, h=h, qb=qb: e.tensor_tensor(out=OT[r0:r0 + 64, h // 2, qb * 512:(qb + 1) * 512], in0=pso[0:64, :], in1=rec[64:128, :], op=ALU.mult),
                     reads=[pso, rec], writes=[("OT", qb)])
        p.release(m2)
        WOUT = p.tile("WOUT", [128, 8, D], BF16)
        p.dma("pool", lambda e: e.dma_start(out=WOUT[:], in_=self.dram["mla_out"].rearrange("(c p) n -> p c n", p=128)), writes=[WOUT])
        for c in range(8):
            p.op("pool", lambda e, c=c: e.tensor_tensor(out=WOUT[:, c, :], in0=WOUT[:, c, :], in1=self.G[:], op=ALU.mult), reads=[WOUT, self.G], writes=[WOUT])
        self.out_proj(OT, WOUT)
        p.release(m0)

    def out_proj(self, OT, WOUT):
        p = self.p
        for t in range(NT):
            for hf in range(2):
                ps = self.PS[(2 * t + hf) % 4]
                for c in range(8):
                    p.op("pe", lambda e, c=c, t=t, hf=hf, ps=ps: e.matmul(ps[:], lhsT=OT[:, c, t * 128:(t + 1) * 128], rhs=WOUT[:, c, hf * 512:(hf + 1) * 512],
                                                                          start=(c == 0), stop=(c == 7)), reads=[("OT", t // 4), WOUT], writes=[ps])
                xs = self.X[:, t, hf * 512:(hf + 1) * 512]
                p.op("dve", lambda e, xs=xs, ps=ps: e.tensor_tensor(out=xs, in0=ps[:], in1=xs, op=ALU.add), reads=[ps, ("X", t)], writes=[("X", t)])


    def mm(self, out, lhsT, rhs, start, stop, reads, writes):
        self.p.op("pe", lambda e: e.matmul(out, lhsT=lhsT, rhs=rhs, start=start, stop=stop), reads=reads, writes=writes)

    def tr(self, out, in_, ident, reads, writes):
        self.p.op("pe", lambda e: e.transpose(out, in_, ident), reads=list(reads) + [ident], writes=writes)

    def gdn_phase(self, s):
        p = self.p
        PS = self.PS
        PSb = [b.bitcast(BF16) for b in PS]
        HTr = [("HT", b) for b in range(NB)]
        m0 = p.mark()
        NEG = -1.0e30
        LTRI = p.tile("LTRI", [128, 128], F32)
        BDm = p.tile("BDm", [128, 128], F32)
        SEL = [p.tile(f"SEL{i}", [128, 128], F32) for i in range(2)]
        MNi = p.tile("MNi", [128, 128], F32)
        MNs = p.tile("MNs", [128, 128], F32)
        hp = p.tile("hp", [128, 24], F32)
        nA = p.tile("nA", [128, 8], F32)
        GON = p.tile("GON", [128, 128], F32)
        WBA = p.tile("WBA", [128, 8, 16], BF16)
        CW = p.tile("CW", [128, 8, 3, 4], F32)
        pl = lambda fn, r=(), w=(): p.op("pool", fn, reads=r, writes=w)
        pl(lambda e: e.memset(LTRI[:], 1.0), w=[LTRI])
        pl(lambda e: e.affine_select(out=LTRI[:], in_=LTRI[:], pattern=[[1, 128]], compare_op=ALU.is_ge, fill=0.0, base=0, channel_multiplier=-1), r=[LTRI], w=[LTRI])
        pl(lambda e: e.memset(LTRI[0:64, 64:128], 0.0), r=[LTRI], w=[LTRI])
        pl(lambda e: e.memset(BDm[:], 0.0), w=[BDm])
        pl(lambda e: e.memset(BDm[0:64, 0:64], 1.0), r=[BDm], w=[BDm])
        pl(lambda e: e.memset(BDm[64:128, 64:128], 1.0), r=[BDm], w=[BDm])
        for i in range(2):
            pl(lambda e, i=i: e.memset(SEL[i][:], 0.0), w=[SEL[i]])
            pl(lambda e, i=i: e.memset(SEL[i][64 * i:64 * i + 1, :], 1.0), r=[SEL[i]], w=[SEL[i]])
        pl(lambda e: e.memset(MNi[:], 0.0), w=[MNi])
        pl(lambda e: e.affine_select(out=MNi[:], in_=MNi[:], pattern=[[1, 128]], compare_op=ALU.is_ge, fill=NEG, base=0, channel_multiplier=-1), r=[MNi], w=[MNi])
        pl(lambda e: e.memset(MNi[0:64, 64:128], NEG), r=[MNi], w=[MNi])
        pl(lambda e: e.memset(MNs[:], 0.0), w=[MNs])
        pl(lambda e: e.affine_select(out=MNs[:], in_=MNs[:], pattern=[[1, 128]], compare_op=ALU.is_gt, fill=NEG, base=0, channel_multiplier=-1), r=[MNs], w=[MNs])
        pl(lambda e: e.memset(MNs[0:64, 64:128], NEG), r=[MNs], w=[MNs])
        p.dma("sp", lambda e: e.dma_start(out=hp[:], in_=self.dram["gdn_hp"]), writes=[hp])
        p.dma("sp", lambda e: e.dma_start(out=GON[:], in_=self.dram["gdn_on"]), writes=[GON])
        p.dma("sp", lambda e: e.dma_start(out=CW[:], in_=self.dram["gdn_conv"]), writes=[CW])
        p.dma("pool", lambda e: e.dma_start(out=WBA[:], in_=self.dram["gdn_ba"]), writes=[WBA])
        p.op("act", lambda e: e.activation(out=nA[:], in_=hp[:, 0:8], func=AF.Exp), reads=[hp], writes=[nA])
        p.op("dve", lambda e: e.tensor_scalar(out=nA[:], in0=nA[:], scalar1=-1.0, scalar2=None, op0=ALU.mult), reads=[nA], writes=[nA])
        BETA = p.tile("BETA", [128, NT, 8], F32)
        GAMS = p.tile("GAMS", [128, NT, 8], F32)
        EGL = p.tile("EGL", [128, NT, 8], F32)
        BG = p.tile("BG", [128, NT, 8], F32)
        DECB = p.tile("DECB", [128, NT, 2, 8], F32)
        GC3 = p.tile("GC3", [128, NT, 3, 8], F32)
        m1 = p.mark()
        ba = p.tile("ba", [128, 16], F32)
        xs = p.tile("gxs", [128, 8], F32)
        ax = p.tile("gax", [128, 8], F32)
        gg = p.tile("ggg", [128, 8], F32)
        lnb = p.tile("lnb", [128, 8], F32)
        gcl = p.tile("gcl", [128, 2, 8], F32)
        dk_s = 128.0 ** -0.5
        import os
        pre = int(os.environ.get("GDN_PRE", "99"))
        if "GDN_OPS" in os.environ:
            p.limit = int(os.environ["GDN_OPS"])
        for t in range(NT if pre >= 2 else pre):
            ts_ = slice(t * 128, (t + 1) * 128)
            ps = PS[t % 2]
            for k in range(8):
                self.mm(ps[:, 0:16], self.HT[:, k, ts_], WBA[:, k, :], k == 0, k == 7, [HTr[t // 4], WBA], [ps])
            p.op("act", lambda e, ps=ps: e.copy(ba[:], ps[:, 0:16]), reads=[ps], writes=[ba])
            p.op("act", lambda e, t=t: e.activation(out=BETA[:, t, :], in_=ba[:, 0:8], func=AF.Sigmoid), reads=[ba], writes=[BETA])
            p.op("act", lambda e, t=t: e.activation(out=lnb[:], in_=BETA[:, t, :], func=AF.Ln), reads=[BETA], writes=[lnb])
            p.op("dve", lambda e: e.tensor_tensor(out=xs[:], in0=ba[:, 8:16], in1=hp[:, 8:16], op=ALU.add), reads=[ba, hp], writes=[xs])
            p.op("act", lambda e: e.activation(out=ax[:], in_=xs[:], func=AF.Abs), reads=[xs], writes=[ax])
            p.op("act", lambda e: e.activation(out=ax[:], in_=ax[:], func=AF.Exp, scale=-1.0), reads=[ax], writes=[ax])
            p.op("act", lambda e: e.activation(out=ax[:], in_=ax[:], func=AF.Ln, bias=self.onesf[:, 0:1], scale=1.0), reads=[ax, self.onesf], writes=[ax])
            p.op("dve", lambda e: e.scalar_tensor_tensor(out=gg[:], in0=xs[:], scalar=0.0, in1=ax[:], op0=ALU.max, op1=ALU.add), reads=[xs, ax], writes=[gg])
            p.op("dve", lambda e: e.tensor_tensor(out=gg[:], in0=gg[:], in1=nA[:], op=ALU.mult), reads=[gg, nA], writes=[gg])
            self.mm(ps[:, 16:24], LTRI[:], gg[:], True, True, [LTRI, gg], [ps])
            self.mm(ps[:, 24:32], BDm[:], gg[:], True, True, [BDm, gg], [ps])
            p.op("act", lambda e, ps=ps: e.copy(gcl[:].rearrange("p a b -> p (a b)"), ps[:, 16:32]), reads=[ps], writes=[gcl])
            p.op("act", lambda e, t=t: e.activation(out=BG[:, t, :], in_=gcl[:, 0, :], func=AF.Exp), reads=[gcl], writes=[BG])
            p.op("dve", lambda e, t=t: e.tensor_scalar(out=GAMS[:, t, :], in0=BG[:, t, :], scalar1=dk_s, scalar2=None, op0=ALU.mult), reads=[BG], writes=[GAMS])
            p.op("dve", lambda e, t=t: e.tensor_tensor(out=BG[:, t, :], in0=BG[:, t, :], in1=BETA[:, t, :], op=ALU.mult), reads=[BG, BETA], writes=[BG])
            p.op("dve", lambda e, t=t: e.tensor_tensor(out=EGL[:, t, :], in0=gcl[:, 1, :], in1=gcl[:, 0, :], op=ALU.subtract), reads=[gcl], writes=[EGL])
            p.op("act", lambda e, t=t: e.activation(out=EGL[:, t, :], in_=EGL[:, t, :], func=AF.Exp), reads=[EGL], writes=[EGL])
            p.op("dve", lambda e, t=t: e.tensor_copy(GC3[:, t, 0, :], gcl[:, 0, :]), reads=[gcl], writes=[GC3])
            p.op("dve", lambda e, t=t: e.tensor_scalar(out=GC3[:, t, 1, :], in0=gcl[:, 0, :], scalar1=-1.0, scalar2=None, op0=ALU.mult), reads=[gcl, GC3], writes=[GC3])
            p.op("dve", lambda e, t=t: e.tensor_tensor(out=GC3[:, t, 2, :], in0=gcl[:, 0, :], in1=lnb[:], op=ALU.add), reads=[gcl, lnb, GC3], writes=[GC3])
            for hf in range(2):
                self.mm(ps[:, 32 + hf * 8:40 + hf * 8], SEL[hf][:], gcl[:, 1, :], True, True, [SEL[hf], gcl], [ps])
            p.op("act", lambda e, t=t, ps=ps: e.activation(out=DECB[:, t, :, :].rearrange("p a b -> p (a b)"), in_=ps[:, 32:48], func=AF.Exp), reads=[ps], writes=[DECB])
        p.limit = None
        p.release(m1)
        NPAR = int(os.environ.get("GDN_NPAR", "2"))
        HB = []
        for sl in range(NPAR):
            b = {}
            b["WH"] = p.tile(f"WH{sl}", [128, 8, 512], BF16)
            b["WO"] = p.tile(f"WO{sl}", [128, D], BF16)
            b["DG"] = p.tile(f"DG{sl}", [128, 12, 128], BF16)
            b["S"] = p.tile(f"S{sl}", [128, 128], F32)
            b["Sb"] = p.tile(f"Sb{sl}", [128, 128], BF16)
            b["XC"] = p.tile(f"XC{sl}", [128, 3, 515], BF16)
            for nm, shp, dt in (("ZS", [128, 128], F32), ("GZ", [128, 128], F32), ("QKV", [128, 3, 128], F32), ("SQ", [128, 2, 128], F32),
                                ("st", [128, 8], F32), ("TOK", [128, 6, 128], BF16), ("DGC", [128, 2, 128], F32), ("KQT", [128, 3, 128], BF16), ("DT2", [128, 2, 128], F32),
                                ("M2", [128, 2, 128], F32), ("ATb", [128, 128], BF16), ("YA0", [128, 2, 128], F32), ("YA1", [128, 2, 128], F32),
                                ("P0", [128, 128], F32), ("P1", [128, 128], F32), ("TTb", [128, 128], BF16), ("U", [128, 128], F32),
                                ("WTb", [128, 128], BF16), ("VNb", [128, 128], BF16), ("OT", [128, 128], F32), ("OG", [128, 128], BF16),
                                ("OGT", [128, 128], BF16), ("junk", [128, 128], BF16)):
                b[nm] = p.tile(f"{nm}{sl}", shp, dt)
            b["bank"] = [PS[4 * sl + i] for i in range(4)]
            b["bankb"] = [PSb[4 * sl + i] for i in range(4)]
            HB.append(b)

        def head_gen(h, b):
            sl = HB.index(b)
            WH, WO, DG, S, Sb, XC = b["WH"], b["WO"], b["DG"], b["S"], b["Sb"], b["XC"]
            bk, bkb = b["bank"], b["bankb"]
            R = lambda i, nm: (f"ps{sl}_{i}")
            r_zc = [R(0, "zc")]
            r_tr, r_d, r_og = [R(1, "tr")], [R(1, "d")], [R(1, "og")]
            r_kk, r_a, r_p = [R(2, "kk")], [R(2, "a")], [R(2, "p")]
            r_ya, r_u, r_wt = [R(3, "ya")], [R(3, "u")], [R(3, "wt")]
            allb = [r_zc, r_tr, r_kk, r_ya]
            p.dma("pool", lambda e: e.dma_start(out=WH[:].rearrange("p k n -> p (k n)"), in_=self.dram["gdn_in"][h].rearrange("p k n -> p (k n)")), writes=[WH])
            p.dma("pool", lambda e: e.dma_start(out=WO[:], in_=self.dram["gdn_out"][h * 128:(h + 1) * 128, :]), writes=[WO])
            p.op("pool", lambda e: e.tensor_tensor(out=WO[:], in0=WO[:], in1=self.G[:], op=ALU.mult), reads=[WO, self.G], writes=[WO])
            for part in range(3):
                for j in range(4):
                    p.op("dve", lambda e, part=part, j=j: e.tensor_scalar(out=DG[:, part * 4 + j, :], in0=self.identb[:], scalar1=CW[:, h, part, j:j + 1], scalar2=None, op0=ALU.mult),
                         reads=[self.identb, CW], writes=[DG])
            p.op("pool", lambda e: e.memset(S[:], 0.0), writes=[S])
            p.op("pool", lambda e: e.memset(Sb[:], 0.0), writes=[Sb])
            p.op("pool", lambda e: e.memset(XC[:, :, 0:3], 0.0), writes=[XC])
            yield
            for blk in range(NB):
                cs = slice(blk * 512, (blk + 1) * 512)
                for part in range(3):
                    ps = bk[part]
                    for k in range(8):
                        self.mm(ps[:], WH[:, k, part * 128:(part + 1) * 128], self.HT[:, k, cs], k == 0, k == 7, [WH, HTr[blk]], allb[part])
                    eng = ("act", "dve", "act")[part]
                    if eng == "act":
                        p.op("act", lambda e, part=part, ps=ps: e.copy(XC[:, part, 3:515], ps[:]), reads=allb[part], writes=[XC])
                    else:
                        p.op("dve", lambda e, part=part, ps=ps: e.tensor_copy(XC[:, part, 3:515], ps[:]), reads=allb[part], writes=[XC])
                yield
                for tt in range(4):
                    t = blk * 4 + tt
                    ts_ = slice(t * 128, (t + 1) * 128)
                    ZS, GZ, QKV, SQ, st, TOK, KQT, DT2, M2, ATb = (b[n] for n in ("ZS", "GZ", "QKV", "SQ", "st", "TOK", "KQT", "DT2", "M2", "ATb"))
                    YA, Pb, TTb, U, WTb, VNb, OTk, OG, OGT = [b["YA0"], b["YA1"]], [b["P0"], b["P1"]], b["TTb"], b["U"], b["WTb"], b["VNb"], b["OT"], b["OG"], b["OGT"]
                    for k in range(8):
                        self.mm(bk[0][:, 0:128], self.HT[:, k, ts_], WH[:, k, 384:512], k == 0, k == 7, [WH, HTr[blk]], r_zc)
                    for part in range(3):
                        for j in range(4):
                            self.mm(bk[0][:, 128 + part * 128:256 + part * 128], XC[:, part, tt * 128 + j:tt * 128 + j + 128], DG[:, part * 4 + j, :], j == 0, j == 3, [XC, DG], r_zc)
                    p.op("act", lambda e: e.activation(out=ZS[:], in_=bk[0][:, 0:128], func=AF.Silu), reads=r_zc, writes=[ZS])
                    p.op("act", lambda e: e.activation(out=QKV[:].rearrange("p a b -> p (a b)"), in_=bk[0][:, 128:512], func=AF.Silu), reads=r_zc, writes=[QKV])
                    p.op("pool", lambda e: e.tensor_tensor(out=GZ[:], in0=ZS[:], in1=GON[:], op=ALU.mult), reads=[ZS, GON], writes=[GZ])
                    yield
                    if "GDN_OPS2" in os.environ and h == 0 and t == 0:
                        p.limit = int(os.environ["GDN_OPS2"])
                    p.op("dve", lambda e: e.tensor_tensor(out=SQ[:], in0=QKV[:, 0:2, :], in1=QKV[:, 0:2, :], op=ALU.mult), reads=[QKV], writes=[SQ])
                    p.op("dve", lambda e: e.tensor_reduce(out=st[:, 0:2], in_=SQ[:], axis=AX.X, op=ALU.add), reads=[SQ], writes=[st])
                    p.op("act", lambda e: e.activation(out=st[:, 0:2], in_=st[:, 0:2], func=AF.Sqrt, bias=self.epsc[:], scale=1.0), reads=[st, self.epsc], writes=[st])
                    p.op("dve", lambda e: e.reciprocal(out=st[:, 2:4], in_=st[:, 0:2]), reads=[st], writes=[st])
                    p.op("act", lambda e: e.activation(out=TOK[:, 0, :], in_=QKV[:, 1, :], func=AF.Copy, scale=st[:, 3:4]), reads=[QKV, st], writes=[(TOK.name, 0)])
                    p.op("dve", lambda e: e.tensor_scalar(out=TOK[:, 1, :], in0=QKV[:, 0, :], scalar1=st[:, 2:3], scalar2=dk_s, op0=ALU.mult, op1=ALU.mult), reads=[QKV, st], writes=[(TOK.name, 0)])
                    p.op("dve", lambda e, t=t: e.tensor_scalar(out=TOK[:, 2, :], in0=QKV[:, 0, :], scalar1=st[:, 2:3], scalar2=GAMS[:, t, h:h + 1], op0=ALU.mult, op1=ALU.mult), reads=[QKV, st, GAMS], writes=[(TOK.name, 0)])
                    p.op("dve", lambda e, t=t: e.tensor_scalar(out=TOK[:, 3, :], in0=QKV[:, 1, :], scalar1=st[:, 3:4], scalar2=EGL[:, t, h:h + 1], op0=ALU.mult, op1=ALU.mult), reads=[QKV, st, EGL], writes=[(TOK.name, 1)])
                    p.op("dve", lambda e, t=t: e.tensor_scalar(out=TOK[:, 4, :], in0=QKV[:, 1, :], scalar1=st[:, 3:4], scalar2=BG[:, t, h:h + 1], op0=ALU.mult, op1=ALU.mult), reads=[QKV, st, BG], writes=[(TOK.name, 1)])
                    p.op("act", lambda e, t=t: e.activation(out=TOK[:, 5, :], in_=QKV[:, 2, :], func=AF.Copy, scale=BETA[:, t, h:h + 1]), reads=[QKV, BETA], writes=[(TOK.name, 1)])
                    for i in range(3):
                        self.tr(bkb[1][:, i * 128:(i + 1) * 128], TOK[:, i, :], self.identb[:], [(TOK.name, 0)], r_tr)
                    p.op("act", lambda e: e.copy(KQT[:].rearrange("p a b -> p (a b)"), bkb[1][:, 0:384]), reads=r_tr, writes=[KQT])
                    d0 = 256
                    DGC = b["DGC"]
                    p.op("act", lambda e, t=t: e.activation(out=DGC[:, 0, :], in_=self.identf[:], func=AF.Copy, scale=GC3[:, t, 0, h:h + 1]), reads=[self.identf, GC3], writes=[DGC])
                    p.op("act", lambda e, t=t: e.activation(out=DGC[:, 1, :], in_=self.identf[:], func=AF.Copy, scale=GC3[:, t, 2, h:h + 1]), reads=[self.identf, GC3], writes=[DGC])
                    self.mm(bk[1][:, d0:d0 + 128], self.onesf[:], DGC[:, 0, :], True, False, [self.onesf, DGC], r_d)
                    self.mm(bk[1][:, d0:d0 + 128], self.identf[:], MNi[:], False, True, [self.identf, MNi], r_d)
                    self.mm(bk[1][:, d0 + 128:d0 + 256], self.onesf[:], DGC[:, 1, :], True, False, [self.onesf, DGC], r_d)
                    self.mm(bk[1][:, d0 + 128:d0 + 256], self.identf[:], MNs[:], False, True, [self.identf, MNs], r_d)
                    p.op("act", lambda e, t=t: e.activation(out=DT2[:].rearrange("p a b -> p (a b)"), in_=bk[1][:, d0:d0 + 256], func=AF.Exp, bias=GC3[:, t, 1, h:h + 1], scale=1.0),
                         reads=r_d + [GC3], writes=[DT2])
                    yield
                    self.mm(bk[2][:, 0:128], KQT[:, 0, :], KQT[:, 1, :], True, True, [KQT], r_kk)
                    self.mm(bk[2][:, 128:256], KQT[:, 0, :], KQT[:, 0, :], True, True, [KQT], r_kk)
                    p.op("dve", lambda e: e.tensor_tensor(out=M2[:].rearrange("p a b -> p (a b)"), in0=bk[2][:, 0:256], in1=DT2[:].rearrange("p a b -> p (a b)"), op=ALU.mult),
                         reads=r_kk + [DT2], writes=[M2])
                    p.op("act", lambda e: e.copy(ATb[:], M2[:, 0, :]), reads=[M2], writes=[ATb])
                    Y = M2[:, 1, :]
                    self.tr(bk[2][:, 256:384], Y, self.identf[:], [M2], r_a)
                    p.op("act", lambda e: e.copy(YA[0][:, 1, :], bk[2][:, 256:384]), reads=r_a, writes=[(YA[0].name, 1)])
                    p.op("pool", lambda e: e.tensor_copy(YA[0][:, 0, :], Y), reads=[M2], writes=[(YA[0].name, 0)])
                    p.op("pool", lambda e: e.tensor_tensor(out=Pb[0][:], in0=self.identf[:], in1=Y, op=ALU.subtract), reads=[M2, self.identf], writes=[Pb[0]])
                    yield
                    for it in range(5):
                        cur, nxt = YA[it % 2], YA[(it + 1) % 2]
                        pc, pn = Pb[it % 2], Pb[(it + 1) % 2]
                        rc = [(cur.name, 0), (cur.name, 1)]
                        if it < 4:
                            self.mm(bk[3][:, 0:128], cur[:, 1, :], cur[:, 0, :], True, True, rc, r_ya)
                        self.mm(bk[3][:, 128:256], cur[:, 0, :], cur[:, 1, :], True, True, rc, r_ya)
                        if it < 4:
                            p.op("act", lambda e, nxt=nxt: e.copy(nxt[:].rearrange("p a b -> p (a b)"), bk[3][:, 0:256]), reads=r_ya, writes=[(nxt.name, 0), (nxt.name, 1)])
                        else:
                            p.op("act", lambda e, nxt=nxt: e.copy(nxt[:, 1, :], bk[3][:, 128:256]), reads=r_ya, writes=[(nxt.name, 1)])
                        self.mm(bk[2][:, 384:512], self.identf[:], pc[:], True, False, [self.identf, pc], r_p)
                        self.mm(bk[2][:, 384:512], nxt[:, 1, :], pc[:], False, True, [(nxt.name, 1), pc], r_p)
                        if it < 4:
                            p.op("dve", lambda e, pn=pn: e.tensor_copy(pn[:], bk[2][:, 384:512]), reads=r_p, writes=[pn])
                        else:
                            p.op("dve", lambda e: e.tensor_copy(TTb[:], bk[2][:, 384:512]), reads=r_p, writes=[TTb])
                        yield
                    if "GDN_OPS3" in os.environ and h == 1 and t == 0:
                        p.limit = int(os.environ["GDN_OPS3"])
                    self.mm(bk[3][:, 256:384], TTb[:], TOK[:, 5, :], True, True, [TTb, (TOK.name, 1)], r_u)
                    self.mm(bk[0][:, 0:128], TOK[:, 4, :], TTb[:], True, True, [TTb, (TOK.name, 1)], r_zc)
                    p.op("act", lambda e: e.copy(U[:], bk[3][:, 256:384]), reads=r_u, writes=[U])
                    p.op("dve", lambda e: e.tensor_copy(WTb[:], bk[0][:, 0:128]), reads=r_zc, writes=[WTb])
                    yield
                    for hf in range(2):
                        r0 = hf * 64
                        rr = slice(r0, r0 + 64)
                        if "GDN_OPS4" in os.environ and h == int(os.environ.get("GDN_H", "0")) and t == 0 and hf == 0:
                            p.limit = int(os.environ["GDN_OPS4"])
                        if os.environ.get("GDN_M128") == "1":
                            self.mm(bk[2][:, 0:128], WTb[:, :], Sb[:], True, True, [WTb, Sb], r_kk)
                        elif os.environ.get("GDN_M128") == "2":
                            self.mm(bk[2][0:64, 0:128], KQT[:, 2, rr], Sb[:], True, True, [KQT, Sb], r_kk)
                        elif os.environ.get("GDN_M128") == "4":
                            self.mm(bk[0][:, 0:128], WTb[:, :], Sb[:], True, True, [WTb, Sb], r_zc)
                        elif os.environ.get("GDN_M128") == "5":
                            self.mm(bk[2][:, 384:512], WTb[:, :], Sb[:], True, True, [WTb, Sb], r_p)
                        elif os.environ.get("GDN_M128") == "3":
                            self.mm(bk[2][0:64, 0:128], WTb[:, rr], KQT[:, 2, :], True, True, [KQT, WTb], r_kk)
                        else:
                            self.mm(bk[2][0:64, 0:128], WTb[:, rr], Sb[:], True, True, [WTb, Sb], r_kk)
                        p.op("dve", lambda e, rr=rr: e.scalar_tensor_tensor(out=VNb[rr, :], in0=bk[2][0:64, 0:128], scalar=-1.0, in1=U[rr, :], op0=ALU.mult, op1=ALU.add), reads=[U] + r_kk, writes=[VNb])
                        self.mm(bk[2][0:64, 128:256], KQT[:, 2, rr], Sb[:], True, False, [KQT, Sb], r_kk)
                        self.mm(bk[2][0:64, 128:256], ATb[rr, rr], VNb[rr, :], False, True, [ATb, VNb], r_kk)
                        p.op("act", lambda e, rr=rr: e.copy(OTk[rr, :], bk[2][0:64, 128:256]), reads=r_kk, writes=[OTk])
                        self.mm(bk[2][:, 256:384], TOK[rr, 3, :], VNb[rr, :], True, True, [(TOK.name, 1), VNb], r_a)
                        p.op("act", lambda e, t=t, hf=hf: e.activation(out=S[:], in_=S[:], func=AF.Copy, scale=DECB[:, t, hf, h:h + 1]), reads=[S, DECB], writes=[S])
                        p.op("dve", lambda e: e.tensor_tensor(out=S[:], in0=bk[2][:, 256:384], in1=S[:], op=ALU.add), reads=[S] + r_a, writes=[S])
                        p.op("act", lambda e: e.copy(Sb[:], S[:]), reads=[S], writes=[Sb])
                        yield
                    p.op("pool", lambda e: e.memset(st[:, 4:5], 0.0), reads=[st], writes=[st])
                    p.op("act", lambda e: e.activation(out=b["junk"][:], in_=OTk[:], func=AF.Square, accum_out=st[:, 4:5]), reads=[OTk, st], writes=[b["junk"], st])
                    p.op("act", lambda e: e.activation(out=st[:, 4:5], in_=st[:, 4:5], func=AF.Sqrt, bias=self.epsc[:], scale=1.0 / 128), reads=[st, self.epsc], writes=[st])
                    p.op("dve", lambda e: e.reciprocal(out=st[:, 5:6], in_=st[:, 4:5]), reads=[st], writes=[st])
                    p.op("dve", lambda e: e.scalar_tensor_tensor(out=OG[:], in0=OTk[:], scalar=st[:, 5:6], in1=GZ[:], op0=ALU.mult, op1=ALU.mult), reads=[OTk, st, GZ], writes=[OG])
                    self.tr(bkb[1][:, 384:512], OG[:], self.identb[:], [OG], r_og)
                    p.op("act", lambda e: e.copy(OGT[:], bkb[1][:, 384:512]), reads=r_og, writes=[OGT])
                    for hf in range(2):
                        self.mm(bk[0][:], OGT[:], WO[:, hf * 512:(hf + 1) * 512], True, True, [OGT, WO], r_zc)
                        xs_ = self.X[:, t, hf * 512:(hf + 1) * 512]
                        p.op("dve", lambda e, xs_=xs_: e.tensor_tensor(out=xs_, in0=bk[0][:], in1=xs_, op=ALU.add), reads=r_zc + [("X", t)], writes=[("X", t)])
                    yield
                p.op("dve", lambda e: e.tensor_copy(XC[:, :, 0:3], XC[:, :, 512:515]), reads=[XC], writes=[XC])
                yield

        import os
        steps = int(os.environ.get("GDN_STEPS", "1000000000"))
        for h0 in range(0, 8, NPAR):
            gens = [head_gen(h0 + i, HB[i]) for i in range(NPAR)]
            alive = list(gens)
            while alive and steps > 0:
                for g_ in list(alive):
                    steps -= 1
                    if steps <= 0:
                        break
                    try:
                        next(g_)
                    except StopIteration:
                        alive.remove(g_)
        if "GDN_DUMP" in os.environ:
            nm = os.environ["GDN_DUMP"]
            src = HB[0][nm]
            self.dump = p.tile("dump", [128, 512], F32)
            n = int(np.prod(src.shape[1:]))
            v = src[:] if len(src.shape) == 2 else src[:].rearrange("p a b -> p (a b)")
            p.op("pool", lambda e: e.memset(self.dump[:], 0.0), writes=[self.dump])
            p.op("dve", lambda e: e.tensor_copy(self.dump[:, 0:min(n, 512)], v[:, 0:min(n, 512)]), reads=[src, self.dump] + [(src.name, i) for i in range(2)], writes=[self.dump])
            p.dma("sp", lambda e: e.dma_start(out=self.y[s, 0:128, 0:512], in_=self.dump[:]), reads=[self.dump], key="dump")
            p.barrier()
            self.skip0 = True
        p.release(m0)

    def build(self):
        p = self.p
        st = self.stages
        self.setup()
        self.prologue_mod()
        final = []
        for s in range(self.nseq):
            self.load_x(s)
            for l in range(2):
                if f"mix{l}" in st:
                    self.norm_phase(l, s, 0)
                    self.make_gate(l, s, 2)
                    (self.mla_phase if l == 0 else self.gdn_phase)(s)
                if f"ffn{l}" in st:
                    self.norm_phase(l, s, 1, router=(l == 1))
                    self.make_gate(l, s, 5)
                    if l == 0:
                        ex = [(self.dram["ffn_gu"][j], self.dram["ffn_dn"][j], None) for j in range(2)]
                    else:
                        ex = [(self.dram["moe_gu"][j], self.dram["moe_dn"][j], j) for j in range(8)]
                    self.ffn_phase(ex)
            final += self.final_out(s)
        p.wait("sp", final)
        p.emit()
        return self.nc


def _cols(v, nk):
    return np.ascontiguousarray(np.asarray(v).reshape(nk, 128).T)


def _gu_layout(w, ff):
    nch = ff // 128
    g = w[:, :ff].reshape(8, 128, nch, 128)
    u = w[:, ff:].reshape(8, 128, nch, 128)
    gu = np.concatenate([g, u], axis=-1)
    return np.ascontiguousarray(gu.transpose(2, 1, 0, 3)).reshape(nch, 128, 8 * 256)


def _rope_consts():
    c = np.zeros((128, 2), np.float64)
    inv = 10000.0 ** (-np.arange(16, dtype=np.float64) / 16.0)
    for j in range(32):
        c[64 + j, 0] = inv[j % 16] / (2 * math.pi); c[64 + j, 1] = 0.25
        c[96 + j, 0] = inv[j % 16] / (2 * math.pi); c[96 + j, 1] = 0.5 if j < 16 else 0.0
    return c.astype(np.float32)


ROPEC = _rope_consts()


def prep_shared(inp):
    f = lambda k: np.asarray(inp[k], dtype=np.float32)
    sh = {}
    sh["ada"] = np.ascontiguousarray(np.concatenate([f("ada_w"), f("ada_b")[:, None, :]], axis=1))
    sh["gmix"] = np.stack([_cols(f("norm_mix")[l], 8) for l in range(2)])
    sh["gffn"] = np.stack([_cols(f("norm_ffn")[l], 8) for l in range(2)])
    sh["gfin"] = np.ascontiguousarray(np.broadcast_to(f("final_norm")[None, :], (128, D)))
    wgu = f("ffn_w_gate_up")[0]
    halves = []
    for j in range(2):
        sub = np.concatenate([wgu[:, j * 1408:(j + 1) * 1408], wgu[:, 2816 + j * 1408:2816 + (j + 1) * 1408]], axis=1)
        halves.append(_gu_layout(sub, 1408))
    sh["ffn_gu"] = np.stack(halves)
    sh["ffn_dn"] = np.ascontiguousarray(f("ffn_w_down")[0].reshape(2, 1408, D))
    sh["moe_gu"] = np.stack([_gu_layout(f("moe_w_gate_up")[0, e], 1408) for e in range(8)])
    sh["moe_dn"] = np.ascontiguousarray(f("moe_w_down")[0])
    sh["moe_r"] = np.ascontiguousarray(f("moe_w_router")[0].reshape(8, 128, 8).transpose(1, 0, 2))

    perm = np.concatenate([np.arange(16, 32), np.arange(0, 16)])
    win = f("mla_w_in")[0]
    kr = win[:, 768:800]
    win2 = np.concatenate([win[:, :768], win[:, 0:64], kr, kr[:, perm]], axis=1)
    sh["mla_in"] = np.ascontiguousarray(win2.reshape(8, 128, 896).transpose(1, 0, 2))
    sh["mla_qn"] = _cols(f("mla_q_norm")[0], 4)
    sh["mla_kvn"] = _cols(f("mla_kv_norm")[0], 2)
    wqb = f("mla_w_qb")[0].reshape(512, 16, 96)
    wqb2 = np.concatenate([wqb[:, :, :64], wqb[:, :, 64:96], wqb[:, :, 64:96][:, :, perm]], axis=2).reshape(512, 2048)
    sh["mla_qb"] = np.ascontiguousarray(wqb2.reshape(4, 128, 2048).transpose(1, 0, 2))
    wkv = f("mla_w_kvb")[0].reshape(256, 16, 128)
    sh["mla_kb"] = np.ascontiguousarray(wkv[:, :, :64].reshape(2, 128, 1024).transpose(1, 0, 2))
    sh["mla_vb"] = np.ascontiguousarray(wkv[:, :, 64:].reshape(2, 128, 1024).transpose(1, 0, 2))
    sh["mla_out"] = np.ascontiguousarray(f("mla_w_out")[0])
    sh["ropec"] = ROPEC
    gw = f("gdn_w_in")[0]
    heads = []
    for h in range(8):
        cols = np.concatenate([np.arange(h * 128, (h + 1) * 128) + off for off in (0, 1024, 2048, 3072)])
        heads.append(gw[:, cols].reshape(8, 128, 512).transpose(1, 0, 2))
    sh["gdn_in"] = np.ascontiguousarray(np.stack(heads))
    sh["gdn_ba"] = np.ascontiguousarray(gw[:, 4096:4112].reshape(8, 128, 16).transpose(1, 0, 2))
    cw = f("gdn_conv_w")[0]
    sh["gdn_conv"] = np.ascontiguousarray(cw.reshape(4, 3, 8, 128).transpose(3, 2, 1, 0))
    hpv = np.zeros((128, 24), np.float32)
    hpv[:, 0:8] = f("gdn_a_log")[0][None, :]
    hpv[:, 8:16] = f("gdn_dt_bias")[0][None, :]
    sh["gdn_hp"] = hpv
    sh["gdn_on"] = np.ascontiguousarray(np.broadcast_to(f("gdn_out_norm")[0][None, :], (128, 128)))
    sh["gdn_out"] = np.ascontiguousarray(f("gdn_w_out")[0])
    return sh


def prep_core(inp, b0, ns):
    x = np.ascontiguousarray(np.asarray(inp["x"], np.float32)[b0:b0 + ns])
    c = np.asarray(inp["c"], np.float32)[b0:b0 + ns]
    cT = np.stack([_cols(c[i], 8) for i in range(ns)])
    pos = np.asarray(inp["positions"]).astype(np.int32)[b0:b0 + ns]
    posr = np.ascontiguousarray(np.broadcast_to(pos[:, None, :], (ns, 128, SEQ)))
    return {"x": x, "cT": cT, "pos": posr}


_CACHE = {}


def run(inp, batches, ns, stages):
    key = (ns, tuple(sorted(stages)))
    if key not in _CACHE:
        _CACHE[key] = Builder(ns, stages).build()
    nc = _CACHE[key]
    sh = prep_shared(inp)
    maps = []
    for b0 in batches:
        mm = dict(sh)
        mm.update(prep_core(inp, b0, ns))
        maps.append(mm)
    res = run_bass_kernel_spmd(nc, maps, core_ids=list(range(len(batches))))
    return [r["y"] for r in res.results]


ALL = ("mix0", "ffn0", "mix1", "ffn1")


def kernel(**inputs):
    ys = run(inputs, [2 * i for i in range(NCORES)], 2, ALL)
    return np.concatenate(ys, axis=0).astype(np.float32)
```

```python
import math
import numpy as np
import concourse.bass as bass
import concourse.mybir as mybir
from concourse.bass_utils import run_bass_kernel_spmd

F32 = mybir.dt.float32
BF16 = mybir.dt.bfloat16
I32 = mybir.dt.int32
AF = mybir.ActivationFunctionType
ALU = mybir.AluOpType
AX = mybir.AxisListType

NCORES = 8
SEQ = 2048
D = 1024
NT = SEQ // 128
NB = SEQ // 512
EPS = 1e-6
GEN = 30000


class Prog:
    ENG = ("pe", "act", "dve", "pool", "sp")

    def __init__(self, nc):
        self.nc = nc
        self.q = {e: [] for e in self.ENG}
        self.cnt = {e: 0 for e in self.ENG}
        self.res = {}
        self.seen = {e: {} for e in self.ENG}
        self.dma_cnt = {}
        self.semkeys = set()
        self.off = 16512
        self.ntile = 0
        self.cap = nc.SBUF_PARTITION_SIZE_BYTES

    def tile(self, name, shape, dt):
        nbytes = int(np.prod(shape[1:])) * (2 if dt == BF16 else 4)
        self.off = (self.off + 63) // 64 * 64
        assert self.off + nbytes <= self.cap, (name, self.off, nbytes)
        self.ntile += 1
        t = self.nc.alloc_sbuf_tensor_at(f"{name}_{self.ntile}", list(shape), dt, offset=self.off)
        self.off += nbytes
        return t

    def mark(self):
        return self.off

    def release(self, m):
        self.barrier()
        self.off = m

    def _key(self, r):
        return r if isinstance(r, (str, tuple)) else r.name

    def _st(self, r):
        r = self._key(r)
        s = self.res.get(r)
        if s is None:
            s = self.res[r] = {"w": None, "r": []}
        return s

    def _deps(self, eng, reads, writes):
        deps = {}

        def add(d):
            if d is None:
                return
            k, v = d
            if eng == "pe" and isinstance(k, tuple) and k[0] == "pe":
                return
            if deps.get(k, 0) < v:
                deps[k] = v
        for r in reads:
            add(self._st(r)["w"])
        for r in writes:
            s = self._st(r)
            add(s["w"])
            for d in s["r"]:
                add(d)
        out = []
        seen = self.seen[eng]
        for k, v in deps.items():
            if seen.get(k, 0) < v:
                seen[k] = v
                out.append((k, v))
        return out

    def _commit(self, token, reads, writes):
        for r in reads:
            lst = self._st(r)["r"]
            lst[:] = [d for d in lst if d[0] != token[0]]
            lst.append(token)
        for r in writes:
            s = self._st(r)
            s["w"] = token
            s["r"] = []

    limit = None

    def op(self, eng, fn, reads=(), writes=()):
        if self.limit is not None:
            if self.limit <= 0:
                return None
            self.limit -= 1
        waits = self._deps(eng, reads, writes)
        if self.limit == 0:
            print("LAST OP", eng, "waits", waits, "cnt", self.cnt, "reads", [self._key(r) for r in reads], "writes", [self._key(r) for r in writes])
        self.cnt[eng] += 1
        n = self.cnt[eng]
        key = (eng, (n - 1) // GEN)
        self.semkeys.add(key)
        token = (key, (n - 1) % GEN + 1)
        self.q[eng].append((waits, fn, (key, 1)))
        self._commit(token, reads, writes)
        return token

    def dma(self, queue, fn, reads=(), writes=(), key=None):
        if key is None:
            r0 = writes[0] if writes else reads[0]
            key = ("dma", self._key(r0))
        self.semkeys.add(key)
        waits = self._deps(queue, reads, writes)
        v = self.dma_cnt.get(key, 0) + 16
        self.dma_cnt[key] = v
        token = (key, v)
        self.q[queue].append((waits, fn, (key, 16)))
        self._commit(token, reads, writes)
        return token

    def barrier(self):
        toks = []
        for e in self.ENG:
            n = self.cnt[e]
            if n:
                toks.append(((e, (n - 1) // GEN), (n - 1) % GEN + 1))
        toks += list(self.dma_cnt.items())
        for e in self.ENG:
            self.wait(e, toks)

    def wait(self, eng, tokens):
        waits = []
        for k, v in tokens:
            if eng == "pe" and isinstance(k, tuple) and k[0] == "pe":
                continue
            if self.seen[eng].get(k, 0) < v:
                self.seen[eng][k] = v
                waits.append((k, v))
        if waits:
            self.q[eng].append((waits, None, None))

    def emit(self):
        import contextlib
        nc = self.nc
        with contextlib.ExitStack() as es:
            sems = {}
            for i, k in enumerate(sorted(self.semkeys, key=str)):
                sems[k] = es.enter_context(nc.semaphore(f"s{i}"))
            block = es.enter_context(nc.Block())
            q = self.q

            def run(engname):
                def body(e):
                    for waits, fn, inc in q[engname]:
                        for k, v in waits:
                            e.wait_ge(sems[k], v)
                        if fn is not None:
                            fn(e).then_inc(sems[inc[0]], inc[1])
                return body
            block.tensor(run("pe"))
            block.scalar(run("act"))
            block.vector(run("dve"))
            block.gpsimd(run("pool"))
            block.sync(run("sp"))


class Builder:
    def __init__(self, nseq, stages):
        self.nseq = nseq
        self.stages = stages
        nc = self.nc = bass.Bass("TRN2", target_bir_lowering=False)
        self.p = Prog(nc)
        self.dram = {}
        self.rr = 0

    def din(self, name, shape, dt=F32):
        a = self.nc.dram_tensor(name, list(shape), dt, kind="ExternalInput").ap()
        self.dram[name] = a
        return a

    def setup(self):
        p, nc, ns = self.p, self.nc, self.nseq
        d = self.din
        d("x", [ns, SEQ, D]); d("cT", [ns, 128, 8]); d("pos", [ns, 128, SEQ], I32)
        d("ada", [2, 1025, 6 * D])
        d("gmix", [2, 128, 8]); d("gffn", [2, 128, 8]); d("gfin", [128, D])
        d("ffn_gu", [2, 11, 128, 8 * 256]); d("ffn_dn", [2, 1408, D])
        d("moe_gu", [8, 11, 128, 8 * 256]); d("moe_dn", [8, 1408, D]); d("moe_r", [128, 8, 8])
        d("mla_in", [128, 8, 896]); d("mla_qn", [128, 4]); d("mla_kvn", [128, 2])
        d("mla_qb", [128, 4, 2048]); d("mla_kb", [128, 2, 1024]); d("mla_vb", [128, 2, 1024])
        d("mla_out", [D, D]); d("ropec", [128, 2])
        d("gdn_in", [8, 128, 8, 512]); d("gdn_ba", [128, 8, 16]); d("gdn_conv", [128, 8, 3, 4])
        d("gdn_hp", [128, 24]); d("gdn_on", [128, 128]); d("gdn_out", [D, D])
        self.y = nc.dram_tensor("y", [ns, SEQ, D], F32, kind="ExternalOutput").ap()

        self.X = p.tile("X", [128, NT, D], F32)
        self.HT = p.tile("HT", [128, 8, SEQ], BF16)
        self.identb = p.tile("identb", [128, 128], BF16)
        self.identf = p.tile("identf", [128, 128], F32)
        self.onesb = p.tile("onesb", [128, 128], BF16)
        self.onesf = p.tile("onesf", [128, 128], F32)
        self.epsc = p.tile("epsc", [128, 1], F32)
        self.MODC = p.tile("MODC", [128, 2, 48, ns], F32)
        self.G = p.tile("G", [128, D], F32)
        self.Acol = p.tile("Acol", [128, 8], F32)
        self.gmix = p.tile("gmix", [128, 2, 8], F32)
        self.gffn = p.tile("gffn", [128, 2, 8], F32)
        self.GF = p.tile("GF", [128, D], F32)
        self.ssq = p.tile("ssq", [128, NT], F32)
        self.rstd = p.tile("rstd", [128, NT], F32)
        self.COMB = p.tile("COMB", [128, NT, 8], F32)
        self.PS = [nc.alloc_psum_tensor(f"ps{i}", [128, 512], F32) for i in range(8)]

        p.op("pool", lambda e: e.memset(self.identf[:], 1.0), writes=[self.identf])
        p.op("pool", lambda e: e.affine_select(out=self.identf[:], in_=self.identf[:], pattern=[[-1, 128]],
                                               compare_op=ALU.is_equal, fill=0.0, base=0, channel_multiplier=1),
             reads=[self.identf], writes=[self.identf])
        p.op("dve", lambda e: e.tensor_copy(self.identb[:], self.identf[:]), reads=[self.identf], writes=[self.identb])
        p.op("pool", lambda e: e.memset(self.onesb[:], 1.0), writes=[self.onesb])
        p.op("pool", lambda e: e.memset(self.onesf[:], 1.0), writes=[self.onesf])
        p.op("pool", lambda e: e.memset(self.epsc[:], EPS), writes=[self.epsc])
        p.dma("sp", lambda e: e.dma_start(out=self.gmix[:], in_=self.dram["gmix"].rearrange("l p k -> p l k")), writes=[self.gmix])
        p.dma("sp", lambda e: e.dma_start(out=self.gffn[:], in_=self.dram["gffn"].rearrange("l p k -> p l k")), writes=[self.gffn])
        p.dma("sp", lambda e: e.dma_start(out=self.GF[:], in_=self.dram["gfin"]), writes=[self.GF])

    def prologue_mod(self):
        p, ns = self.p, self.nseq
        m = p.mark()
        cT = p.tile("cTs", [128, ns, 8], F32)
        condb = p.tile("condb", [128, 8, ns], BF16)
        for s in range(ns):
            p.dma("sp", lambda e, s=s: e.dma_start(out=cT[:, s, :], in_=self.dram["cT"][s]), writes=[cT], key=("dma", "cT", s))
        for s in range(ns):
            p.op("act", lambda e, s=s: e.activation(out=condb[:, :, s], in_=cT[:, s, :], func=AF.Silu), reads=[cT], writes=[condb])
        W = [p.tile(f"adaw{i}", [128, 8, 512], BF16) for i in range(2)]
        Bv = [p.tile(f"adab{i}", [1, 512], BF16) for i in range(2)]
        ada = self.dram["ada"]
        it = 0
        for l in range(2):
            ps = self.PS[l]
            psv = ps[:, 0:48 * ns].rearrange("p (b s) -> p b s", s=ns)
            for ch in range(12):
                w, bv = W[it % 2], Bv[it % 2]
                it += 1
                cols = slice(ch * 512, (ch + 1) * 512)
                p.dma("pool", lambda e, w=w, l=l, cols=cols: e.dma_start(
                    out=w[:], in_=ada[l, 0:1024, cols].rearrange("(k p) n -> p k n", p=128)), writes=[w])
                p.dma("pool", lambda e, bv=bv, l=l, cols=cols: e.dma_start(out=bv[:], in_=ada[l, 1024:1025, cols]), writes=[bv])
                for qd in range(4):
                    blk = ch * 4 + qd
                    for k in range(8):
                        p.op("pe", lambda e, w=w, k=k, qd=qd, blk=blk, psv=psv: e.matmul(
                            psv[:, blk, :], lhsT=w[:, k, qd * 128:(qd + 1) * 128], rhs=condb[:, k, :], start=(k == 0), stop=False),
                            reads=[w, condb], writes=[ps])
                    p.op("pe", lambda e, bv=bv, qd=qd, blk=blk, psv=psv: e.matmul(
                        psv[:, blk, :], lhsT=bv[0:1, qd * 128:(qd + 1) * 128], rhs=self.onesb[0:1, 0:ns], start=False, stop=True),
                        reads=[bv, self.onesb], writes=[ps])
            p.op("act", lambda e, l=l, psv=psv: e.copy(self.MODC[:, l, :, :], psv), reads=[ps], writes=[self.MODC])
        p.release(m)

    def modc(self, l, s, j, k=None):
        if k is None:
            return self.MODC[:, l, j * 8:(j + 1) * 8, s]
        return self.MODC[:, l, j * 8 + k, s:s + 1]

    def make_gate(self, l, s, j):
        p = self.p
        m = p.mark()
        dg = [p.tile(f"dg{i}", [128, 128], F32) for i in range(2)]
        for k in range(8):
            t = dg[k % 2]
            ps = self.PS[k // 4]
            p.op("dve", lambda e, t=t, k=k: e.tensor_scalar(out=t[:], in0=self.identf[:], scalar1=self.modc(l, s, j, k), scalar2=None, op0=ALU.mult),
                 reads=[self.identf, self.MODC], writes=[t])
            p.op("pe", lambda e, t=t, k=k, ps=ps: e.matmul(ps[:, (k % 4) * 128:(k % 4 + 1) * 128], lhsT=self.onesf[:], rhs=t[:], start=True, stop=True),
                 reads=[t, self.onesf], writes=[ps])
        for h in range(2):
            p.op("act", lambda e, h=h: e.copy(self.G[:, h * 512:(h + 1) * 512], self.PS[h][:]), reads=[self.PS[h]], writes=[self.G])
        p.release(m)

    def load_x(self, s):
        p = self.p
        for t in range(NT):
            q = "sp" if t % 2 == 0 else "act"
            p.dma(q, lambda e, t=t: e.dma_start(out=self.X[:, t, :], in_=self.dram["x"][s, t * 128:(t + 1) * 128, :]),
                  writes=[("X", t)])

    def final_out(self, s):
        p = self.p
        m = p.mark()
        ob = [p.tile(f"ob{i}", [128, D], F32) for i in range(2)]
        junk = p.tile("junkf", [128, D], BF16)
        toks = []
        for t in range(NT):
            if t == 0 and getattr(self, "skip0", False):
                continue
            self.rms_stats(t, junk)
            o = ob[t % 2]
            p.op("dve", lambda e, t=t, o=o: e.scalar_tensor_tensor(out=o[:], in0=self.X[:, t, :], scalar=self.rstd[:, t:t + 1], in1=self.GF[:],
                                                                  op0=ALU.mult, op1=ALU.mult),
                 reads=[("X", t), ("rstd", t), self.GF], writes=[o])
            toks.append(p.dma("sp", lambda e, t=t, o=o: e.dma_start(out=self.y[s, t * 128:(t + 1) * 128, :], in_=o[:]), reads=[o]))
        p.release(m)
        return toks

    def rms_stats(self, t, junk):
        p = self.p
        p.op("pool", lambda e: e.memset(self.ssq[:, t:t + 1], 0.0), writes=[("ssq", t)])
        p.op("act", lambda e: e.activation(out=junk[:], in_=self.X[:, t, :], func=AF.Square, accum_out=self.ssq[:, t:t + 1]),
             reads=[("X", t)], writes=[junk, ("ssq", t)])
        p.op("act", lambda e: e.activation(out=self.ssq[:, t:t + 1], in_=self.ssq[:, t:t + 1], func=AF.Sqrt, bias=self.epsc[:], scale=1.0 / D),
             reads=[("ssq", t), self.epsc], writes=[("ssq", t)])
        p.op("dve", lambda e: e.reciprocal(out=self.rstd[:, t:t + 1], in_=self.ssq[:, t:t + 1]), reads=[("ssq", t)], writes=[("rstd", t)])

    def norm_phase(self, l, s, which, router=False):
        p = self.p
        m = p.mark()
        jsh, jsc = (0, 1) if which == 0 else (3, 4)
        gcol = (self.gmix if which == 0 else self.gffn)[:, l, :]
        p.op("dve", lambda e: e.tensor_scalar(out=self.Acol[:], in0=self.modc(l, s, jsc), scalar1=1.0, scalar2=None, op0=ALU.add),
             reads=[self.MODC], writes=[self.Acol])
        p.op("dve", lambda e: e.tensor_tensor(out=self.Acol[:], in0=self.Acol[:], in1=gcol, op=ALU.mult),
             reads=[self.Acol, self.gmix, self.gffn], writes=[self.Acol])
        junk = p.tile("junk", [128, D], BF16)
        xn = [p.tile(f"xn{i}", [128, D], BF16) for i in range(2)]
        if router:
            xnf = p.tile("xnf", [128, D], F32)
            hTf = p.tile("hTf", [128, 8, 128], F32)
            wr = p.tile("wr", [128, 8, 8], F32)
            lg = p.tile("lg", [128, 8], F32)
            mx = p.tile("mx", [128, 8], F32)
            sc = p.tile("rsc", [128, 4], F32)
            c1 = p.tile("c1", [128, 8], F32)
            p.dma("sp", lambda e: e.dma_start(out=wr[:], in_=self.dram["moe_r"]), writes=[wr])
        psT = [self.PS[i].bitcast(BF16) for i in range(4)]
        for blk in range(NB):
            for tt in range(4):
                t = blk * 4 + tt
                self.rms_stats(t, junk)
                x_ = xn[t % 2]
                p.op("dve", lambda e, t=t, x_=x_: e.tensor_scalar(out=x_[:], in0=self.X[:, t, :], scalar1=self.rstd[:, t:t + 1], scalar2=None, op0=ALU.mult),
                     reads=[("X", t), ("rstd", t)], writes=[x_])
                for k in range(8):
                    p.op("pe", lambda e, k=k, tt=tt, x_=x_: e.transpose(psT[k // 2][:, (k % 2) * 512 + tt * 128:(k % 2) * 512 + (tt + 1) * 128],
                                                                       x_[:, k * 128:(k + 1) * 128], self.identb[:]),
                         reads=[x_, self.identb], writes=[self.PS[k // 2]])
                if router:
                    p.op("dve", lambda e, t=t: e.tensor_scalar(out=xnf[:], in0=self.X[:, t, :], scalar1=self.rstd[:, t:t + 1], scalar2=None, op0=ALU.mult),
                         reads=[("X", t), ("rstd", t)], writes=[xnf])
                    for k in range(8):
                        ps = self.PS[4 + k // 4]
                        p.op("pe", lambda e, k=k, ps=ps: e.transpose(ps[:, (k % 4) * 128:(k % 4 + 1) * 128], xnf[:, k * 128:(k + 1) * 128], self.identf[:]),
                             reads=[xnf, self.identf], writes=[ps])
                    for k in range(8):
                        ps = self.PS[4 + k // 4]
                        p.op("act", lambda e, k=k, ps=ps: e.activation(out=hTf[:, k, :], in_=ps[:, (k % 4) * 128:(k % 4 + 1) * 128], func=AF.Identity,
                                                                      bias=self.modc(l, s, jsh, k), scale=self.Acol[:, k:k + 1]),
                             reads=[ps, self.Acol, self.MODC], writes=[hTf])
                    for k in range(8):
                        p.op("pe", lambda e, k=k: e.matmul(self.PS[6][:, 0:8], lhsT=hTf[:, k, :], rhs=wr[:, k, :], start=(k == 0), stop=(k == 7)),
                             reads=[hTf, wr], writes=[self.PS[6]])
                    p.op("act", lambda e: e.copy(lg[:], self.PS[6][:, 0:8]), reads=[self.PS[6]], writes=[lg])
                    p.op("dve", lambda e: e.max(out=mx[:], in_=lg[:]), reads=[lg], writes=[mx])
                    p.op("dve", lambda e: e.tensor_tensor(out=sc[:, 0:1], in0=mx[:, 1:2], in1=mx[:, 0:1], op=ALU.subtract), reads=[mx], writes=[sc])
                    p.op("act", lambda e: e.activation(out=sc[:, 0:1], in_=sc[:, 0:1], func=AF.Exp), reads=[sc], writes=[sc])
                    p.op("dve", lambda e: e.tensor_scalar(out=sc[:, 1:2], in0=sc[:, 0:1], scalar1=1.0, scalar2=None, op0=ALU.add), reads=[sc], writes=[sc])
                    p.op("dve", lambda e: e.reciprocal(out=sc[:, 1:2], in_=sc[:, 1:2]), reads=[sc], writes=[sc])
                    p.op("dve", lambda e: e.tensor_tensor(out=sc[:, 2:3], in0=sc[:, 0:1], in1=sc[:, 1:2], op=ALU.mult), reads=[sc], writes=[sc])
                    p.op("dve", lambda e: e.tensor_scalar(out=c1[:], in0=lg[:], scalar1=mx[:, 0:1], scalar2=sc[:, 1:2], op0=ALU.is_equal, op1=ALU.mult),
                         reads=[lg, mx, sc], writes=[c1])
                    p.op("dve", lambda e, t=t: e.tensor_scalar(out=self.COMB[:, t, :], in0=lg[:], scalar1=mx[:, 1:2], scalar2=sc[:, 2:3], op0=ALU.is_equal, op1=ALU.mult),
                         reads=[lg, mx, sc], writes=[("COMB", t)])
                    p.op("dve", lambda e, t=t: e.tensor_tensor(out=self.COMB[:, t, :], in0=self.COMB[:, t, :], in1=c1[:], op=ALU.add),
                         reads=[c1, ("COMB", t)], writes=[("COMB", t)])
            for k in range(8):
                src = psT[k // 2][:, (k % 2) * 512:(k % 2 + 1) * 512]
                dst = self.HT[:, k, blk * 512:(blk + 1) * 512]
                if k % 2 == 0:
                    p.op("act", lambda e, k=k, src=src, dst=dst: e.activation(out=dst, in_=src, func=AF.Identity, bias=self.modc(l, s, jsh, k),
                                                                            scale=self.Acol[:, k:k + 1]),
                         reads=[self.PS[k // 2], self.Acol, self.MODC], writes=[("HT", blk)])
                else:
                    p.op("dve", lambda e, k=k, src=src, dst=dst: e.tensor_scalar(out=dst, in0=src, scalar1=self.Acol[:, k:k + 1], scalar2=self.modc(l, s, jsh, k),
                                                                               op0=ALU.mult, op1=ALU.add),
                         reads=[self.PS[k // 2], self.Acol, self.MODC], writes=[("HT", blk)])
        p.release(m)

    def ffn_phase(self, experts):
        p = self.p
        m = p.mark()
        ACT_ = p.tile("actT", [128, 11, SEQ], BF16)
        WGU = [p.tile(f"wgu{i}", [128, 8, 256], BF16) for i in range(3)]
        WD = p.tile("wd", [128, 11, D], BF16)
        SG = [p.tile(f"sg{i}", [128, 512], BF16) for i in range(2)]
        HTr = [("HT", b) for b in range(NB)]
        it = 0
        for (gu, dn, ci) in experts:
            p.dma("pool", lambda e, dn=dn: e.dma_start(out=WD[:], in_=dn.rearrange("(c p) n -> p c n", p=128)), writes=[WD])
            for c in range(11):
                w = WGU[c % 3]
                p.dma("pool", lambda e, w=w, c=c, gu=gu: e.dma_start(out=w[:].rearrange("p k n -> p (k n)"), in_=gu[c]), writes=[w])
                for blk in range(NB):
                    psg, psu = self.PS[it % 2], self.PS[2 + it % 2]
                    sg = SG[it % 2]
                    it += 1
                    for k in range(8):
                        p.op("pe", lambda e, w=w, k=k, blk=blk, psg=psg: e.matmul(psg[:], lhsT=w[:, k, 0:128], rhs=self.HT[:, k, blk * 512:(blk + 1) * 512],
                                                                                  start=(k == 0), stop=(k == 7)), reads=[w, HTr[blk]], writes=[psg])
                    for k in range(8):
                        p.op("pe", lambda e, w=w, k=k, blk=blk, psu=psu: e.matmul(psu[:], lhsT=w[:, k, 128:256], rhs=self.HT[:, k, blk * 512:(blk + 1) * 512],
                                                                                  start=(k == 0), stop=(k == 7)), reads=[w, HTr[blk]], writes=[psu])
                    p.op("act", lambda e, sg=sg, psg=psg: e.activation(out=sg[:], in_=psg[:], func=AF.Silu), reads=[psg], writes=[sg])
                    p.op("dve", lambda e, sg=sg, psu=psu, c=c, blk=blk: e.tensor_tensor(out=ACT_[:, c, blk * 512:(blk + 1) * 512], in0=psu[:], in1=sg[:], op=ALU.mult),
                         reads=[sg, psu], writes=[("actT", c)])
                if c == 5:
                    for cc in range(11):
                        p.op("pool", lambda e, cc=cc: e.tensor_tensor(out=WD[:, cc, :], in0=WD[:, cc, :], in1=self.G[:], op=ALU.mult),
                             reads=[WD, self.G], writes=[WD])
            for t in range(NT):
                for hf in range(2):
                    ps = self.PS[4 + (2 * t + hf) % 4]
                    for c in range(11):
                        p.op("pe", lambda e, c=c, t=t, hf=hf, ps=ps: e.matmul(ps[:], lhsT=ACT_[:, c, t * 128:(t + 1) * 128], rhs=WD[:, c, hf * 512:(hf + 1) * 512],
                                                                              start=(c == 0), stop=(c == 10)), reads=[("actT", c), WD], writes=[ps])
                    xs = self.X[:, t, hf * 512:(hf + 1) * 512]
                    if ci is None:
                        p.op("dve", lambda e, xs=xs, ps=ps: e.tensor_tensor(out=xs, in0=ps[:], in1=xs, op=ALU.add), reads=[ps, ("X", t)], writes=[("X", t)])
                    else:
                        p.op("dve", lambda e, xs=xs, ps=ps, t=t, ci=ci: e.scalar_tensor_tensor(out=xs, in0=ps[:], scalar=self.COMB[:, t, ci:ci + 1], in1=xs,
                                                                                              op0=ALU.mult, op1=ALU.add),
                             reads=[ps, ("X", t), ("COMB", t)], writes=[("X", t)])
        p.release(m)


    def mla_phase(self, s):
        p = self.p
        PS = self.PS
        m0 = p.mark()
        QNT = p.tile("QNT", [128, 4, SEQ], BF16)
        KVNT = p.tile("KVNT", [128, 2, SEQ], BF16)
        ROPE = p.tile("ROPE", [128, SEQ], F32)
        KT = p.tile("KT", [128, SEQ], BF16)
        QT = p.tile("QT", [128, SEQ], BF16)
        VA = p.tile("VA", [128, NT, 128], BF16)
        PT = [p.tile(f"PT{i}", [128, 512], BF16) for i in range(4)]
        TRI = p.tile("TRI", [128, 128], BF16)
        tA = p.tile("tA", [128, 512], F32)
        tB = p.tile("tB", [128, 512], F32)
        rec = p.tile("rec", [128, 512], F32)
        qnc = p.tile("qnc", [128, 4], F32)
        kvnc = p.tile("kvnc", [128, 2], F32)
        ropec = p.tile("ropec", [128, 2], F32)
        st = p.tile("mst", [128, 4], F32)
        p.dma("sp", lambda e: e.dma_start(out=qnc[:], in_=self.dram["mla_qn"]), writes=[qnc])
        p.dma("sp", lambda e: e.dma_start(out=kvnc[:], in_=self.dram["mla_kvn"]), writes=[kvnc])
        p.dma("sp", lambda e: e.dma_start(out=ropec[:], in_=self.dram["ropec"]), writes=[ropec])
        p.op("pool", lambda e: e.memset(TRI[:], 1.0), writes=[TRI])
        p.op("pool", lambda e: e.affine_select(out=TRI[:], in_=TRI[:], pattern=[[1, 128]], compare_op=ALU.is_ge, fill=0.0, base=0, channel_multiplier=-1),
             reads=[TRI], writes=[TRI])
        p.op("pool", lambda e: e.memset(VA[:, :, 64:128], 1.0), writes=[("VA", 0), ("VA", 1)])
        m1 = p.mark()
        WIN = p.tile("WIN", [128, 8, 896], BF16)
        p.dma("pool", lambda e: e.dma_start(out=WIN[:], in_=self.dram["mla_in"]), writes=[WIN])
        posi = p.tile("posi", [128, 512], I32)
        u = p.tile("ropu", [128, 512], F32)
        ki = p.tile("ropk", [128, 512], I32)
        kf = p.tile("ropkf", [128, 512], F32)
        qtok = [p.tile(f"qtok{i}", [128, 512], BF16) for i in range(2)]
        kvtok = [p.tile(f"kvtok{i}", [128, 256], BF16) for i in range(2)]
        junk = p.tile("mjunk", [128, 512], BF16)
        for blk in range(NB):
            cs = slice(blk * 512, (blk + 1) * 512)
            p.dma("sp", lambda e, cs=cs: e.dma_start(out=posi[:], in_=self.dram["pos"][s, :, cs]), writes=[posi])
            p.op("dve", lambda e: e.tensor_copy(u[:], posi[:]), reads=[posi], writes=[u])
            p.op("dve", lambda e: e.tensor_scalar(out=u[:], in0=u[:], scalar1=ropec[:, 0:1], scalar2=ropec[:, 1:2], op0=ALU.mult, op1=ALU.add),
                 reads=[u, ropec], writes=[u])
            p.op("dve", lambda e: e.tensor_copy(ki[:], u[:]), reads=[u], writes=[ki])
            p.op("dve", lambda e: e.tensor_copy(kf[:], ki[:]), reads=[ki], writes=[kf])
            p.op("dve", lambda e: e.tensor_tensor(out=u[:], in0=u[:], in1=kf[:], op=ALU.subtract), reads=[u, kf], writes=[u])
            p.op("act", lambda e, cs=cs: e.activation(out=ROPE[:, cs], in_=u[:], func=AF.Sin, scale=2.0 * math.pi * (1.0 - 1e-6)),
                 reads=[u], writes=[("ROPE", blk)])
        psTb = PS[4].bitcast(BF16)
        for t in range(NT):
            ts_ = slice(t * 128, (t + 1) * 128)
            blk = t // 4
            pa, pb = PS[(t % 2) * 2], PS[(t % 2) * 2 + 1]
            for k in range(8):
                p.op("pe", lambda e, k=k, ts_=ts_, pa=pa: e.matmul(pa[:], lhsT=self.HT[:, k, ts_], rhs=WIN[:, k, 0:512], start=(k == 0), stop=(k == 7)),
                     reads=[("HT", blk), WIN], writes=[pa])
            for k in range(8):
                p.op("pe", lambda e, k=k, ts_=ts_, pb=pb: e.matmul(pb[:, 0:256], lhsT=self.HT[:, k, ts_], rhs=WIN[:, k, 512:768], start=(k == 0), stop=(k == 7)),
                     reads=[("HT", blk), WIN], writes=[pb])
            p.op("pool", lambda e: e.memset(st[:, 0:2], 0.0), writes=[st])
            p.op("act", lambda e, pa=pa: e.activation(out=junk[:], in_=pa[:], func=AF.Square, accum_out=st[:, 0:1]), reads=[pa], writes=[junk, st])
            p.op("act", lambda e, pb=pb: e.activation(out=junk[:, 0:256], in_=pb[:, 0:256], func=AF.Square, accum_out=st[:, 1:2]), reads=[pb], writes=[junk, st])
            p.op("act", lambda e: e.activation(out=st[:, 0:1], in_=st[:, 0:1], func=AF.Sqrt, bias=self.epsc[:], scale=1.0 / 512), reads=[st, self.epsc], writes=[st])
            p.op("act", lambda e: e.activation(out=st[:, 1:2], in_=st[:, 1:2], func=AF.Sqrt, bias=self.epsc[:], scale=1.0 / 256), reads=[st, self.epsc], writes=[st])
            p.op("dve", lambda e: e.reciprocal(out=st[:, 2:4], in_=st[:, 0:2]), reads=[st], writes=[st])
            qt_, kvt_ = qtok[t % 2], kvtok[t % 2]
            p.op("dve", lambda e, pa=pa, qt_=qt_: e.tensor_scalar(out=qt_[:], in0=pa[:], scalar1=st[:, 2:3], scalar2=None, op0=ALU.mult), reads=[pa, st], writes=[qt_])
            p.op("act", lambda e, pb=pb, kvt_=kvt_: e.activation(out=kvt_[:], in_=pb[:, 0:256], func=AF.Copy, scale=st[:, 3:4]), reads=[pb, st], writes=[kvt_])
            for c in range(4):
                p.op("pe", lambda e, c=c, qt_=qt_: e.transpose(psTb[:, c * 128:(c + 1) * 128], qt_[:, c * 128:(c + 1) * 128], self.identb[:]),
                     reads=[qt_, self.identb], writes=[PS[4]])
            for c in range(2):
                p.op("pe", lambda e, c=c, kvt_=kvt_: e.transpose(psTb[:, 512 + c * 128:512 + (c + 1) * 128], kvt_[:, c * 128:(c + 1) * 128], self.identb[:]),
                     reads=[kvt_, self.identb], writes=[PS[4]])
            for c in range(4):
                if c % 2 == 0:
                    p.op("act", lambda e, c=c, ts_=ts_: e.activation(out=QNT[:, c, ts_], in_=psTb[:, c * 128:(c + 1) * 128], func=AF.Copy, scale=qnc[:, c:c + 1]),
                         reads=[PS[4], qnc], writes=[("QNT", blk)])
                else:
                    p.op("dve", lambda e, c=c, ts_=ts_: e.tensor_scalar(out=QNT[:, c, ts_], in0=psTb[:, c * 128:(c + 1) * 128], scalar1=qnc[:, c:c + 1], scalar2=None, op0=ALU.mult),
                         reads=[PS[4], qnc], writes=[("QNT", blk)])
            for c in range(2):
                if c % 2 == 0:
                    p.op("act", lambda e, c=c, ts_=ts_: e.activation(out=KVNT[:, c, ts_], in_=psTb[:, 512 + c * 128:512 + (c + 1) * 128], func=AF.Copy, scale=kvnc[:, c:c + 1]),
                         reads=[PS[4], kvnc], writes=[("KVNT", blk)])
                else:
                    p.op("dve", lambda e, c=c, ts_=ts_: e.tensor_scalar(out=KVNT[:, c, ts_], in0=psTb[:, 512 + c * 128:512 + (c + 1) * 128], scalar1=kvnc[:, c:c + 1], scalar2=None, op0=ALU.mult),
                         reads=[PS[4], kvnc], writes=[("KVNT", blk)])
        for blk in range(NB):
            cs = slice(blk * 512, (blk + 1) * 512)
            ps = PS[5 + blk % 2]
            for k in range(8):
                p.op("pe", lambda e, k=k, cs=cs, ps=ps: e.matmul(ps[:], lhsT=WIN[:, k, 768:896], rhs=self.HT[:, k, cs], start=(k == 0), stop=(k == 7)),
                     reads=[("HT", blk), WIN], writes=[ps])
            p.op("dve", lambda e, cs=cs, ps=ps: e.tensor_tensor(out=tA[64:96, :], in0=ps[64:96, :], in1=ROPE[64:96, cs], op=ALU.mult), reads=[ps, ("ROPE", blk)], writes=[tA])
            p.op("dve", lambda e, cs=cs, ps=ps: e.tensor_tensor(out=tB[64:96, :], in0=ps[96:128, :], in1=ROPE[96:128, cs], op=ALU.mult), reads=[ps, ("ROPE", blk)], writes=[tB])
            p.op("pool", lambda e, cs=cs: e.tensor_tensor(out=KT[64:96, cs], in0=tA[64:96, :], in1=tB[64:96, :], op=ALU.add), reads=[tA, tB], writes=[("KTr", blk)])
        p.release(m1)
        m2 = p.mark()
        WQB = p.tile("WQB", [128, 4, 2048], BF16)
        WKB = p.tile("WKB", [128, 2, 1024], BF16)
        WVB = p.tile("WVB", [128, 2, 1024], BF16)
        p.dma("pool", lambda e: e.dma_start(out=WQB[:], in_=self.dram["mla_qb"]), writes=[WQB])
        p.dma("pool", lambda e: e.dma_start(out=WKB[:], in_=self.dram["mla_kb"]), writes=[WKB])
        p.dma("pool", lambda e: e.dma_start(out=WVB[:], in_=self.dram["mla_vb"]), writes=[WVB])
        OT = self.HT
        scale = 96.0 ** -0.5
        si = 0
        oi = 0
        pi_ = 0
        for h in range(16):
            for blk in range(NB):
                cs = slice(blk * 512, (blk + 1) * 512)
                ps = PS[blk % 2]
                for c in range(4):
                    p.op("pe", lambda e, c=c, cs=cs, ps=ps, h=h: e.matmul(ps[:], lhsT=WQB[:, c, h * 128:(h + 1) * 128], rhs=QNT[:, c, cs], start=(c == 0), stop=(c == 3)),
                         reads=[WQB, ("QNT", blk)], writes=[ps])
                p.op("act", lambda e, cs=cs, ps=ps: e.copy(QT[0:64, cs], ps[0:64, :]), reads=[ps], writes=[("QT", blk)])
                p.op("dve", lambda e, cs=cs, ps=ps: e.tensor_tensor(out=tA[64:96, :], in0=ps[64:96, :], in1=ROPE[64:96, cs], op=ALU.mult), reads=[ps, ("ROPE", blk)], writes=[tA])
                p.op("dve", lambda e, cs=cs, ps=ps: e.tensor_tensor(out=tB[64:96, :], in0=ps[96:128, :], in1=ROPE[96:128, cs], op=ALU.mult), reads=[ps, ("ROPE", blk)], writes=[tB])
                p.op("pool", lambda e, cs=cs: e.tensor_tensor(out=QT[64:96, cs], in0=tA[64:96, :], in1=tB[64:96, :], op=ALU.add), reads=[tA, tB], writes=[("QT", blk)])
            for blk in range(NB):
                cs = slice(blk * 512, (blk + 1) * 512)
                ps = PS[blk % 2]
                for c in range(2):
                    p.op("pe", lambda e, c=c, cs=cs, ps=ps, h=h: e.matmul(ps[0:64, :], lhsT=WKB[:, c, h * 64:(h + 1) * 64], rhs=KVNT[:, c, cs], start=(c == 0), stop=(c == 1)),
                         reads=[WKB, ("KVNT", blk)], writes=[ps])
                p.op("act", lambda e, cs=cs, ps=ps: e.copy(KT[0:64, cs], ps[0:64, :]), reads=[ps], writes=[("KTn", blk)])
            for half in range(2):
                ps = PS[half]
                psv = ps[:].rearrange("p (a b) -> p a b", b=64)
                for kk in range(8):
                    kt = half * 8 + kk
                    for c in range(2):
                        p.op("pe", lambda e, c=c, kt=kt, kk=kk, psv=psv, h=h: e.matmul(psv[:, kk, :], lhsT=KVNT[:, c, kt * 128:(kt + 1) * 128], rhs=WVB[:, c, h * 64:(h + 1) * 64],
                                                                                   start=(c == 0), stop=(c == 1)), reads=[WVB, ("KVNT", kt // 4)], writes=[ps])
                p.op("dve", lambda e, half=half, psv=psv: e.tensor_copy(VA[:, half * 8:(half + 1) * 8, 0:64], psv), reads=[ps], writes=[("VA", half)])
            steps = []
            for qb in range(NB):
                pso = PS[5 + oi % 2]
                oi += 1
                last = 4 * qb + 3
                for kt in range(last + 1):
                    steps.append((qb, kt, max(0, kt - 4 * qb) * 128, pso, last))
            LA = 2
            slot = {}

            def emit_s(i):
                qb, kt, col0, pso, last = steps[i]
                pss = PS[2 + (si + i) % 3]
                pt = PT[(pi_ + i) % 4]
                slot[i] = (pss, pt)
                p.op("pe", lambda e: e.matmul(pss[:, col0:512], lhsT=KT[0:96, kt * 128:(kt + 1) * 128],
                                              rhs=QT[0:96, qb * 512 + col0:(qb + 1) * 512], start=True, stop=True),
                     reads=[("KTn", kt // 4), ("KTr", kt // 4), ("QT", qb)], writes=[pss])
                p.op("act", lambda e: e.activation(out=pt[:, col0:512], in_=pss[:, col0:512], func=AF.Exp, scale=scale), reads=[pss], writes=[pt])
                if kt >= 4 * qb:
                    dc = (kt - 4 * qb) * 128
                    p.op("pool", lambda e: e.tensor_tensor(out=pt[:, dc:dc + 128], in0=pt[:, dc:dc + 128], in1=TRI[:], op=ALU.mult), reads=[pt, TRI], writes=[pt])

            def emit_pv(i):
                qb, kt, col0, pso, last = steps[i]
                pss, pt = slot.pop(i)
                p.op("pe", lambda e: e.matmul(pso[:, col0:512], lhsT=VA[:, kt, :], rhs=pt[:, col0:512], start=(kt == 0), stop=(kt == last)),
                     reads=[("VA", kt // 8), pt], writes=[pso])
                if kt == last:
                    p.op("dve", lambda e: e.reciprocal(out=rec[64:128, :], in_=pso[64:128, :]), reads=[pso], writes=[rec])
                    r0 = (h % 2) * 64
                    hh = h // 2
                    p.op("dve", lambda e: e.tensor_tensor(out=OT[r0:r0 + 64, hh, qb * 512:(qb + 1) * 512], in0=pso[0:64, :], in1=rec[64:128, :], op=ALU.mult),
                         reads=[pso, rec], writes=[("OT", qb)])
            for i in range(len(steps) + LA):
                if i < len(steps):
                    emit_s(i)
                if i - LA >= 0:
                    emit_pv(i - LA)
            si += len(steps)
            pi_ += len(steps)
        p.release(m2)
        WOUT = p.tile("WOUT", [128, 8, D], BF16)
        p.dma("pool", lambda e: e.dma_start(out=WOUT[:], in_=self.dram["mla_out"].rearrange("(c p) n -> p c n", p=128)), writes=[WOUT])
        for c in range(8):
            p.op("pool", lambda e, c=c: e.tensor_tensor(out=WOUT[:, c, :], in0=WOUT[:, c, :], in1=self.G[:], op=ALU.mult), reads=[WOUT, self.G], writes=[WOUT])
        self.out_proj(OT, WOUT)
        p.release(m0)

    def out_proj(self, OT, WOUT):
        p = self.p
        for t in range(NT):
            for hf in range(2):
                ps = self.PS[(2 * t + hf) % 4]
                for c in range(8):
                    p.op("pe", lambda e, c=c, t=t, hf=hf, ps=ps: e.matmul(ps[:], lhsT=OT[:, c, t * 128:(t + 1) * 128], rhs=WOUT[:, c, hf * 512:(hf + 1) * 512],
                                                                          start=(c == 0), stop=(c == 7)), reads=[("OT", t // 4), WOUT], writes=[ps])
                xs = self.X[:, t, hf * 512:(hf + 1) * 512]
                p.op("dve", lambda e, xs=xs, ps=ps: e.tensor_tensor(out=xs, in0=ps[:], in1=xs, op=ALU.add), reads=[ps, ("X", t)], writes=[("X", t)])


    def mm(self, out, lhsT, rhs, start, stop, reads, writes):
        self.p.op("pe", lambda e: e.matmul(out, lhsT=lhsT, rhs=rhs, start=start, stop=stop), reads=reads, writes=writes)

    def tr(self, out, in_, ident, reads, writes):
        self.p.op("pe", lambda e: e.transpose(out, in_, ident), reads=list(reads) + [ident], writes=writes)

    def gdn_phase(self, s):
        p = self.p
        PS = self.PS
        PSb = [b.bitcast(BF16) for b in PS]
        HTr = [("HT", b) for b in range(NB)]
        m0 = p.mark()
        NEG = -1.0e30
        LTRI = p.tile("LTRI", [128, 128], F32)
        BDm = p.tile("BDm", [128, 128], F32)
        SEL = [p.tile(f"SEL{i}", [128, 128], F32) for i in range(2)]
        MNi = p.tile("MNi", [128, 128], F32)
        MNs = p.tile("MNs", [128, 128], F32)
        hp = p.tile("hp", [128, 24], F32)
        nA = p.tile("nA", [128, 8], F32)
        GON = p.tile("GON", [128, 128], F32)
        WBA = p.tile("WBA", [128, 8, 16], BF16)
        CW = p.tile("CW", [128, 8, 3, 4], F32)
        pl = lambda fn, r=(), w=(): p.op("pool", fn, reads=r, writes=w)
        pl(lambda e: e.memset(LTRI[:], 1.0), w=[LTRI])
        pl(lambda e: e.affine_select(out=LTRI[:], in_=LTRI[:], pattern=[[1, 128]], compare_op=ALU.is_ge, fill=0.0, base=0, channel_multiplier=-1), r=[LTRI], w=[LTRI])
        pl(lambda e: e.memset(LTRI[0:64, 64:128], 0.0), r=[LTRI], w=[LTRI])
        pl(lambda e: e.memset(BDm[:], 0.0), w=[BDm])
        pl(lambda e: e.memset(BDm[0:64, 0:64], 1.0), r=[BDm], w=[BDm])
        pl(lambda e: e.memset(BDm[64:128, 64:128], 1.0), r=[BDm], w=[BDm])
        for i in range(2):
            pl(lambda e, i=i: e.memset(SEL[i][:], 0.0), w=[SEL[i]])
            pl(lambda e, i=i: e.memset(SEL[i][64 * i:64 * i + 1, :], 1.0), r=[SEL[i]], w=[SEL[i]])
        pl(lambda e: e.memset(MNi[:], 0.0), w=[MNi])
        pl(lambda e: e.affine_select(out=MNi[:], in_=MNi[:], pattern=[[1, 128]], compare_op=ALU.is_ge, fill=NEG, base=0, channel_multiplier=-1), r=[MNi], w=[MNi])
        pl(lambda e: e.memset(MNi[0:64, 64:128], NEG), r=[MNi], w=[MNi])
        pl(lambda e: e.memset(MNs[:], 0.0), w=[MNs])
        pl(lambda e: e.affine_select(out=MNs[:], in_=MNs[:], pattern=[[1, 128]], compare_op=ALU.is_gt, fill=NEG, base=0, channel_multiplier=-1), r=[MNs], w=[MNs])
        pl(lambda e: e.memset(MNs[0:64, 64:128], NEG), r=[MNs], w=[MNs])
        p.dma("sp", lambda e: e.dma_start(out=hp[:], in_=self.dram["gdn_hp"]), writes=[hp])
        p.dma("sp", lambda e: e.dma_start(out=GON[:], in_=self.dram["gdn_on"]), writes=[GON])
        p.dma("sp", lambda e: e.dma_start(out=CW[:], in_=self.dram["gdn_conv"]), writes=[CW])
        p.dma("pool", lambda e: e.dma_start(out=WBA[:], in_=self.dram["gdn_ba"]), writes=[WBA])
        p.op("act", lambda e: e.activation(out=nA[:], in_=hp[:, 0:8], func=AF.Exp), reads=[hp], writes=[nA])
        p.op("dve", lambda e: e.tensor_scalar(out=nA[:], in0=nA[:], scalar1=-1.0, scalar2=None, op0=ALU.mult), reads=[nA], writes=[nA])
        BETA = p.tile("BETA", [128, NT, 8], F32)
        GAMS = p.tile("GAMS", [128, NT, 8], F32)
        EGL = p.tile("EGL", [128, NT, 8], F32)
        BG = p.tile("BG", [128, NT, 8], F32)
        DECB = p.tile("DECB", [128, NT, 2, 8], F32)
        GC3 = p.tile("GC3", [128, NT, 3, 8], F32)
        m1 = p.mark()
        ba = p.tile("ba", [128, 16], F32)
        xs = p.tile("gxs", [128, 8], F32)
        ax = p.tile("gax", [128, 8], F32)
        gg = p.tile("ggg", [128, 8], F32)
        lnb = p.tile("lnb", [128, 8], F32)
        gcl = p.tile("gcl", [128, 2, 8], F32)
        dk_s = 128.0 ** -0.5
        import os
        pre = int(os.environ.get("GDN_PRE", "99"))
        if "GDN_OPS" in os.environ:
            p.limit = int(os.environ["GDN_OPS"])
        for t in range(NT if pre >= 2 else pre):
            ts_ = slice(t * 128, (t + 1) * 128)
            ps = PS[t % 2]
            for k in range(8):
                self.mm(ps[:, 0:16], self.HT[:, k, ts_], WBA[:, k, :], k == 0, k == 7, [HTr[t // 4], WBA], [ps])
            p.op("act", lambda e, ps=ps: e.copy(ba[:], ps[:, 0:16]), reads=[ps], writes=[ba])
            p.op("act", lambda e, t=t: e.activation(out=BETA[:, t, :], in_=ba[:, 0:8], func=AF.Sigmoid), reads=[ba], writes=[BETA])
            p.op("act", lambda e, t=t: e.activation(out=lnb[:], in_=BETA[:, t, :], func=AF.Ln), reads=[BETA], writes=[lnb])
            p.op("dve", lambda e: e.tensor_tensor(out=xs[:], in0=ba[:, 8:16], in1=hp[:, 8:16], op=ALU.add), reads=[ba, hp], writes=[xs])
            p.op("act", lambda e: e.activation(out=ax[:], in_=xs[:], func=AF.Abs), reads=[xs], writes=[ax])
            p.op("act", lambda e: e.activation(out=ax[:], in_=ax[:], func=AF.Exp, scale=-1.0), reads=[ax], writes=[ax])
            p.op("act", lambda e: e.activation(out=ax[:], in_=ax[:], func=AF.Ln, bias=self.onesf[:, 0:1], scale=1.0), reads=[ax, self.onesf], writes=[ax])
            p.op("dve", lambda e: e.scalar_tensor_tensor(out=gg[:], in0=xs[:], scalar=0.0, in1=ax[:], op0=ALU.max, op1=ALU.add), reads=[xs, ax], writes=[gg])
            p.op("dve", lambda e: e.tensor_tensor(out=gg[:], in0=gg[:], in1=nA[:], op=ALU.mult), reads=[gg, nA], writes=[gg])
            self.mm(ps[:, 16:24], LTRI[:], gg[:], True, True, [LTRI, gg], [ps])
            self.mm(ps[:, 24:32], BDm[:], gg[:], True, True, [BDm, gg], [ps])
            p.op("act", lambda e, ps=ps: e.copy(gcl[:].rearrange("p a b -> p (a b)"), ps[:, 16:32]), reads=[ps], writes=[gcl])
            p.op("act", lambda e, t=t: e.activation(out=BG[:, t, :], in_=gcl[:, 0, :], func=AF.Exp), reads=[gcl], writes=[BG])
            p.op("dve", lambda e, t=t: e.tensor_scalar(out=GAMS[:, t, :], in0=BG[:, t, :], scalar1=dk_s, scalar2=None, op0=ALU.mult), reads=[BG], writes=[GAMS])
            p.op("dve", lambda e, t=t: e.tensor_tensor(out=BG[:, t, :], in0=BG[:, t, :], in1=BETA[:, t, :], op=ALU.mult), reads=[BG, BETA], writes=[BG])
            p.op("dve", lambda e, t=t: e.tensor_tensor(out=EGL[:, t, :], in0=gcl[:, 1, :], in1=gcl[:, 0, :], op=ALU.subtract), reads=[gcl], writes=[EGL])
            p.op("act", lambda e, t=t: e.activation(out=EGL[:, t, :], in_=EGL[:, t, :], func=AF.Exp), reads=[EGL], writes=[EGL])
            p.op("dve", lambda e, t=t: e.tensor_copy(GC3[:, t, 0, :], gcl[:, 0, :]), reads=[gcl], writes=[GC3])
            p.op("dve", lambda e, t=t: e.tensor_scalar(out=GC3[:, t, 1, :], in0=gcl[:, 0, :], scalar1=-1.0, scalar2=None, op0=ALU.mult), reads=[gcl, GC3], writes=[GC3])
            p.op("dve", lambda e, t=t: e.tensor_tensor(out=GC3[:, t, 2, :], in0=gcl[:, 0, :], in1=lnb[:], op=ALU.add), reads=[gcl, lnb, GC3], writes=[GC3])
            for hf in range(2):
                self.mm(ps[:, 32 + hf * 8:40 + hf * 8], SEL[hf][:], gcl[:, 1, :], True, True, [SEL[hf], gcl], [ps])
            p.op("act", lambda e, t=t, ps=ps: e.activation(out=DECB[:, t, :, :].rearrange("p a b -> p (a b)"), in_=ps[:, 32:48], func=AF.Exp), reads=[ps], writes=[DECB])
        p.limit = None
        p.release(m1)
        NPAR = int(os.environ.get("GDN_NPAR", "2"))
        HB = []
        for sl in range(NPAR):
            b = {}
            b["WH"] = p.tile(f"WH{sl}", [128, 8, 512], BF16)
            b["WO"] = p.tile(f"WO{sl}", [128, D], BF16)
            b["DG"] = p.tile(f"DG{sl}", [128, 12, 128], BF16)
            b["S"] = p.tile(f"S{sl}", [128, 128], F32)
            b["Sb"] = p.tile(f"Sb{sl}", [128, 128], BF16)
            b["XC"] = p.tile(f"XC{sl}", [128, 3, 515], BF16)
            for nm, shp, dt in (("ZS", [128, 128], F32), ("GZ", [128, 128], F32), ("QKV", [128, 3, 128], F32), ("SQ", [128, 2, 128], F32),
                                ("st", [128, 8], F32), ("TOK", [128, 6, 128], BF16), ("DGC", [128, 2, 128], F32), ("KQT", [128, 3, 128], BF16), ("DT2", [128, 2, 128], F32),
                                ("M2", [128, 2, 128], F32), ("ATb", [128, 128], BF16), ("YA0", [128, 2, 128], F32), ("YA1", [128, 2, 128], F32),
                                ("P0", [128, 128], F32), ("P1", [128, 128], F32), ("TTb", [128, 128], BF16), ("U", [128, 128], F32),
                                ("WTb", [128, 128], BF16), ("VNb", [128, 128], BF16), ("OT", [128, 128], F32), ("OG", [128, 128], BF16),
                                ("OGT", [128, 128], BF16), ("junk", [128, 128], BF16)):
                b[nm] = p.tile(f"{nm}{sl}", shp, dt)
            b["bank"] = [PS[4 * sl + i] for i in range(4)]
            b["bankb"] = [PSb[4 * sl + i] for i in range(4)]
            HB.append(b)

        def head_gen(h, b):
            sl = HB.index(b)
            WH, WO, DG, S, Sb, XC = b["WH"], b["WO"], b["DG"], b["S"], b["Sb"], b["XC"]
            bk, bkb = b["bank"], b["bankb"]
            R = lambda i, nm: (f"ps{sl}_{i}")
            r_zc = [R(0, "zc")]
            r_tr, r_d, r_og = [R(1, "tr")], [R(1, "d")], [R(1, "og")]
            r_kk, r_a, r_p = [R(2, "kk")], [R(2, "a")], [R(2, "p")]
            r_ya, r_u, r_wt = [R(3, "ya")], [R(3, "u")], [R(3, "wt")]
            allb = [r_zc, r_tr, r_kk, r_ya]
            p.dma("pool", lambda e: e.dma_start(out=WH[:].rearrange("p k n -> p (k n)"), in_=self.dram["gdn_in"][h].rearrange("p k n -> p (k n)")), writes=[WH])
            p.dma("pool", lambda e: e.dma_start(out=WO[:], in_=self.dram["gdn_out"][h * 128:(h + 1) * 128, :]), writes=[WO])
            p.op("pool", lambda e: e.tensor_tensor(out=WO[:], in0=WO[:], in1=self.G[:], op=ALU.mult), reads=[WO, self.G], writes=[WO])
            for part in range(3):
                for j in range(4):
                    p.op("dve", lambda e, part=part, j=j: e.tensor_scalar(out=DG[:, part * 4 + j, :], in0=self.identb[:], scalar1=CW[:, h, part, j:j + 1], scalar2=None, op0=ALU.mult),
                         reads=[self.identb, CW], writes=[DG])
            p.op("pool", lambda e: e.memset(S[:], 0.0), writes=[S])
            p.op("pool", lambda e: e.memset(Sb[:], 0.0), writes=[Sb])
            p.op("pool", lambda e: e.memset(XC[:, :, 0:3], 0.0), writes=[XC])
            yield
            for blk in range(NB):
                cs = slice(blk * 512, (blk + 1) * 512)
                for part in range(3):
                    ps = bk[part]
                    for k in range(8):
                        self.mm(ps[:], WH[:, k, part * 128:(part + 1) * 128], self.HT[:, k, cs], k == 0, k == 7, [WH, HTr[blk]], allb[part])
                    eng = ("act", "dve", "act")[part]
                    if eng == "act":
                        p.op("act", lambda e, part=part, ps=ps: e.copy(XC[:, part, 3:515], ps[:]), reads=allb[part], writes=[XC])
                    else:
                        p.op("dve", lambda e, part=part, ps=ps: e.tensor_copy(XC[:, part, 3:515], ps[:]), reads=allb[part], writes=[XC])
                yield
                for tt in range(4):
                    t = blk * 4 + tt
                    ts_ = slice(t * 128, (t + 1) * 128)
                    ZS, GZ, QKV, SQ, st, TOK, KQT, DT2, M2, ATb = (b[n] for n in ("ZS", "GZ", "QKV", "SQ", "st", "TOK", "KQT", "DT2", "M2", "ATb"))
                    YA, Pb, TTb, U, WTb, VNb, OTk, OG, OGT = [b["YA0"], b["YA1"]], [b["P0"], b["P1"]], b["TTb"], b["U"], b["WTb"], b["VNb"], b["OT"], b["OG"], b["OGT"]
                    for k in range(8):
                        self.mm(bk[0][:, 0:128], self.HT[:, k, ts_], WH[:, k, 384:512], k == 0, k == 7, [WH, HTr[blk]], r_zc)
                    for part in range(3):
                        for j in range(4):
                            self.mm(bk[0][:, 128 + part * 128:256 + part * 128], XC[:, part, tt * 128 + j:tt * 128 + j + 128], DG[:, part * 4 + j, :], j == 0, j == 3, [XC, DG], r_zc)
                    p.op("act", lambda e: e.activation(out=ZS[:], in_=bk[0][:, 0:128], func=AF.Silu), reads=r_zc, writes=[ZS])
                    p.op("act", lambda e: e.activation(out=QKV[:].rearrange("p a b -> p (a b)"), in_=bk[0][:, 128:512], func=AF.Silu), reads=r_zc, writes=[QKV])
                    p.op("pool", lambda e: e.tensor_tensor(out=GZ[:], in0=ZS[:], in1=GON[:], op=ALU.mult), reads=[ZS, GON], writes=[GZ])
                    yield
                    if "GDN_OPS2" in os.environ and h == 0 and t == 0:
                        p.limit = int(os.environ["GDN_OPS2"])
                    p.op("dve", lambda e: e.tensor_tensor(out=SQ[:], in0=QKV[:, 0:2, :], in1=QKV[:, 0:2, :], op=ALU.mult), reads=[QKV], writes=[SQ])
                    p.op("dve", lambda e: e.tensor_reduce(out=st[:, 0:2], in_=SQ[:], axis=AX.X, op=ALU.add), reads=[SQ], writes=[st])
                    p.op("act", lambda e: e.activation(out=st[:, 0:2], in_=st[:, 0:2], func=AF.Sqrt, bias=self.epsc[:], scale=1.0), reads=[st, self.epsc], writes=[st])
                    p.op("dve", lambda e: e.reciprocal(out=st[:, 2:4], in_=st[:, 0:2]), reads=[st], writes=[st])
                    p.op("act", lambda e: e.activation(out=TOK[:, 0, :], in_=QKV[:, 1, :], func=AF.Copy, scale=st[:, 3:4]), reads=[QKV, st], writes=[(TOK.name, 0)])
                    p.op("dve", lambda e: e.tensor_scalar(out=TOK[:, 1, :], in0=QKV[:, 0, :], scalar1=st[:, 2:3], scalar2=dk_s, op0=ALU.mult, op1=ALU.mult), reads=[QKV, st], writes=[(TOK.name, 0)])
                    p.op("dve", lambda e, t=t: e.tensor_scalar(out=TOK[:, 2, :], in0=QKV[:, 0, :], scalar1=st[:, 2:3], scalar2=GAMS[:, t, h:h + 1], op0=ALU.mult, op1=ALU.mult), reads=[QKV, st, GAMS], writes=[(TOK.name, 0)])
                    p.op("dve", lambda e, t=t: e.tensor_scalar(out=TOK[:, 3, :], in0=QKV[:, 1, :], scalar1=st[:, 3:4], scalar2=EGL[:, t, h:h + 1], op0=ALU.mult, op1=ALU.mult), reads=[QKV, st, EGL], writes=[(TOK.name, 1)])
                    p.op("dve", lambda e, t=t: e.tensor_scalar(out=TOK[:, 4, :], in0=QKV[:, 1, :], scalar1=st[:, 3:4], scalar2=BG[:, t, h:h + 1], op0=ALU.mult, op1=ALU.mult), reads=[QKV, st, BG], writes=[(TOK.name, 1)])
                    p.op("act", lambda e, t=t: e.activation(out=TOK[:, 5, :], in_=QKV[:, 2, :], func=AF.Copy, scale=BETA[:, t, h:h + 1]), reads=[QKV, BETA], writes=[(TOK.name, 1)])
                    for i in range(3):
                        self.tr(bkb[1][:, i * 128:(i + 1) * 128], TOK[:, i, :], self.identb[:], [(TOK.name, 0)], r_tr)
                    p.op("act", lambda e: e.copy(KQT[:].rearrange("p a b -> p (a b)"), bkb[1][:, 0:384]), reads=r_tr, writes=[KQT])
                    yield
                    d0 = 256
                    DGC = b["DGC"]
                    p.op("act", lambda e, t=t: e.activation(out=DGC[:, 0, :], in_=self.identf[:], func=AF.Copy, scale=GC3[:, t, 0, h:h + 1]), reads=[self.identf, GC3], writes=[DGC])
                    p.op("act", lambda e, t=t: e.activation(out=DGC[:, 1, :], in_=self.identf[:], func=AF.Copy, scale=GC3[:, t, 2, h:h + 1]), reads=[self.identf, GC3], writes=[DGC])
                    self.mm(bk[1][:, d0:d0 + 128], self.onesf[:], DGC[:, 0, :], True, False, [self.onesf, DGC], r_d)
                    self.mm(bk[1][:, d0:d0 + 128], self.identf[:], MNi[:], False, True, [self.identf, MNi], r_d)
                    self.mm(bk[1][:, d0 + 128:d0 + 256], self.onesf[:], DGC[:, 1, :], True, False, [self.onesf, DGC], r_d)
                    self.mm(bk[1][:, d0 + 128:d0 + 256], self.identf[:], MNs[:], False, True, [self.identf, MNs], r_d)
                    p.op("act", lambda e, t=t: e.activation(out=DT2[:].rearrange("p a b -> p (a b)"), in_=bk[1][:, d0:d0 + 256], func=AF.Exp, bias=GC3[:, t, 1, h:h + 1], scale=1.0),
                         reads=r_d + [GC3], writes=[DT2])
                    yield
                    self.mm(bk[2][:, 0:128], KQT[:, 0, :], KQT[:, 1, :], True, True, [KQT], r_kk)
                    self.mm(bk[2][:, 128:256], KQT[:, 0, :], KQT[:, 0, :], True, True, [KQT], r_kk)
                    p.op("dve", lambda e: e.tensor_tensor(out=M2[:].rearrange("p a b -> p (a b)"), in0=bk[2][:, 0:256], in1=DT2[:].rearrange("p a b -> p (a b)"), op=ALU.mult),
                         reads=r_kk + [DT2], writes=[M2])
                    p.op("act", lambda e: e.copy(ATb[:], M2[:, 0, :]), reads=[M2], writes=[ATb])
                    yield
                    Y = M2[:, 1, :]
                    self.tr(bk[2][:, 256:384], Y, self.identf[:], [M2], r_a)
                    p.op("act", lambda e: e.copy(YA[0][:, 1, :], bk[2][:, 256:384]), reads=r_a, writes=[(YA[0].name, 1)])
                    p.op("pool", lambda e: e.tensor_copy(YA[0][:, 0, :], Y), reads=[M2], writes=[(YA[0].name, 0)])
                    p.op("pool", lambda e: e.tensor_tensor(out=Pb[0][:], in0=self.identf[:], in1=Y, op=ALU.subtract), reads=[M2, self.identf], writes=[Pb[0]])
                    yield
                    for it in range(5):
                        cur, nxt = YA[it % 2], YA[(it + 1) % 2]
                        pc, pn = Pb[it % 2], Pb[(it + 1) % 2]
                        rc = [(cur.name, 0), (cur.name, 1)]
                        if it < 4:
                            self.mm(bk[3][:, 0:128], cur[:, 1, :], cur[:, 0, :], True, True, rc, r_ya)
                        self.mm(bk[3][:, 128:256], cur[:, 0, :], cur[:, 1, :], True, True, rc, r_ya)
                        if it < 4:
                            p.op("act", lambda e, nxt=nxt: e.copy(nxt[:].rearrange("p a b -> p (a b)"), bk[3][:, 0:256]), reads=r_ya, writes=[(nxt.name, 0), (nxt.name, 1)])
                        else:
                            p.op("act", lambda e, nxt=nxt: e.copy(nxt[:, 1, :], bk[3][:, 128:256]), reads=r_ya, writes=[(nxt.name, 1)])
                        yield
                        self.mm(bk[2][:, 384:512], self.identf[:], pc[:], True, False, [self.identf, pc], r_p)
                        self.mm(bk[2][:, 384:512], nxt[:, 1, :], pc[:], False, True, [(nxt.name, 1), pc], r_p)
                        if it < 4:
                            p.op("dve", lambda e, pn=pn: e.tensor_copy(pn[:], bk[2][:, 384:512]), reads=r_p, writes=[pn])
                        else:
                            p.op("dve", lambda e: e.tensor_copy(TTb[:], bk[2][:, 384:512]), reads=r_p, writes=[TTb])
                        yield
                    if "GDN_OPS3" in os.environ and h == 1 and t == 0:
                        p.limit = int(os.environ["GDN_OPS3"])
                    self.mm(bk[3][:, 256:384], TTb[:], TOK[:, 5, :], True, True, [TTb, (TOK.name, 1)], r_u)
                    self.mm(bk[0][:, 0:128], TOK[:, 4, :], TTb[:], True, True, [TTb, (TOK.name, 1)], r_zc)
                    p.op("act", lambda e: e.copy(U[:], bk[3][:, 256:384]), reads=r_u, writes=[U])
                    p.op("dve", lambda e: e.tensor_copy(WTb[:], bk[0][:, 0:128]), reads=r_zc, writes=[WTb])
                    yield
                    for hf in range(2):
                        r0 = hf * 64
                        rr = slice(r0, r0 + 64)
                        if "GDN_OPS4" in os.environ and h == int(os.environ.get("GDN_H", "0")) and t == 0 and hf == 0:
                            p.limit = int(os.environ["GDN_OPS4"])
                        if os.environ.get("GDN_M128") == "1":
                            self.mm(bk[2][:, 0:128], WTb[:, :], Sb[:], True, True, [WTb, Sb], r_kk)
                        elif os.environ.get("GDN_M128") == "2":
                            self.mm(bk[2][0:64, 0:128], KQT[:, 2, rr], Sb[:], True, True, [KQT, Sb], r_kk)
                        elif os.environ.get("GDN_M128") == "4":
                            self.mm(bk[0][:, 0:128], WTb[:, :], Sb[:], True, True, [WTb, Sb], r_zc)
                        elif os.environ.get("GDN_M128") == "5":
                            self.mm(bk[2][:, 384:512], WTb[:, :], Sb[:], True, True, [WTb, Sb], r_p)
                        elif os.environ.get("GDN_M128") == "3":
                            self.mm(bk[2][0:64, 0:128], WTb[:, rr], KQT[:, 2, :], True, True, [KQT, WTb], r_kk)
                        else:
                            self.mm(bk[2][0:64, 0:128], WTb[:, rr], Sb[:], True, True, [WTb, Sb], r_kk)
                        p.op("dve", lambda e, rr=rr: e.scalar_tensor_tensor(out=VNb[rr, :], in0=bk[2][0:64, 0:128], scalar=-1.0, in1=U[rr, :], op0=ALU.mult, op1=ALU.add), reads=[U] + r_kk, writes=[VNb])
                        yield
                        self.mm(bk[2][0:64, 128:256], KQT[:, 2, rr], Sb[:], True, False, [KQT, Sb], r_kk)
                        self.mm(bk[2][0:64, 128:256], ATb[rr, rr], VNb[rr, :], False, True, [ATb, VNb], r_kk)
                        p.op("act", lambda e, rr=rr: e.copy(OTk[rr, :], bk[2][0:64, 128:256]), reads=r_kk, writes=[OTk])
                        self.mm(bk[2][:, 256:384], TOK[rr, 3, :], VNb[rr, :], True, True, [(TOK.name, 1), VNb], r_a)
                        p.op("act", lambda e, t=t, hf=hf: e.activation(out=S[:], in_=S[:], func=AF.Copy, scale=DECB[:, t, hf, h:h + 1]), reads=[S, DECB], writes=[S])
                        p.op("dve", lambda e: e.tensor_tensor(out=S[:], in0=bk[2][:, 256:384], in1=S[:], op=ALU.add), reads=[S] + r_a, writes=[S])
                        p.op("act", lambda e: e.copy(Sb[:], S[:]), reads=[S], writes=[Sb])
                        yield
                    p.op("pool", lambda e: e.memset(st[:, 4:5], 0.0), reads=[st], writes=[st])
                    p.op("act", lambda e: e.activation(out=b["junk"][:], in_=OTk[:], func=AF.Square, accum_out=st[:, 4:5]), reads=[OTk, st], writes=[b["junk"], st])
                    p.op("act", lambda e: e.activation(out=st[:, 4:5], in_=st[:, 4:5], func=AF.Sqrt, bias=self.epsc[:], scale=1.0 / 128), reads=[st, self.epsc], writes=[st])
                    p.op("dve", lambda e: e.reciprocal(out=st[:, 5:6], in_=st[:, 4:5]), reads=[st], writes=[st])
                    p.op("dve", lambda e: e.scalar_tensor_tensor(out=OG[:], in0=OTk[:], scalar=st[:, 5:6], in1=GZ[:], op0=ALU.mult, op1=ALU.mult), reads=[OTk, st, GZ], writes=[OG])
                    yield
                    self.tr(bkb[1][:, 384:512], OG[:], self.identb[:], [OG], r_og)
                    p.op("act", lambda e: e.copy(OGT[:], bkb[1][:, 384:512]), reads=r_og, writes=[OGT])
                    yield
                    for hf in range(2):
                        self.mm(bk[0][:], OGT[:], WO[:, hf * 512:(hf + 1) * 512], True, True, [OGT, WO], r_zc)
                        xs_ = self.X[:, t, hf * 512:(hf + 1) * 512]
                        p.op("dve", lambda e, xs_=xs_: e.tensor_tensor(out=xs_, in0=bk[0][:], in1=xs_, op=ALU.add), reads=r_zc + [("X", t)], writes=[("X", t)])
                    yield
                p.op("dve", lambda e: e.tensor_copy(XC[:, :, 0:3], XC[:, :, 512:515]), reads=[XC], writes=[XC])
                yield

        import os
        steps = int(os.environ.get("GDN_STEPS", "1000000000"))
        for h0 in range(0, 8, NPAR):
            gens = [head_gen(h0 + i, HB[i]) for i in range(NPAR)]
            alive = list(gens)
            while alive and steps > 0:
                for g_ in list(alive):
                    steps -= 1
                    if steps <= 0:
                        break
                    try:
                        next(g_)
                    except StopIteration:
                        alive.remove(g_)
        if "GDN_DUMP" in os.environ:
            nm = os.environ["GDN_DUMP"]
            src = HB[0][nm]
            self.dump = p.tile("dump", [128, 512], F32)
            n = int(np.prod(src.shape[1:]))
            v = src[:] if len(src.shape) == 2 else src[:].rearrange("p a b -> p (a b)")
            p.op("pool", lambda e: e.memset(self.dump[:], 0.0), writes=[self.dump])
            p.op("dve", lambda e: e.tensor_copy(self.dump[:, 0:min(n, 512)], v[:, 0:min(n, 512)]), reads=[src, self.dump] + [(src.name, i) for i in range(2)], writes=[self.dump])
            p.dma("sp", lambda e: e.dma_start(out=self.y[s, 0:128, 0:512], in_=self.dump[:]), reads=[self.dump], key="dump")
            p.barrier()
            self.skip0 = True
        p.release(m0)

    def build(self):
        p = self.p
        st = self.stages
        self.setup()
        self.prologue_mod()
        final = []
        for s in range(self.nseq):
            self.load_x(s)
            for l in range(2):
                if f"mix{l}" in st:
                    self.norm_phase(l, s, 0)
                    self.make_gate(l, s, 2)
                    (self.mla_phase if l == 0 else self.gdn_phase)(s)
                if f"ffn{l}" in st:
                    self.norm_phase(l, s, 1, router=(l == 1))
                    self.make_gate(l, s, 5)
                    if l == 0:
                        ex = [(self.dram["ffn_gu"][j], self.dram["ffn_dn"][j], None) for j in range(2)]
                    else:
                        ex = [(self.dram["moe_gu"][j], self.dram["moe_dn"][j], j) for j in range(8)]
                    self.ffn_phase(ex)
            final += self.final_out(s)
        p.wait("sp", final)
        p.emit()
        return self.nc


def _cols(v, nk):
    return np.ascontiguousarray(np.asarray(v).reshape(nk, 128).T)


def _gu_layout(w, ff):
    nch = ff // 128
    g = w[:, :ff].reshape(8, 128, nch, 128)
    u = w[:, ff:].reshape(8, 128, nch, 128)
    gu = np.concatenate([g, u], axis=-1)
    return np.ascontiguousarray(gu.transpose(2, 1, 0, 3)).reshape(nch, 128, 8 * 256)


def _rope_consts():
    c = np.zeros((128, 2), np.float64)
    inv = 10000.0 ** (-np.arange(16, dtype=np.float64) / 16.0)
    for j in range(32):
        c[64 + j, 0] = inv[j % 16] / (2 * math.pi); c[64 + j, 1] = 0.25
        c[96 + j, 0] = inv[j % 16] / (2 * math.pi); c[96 + j, 1] = 0.5 if j < 16 else 0.0
    return c.astype(np.float32)


ROPEC = _rope_consts()


def prep_shared(inp):
    f = lambda k: np.asarray(inp[k], dtype=np.float32)
    sh = {}
    sh["ada"] = np.ascontiguousarray(np.concatenate([f("ada_w"), f("ada_b")[:, None, :]], axis=1))
    sh["gmix"] = np.stack([_cols(f("norm_mix")[l], 8) for l in range(2)])
    sh["gffn"] = np.stack([_cols(f("norm_ffn")[l], 8) for l in range(2)])
    sh["gfin"] = np.ascontiguousarray(np.broadcast_to(f("final_norm")[None, :], (128, D)))
    wgu = f("ffn_w_gate_up")[0]
    halves = []
    for j in range(2):
        sub = np.concatenate([wgu[:, j * 1408:(j + 1) * 1408], wgu[:, 2816 + j * 1408:2816 + (j + 1) * 1408]], axis=1)
        halves.append(_gu_layout(sub, 1408))
    sh["ffn_gu"] = np.stack(halves)
    sh["ffn_dn"] = np.ascontiguousarray(f("ffn_w_down")[0].reshape(2, 1408, D))
    sh["moe_gu"] = np.stack([_gu_layout(f("moe_w_gate_up")[0, e], 1408) for e in range(8)])
    sh["moe_dn"] = np.ascontiguousarray(f("moe_w_down")[0])
    sh["moe_r"] = np.ascontiguousarray(f("moe_w_router")[0].reshape(8, 128, 8).transpose(1, 0, 2))

    perm = np.concatenate([np.arange(16, 32), np.arange(0, 16)])
    win = f("mla_w_in")[0]
    kr = win[:, 768:800]
    win2 = np.concatenate([win[:, :768], win[:, 0:64], kr, kr[:, perm]], axis=1)
    sh["mla_in"] = np.ascontiguousarray(win2.reshape(8, 128, 896).transpose(1, 0, 2))
    sh["mla_qn"] = _cols(f("mla_q_norm")[0], 4)
    sh["mla_kvn"] = _cols(f("mla_kv_norm")[0], 2)
    wqb = f("mla_w_qb")[0].reshape(512, 16, 96)
    wqb2 = np.concatenate([wqb[:, :, :64], wqb[:, :, 64:96], wqb[:, :, 64:96][:, :, perm]], axis=2).reshape(512, 2048)
    sh["mla_qb"] = np.ascontiguousarray(wqb2.reshape(4, 128, 2048).transpose(1, 0, 2))
    wkv = f("mla_w_kvb")[0].reshape(256, 16, 128)
    sh["mla_kb"] = np.ascontiguousarray(wkv[:, :, :64].reshape(2, 128, 1024).transpose(1, 0, 2))
    sh["mla_vb"] = np.ascontiguousarray(wkv[:, :, 64:].reshape(2, 128, 1024).transpose(1, 0, 2))
    sh["mla_out"] = np.ascontiguousarray(f("mla_w_out")[0])
    sh["ropec"] = ROPEC
    gw = f("gdn_w_in")[0]
    heads = []
    for h in range(8):
        cols = np.concatenate([np.arange(h * 128, (h + 1) * 128) + off for off in (0, 1024, 2048, 3072)])
        heads.append(gw[:, cols].reshape(8, 128, 512).transpose(1, 0, 2))
    sh["gdn_in"] = np.ascontiguousarray(np.stack(heads))
    sh["gdn_ba"] = np.ascontiguousarray(gw[:, 4096:4112].reshape(8, 128, 16).transpose(1, 0, 2))
    cw = f("gdn_conv_w")[0]
    sh["gdn_conv"] = np.ascontiguousarray(cw.reshape(4, 3, 8, 128).transpose(3, 2, 1, 0))
    hpv = np.zeros((128, 24), np.float32)
    hpv[:, 0:8] = f("gdn_a_log")[0][None, :]
    hpv[:, 8:16] = f("gdn_dt_bias")[0][None, :]
    sh["gdn_hp"] = hpv
    sh["gdn_on"] = np.ascontiguousarray(np.broadcast_to(f("gdn_out_norm")[0][None, :], (128, 128)))
    sh["gdn_out"] = np.ascontiguousarray(f("gdn_w_out")[0])
    return sh


def prep_core(inp, b0, ns):
    x = np.ascontiguousarray(np.asarray(inp["x"], np.float32)[b0:b0 + ns])
    c = np.asarray(inp["c"], np.float32)[b0:b0 + ns]
    cT = np.stack([_cols(c[i], 8) for i in range(ns)])
    pos = np.asarray(inp["positions"]).astype(np.int32)[b0:b0 + ns]
    posr = np.ascontiguousarray(np.broadcast_to(pos[:, None, :], (ns, 128, SEQ)))
    return {"x": x, "cT": cT, "pos": posr}


_CACHE = {}


def run(inp, batches, ns, stages):
    key = (ns, tuple(sorted(stages)))
    if key not in _CACHE:
        _CACHE[key] = Builder(ns, stages).build()
    nc = _CACHE[key]
    sh = prep_shared(inp)
    maps = []
    for b0 in batches:
        mm = dict(sh)
        mm.update(prep_core(inp, b0, ns))
        maps.append(mm)
    res = run_bass_kernel_spmd(nc, maps, core_ids=list(range(len(batches))))
    return [r["y"] for r in res.results]


ALL = ("mix0", "ffn0", "mix1", "ffn1")


def kernel(**inputs):
    ys = run(inputs, [2 * i for i in range(NCORES)], 2, ALL)
    return np.concatenate(ys, axis=0).astype(np.float32)
```

```python
import math
import numpy as np
import concourse.bass as bass
import concourse.mybir as mybir
from concourse.bass_utils import run_bass_kernel_spmd

F32 = mybir.dt.float32
BF16 = mybir.dt.bfloat16
I32 = mybir.dt.int32
AF = mybir.ActivationFunctionType
ALU = mybir.AluOpType
AX = mybir.AxisListType

NCORES = 8
SEQ = 2048
D = 1024
NT = SEQ // 128
NB = SEQ // 512
EPS = 1e-6
GEN = 30000


class Prog:
    ENG = ("pe", "act", "dve", "pool", "sp")

    def __init__(self, nc):
        self.nc = nc
        self.q = {e: [] for e in self.ENG}
        self.cnt = {e: 0 for e in self.ENG}
        self.res = {}
        self.seen = {e: {} for e in self.ENG}
        self.dma_cnt = {}
        self.semkeys = set()
        self.off = 16512
        self.ntile = 0
        self.cap = nc.SBUF_PARTITION_SIZE_BYTES

    def tile(self, name, shape, dt):
        nbytes = int(np.prod(shape[1:])) * (2 if dt == BF16 else 4)
        self.off = (self.off + 63) // 64 * 64
        assert self.off + nbytes <= self.cap, (name, self.off, nbytes)
        self.ntile += 1
        t = self.nc.alloc_sbuf_tensor_at(f"{name}_{self.ntile}", list(shape), dt, offset=self.off)
        self.off += nbytes
        return t

    def mark(self):
        return self.off

    def release(self, m):
        self.barrier()
        self.off = m

    def _key(self, r):
        return r if isinstance(r, (str, tuple)) else r.name

    def _st(self, r):
        r = self._key(r)
        s = self.res.get(r)
        if s is None:
            s = self.res[r] = {"w": None, "r": []}
        return s

    def _deps(self, eng, reads, writes):
        deps = {}

        def add(d):
            if d is None:
                return
            k, v = d
            if eng == "pe" and isinstance(k, tuple) and k[0] == "pe":
                return
            if deps.get(k, 0) < v:
                deps[k] = v
        for r in reads:
            add(self._st(r)["w"])
        for r in writes:
            s = self._st(r)
            add(s["w"])
            for d in s["r"]:
                add(d)
        out = []
        seen = self.seen[eng]
        for k, v in deps.items():
            if seen.get(k, 0) < v:
                seen[k] = v
                out.append((k, v))
        return out

    def _commit(self, token, reads, writes):
        for r in reads:
            lst = self._st(r)["r"]
            lst[:] = [d for d in lst if d[0] != token[0]]
            lst.append(token)
        for r in writes:
            s = self._st(r)
            s["w"] = token
            s["r"] = []

    limit = None

    def op(self, eng, fn, reads=(), writes=()):
        if self.limit is not None:
            if self.limit <= 0:
                return None
            self.limit -= 1
        waits = self._deps(eng, reads, writes)
        if self.limit == 0:
            print("LAST OP", eng, "waits", waits, "cnt", self.cnt, "reads", [self._key(r) for r in reads], "writes", [self._key(r) for r in writes])
        self.cnt[eng] += 1
        n = self.cnt[eng]
        key = (eng, (n - 1) // GEN)
        self.semkeys.add(key)
        token = (key, (n - 1) % GEN + 1)
        self.q[eng].append((waits, fn, (key, 1)))
        self._commit(token, reads, writes)
        return token

    def dma(self, queue, fn, reads=(), writes=(), key=None):
        if key is None:
            r0 = writes[0] if writes else reads[0]
            key = ("dma", self._key(r0))
        self.semkeys.add(key)
        waits = self._deps(queue, reads, writes)
        v = self.dma_cnt.get(key, 0) + 16
        self.dma_cnt[key] = v
        token = (key, v)
        self.q[queue].append((waits, fn, (key, 16)))
        self._commit(token, reads, writes)
        return token

    def barrier(self):
        toks = []
        for e in self.ENG:
            n = self.cnt[e]
            if n:
                toks.append(((e, (n - 1) // GEN), (n - 1) % GEN + 1))
        toks += list(self.dma_cnt.items())
        for e in self.ENG:
            self.wait(e, toks)

    def wait(self, eng, tokens):
        waits = []
        for k, v in tokens:
            if eng == "pe" and isinstance(k, tuple) and k[0] == "pe":
                continue
            if self.seen[eng].get(k, 0) < v:
                self.seen[eng][k] = v
                waits.append((k, v))
        if waits:
            self.q[eng].append((waits, None, None))

    def emit(self):
        import contextlib
        nc = self.nc
        with contextlib.ExitStack() as es:
            sems = {}
            for i, k in enumerate(sorted(self.semkeys, key=str)):
                sems[k] = es.enter_context(nc.semaphore(f"s{i}"))
            block = es.enter_context(nc.Block())
            q = self.q

            def run(engname):
                def body(e):
                    for waits, fn, inc in q[engname]:
                        for k, v in waits:
                            e.wait_ge(sems[k], v)
                        if fn is not None:
                            fn(e).then_inc(sems[inc[0]], inc[1])
                return body
            block.tensor(run("pe"))
            block.scalar(run("act"))
            block.vector(run("dve"))
            block.gpsimd(run("pool"))
            block.sync(run("sp"))


class Builder:
    def __init__(self, nseq, stages):
        self.nseq = nseq
        self.stages = stages
        nc = self.nc = bass.Bass("TRN2", target_bir_lowering=False)
        self.p = Prog(nc)
        self.dram = {}
        self.rr = 0

    def din(self, name, shape, dt=F32):
        a = self.nc.dram_tensor(name, list(shape), dt, kind="ExternalInput").ap()
        self.dram[name] = a
        return a

    def setup(self):
        p, nc, ns = self.p, self.nc, self.nseq
        d = self.din
        d("x", [ns, SEQ, D]); d("cT", [ns, 128, 8]); d("pos", [ns, 128, SEQ], I32)
        d("ada", [2, 1025, 6 * D])
        d("gmix", [2, 128, 8]); d("gffn", [2, 128, 8]); d("gfin", [128, D])
        d("ffn_gu", [2, 11, 128, 8 * 256]); d("ffn_dn", [2, 1408, D])
        d("moe_gu", [8, 11, 128, 8 * 256]); d("moe_dn", [8, 1408, D]); d("moe_r", [128, 8, 8])
        d("mla_in", [128, 8, 896]); d("mla_qn", [128, 4]); d("mla_kvn", [128, 2])
        d("mla_qb", [128, 4, 2048]); d("mla_kb", [128, 2, 1024]); d("mla_vb", [128, 2, 1024])
        d("mla_out", [D, D]); d("ropec", [128, 2])
        d("gdn_in", [8, 128, 8, 512]); d("gdn_ba", [128, 8, 16]); d("gdn_conv", [128, 8, 3, 4])
        d("gdn_hp", [128, 24]); d("gdn_on", [128, 128]); d("gdn_out", [D, D])
        self.y = nc.dram_tensor("y", [ns, SEQ, D], F32, kind="ExternalOutput").ap()

        self.X = p.tile("X", [128, NT, D], F32)
        self.HT = p.tile("HT", [128, 8, SEQ], BF16)
        self.identb = p.tile("identb", [128, 128], BF16)
        self.identf = p.tile("identf", [128, 128], F32)
        self.onesb = p.tile("onesb", [128, 128], BF16)
        self.onesf = p.tile("onesf", [128, 128], F32)
        self.epsc = p.tile("epsc", [128, 1], F32)
        self.MODC = p.tile("MODC", [128, 2, 48, ns], F32)
        self.G = p.tile("G", [128, D], F32)
        self.Acol = p.tile("Acol", [128, 8], F32)
        self.gmix = p.tile("gmix", [128, 2, 8], F32)
        self.gffn = p.tile("gffn", [128, 2, 8], F32)
        self.GF = p.tile("GF", [128, D], F32)
        self.ssq = p.tile("ssq", [128, NT], F32)
        self.rstd = p.tile("rstd", [128, NT], F32)
        self.COMB = p.tile("COMB", [128, NT, 8], F32)
        self.PS = [nc.alloc_psum_tensor(f"ps{i}", [128, 512], F32) for i in range(8)]

        p.op("pool", lambda e: e.memset(self.identf[:], 1.0), writes=[self.identf])
        p.op("pool", lambda e: e.affine_select(out=self.identf[:], in_=self.identf[:], pattern=[[-1, 128]],
                                               compare_op=ALU.is_equal, fill=0.0, base=0, channel_multiplier=1),
             reads=[self.identf], writes=[self.identf])
        p.op("dve", lambda e: e.tensor_copy(self.identb[:], self.identf[:]), reads=[self.identf], writes=[self.identb])
        p.op("pool", lambda e: e.memset(self.onesb[:], 1.0), writes=[self.onesb])
        p.op("pool", lambda e: e.memset(self.onesf[:], 1.0), writes=[self.onesf])
        p.op("pool", lambda e: e.memset(self.epsc[:], EPS), writes=[self.epsc])
        p.dma("sp", lambda e: e.dma_start(out=self.gmix[:], in_=self.dram["gmix"].rearrange("l p k -> p l k")), writes=[self.gmix])
        p.dma("sp", lambda e: e.dma_start(out=self.gffn[:], in_=self.dram["gffn"].rearrange("l p k -> p l k")), writes=[self.gffn])
        p.dma("sp", lambda e: e.dma_start(out=self.GF[:], in_=self.dram["gfin"]), writes=[self.GF])

    def prologue_mod(self):
        p, ns = self.p, self.nseq
        m = p.mark()
        cT = p.tile("cTs", [128, ns, 8], F32)
        condb = p.tile("condb", [128, 8, ns], BF16)
        for s in range(ns):
            p.dma("sp", lambda e, s=s: e.dma_start(out=cT[:, s, :], in_=self.dram["cT"][s]), writes=[cT], key=("dma", "cT", s))
        for s in range(ns):
            p.op("act", lambda e, s=s: e.activation(out=condb[:, :, s], in_=cT[:, s, :], func=AF.Silu), reads=[cT], writes=[condb])
        W = [p.tile(f"adaw{i}", [128, 8, 512], BF16) for i in range(2)]
        Bv = [p.tile(f"adab{i}", [1, 512], BF16) for i in range(2)]
        ada = self.dram["ada"]
        it = 0
        for l in range(2):
            ps = self.PS[l]
            psv = ps[:, 0:48 * ns].rearrange("p (b s) -> p b s", s=ns)
            for ch in range(12):
                w, bv = W[it % 2], Bv[it % 2]
                it += 1
                cols = slice(ch * 512, (ch + 1) * 512)
                p.dma("pool", lambda e, w=w, l=l, cols=cols: e.dma_start(
                    out=w[:], in_=ada[l, 0:1024, cols].rearrange("(k p) n -> p k n", p=128)), writes=[w])
                p.dma("pool", lambda e, bv=bv, l=l, cols=cols: e.dma_start(out=bv[:], in_=ada[l, 1024:1025, cols]), writes=[bv])
                for qd in range(4):
                    blk = ch * 4 + qd
                    for k in range(8):
                        p.op("pe", lambda e, w=w, k=k, qd=qd, blk=blk, psv=psv: e.matmul(
                            psv[:, blk, :], lhsT=w[:, k, qd * 128:(qd + 1) * 128], rhs=condb[:, k, :], start=(k == 0), stop=False),
                            reads=[w, condb], writes=[ps])
                    p.op("pe", lambda e, bv=bv, qd=qd, blk=blk, psv=psv: e.matmul(
                        psv[:, blk, :], lhsT=bv[0:1, qd * 128:(qd + 1) * 128], rhs=self.onesb[0:1, 0:ns], start=False, stop=True),
                        reads=[bv, self.onesb], writes=[ps])
            p.op("act", lambda e, l=l, psv=psv: e.copy(self.MODC[:, l, :, :], psv), reads=[ps], writes=[self.MODC])
        p.release(m)

    def modc(self, l, s, j, k=None):
        if k is None:
            return self.MODC[:, l, j * 8:(j + 1) * 8, s]
        return self.MODC[:, l, j * 8 + k, s:s + 1]

    def make_gate(self, l, s, j):
        p = self.p
        m = p.mark()
        dg = [p.tile(f"dg{i}", [128, 128], F32) for i in range(2)]
        for k in range(8):
            t = dg[k % 2]
            ps = self.PS[k // 4]
            p.op("dve", lambda e, t=t, k=k: e.tensor_scalar(out=t[:], in0=self.identf[:], scalar1=self.modc(l, s, j, k), scalar2=None, op0=ALU.mult),
                 reads=[self.identf, self.MODC], writes=[t])
            p.op("pe", lambda e, t=t, k=k, ps=ps: e.matmul(ps[:, (k % 4) * 128:(k % 4 + 1) * 128], lhsT=self.onesf[:], rhs=t[:], start=True, stop=True),
                 reads=[t, self.onesf], writes=[ps])
        for h in range(2):
            p.op("act", lambda e, h=h: e.copy(self.G[:, h * 512:(h + 1) * 512], self.PS[h][:]), reads=[self.PS[h]], writes=[self.G])
        p.release(m)

    def load_x(self, s):
        p = self.p
        for t in range(NT):
            q = "sp" if t % 2 == 0 else "act"
            p.dma(q, lambda e, t=t: e.dma_start(out=self.X[:, t, :], in_=self.dram["x"][s, t * 128:(t + 1) * 128, :]),
                  writes=[("X", t)])

    def final_out(self, s):
        p = self.p
        m = p.mark()
        ob = [p.tile(f"ob{i}", [128, D], F32) for i in range(2)]
        junk = p.tile("junkf", [128, D], BF16)
        toks = []
        for t in range(NT):
            if t == 0 and getattr(self, "skip0", False):
                continue
            self.rms_stats(t, junk)
            o = ob[t % 2]
            p.op("dve", lambda e, t=t, o=o: e.scalar_tensor_tensor(out=o[:], in0=self.X[:, t, :], scalar=self.rstd[:, t:t + 1], in1=self.GF[:],
                                                                  op0=ALU.mult, op1=ALU.mult),
                 reads=[("X", t), ("rstd", t), self.GF], writes=[o])
            toks.append(p.dma("sp", lambda e, t=t, o=o: e.dma_start(out=self.y[s, t * 128:(t + 1) * 128, :], in_=o[:]), reads=[o]))
        p.release(m)
        return toks

    def rms_stats(self, t, junk):
        p = self.p
        p.op("pool", lambda e: e.memset(self.ssq[:, t:t + 1], 0.0), writes=[("ssq", t)])
        p.op("act", lambda e: e.activation(out=junk[:], in_=self.X[:, t, :], func=AF.Square, accum_out=self.ssq[:, t:t + 1]),
             reads=[("X", t)], writes=[junk, ("ssq", t)])
        p.op("act", lambda e: e.activation(out=self.ssq[:, t:t + 1], in_=self.ssq[:, t:t + 1], func=AF.Sqrt, bias=self.epsc[:], scale=1.0 / D),
             reads=[("ssq", t), self.epsc], writes=[("ssq", t)])
        p.op("dve", lambda e: e.reciprocal(out=self.rstd[:, t:t + 1], in_=self.ssq[:, t:t + 1]), reads=[("ssq", t)], writes=[("rstd", t)])

    def norm_phase(self, l, s, which, router=False):
        p = self.p
        m = p.mark()
        jsh, jsc = (0, 1) if which == 0 else (3, 4)
        gcol = (self.gmix if which == 0 else self.gffn)[:, l, :]
        p.op("dve", lambda e: e.tensor_scalar(out=self.Acol[:], in0=self.modc(l, s, jsc), scalar1=1.0, scalar2=None, op0=ALU.add),
             reads=[self.MODC], writes=[self.Acol])
        p.op("dve", lambda e: e.tensor_tensor(out=self.Acol[:], in0=self.Acol[:], in1=gcol, op=ALU.mult),
             reads=[self.Acol, self.gmix, self.gffn], writes=[self.Acol])
        junk = p.tile("junk", [128, D], BF16)
        xn = [p.tile(f"xn{i}", [128, D], BF16) for i in range(2)]
        if router:
            xnf2 = [p.tile(f"xnf{i}", [128, D], F32) for i in range(2)]
            hTf2 = [p.tile(f"hTf{i}", [128, 8, 128], F32) for i in range(2)]
            wr = p.tile("wr", [128, 8, 8], F32)
            lg2 = [p.tile(f"lg{i}", [128, 8], F32) for i in range(2)]
            mx2 = [p.tile(f"mx{i}", [128, 8], F32) for i in range(2)]
            sc2 = [p.tile(f"rsc{i}", [128, 4], F32) for i in range(2)]
            c12 = [p.tile(f"c1{i}", [128, 8], F32) for i in range(2)]
            p.dma("sp", lambda e: e.dma_start(out=wr[:], in_=self.dram["moe_r"]), writes=[wr])
        psT = [self.PS[i].bitcast(BF16) for i in range(4)]
        for blk in range(NB):
            for tt in range(4):
                t = blk * 4 + tt
                self.rms_stats(t, junk)
                x_ = xn[t % 2]
                p.op("dve", lambda e, t=t, x_=x_: e.tensor_scalar(out=x_[:], in0=self.X[:, t, :], scalar1=self.rstd[:, t:t + 1], scalar2=None, op0=ALU.mult),
                     reads=[("X", t), ("rstd", t)], writes=[x_])
                for k in range(8):
                    p.op("pe", lambda e, k=k, tt=tt, x_=x_: e.transpose(psT[k // 2][:, (k % 2) * 512 + tt * 128:(k % 2) * 512 + (tt + 1) * 128],
                                                                       x_[:, k * 128:(k + 1) * 128], self.identb[:]),
                         reads=[x_, self.identb], writes=[self.PS[k // 2]])
                if router:
                    def router_ops(t, xnf, hTf, lg, mx, sc, c1, part):
                        if part == 0:
                            p.op("dve", lambda e, t=t, xnf=xnf: e.tensor_scalar(out=xnf[:], in0=self.X[:, t, :], scalar1=self.rstd[:, t:t + 1], scalar2=None, op0=ALU.mult),
                                 reads=[("X", t), ("rstd", t)], writes=[xnf])
                            for k in range(8):
                                ps = self.PS[4 + k // 4]
                                p.op("pe", lambda e, k=k, ps=ps: e.transpose(ps[:, (k % 4) * 128:(k % 4 + 1) * 128], xnf[:, k * 128:(k + 1) * 128], self.identf[:]),
                                     reads=[xnf, self.identf], writes=[ps])
                            for k in range(8):
                                ps = self.PS[4 + k // 4]
                                p.op("act", lambda e, k=k, ps=ps: e.activation(out=hTf[:, k, :], in_=ps[:, (k % 4) * 128:(k % 4 + 1) * 128], func=AF.Identity,
                                                                              bias=self.modc(l, s, jsh, k), scale=self.Acol[:, k:k + 1]),
                                     reads=[ps, self.Acol, self.MODC], writes=[hTf])
                            return
                        for k in range(8):
                            p.op("pe", lambda e, k=k: e.matmul(self.PS[6][:, 0:8], lhsT=hTf[:, k, :], rhs=wr[:, k, :], start=(k == 0), stop=(k == 7)),
                                 reads=[hTf, wr], writes=[self.PS[6]])
                        p.op("act", lambda e: e.copy(lg[:], self.PS[6][:, 0:8]), reads=[self.PS[6]], writes=[lg])
                        p.op("dve", lambda e: e.max(out=mx[:], in_=lg[:]), reads=[lg], writes=[mx])
                        p.op("dve", lambda e: e.tensor_tensor(out=sc[:, 0:1], in0=mx[:, 1:2], in1=mx[:, 0:1], op=ALU.subtract), reads=[mx], writes=[sc])
                        p.op("act", lambda e: e.activation(out=sc[:, 0:1], in_=sc[:, 0:1], func=AF.Exp), reads=[sc], writes=[sc])
                        p.op("dve", lambda e: e.tensor_scalar(out=sc[:, 1:2], in0=sc[:, 0:1], scalar1=1.0, scalar2=None, op0=ALU.add), reads=[sc], writes=[sc])
                        p.op("dve", lambda e: e.reciprocal(out=sc[:, 1:2], in_=sc[:, 1:2]), reads=[sc], writes=[sc])
                        p.op("dve", lambda e: e.tensor_tensor(out=sc[:, 2:3], in0=sc[:, 0:1], in1=sc[:, 1:2], op=ALU.mult), reads=[sc], writes=[sc])
                        p.op("dve", lambda e: e.tensor_scalar(out=c1[:], in0=lg[:], scalar1=mx[:, 0:1], scalar2=sc[:, 1:2], op0=ALU.is_equal, op1=ALU.mult),
                             reads=[lg, mx, sc], writes=[c1])
                        p.op("dve", lambda e, t=t: e.tensor_scalar(out=self.COMB[:, t, :], in0=lg[:], scalar1=mx[:, 1:2], scalar2=sc[:, 2:3], op0=ALU.is_equal, op1=ALU.mult),
                             reads=[lg, mx, sc], writes=[("COMB", t)])
                        p.op("dve", lambda e, t=t: e.tensor_tensor(out=self.COMB[:, t, :], in0=self.COMB[:, t, :], in1=c1[:], op=ALU.add),
                             reads=[c1, ("COMB", t)], writes=[("COMB", t)])
                    ra = lambda u, part: router_ops(u, xnf2[u % 2], hTf2[u % 2], lg2[u % 2], mx2[u % 2], sc2[u % 2], c12[u % 2], part)
                    if t >= 1:
                        ra(t - 1, 0)
                    if t >= 2:
                        ra(t - 2, 1)
                    if t == NT - 1:
                        ra(t, 0)
                        ra(t - 1, 1)
                        ra(t, 1)
            for k in range(8):
                src = psT[k // 2][:, (k % 2) * 512:(k % 2 + 1) * 512]
                dst = self.HT[:, k, blk * 512:(blk + 1) * 512]
                if k % 2 == 0:
                    p.op("act", lambda e, k=k, src=src, dst=dst: e.activation(out=dst, in_=src, func=AF.Identity, bias=self.modc(l, s, jsh, k),
                                                                            scale=self.Acol[:, k:k + 1]),
                         reads=[self.PS[k // 2], self.Acol, self.MODC], writes=[("HT", blk)])
                else:
                    p.op("dve", lambda e, k=k, src=src, dst=dst: e.tensor_scalar(out=dst, in0=src, scalar1=self.Acol[:, k:k + 1], scalar2=self.modc(l, s, jsh, k),
                                                                               op0=ALU.mult, op1=ALU.add),
                         reads=[self.PS[k // 2], self.Acol, self.MODC], writes=[("HT", blk)])
        p.release(m)

    def ffn_phase(self, experts):
        p = self.p
        m = p.mark()
        ACT_ = p.tile("actT", [128, 11, SEQ], BF16)
        WGU = [p.tile(f"wgu{i}", [128, 8, 256], BF16) for i in range(3)]
        WD = p.tile("wd", [128, 11, D], BF16)
        SG = [p.tile(f"sg{i}", [128, 512], BF16) for i in range(2)]
        HTr = [("HT", b) for b in range(NB)]
        it = 0
        for (gu, dn, ci) in experts:
            p.dma("pool", lambda e, dn=dn: e.dma_start(out=WD[:], in_=dn.rearrange("(c p) n -> p c n", p=128)), writes=[WD])
            for c in range(11):
                w = WGU[c % 3]
                p.dma("pool", lambda e, w=w, c=c, gu=gu: e.dma_start(out=w[:].rearrange("p k n -> p (k n)"), in_=gu[c]), writes=[w])
                for blk in range(NB):
                    psg, psu = self.PS[it % 2], self.PS[2 + it % 2]
                    sg = SG[it % 2]
                    it += 1
                    for k in range(8):
                        p.op("pe", lambda e, w=w, k=k, blk=blk, psg=psg: e.matmul(psg[:], lhsT=w[:, k, 0:128], rhs=self.HT[:, k, blk * 512:(blk + 1) * 512],
                                                                                  start=(k == 0), stop=(k == 7)), reads=[w, HTr[blk]], writes=[psg])
                    for k in range(8):
                        p.op("pe", lambda e, w=w, k=k, blk=blk, psu=psu: e.matmul(psu[:], lhsT=w[:, k, 128:256], rhs=self.HT[:, k, blk * 512:(blk + 1) * 512],
                                                                                  start=(k == 0), stop=(k == 7)), reads=[w, HTr[blk]], writes=[psu])
                    p.op("act", lambda e, sg=sg, psg=psg: e.activation(out=sg[:], in_=psg[:], func=AF.Silu), reads=[psg], writes=[sg])
                    p.op("dve", lambda e, sg=sg, psu=psu, c=c, blk=blk: e.tensor_tensor(out=ACT_[:, c, blk * 512:(blk + 1) * 512], in0=psu[:], in1=sg[:], op=ALU.mult),
                         reads=[sg, psu], writes=[("actT", c)])
                if c == 5:
                    for cc in range(11):
                        p.op("pool", lambda e, cc=cc: e.tensor_tensor(out=WD[:, cc, :], in0=WD[:, cc, :], in1=self.G[:], op=ALU.mult),
                             reads=[WD, self.G], writes=[WD])
            for t in range(NT):
                for hf in range(2):
                    ps = self.PS[4 + (2 * t + hf) % 4]
                    for c in range(11):
                        p.op("pe", lambda e, c=c, t=t, hf=hf, ps=ps: e.matmul(ps[:], lhsT=ACT_[:, c, t * 128:(t + 1) * 128], rhs=WD[:, c, hf * 512:(hf + 1) * 512],
                                                                              start=(c == 0), stop=(c == 10)), reads=[("actT", c), WD], writes=[ps])
                    xs = self.X[:, t, hf * 512:(hf + 1) * 512]
                    if ci is None:
                        p.op("dve", lambda e, xs=xs, ps=ps: e.tensor_tensor(out=xs, in0=ps[:], in1=xs, op=ALU.add), reads=[ps, ("X", t)], writes=[("X", t)])
                    else:
                        p.op("dve", lambda e, xs=xs, ps=ps, t=t, ci=ci: e.scalar_tensor_tensor(out=xs, in0=ps[:], scalar=self.COMB[:, t, ci:ci + 1], in1=xs,
                                                                                              op0=ALU.mult, op1=ALU.add),
                             reads=[ps, ("X", t), ("COMB", t)], writes=[("X", t)])
        p.release(m)


    def mla_phase(self, s):
        p = self.p
        PS = self.PS
        m0 = p.mark()
        QNT = p.tile("QNT", [128, 4, SEQ], BF16)
        KVNT = p.tile("KVNT", [128, 2, SEQ], BF16)
        ROPE = p.tile("ROPE", [128, SEQ], F32)
        KT = p.tile("KT", [128, SEQ], BF16)
        QT = p.tile("QT", [128, SEQ], BF16)
        VA = p.tile("VA", [128, NT, 128], BF16)
        PT = [p.tile(f"PT{i}", [128, 512], BF16) for i in range(4)]
        TRI = p.tile("TRI", [128, 128], BF16)
        tA = p.tile("tA", [128, 512], F32)
        tB = p.tile("tB", [128, 512], F32)
        rec = p.tile("rec", [128, 512], F32)
        qnc = p.tile("qnc", [128, 4], F32)
        kvnc = p.tile("kvnc", [128, 2], F32)
        ropec = p.tile("ropec", [128, 2], F32)
        st = p.tile("mst", [128, 4], F32)
        p.dma("sp", lambda e: e.dma_start(out=qnc[:], in_=self.dram["mla_qn"]), writes=[qnc])
        p.dma("sp", lambda e: e.dma_start(out=kvnc[:], in_=self.dram["mla_kvn"]), writes=[kvnc])
        p.dma("sp", lambda e: e.dma_start(out=ropec[:], in_=self.dram["ropec"]), writes=[ropec])
        p.op("pool", lambda e: e.memset(TRI[:], 1.0), writes=[TRI])
        p.op("pool", lambda e: e.affine_select(out=TRI[:], in_=TRI[:], pattern=[[1, 128]], compare_op=ALU.is_ge, fill=0.0, base=0, channel_multiplier=-1),
             reads=[TRI], writes=[TRI])
        p.op("pool", lambda e: e.memset(VA[:, :, 64:128], 1.0), writes=[("VA", 0), ("VA", 1)])
        m1 = p.mark()
        WIN = p.tile("WIN", [128, 8, 896], BF16)
        p.dma("pool", lambda e: e.dma_start(out=WIN[:], in_=self.dram["mla_in"]), writes=[WIN])
        posi = p.tile("posi", [128, 512], I32)
        u = p.tile("ropu", [128, 512], F32)
        ki = p.tile("ropk", [128, 512], I32)
        kf = p.tile("ropkf", [128, 512], F32)
        qtok = [p.tile(f"qtok{i}", [128, 512], BF16) for i in range(2)]
        kvtok = [p.tile(f"kvtok{i}", [128, 256], BF16) for i in range(2)]
        junk = p.tile("mjunk", [128, 512], BF16)
        for blk in range(NB):
            cs = slice(blk * 512, (blk + 1) * 512)
            p.dma("sp", lambda e, cs=cs: e.dma_start(out=posi[:], in_=self.dram["pos"][s, :, cs]), writes=[posi])
            p.op("dve", lambda e: e.tensor_copy(u[:], posi[:]), reads=[posi], writes=[u])
            p.op("dve", lambda e: e.tensor_scalar(out=u[:], in0=u[:], scalar1=ropec[:, 0:1], scalar2=ropec[:, 1:2], op0=ALU.mult, op1=ALU.add),
                 reads=[u, ropec], writes=[u])
            p.op("dve", lambda e: e.tensor_copy(ki[:], u[:]), reads=[u], writes=[ki])
            p.op("dve", lambda e: e.tensor_copy(kf[:], ki[:]), reads=[ki], writes=[kf])
            p.op("dve", lambda e: e.tensor_tensor(out=u[:], in0=u[:], in1=kf[:], op=ALU.subtract), reads=[u, kf], writes=[u])
            p.op("act", lambda e, cs=cs: e.activation(out=ROPE[:, cs], in_=u[:], func=AF.Sin, scale=2.0 * math.pi * (1.0 - 1e-6)),
                 reads=[u], writes=[("ROPE", blk)])
        psTb = PS[4].bitcast(BF16)
        def mla_proj(t):
            ts_ = slice(t * 128, (t + 1) * 128)
            blk = t // 4
            pa, pb = PS[(t % 2) * 2], PS[(t % 2) * 2 + 1]
            for k in range(8):
                p.op("pe", lambda e, k=k, ts_=ts_, pa=pa: e.matmul(pa[:], lhsT=self.HT[:, k, ts_], rhs=WIN[:, k, 0:512], start=(k == 0), stop=(k == 7)),
                     reads=[("HT", blk), WIN], writes=[pa])
            for k in range(8):
                p.op("pe", lambda e, k=k, ts_=ts_, pb=pb: e.matmul(pb[:, 0:256], lhsT=self.HT[:, k, ts_], rhs=WIN[:, k, 512:768], start=(k == 0), stop=(k == 7)),
                     reads=[("HT", blk), WIN], writes=[pb])

        def mla_rest(t):
            ts_ = slice(t * 128, (t + 1) * 128)
            blk = t // 4
            pa, pb = PS[(t % 2) * 2], PS[(t % 2) * 2 + 1]
            p.op("pool", lambda e: e.memset(st[:, 0:2], 0.0), writes=[st])
            p.op("act", lambda e, pa=pa: e.activation(out=junk[:], in_=pa[:], func=AF.Square, accum_out=st[:, 0:1]), reads=[pa], writes=[junk, st])
            p.op("act", lambda e, pb=pb: e.activation(out=junk[:, 0:256], in_=pb[:, 0:256], func=AF.Square, accum_out=st[:, 1:2]), reads=[pb], writes=[junk, st])
            p.op("act", lambda e: e.activation(out=st[:, 0:1], in_=st[:, 0:1], func=AF.Sqrt, bias=self.epsc[:], scale=1.0 / 512), reads=[st, self.epsc], writes=[st])
            p.op("act", lambda e: e.activation(out=st[:, 1:2], in_=st[:, 1:2], func=AF.Sqrt, bias=self.epsc[:], scale=1.0 / 256), reads=[st, self.epsc], writes=[st])
            p.op("dve", lambda e: e.reciprocal(out=st[:, 2:4], in_=st[:, 0:2]), reads=[st], writes=[st])
            qt_, kvt_ = qtok[t % 2], kvtok[t % 2]
            p.op("dve", lambda e, pa=pa, qt_=qt_: e.tensor_scalar(out=qt_[:], in0=pa[:], scalar1=st[:, 2:3], scalar2=None, op0=ALU.mult), reads=[pa, st], writes=[qt_])
            p.op("act", lambda e, pb=pb, kvt_=kvt_: e.activation(out=kvt_[:], in_=pb[:, 0:256], func=AF.Copy, scale=st[:, 3:4]), reads=[pb, st], writes=[kvt_])
            for c in range(4):
                p.op("pe", lambda e, c=c, qt_=qt_: e.transpose(psTb[:, c * 128:(c + 1) * 128], qt_[:, c * 128:(c + 1) * 128], self.identb[:]),
                     reads=[qt_, self.identb], writes=[PS[4]])
            for c in range(2):
                p.op("pe", lambda e, c=c, kvt_=kvt_: e.transpose(psTb[:, 512 + c * 128:512 + (c + 1) * 128], kvt_[:, c * 128:(c + 1) * 128], self.identb[:]),
                     reads=[kvt_, self.identb], writes=[PS[4]])
            for c in range(4):
                if c % 2 == 0:
                    p.op("act", lambda e, c=c, ts_=ts_: e.activation(out=QNT[:, c, ts_], in_=psTb[:, c * 128:(c + 1) * 128], func=AF.Copy, scale=qnc[:, c:c + 1]),
                         reads=[PS[4], qnc], writes=[("QNT", blk)])
                else:
                    p.op("dve", lambda e, c=c, ts_=ts_: e.tensor_scalar(out=QNT[:, c, ts_], in0=psTb[:, c * 128:(c + 1) * 128], scalar1=qnc[:, c:c + 1], scalar2=None, op0=ALU.mult),
                         reads=[PS[4], qnc], writes=[("QNT", blk)])
            for c in range(2):
                if c % 2 == 0:
                    p.op("act", lambda e, c=c, ts_=ts_: e.activation(out=KVNT[:, c, ts_], in_=psTb[:, 512 + c * 128:512 + (c + 1) * 128], func=AF.Copy, scale=kvnc[:, c:c + 1]),
                         reads=[PS[4], kvnc], writes=[("KVNT", blk)])
                else:
                    p.op("dve", lambda e, c=c, ts_=ts_: e.tensor_scalar(out=KVNT[:, c, ts_], in0=psTb[:, 512 + c * 128:512 + (c + 1) * 128], scalar1=kvnc[:, c:c + 1], scalar2=None, op0=ALU.mult),
                         reads=[PS[4], kvnc], writes=[("KVNT", blk)])
        mla_proj(0)
        for t in range(NT):
            if t + 1 < NT:
                mla_proj(t + 1)
            mla_rest(t)
        for blk in range(NB):
            cs = slice(blk * 512, (blk + 1) * 512)
            ps = PS[5 + blk % 2]
            for k in range(8):
                p.op("pe", lambda e, k=k, cs=cs, ps=ps: e.matmul(ps[:], lhsT=WIN[:, k, 768:896], rhs=self.HT[:, k, cs], start=(k == 0), stop=(k == 7)),
                     reads=[("HT", blk), WIN], writes=[ps])
            p.op("dve", lambda e, cs=cs, ps=ps: e.tensor_tensor(out=tA[64:96, :], in0=ps[64:96, :], in1=ROPE[64:96, cs], op=ALU.mult), reads=[ps, ("ROPE", blk)], writes=[tA])
            p.op("dve", lambda e, cs=cs, ps=ps: e.tensor_tensor(out=tB[64:96, :], in0=ps[96:128, :], in1=ROPE[96:128, cs], op=ALU.mult), reads=[ps, ("ROPE", blk)], writes=[tB])
            p.op("pool", lambda e, cs=cs: e.tensor_tensor(out=KT[64:96, cs], in0=tA[64:96, :], in1=tB[64:96, :], op=ALU.add), reads=[tA, tB], writes=[("KTr", blk)])
        p.release(m1)
        m2 = p.mark()
        WQB = p.tile("WQB", [128, 4, 2048], BF16)
        WKB = p.tile("WKB", [128, 2, 1024], BF16)
        WVB = p.tile("WVB", [128, 2, 1024], BF16)
        p.dma("pool", lambda e: e.dma_start(out=WQB[:], in_=self.dram["mla_qb"]), writes=[WQB])
        p.dma("pool", lambda e: e.dma_start(out=WKB[:], in_=self.dram["mla_kb"]), writes=[WKB])
        p.dma("pool", lambda e: e.dma_start(out=WVB[:], in_=self.dram["mla_vb"]), writes=[WVB])
        OT = self.HT
        scale = 96.0 ** -0.5
        si = 0
        oi = 0
        pi_ = 0
        for h in range(16):
            for blk in range(NB):
                cs = slice(blk * 512, (blk + 1) * 512)
                ps = PS[blk % 2]
                for c in range(4):
                    p.op("pe", lambda e, c=c, cs=cs, ps=ps, h=h: e.matmul(ps[:], lhsT=WQB[:, c, h * 128:(h + 1) * 128], rhs=QNT[:, c, cs], start=(c == 0), stop=(c == 3)),
                         reads=[WQB, ("QNT", blk)], writes=[ps])
                p.op("act", lambda e, cs=cs, ps=ps: e.copy(QT[0:64, cs], ps[0:64, :]), reads=[ps], writes=[("QT", blk)])
                p.op("dve", lambda e, cs=cs, ps=ps: e.tensor_tensor(out=tA[64:96, :], in0=ps[64:96, :], in1=ROPE[64:96, cs], op=ALU.mult), reads=[ps, ("ROPE", blk)], writes=[tA])
                p.op("dve", lambda e, cs=cs, ps=ps: e.tensor_tensor(out=tB[64:96, :], in0=ps[96:128, :], in1=ROPE[96:128, cs], op=ALU.mult), reads=[ps, ("ROPE", blk)], writes=[tB])
                p.op("pool", lambda e, cs=cs: e.tensor_tensor(out=QT[64:96, cs], in0=tA[64:96, :], in1=tB[64:96, :], op=ALU.add), reads=[tA, tB], writes=[("QT", blk)])
            for blk in range(NB):
                cs = slice(blk * 512, (blk + 1) * 512)
                ps = PS[blk % 2]
                for c in range(2):
                    p.op("pe", lambda e, c=c, cs=cs, ps=ps, h=h: e.matmul(ps[0:64, :], lhsT=WKB[:, c, h * 64:(h + 1) * 64], rhs=KVNT[:, c, cs], start=(c == 0), stop=(c == 1)),
                         reads=[WKB, ("KVNT", blk)], writes=[ps])
                p.op("act", lambda e, cs=cs, ps=ps: e.copy(KT[0:64, cs], ps[0:64, :]), reads=[ps], writes=[("KTn", blk)])
            for half in range(2):
                ps = PS[half]
                psv = ps[:].rearrange("p (a b) -> p a b", b=64)
                for kk in range(8):
                    kt = half * 8 + kk
                    for c in range(2):
                        p.op("pe", lambda e, c=c, kt=kt, kk=kk, psv=psv, h=h: e.matmul(psv[:, kk, :], lhsT=KVNT[:, c, kt * 128:(kt + 1) * 128], rhs=WVB[:, c, h * 64:(h + 1) * 64],
                                                                                   start=(c == 0), stop=(c == 1)), reads=[WVB, ("KVNT", kt // 4)], writes=[ps])
                p.op("dve", lambda e, half=half, psv=psv: e.tensor_copy(VA[:, half * 8:(half + 1) * 8, 0:64], psv), reads=[ps], writes=[("VA", half)])
            steps = []
            for qb in range(NB):
                pso = PS[5 + oi % 2]
                oi += 1
                last = 4 * qb + 3
                for kt in range(last + 1):
                    steps.append((qb, kt, max(0, kt - 4 * qb) * 128, pso, last))
            LA = 2
            slot = {}

            def emit_s(i):
                qb, kt, col0, pso, last = steps[i]
                pss = PS[2 + (si + i) % 3]
                pt = PT[(pi_ + i) % 4]
                slot[i] = (pss, pt)
                p.op("pe", lambda e: e.matmul(pss[:, col0:512], lhsT=KT[0:96, kt * 128:(kt + 1) * 128],
                                              rhs=QT[0:96, qb * 512 + col0:(qb + 1) * 512], start=True, stop=True),
                     reads=[("KTn", kt // 4), ("KTr", kt // 4), ("QT", qb)], writes=[pss])
                p.op("act", lambda e: e.activation(out=pt[:, col0:512], in_=pss[:, col0:512], func=AF.Exp, scale=scale), reads=[pss], writes=[pt])
                if kt >= 4 * qb:
                    dc = (kt - 4 * qb) * 128
                    p.op("pool", lambda e: e.tensor_tensor(out=pt[:, dc:dc + 128], in0=pt[:, dc:dc + 128], in1=TRI[:], op=ALU.mult), reads=[pt, TRI], writes=[pt])

            def emit_pv(i):
                qb, kt, col0, pso, last = steps[i]
                pss, pt = slot.pop(i)
                p.op("pe", lambda e: e.matmul(pso[:, col0:512], lhsT=VA[:, kt, :], rhs=pt[:, col0:512], start=(kt == 0), stop=(kt == last)),
                     reads=[("VA", kt // 8), pt], writes=[pso])
                if kt == last:
                    p.op("dve", lambda e: e.reciprocal(out=rec[64:128, :], in_=pso[64:128, :]), reads=[pso], writes=[rec])
                    r0 = (h % 2) * 64
                    hh = h // 2
                    p.op("dve", lambda e: e.tensor_tensor(out=OT[r0:r0 + 64, hh, qb * 512:(qb + 1) * 512], in0=pso[0:64, :], in1=rec[64:128, :], op=ALU.mult),
                         reads=[pso, rec], writes=[("OT", qb)])
            for i in range(len(steps) + LA):
                if i < len(steps):
                    emit_s(i)
                if i - LA >= 0:
                    emit_pv(i - LA)
            si += len(steps)
            pi_ += len(steps)
        p.release(m2)
        WOUT = p.tile("WOUT", [128, 8, D], BF16)
        p.dma("pool", lambda e: e.dma_start(out=WOUT[:], in_=self.dram["mla_out"].rearrange("(c p) n -> p c n", p=128)), writes=[WOUT])
        for c in range(8):
            p.op("pool", lambda e, c=c: e.tensor_tensor(out=WOUT[:, c, :], in0=WOUT[:, c, :], in1=self.G[:], op=ALU.mult), reads=[WOUT, self.G], writes=[WOUT])
        self.out_proj(OT, WOUT)
        p.release(m0)

    def out_proj(self, OT, WOUT):
        p = self.p
        for t in range(NT):
            for hf in range(2):
                ps = self.PS[(2 * t + hf) % 4]
                for c in range(8):
                    p.op("pe", lambda e, c=c, t=t, hf=hf, ps=ps: e.matmul(ps[:], lhsT=OT[:, c, t * 128:(t + 1) * 128], rhs=WOUT[:, c, hf * 512:(hf + 1) * 512],
                                                                          start=(c == 0), stop=(c == 7)), reads=[("OT", t // 4), WOUT], writes=[ps])
                xs = self.X[:, t, hf * 512:(hf + 1) * 512]
                p.op("dve", lambda e, xs=xs, ps=ps: e.tensor_tensor(out=xs, in0=ps[:], in1=xs, op=ALU.add), reads=[ps, ("X", t)], writes=[("X", t)])


    def mm(self, out, lhsT, rhs, start, stop, reads, writes):
        self.p.op("pe", lambda e: e.matmul(out, lhsT=lhsT, rhs=rhs, start=start, stop=stop), reads=reads, writes=writes)

    def tr(self, out, in_, ident, reads, writes):
        self.p.op("pe", lambda e: e.transpose(out, in_, ident), reads=list(reads) + [ident], writes=writes)

    def gdn_phase(self, s):
        p = self.p
        PS = self.PS
        PSb = [b.bitcast(BF16) for b in PS]
        HTr = [("HT", b) for b in range(NB)]
        m0 = p.mark()
        NEG = -1.0e30
        LTRI = p.tile("LTRI", [128, 128], F32)
        BDm = p.tile("BDm", [128, 128], F32)
        SEL = [p.tile(f"SEL{i}", [128, 128], F32) for i in range(2)]
        MNi = p.tile("MNi", [128, 128], F32)
        MNs = p.tile("MNs", [128, 128], F32)
        hp = p.tile("hp", [128, 24], F32)
        nA = p.tile("nA", [128, 8], F32)
        GON = p.tile("GON", [128, 128], F32)
        WBA = p.tile("WBA", [128, 8, 16], BF16)
        CW = p.tile("CW", [128, 8, 3, 4], F32)
        pl = lambda fn, r=(), w=(): p.op("pool", fn, reads=r, writes=w)
        pl(lambda e: e.memset(LTRI[:], 1.0), w=[LTRI])
        pl(lambda e: e.affine_select(out=LTRI[:], in_=LTRI[:], pattern=[[1, 128]], compare_op=ALU.is_ge, fill=0.0, base=0, channel_multiplier=-1), r=[LTRI], w=[LTRI])
        pl(lambda e: e.memset(LTRI[0:64, 64:128], 0.0), r=[LTRI], w=[LTRI])
        pl(lambda e: e.memset(BDm[:], 0.0), w=[BDm])
        pl(lambda e: e.memset(BDm[0:64, 0:64], 1.0), r=[BDm], w=[BDm])
        pl(lambda e: e.memset(BDm[64:128, 64:128], 1.0), r=[BDm], w=[BDm])
        for i in range(2):
            pl(lambda e, i=i: e.memset(SEL[i][:], 0.0), w=[SEL[i]])
            pl(lambda e, i=i: e.memset(SEL[i][64 * i:64 * i + 1, :], 1.0), r=[SEL[i]], w=[SEL[i]])
        pl(lambda e: e.memset(MNi[:], 0.0), w=[MNi])
        pl(lambda e: e.affine_select(out=MNi[:], in_=MNi[:], pattern=[[1, 128]], compare_op=ALU.is_ge, fill=NEG, base=0, channel_multiplier=-1), r=[MNi], w=[MNi])
        pl(lambda e: e.memset(MNi[0:64, 64:128], NEG), r=[MNi], w=[MNi])
        pl(lambda e: e.memset(MNs[:], 0.0), w=[MNs])
        pl(lambda e: e.affine_select(out=MNs[:], in_=MNs[:], pattern=[[1, 128]], compare_op=ALU.is_gt, fill=NEG, base=0, channel_multiplier=-1), r=[MNs], w=[MNs])
        pl(lambda e: e.memset(MNs[0:64, 64:128], NEG), r=[MNs], w=[MNs])
        p.dma("sp", lambda e: e.dma_start(out=hp[:], in_=self.dram["gdn_hp"]), writes=[hp])
        p.dma("sp", lambda e: e.dma_start(out=GON[:], in_=self.dram["gdn_on"]), writes=[GON])
        p.dma("sp", lambda e: e.dma_start(out=CW[:], in_=self.dram["gdn_conv"]), writes=[CW])
        p.dma("pool", lambda e: e.dma_start(out=WBA[:], in_=self.dram["gdn_ba"]), writes=[WBA])
        p.op("act", lambda e: e.activation(out=nA[:], in_=hp[:, 0:8], func=AF.Exp), reads=[hp], writes=[nA])
        p.op("dve", lambda e: e.tensor_scalar(out=nA[:], in0=nA[:], scalar1=-1.0, scalar2=None, op0=ALU.mult), reads=[nA], writes=[nA])
        BETA = p.tile("BETA", [128, NT, 8], F32)
        GAMS = p.tile("GAMS", [128, NT, 8], F32)
        EGL = p.tile("EGL", [128, NT, 8], F32)
        BG = p.tile("BG", [128, NT, 8], F32)
        DECB = p.tile("DECB", [128, NT, 2, 8], F32)
        GC3 = p.tile("GC3", [128, NT, 3, 8], F32)
        m1 = p.mark()
        ba = p.tile("ba", [128, 16], F32)
        xs = p.tile("gxs", [128, 8], F32)
        ax = p.tile("gax", [128, 8], F32)
        gg = p.tile("ggg", [128, 8], F32)
        lnb = p.tile("lnb", [128, 8], F32)
        gcl = p.tile("gcl", [128, 2, 8], F32)
        dk_s = 128.0 ** -0.5
        import os
        pre = int(os.environ.get("GDN_PRE", "99"))
        if "GDN_OPS" in os.environ:
            p.limit = int(os.environ["GDN_OPS"])
        for t in range(NT if pre >= 2 else pre):
            ts_ = slice(t * 128, (t + 1) * 128)
            ps = PS[t % 2]
            for k in range(8):
                self.mm(ps[:, 0:16], self.HT[:, k, ts_], WBA[:, k, :], k == 0, k == 7, [HTr[t // 4], WBA], [ps])
            p.op("act", lambda e, ps=ps: e.copy(ba[:], ps[:, 0:16]), reads=[ps], writes=[ba])
            p.op("act", lambda e, t=t: e.activation(out=BETA[:, t, :], in_=ba[:, 0:8], func=AF.Sigmoid), reads=[ba], writes=[BETA])
            p.op("act", lambda e, t=t: e.activation(out=lnb[:], in_=BETA[:, t, :], func=AF.Ln), reads=[BETA], writes=[lnb])
            p.op("dve", lambda e: e.tensor_tensor(out=xs[:], in0=ba[:, 8:16], in1=hp[:, 8:16], op=ALU.add), reads=[ba, hp], writes=[xs])
            p.op("act", lambda e: e.activation(out=ax[:], in_=xs[:], func=AF.Abs), reads=[xs], writes=[ax])
            p.op("act", lambda e: e.activation(out=ax[:], in_=ax[:], func=AF.Exp, scale=-1.0), reads=[ax], writes=[ax])
            p.op("act", lambda e: e.activation(out=ax[:], in_=ax[:], func=AF.Ln, bias=self.onesf[:, 0:1], scale=1.0), reads=[ax, self.onesf], writes=[ax])
            p.op("dve", lambda e: e.scalar_tensor_tensor(out=gg[:], in0=xs[:], scalar=0.0, in1=ax[:], op0=ALU.max, op1=ALU.add), reads=[xs, ax], writes=[gg])
            p.op("dve", lambda e: e.tensor_tensor(out=gg[:], in0=gg[:], in1=nA[:], op=ALU.mult), reads=[gg, nA], writes=[gg])
            self.mm(ps[:, 16:24], LTRI[:], gg[:], True, True, [LTRI, gg], [ps])
            self.mm(ps[:, 24:32], BDm[:], gg[:], True, True, [BDm, gg], [ps])
            p.op("act", lambda e, ps=ps: e.copy(gcl[:].rearrange("p a b -> p (a b)"), ps[:, 16:32]), reads=[ps], writes=[gcl])
            p.op("act", lambda e, t=t: e.activation(out=BG[:, t, :], in_=gcl[:, 0, :], func=AF.Exp), reads=[gcl], writes=[BG])
            p.op("dve", lambda e, t=t: e.tensor_scalar(out=GAMS[:, t, :], in0=BG[:, t, :], scalar1=dk_s, scalar2=None, op0=ALU.mult), reads=[BG], writes=[GAMS])
            p.op("dve", lambda e, t=t: e.tensor_tensor(out=BG[:, t, :], in0=BG[:, t, :], in1=BETA[:, t, :], op=ALU.mult), reads=[BG, BETA], writes=[BG])
            p.op("dve", lambda e, t=t: e.tensor_tensor(out=EGL[:, t, :], in0=gcl[:, 1, :], in1=gcl[:, 0, :], op=ALU.subtract), reads=[gcl], writes=[EGL])
            p.op("act", lambda e, t=t: e.activation(out=EGL[:, t, :], in_=EGL[:, t, :], func=AF.Exp), reads=[EGL], writes=[EGL])
            p.op("dve", lambda e, t=t: e.tensor_copy(GC3[:, t, 0, :], gcl[:, 0, :]), reads=[gcl], writes=[GC3])
            p.op("dve", lambda e, t=t: e.tensor_scalar(out=GC3[:, t, 1, :], in0=gcl[:, 0, :], scalar1=-1.0, scalar2=None, op0=ALU.mult), reads=[gcl, GC3], writes=[GC3])
            p.op("dve", lambda e, t=t: e.tensor_tensor(out=GC3[:, t, 2, :], in0=gcl[:, 0, :], in1=lnb[:], op=ALU.add), reads=[gcl, lnb, GC3], writes=[GC3])
            for hf in range(2):
                self.mm(ps[:, 32 + hf * 8:40 + hf * 8], SEL[hf][:], gcl[:, 1, :], True, True, [SEL[hf], gcl], [ps])
            p.op("act", lambda e, t=t, ps=ps: e.activation(out=DECB[:, t, :, :].rearrange("p a b -> p (a b)"), in_=ps[:, 32:48], func=AF.Exp), reads=[ps], writes=[DECB])
        p.limit = None
        p.release(m1)
        NPAR = int(os.environ.get("GDN_NPAR", "2"))
        HB = []
        for sl in range(NPAR):
            b = {}
            b["WH"] = p.tile(f"WH{sl}", [128, 8, 512], BF16)
            b["WO"] = p.tile(f"WO{sl}", [128, D], BF16)
            b["DG"] = p.tile(f"DG{sl}", [128, 12, 128], BF16)
            b["S"] = p.tile(f"S{sl}", [128, 128], F32)
            b["Sb"] = p.tile(f"Sb{sl}", [128, 128], BF16)
            b["XC"] = p.tile(f"XC{sl}", [128, 3, 515], BF16)
            for nm, shp, dt in (("ZS", [128, 128], F32), ("GZ", [128, 128], F32), ("QKV", [128, 3, 128], F32), ("SQ", [128, 2, 128], F32),
                                ("st", [128, 8], F32), ("TOK", [128, 6, 128], BF16), ("DGC", [128, 2, 128], F32), ("KQT", [128, 3, 128], BF16), ("DT2", [128, 2, 128], F32),
                                ("M2", [128, 2, 128], F32), ("ATb", [128, 128], BF16), ("YA0", [128, 2, 128], F32), ("YA1", [128, 2, 128], F32),
                                ("P0", [128, 128], F32), ("P1", [128, 128], F32), ("TTb", [128, 128], BF16), ("U", [128, 128], F32),
                                ("WTb", [128, 128], BF16), ("VNb", [128, 128], BF16), ("OT", [128, 128], F32), ("OG", [128, 128], BF16),
                                ("OGT", [128, 128], BF16), ("junk", [128, 128], BF16)):
                b[nm] = p.tile(f"{nm}{sl}", shp, dt)
            b["bank"] = [PS[4 * sl + i] for i in range(4)]
            b["bankb"] = [PSb[4 * sl + i] for i in range(4)]
            HB.append(b)

        def head_gen(h, b):
            sl = HB.index(b)
            WH, WO, DG, S, Sb, XC = b["WH"], b["WO"], b["DG"], b["S"], b["Sb"], b["XC"]
            bk, bkb = b["bank"], b["bankb"]
            R = lambda i, nm: (f"ps{sl}_{i}")
            r_zc = [R(0, "zc")]
            r_tr, r_d, r_og = [R(1, "tr")], [R(1, "d")], [R(1, "og")]
            r_kk, r_a, r_p = [R(2, "kk")], [R(2, "a")], [R(2, "p")]
            r_ya, r_u, r_wt = [R(3, "ya")], [R(3, "u")], [R(3, "wt")]
            allb = [r_zc, r_tr, r_kk, r_ya]
            p.dma("pool", lambda e: e.dma_start(out=WH[:].rearrange("p k n -> p (k n)"), in_=self.dram["gdn_in"][h].rearrange("p k n -> p (k n)")), writes=[WH])
            p.dma("pool", lambda e: e.dma_start(out=WO[:], in_=self.dram["gdn_out"][h * 128:(h + 1) * 128, :]), writes=[WO])
            p.op("pool", lambda e: e.tensor_tensor(out=WO[:], in0=WO[:], in1=self.G[:], op=ALU.mult), reads=[WO, self.G], writes=[WO])
            for part in range(3):
                for j in range(4):
                    p.op("dve", lambda e, part=part, j=j: e.tensor_scalar(out=DG[:, part * 4 + j, :], in0=self.identb[:], scalar1=CW[:, h, part, j:j + 1], scalar2=None, op0=ALU.mult),
                         reads=[self.identb, CW], writes=[DG])
            p.op("pool", lambda e: e.memset(S[:], 0.0), writes=[S])
            p.op("pool", lambda e: e.memset(Sb[:], 0.0), writes=[Sb])
            p.op("pool", lambda e: e.memset(XC[:, :, 0:3], 0.0), writes=[XC])
            yield
            for blk in range(NB):
                cs = slice(blk * 512, (blk + 1) * 512)
                for part in range(3):
                    ps = bk[part]
                    for k in range(8):
                        self.mm(ps[:], WH[:, k, part * 128:(part + 1) * 128], self.HT[:, k, cs], k == 0, k == 7, [WH, HTr[blk]], allb[part])
                    eng = ("act", "dve", "act")[part]
                    if eng == "act":
                        p.op("act", lambda e, part=part, ps=ps: e.copy(XC[:, part, 3:515], ps[:]), reads=allb[part], writes=[XC])
                    else:
                        p.op("dve", lambda e, part=part, ps=ps: e.tensor_copy(XC[:, part, 3:515], ps[:]), reads=allb[part], writes=[XC])
                yield
                for tt in range(4):
                    t = blk * 4 + tt
                    ts_ = slice(t * 128, (t + 1) * 128)
                    ZS, GZ, QKV, SQ, st, TOK, KQT, DT2, M2, ATb = (b[n] for n in ("ZS", "GZ", "QKV", "SQ", "st", "TOK", "KQT", "DT2", "M2", "ATb"))
                    YA, Pb, TTb, U, WTb, VNb, OTk, OG, OGT = [b["YA0"], b["YA1"]], [b["P0"], b["P1"]], b["TTb"], b["U"], b["WTb"], b["VNb"], b["OT"], b["OG"], b["OGT"]
                    for k in range(8):
                        self.mm(bk[0][:, 0:128], self.HT[:, k, ts_], WH[:, k, 384:512], k == 0, k == 7, [WH, HTr[blk]], r_zc)
                    for part in range(3):
                        for j in range(4):
                            self.mm(bk[0][:, 128 + part * 128:256 + part * 128], XC[:, part, tt * 128 + j:tt * 128 + j + 128], DG[:, part * 4 + j, :], j == 0, j == 3, [XC, DG], r_zc)
                    p.op("act", lambda e: e.activation(out=ZS[:], in_=bk[0][:, 0:128], func=AF.Silu), reads=r_zc, writes=[ZS])
                    p.op("act", lambda e: e.activation(out=QKV[:].rearrange("p a b -> p (a b)"), in_=bk[0][:, 128:512], func=AF.Silu), reads=r_zc, writes=[QKV])
                    p.op("pool", lambda e: e.tensor_tensor(out=GZ[:], in0=ZS[:], in1=GON[:], op=ALU.mult), reads=[ZS, GON], writes=[GZ])
                    yield
                    if "GDN_OPS2" in os.environ and h == 0 and t == 0:
                        p.limit = int(os.environ["GDN_OPS2"])
                    p.op("dve", lambda e: e.tensor_tensor(out=SQ[:], in0=QKV[:, 0:2, :], in1=QKV[:, 0:2, :], op=ALU.mult), reads=[QKV], writes=[SQ])
                    p.op("dve", lambda e: e.tensor_reduce(out=st[:, 0:2], in_=SQ[:], axis=AX.X, op=ALU.add), reads=[SQ], writes=[st])
                    p.op("act", lambda e: e.activation(out=st[:, 0:2], in_=st[:, 0:2], func=AF.Sqrt, bias=self.epsc[:], scale=1.0), reads=[st, self.epsc], writes=[st])
                    p.op("dve", lambda e: e.reciprocal(out=st[:, 2:4], in_=st[:, 0:2]), reads=[st], writes=[st])
                    p.op("act", lambda e: e.activation(out=TOK[:, 0, :], in_=QKV[:, 1, :], func=AF.Copy, scale=st[:, 3:4]), reads=[QKV, st], writes=[(TOK.name, 0)])
                    p.op("dve", lambda e: e.tensor_scalar(out=TOK[:, 1, :], in0=QKV[:, 0, :], scalar1=st[:, 2:3], scalar2=dk_s, op0=ALU.mult, op1=ALU.mult), reads=[QKV, st], writes=[(TOK.name, 0)])
                    p.op("dve", lambda e, t=t: e.tensor_scalar(out=TOK[:, 2, :], in0=QKV[:, 0, :], scalar1=st[:, 2:3], scalar2=GAMS[:, t, h:h + 1], op0=ALU.mult, op1=ALU.mult), reads=[QKV, st, GAMS], writes=[(TOK.name, 0)])
                    p.op("dve", lambda e, t=t: e.tensor_scalar(out=TOK[:, 3, :], in0=QKV[:, 1, :], scalar1=st[:, 3:4], scalar2=EGL[:, t, h:h + 1], op0=ALU.mult, op1=ALU.mult), reads=[QKV, st, EGL], writes=[(TOK.name, 1)])
                    p.op("dve", lambda e, t=t: e.tensor_scalar(out=TOK[:, 4, :], in0=QKV[:, 1, :], scalar1=st[:, 3:4], scalar2=BG[:, t, h:h + 1], op0=ALU.mult, op1=ALU.mult), reads=[QKV, st, BG], writes=[(TOK.name, 1)])
                    p.op("act", lambda e, t=t: e.activation(out=TOK[:, 5, :], in_=QKV[:, 2, :], func=AF.Copy, scale=BETA[:, t, h:h + 1]), reads=[QKV, BETA], writes=[(TOK.name, 1)])
                    for i in range(3):
                        self.tr(bkb[1][:, i * 128:(i + 1) * 128], TOK[:, i, :], self.identb[:], [(TOK.name, 0)], r_tr)
                    p.op("act", lambda e: e.copy(KQT[:].rearrange("p a b -> p (a b)"), bkb[1][:, 0:384]), reads=r_tr, writes=[KQT])
                    yield
                    d0 = 256
                    DGC = b["DGC"]
                    p.op("act", lambda e, t=t: e.activation(out=DGC[:, 0, :], in_=self.identf[:], func=AF.Copy, scale=GC3[:, t, 0, h:h + 1]), reads=[self.identf, GC3], writes=[DGC])
                    p.op("act", lambda e, t=t: e.activation(out=DGC[:, 1, :], in_=self.identf[:], func=AF.Copy, scale=GC3[:, t, 2, h:h + 1]), reads=[self.identf, GC3], writes=[DGC])
                    self.mm(bk[1][:, d0:d0 + 128], self.onesf[:], DGC[:, 0, :], True, False, [self.onesf, DGC], r_d)
                    self.mm(bk[1][:, d0:d0 + 128], self.identf[:], MNi[:], False, True, [self.identf, MNi], r_d)
                    self.mm(bk[1][:, d0 + 128:d0 + 256], self.onesf[:], DGC[:, 1, :], True, False, [self.onesf, DGC], r_d)
                    self.mm(bk[1][:, d0 + 128:d0 + 256], self.identf[:], MNs[:], False, True, [self.identf, MNs], r_d)
                    p.op("act", lambda e, t=t: e.activation(out=DT2[:].rearrange("p a b -> p (a b)"), in_=bk[1][:, d0:d0 + 256], func=AF.Exp, bias=GC3[:, t, 1, h:h + 1], scale=1.0),
                         reads=r_d + [GC3], writes=[DT2])
                    yield
                    self.mm(bk[2][:, 0:128], KQT[:, 0, :], KQT[:, 1, :], True, True, [KQT], r_kk)
                    self.mm(bk[2][:, 128:256], KQT[:, 0, :], KQT[:, 0, :], True, True, [KQT], r_kk)
                    p.op("dve", lambda e: e.tensor_tensor(out=M2[:].rearrange("p a b -> p (a b)"), in0=bk[2][:, 0:256], in1=DT2[:].rearrange("p a b -> p (a b)"), op=ALU.mult),
                         reads=r_kk + [DT2], writes=[M2])
                    p.op("act", lambda e: e.copy(ATb[:], M2[:, 0, :]), reads=[M2], writes=[ATb])
                    yield
                    Y = M2[:, 1, :]
                    self.tr(bk[2][:, 256:384], Y, self.identf[:], [M2], r_a)
                    p.op("act", lambda e: e.copy(YA[0][:, 1, :], bk[2][:, 256:384]), reads=r_a, writes=[(YA[0].name, 1)])
                    p.op("pool", lambda e: e.tensor_copy(YA[0][:, 0, :], Y), reads=[M2], writes=[(YA[0].name, 0)])
                    p.op("pool", lambda e: e.tensor_tensor(out=Pb[0][:], in0=self.identf[:], in1=Y, op=ALU.subtract), reads=[M2, self.identf], writes=[Pb[0]])
                    yield
                    for it in range(5):
                        cur, nxt = YA[it % 2], YA[(it + 1) % 2]
                        pc, pn = Pb[it % 2], Pb[(it + 1) % 2]
                        rc = [(cur.name, 0), (cur.name, 1)]
                        if it < 4:
                            self.mm(bk[3][:, 0:128], cur[:, 1, :], cur[:, 0, :], True, True, rc, r_ya)
                        self.mm(bk[3][:, 128:256], cur[:, 0, :], cur[:, 1, :], True, True, rc, r_ya)
                        if it < 4:
                            p.op("act", lambda e, nxt=nxt: e.copy(nxt[:].rearrange("p a b -> p (a b)"), bk[3][:, 0:256]), reads=r_ya, writes=[(nxt.name, 0), (nxt.name, 1)])
                        else:
                            p.op("act", lambda e, nxt=nxt: e.copy(nxt[:, 1, :], bk[3][:, 128:256]), reads=r_ya, writes=[(nxt.name, 1)])
                        yield
                        self.mm(bk[2][:, 384:512], self.identf[:], pc[:], True, False, [self.identf, pc], r_p)
                        self.mm(bk[2][:, 384:512], nxt[:, 1, :], pc[:], False, True, [(nxt.name, 1), pc], r_p)
                        if it < 4:
                            p.op("dve", lambda e, pn=pn: e.tensor_copy(pn[:], bk[2][:, 384:512]), reads=r_p, writes=[pn])
                        else:
                            p.op("dve", lambda e: e.tensor_copy(TTb[:], bk[2][:, 384:512]), reads=r_p, writes=[TTb])
                        yield
                    if "GDN_OPS3" in os.environ and h == 1 and t == 0:
                        p.limit = int(os.environ["GDN_OPS3"])
                    self.mm(bk[3][:, 256:384], TTb[:], TOK[:, 5, :], True, True, [TTb, (TOK.name, 1)], r_u)
                    self.mm(bk[0][:, 0:128], TOK[:, 4, :], TTb[:], True, True, [TTb, (TOK.name, 1)], r_zc)
                    p.op("act", lambda e: e.copy(U[:], bk[3][:, 256:384]), reads=r_u, writes=[U])
                    p.op("dve", lambda e: e.tensor_copy(WTb[:], bk[0][:, 0:128]), reads=r_zc, writes=[WTb])
                    yield
                    for hf in range(2):
                        r0 = hf * 64
                        rr = slice(r0, r0 + 64)
                        if "GDN_OPS4" in os.environ and h == int(os.environ.get("GDN_H", "0")) and t == 0 and hf == 0:
                            p.limit = int(os.environ["GDN_OPS4"])
                        if os.environ.get("GDN_M128") == "1":
                            self.mm(bk[2][:, 0:128], WTb[:, :], Sb[:], True, True, [WTb, Sb], r_kk)
                        elif os.environ.get("GDN_M128") == "2":
                            self.mm(bk[2][0:64, 0:128], KQT[:, 2, rr], Sb[:], True, True, [KQT, Sb], r_kk)
                        elif os.environ.get("GDN_M128") == "4":
                            self.mm(bk[0][:, 0:128], WTb[:, :], Sb[:], True, True, [WTb, Sb], r_zc)
                        elif os.environ.get("GDN_M128") == "5":
                            self.mm(bk[2][:, 384:512], WTb[:, :], Sb[:], True, True, [WTb, Sb], r_p)
                        elif os.environ.get("GDN_M128") == "3":
                            self.mm(bk[2][0:64, 0:128], WTb[:, rr], KQT[:, 2, :], True, True, [KQT, WTb], r_kk)
                        else:
                            self.mm(bk[2][0:64, 0:128], WTb[:, rr], Sb[:], True, True, [WTb, Sb], r_kk)
                        p.op("dve", lambda e, rr=rr: e.scalar_tensor_tensor(out=VNb[rr, :], in0=bk[2][0:64, 0:128], scalar=-1.0, in1=U[rr, :], op0=ALU.mult, op1=ALU.add), reads=[U] + r_kk, writes=[VNb])
                        yield
                        self.mm(bk[2][0:64, 128:256], KQT[:, 2, rr], Sb[:], True, False, [KQT, Sb], r_kk)
                        self.mm(bk[2][0:64, 128:256], ATb[rr, rr], VNb[rr, :], False, True, [ATb, VNb], r_kk)
                        p.op("act", lambda e, rr=rr: e.copy(OTk[rr, :], bk[2][0:64, 128:256]), reads=r_kk, writes=[OTk])
                        self.mm(bk[2][:, 256:384], TOK[rr, 3, :], VNb[rr, :], True, True, [(TOK.name, 1), VNb], r_a)
                        p.op("act", lambda e, t=t, hf=hf: e.activation(out=S[:], in_=S[:], func=AF.Copy, scale=DECB[:, t, hf, h:h + 1]), reads=[S, DECB], writes=[S])
                        p.op("dve", lambda e: e.tensor_tensor(out=S[:], in0=bk[2][:, 256:384], in1=S[:], op=ALU.add), reads=[S] + r_a, writes=[S])
                        p.op("act", lambda e: e.copy(Sb[:], S[:]), reads=[S], writes=[Sb])
                        yield
                    p.op("pool", lambda e: e.memset(st[:, 4:5], 0.0), reads=[st], writes=[st])
                    p.op("act", lambda e: e.activation(out=b["junk"][:], in_=OTk[:], func=AF.Square, accum_out=st[:, 4:5]), reads=[OTk, st], writes=[b["junk"], st])
                    p.op("act", lambda e: e.activation(out=st[:, 4:5], in_=st[:, 4:5], func=AF.Sqrt, bias=self.epsc[:], scale=1.0 / 128), reads=[st, self.epsc], writes=[st])
                    p.op("dve", lambda e: e.reciprocal(out=st[:, 5:6], in_=st[:, 4:5]), reads=[st], writes=[st])
                    p.op("dve", lambda e: e.scalar_tensor_tensor(out=OG[:], in0=OTk[:], scalar=st[:, 5:6], in1=GZ[:], op0=ALU.mult, op1=ALU.mult), reads=[OTk, st, GZ], writes=[OG])
                    yield
                    self.tr(bkb[1][:, 384:512], OG[:], self.identb[:], [OG], r_og)
                    p.op("act", lambda e: e.copy(OGT[:], bkb[1][:, 384:512]), reads=r_og, writes=[OGT])
                    yield
                    for hf in range(2):
                        self.mm(bk[0][:], OGT[:], WO[:, hf * 512:(hf + 1) * 512], True, True, [OGT, WO], r_zc)
                        xs_ = self.X[:, t, hf * 512:(hf + 1) * 512]
                        p.op("dve", lambda e, xs_=xs_: e.tensor_tensor(out=xs_, in0=bk[0][:], in1=xs_, op=ALU.add), reads=r_zc + [("X", t)], writes=[("X", t)])
                    yield
                p.op("dve", lambda e: e.tensor_copy(XC[:, :, 0:3], XC[:, :, 512:515]), reads=[XC], writes=[XC])
                yield

        import os
        steps = int(os.environ.get("GDN_STEPS", "1000000000"))
        for h0 in range(0, 8, NPAR):
            gens = [head_gen(h0 + i, HB[i]) for i in range(NPAR)]
            alive = list(gens)
            while alive and steps > 0:
                for g_ in list(alive):
                    steps -= 1
                    if steps <= 0:
                        break
                    try:
                        next(g_)
                    except StopIteration:
                        alive.remove(g_)
        if "GDN_DUMP" in os.environ:
            nm = os.environ["GDN_DUMP"]
            src = HB[0][nm]
            self.dump = p.tile("dump", [128, 512], F32)
            n = int(np.prod(src.shape[1:]))
            v = src[:] if len(src.shape) == 2 else src[:].rearrange("p a b -> p (a b)")
            p.op("pool", lambda e: e.memset(self.dump[:], 0.0), writes=[self.dump])
            p.op("dve", lambda e: e.tensor_copy(self.dump[:, 0:min(n, 512)], v[:, 0:min(n, 512)]), reads=[src, self.dump] + [(src.name, i) for i in range(2)], writes=[self.dump])
            p.dma("sp", lambda e: e.dma_start(out=self.y[s, 0:128, 0:512], in_=self.dump[:]), reads=[self.dump], key="dump")
            p.barrier()
            self.skip0 = True
        p.release(m0)

    def build(self):
        p = self.p
        st = self.stages
        self.setup()
        self.prologue_mod()
        final = []
        for s in range(self.nseq):
            self.load_x(s)
            for l in range(2):
                if f"mix{l}" in st:
                    self.norm_phase(l, s, 0)
                    self.make_gate(l, s, 2)
                    (self.mla_phase if l == 0 else self.gdn_phase)(s)
                if f"ffn{l}" in st:
                    self.norm_phase(l, s, 1, router=(l == 1))
                    self.make_gate(l, s, 5)
                    if l == 0:
                        ex = [(self.dram["ffn_gu"][j], self.dram["ffn_dn"][j], None) for j in range(2)]
                    else:
                        ex = [(self.dram["moe_gu"][j], self.dram["moe_dn"][j], j) for j in range(8)]
                    self.ffn_phase(ex)
            final += self.final_out(s)
        p.wait("sp", final)
        p.emit()
        return self.nc


def _cols(v, nk):
    return np.ascontiguousarray(np.asarray(v).reshape(nk, 128).T)


def _gu_layout(w, ff):
    nch = ff // 128
    g = w[:, :ff].reshape(8, 128, nch, 128)
    u = w[:, ff:].reshape(8, 128, nch, 128)
    gu = np.concatenate([g, u], axis=-1)
    return np.ascontiguousarray(gu.transpose(2, 1, 0, 3)).reshape(nch, 128, 8 * 256)


def _rope_consts():
    c = np.zeros((128, 2), np.float64)
    inv = 10000.0 ** (-np.arange(16, dtype=np.float64) / 16.0)
    for j in range(32):
        c[64 + j, 0] = inv[j % 16] / (2 * math.pi); c[64 + j, 1] = 0.25
        c[96 + j, 0] = inv[j % 16] / (2 * math.pi); c[96 + j, 1] = 0.5 if j < 16 else 0.0
    return c.astype(np.float32)


ROPEC = _rope_consts()


def prep_shared(inp):
    f = lambda k: np.asarray(inp[k], dtype=np.float32)
    sh = {}
    sh["ada"] = np.ascontiguousarray(np.concatenate([f("ada_w"), f("ada_b")[:, None, :]], axis=1))
    sh["gmix"] = np.stack([_cols(f("norm_mix")[l], 8) for l in range(2)])
    sh["gffn"] = np.stack([_cols(f("norm_ffn")[l], 8) for l in range(2)])
    sh["gfin"] = np.ascontiguousarray(np.broadcast_to(f("final_norm")[None, :], (128, D)))
    wgu = f("ffn_w_gate_up")[0]
    halves = []
    for j in range(2):
        sub = np.concatenate([wgu[:, j * 1408:(j + 1) * 1408], wgu[:, 2816 + j * 1408:2816 + (j + 1) * 1408]], axis=1)
        halves.append(_gu_layout(sub, 1408))
    sh["ffn_gu"] = np.stack(halves)
    sh["ffn_dn"] = np.ascontiguousarray(f("ffn_w_down")[0].reshape(2, 1408, D))
    sh["moe_gu"] = np.stack([_gu_layout(f("moe_w_gate_up")[0, e], 1408) for e in range(8)])
    sh["moe_dn"] = np.ascontiguousarray(f("moe_w_down")[0])
    sh["moe_r"] = np.ascontiguousarray(f("moe_w_router")[0].reshape(8, 128, 8).transpose(1, 0, 2))

    perm = np.concatenate([np.arange(16, 32), np.arange(0, 16)])
    win = f("mla_w_in")[0]
    kr = win[:, 768:800]
    win2 = np.concatenate([win[:, :768], win[:, 0:64], kr, kr[:, perm]], axis=1)
    sh["mla_in"] = np.ascontiguousarray(win2.reshape(8, 128, 896).transpose(1, 0, 2))
    sh["mla_qn"] = _cols(f("mla_q_norm")[0], 4)
    sh["mla_kvn"] = _cols(f("mla_kv_norm")[0], 2)
    wqb = f("mla_w_qb")[0].reshape(512, 16, 96)
    wqb2 = np.concatenate([wqb[:, :, :64], wqb[:, :, 64:96], wqb[:, :, 64:96][:, :, perm]], axis=2).reshape(512, 2048)
    sh["mla_qb"] = np.ascontiguousarray(wqb2.reshape(4, 128, 2048).transpose(1, 0, 2))
    wkv = f("mla_w_kvb")[0].reshape(256, 16, 128)
    sh["mla_kb"] = np.ascontiguousarray(wkv[:, :, :64].reshape(2, 128, 1024).transpose(1, 0, 2))
    sh["mla_vb"] = np.ascontiguousarray(wkv[:, :, 64:].reshape(2, 128, 1024).transpose(1, 0, 2))
    sh["mla_out"] = np.ascontiguousarray(f("mla_w_out")[0])
    sh["ropec"] = ROPEC
    gw = f("gdn_w_in")[0]
    heads = []
    for h in range(8):
        cols = np.concatenate([np.arange(h * 128, (h + 1) * 128) + off for off in (0, 1024, 2048, 3072)])
        heads.append(gw[:, cols].reshape(8, 128, 512).transpose(1, 0, 2))
    sh["gdn_in"] = np.ascontiguousarray(np.stack(heads))
    sh["gdn_ba"] = np.ascontiguousarray(gw[:, 4096:4112].reshape(8, 128, 16).transpose(1, 0, 2))
    cw = f("gdn_conv_w")[0]
    sh["gdn_conv"] = np.ascontiguousarray(cw.reshape(4, 3, 8, 128).transpose(3, 2, 1, 0))
    hpv = np.zeros((128, 24), np.float32)
    hpv[:, 0:8] = f("gdn_a_log")[0][None, :]
    hpv[:, 8:16] = f("gdn_dt_bias")[0][None, :]
    sh["gdn_hp"] = hpv
    sh["gdn_on"] = np.ascontiguousarray(np.broadcast_to(f("gdn_out_norm")[0][None, :], (128, 128)))
    sh["gdn_out"] = np.ascontiguousarray(f("gdn_w_out")[0])
    return sh


def prep_core(inp, b0, ns):
    x = np.ascontiguousarray(np.asarray(inp["x"], np.float32)[b0:b0 + ns])
    c = np.asarray(inp["c"], np.float32)[b0:b0 + ns]
    cT = np.stack([_cols(c[i], 8) for i in range(ns)])
    pos = np.asarray(inp["positions"]).astype(np.int32)[b0:b0 + ns]
    posr = np.ascontiguousarray(np.broadcast_to(pos[:, None, :], (ns, 128, SEQ)))
    return {"x": x, "cT": cT, "pos": posr}


_CACHE = {}


def run(inp, batches, ns, stages):
    key = (ns, tuple(sorted(stages)))
    if key not in _CACHE:
        _CACHE[key] = Builder(ns, stages).build()
    nc = _CACHE[key]
    sh = prep_shared(inp)
    maps = []
    for b0 in batches:
        mm = dict(sh)
        mm.update(prep_core(inp, b0, ns))
        maps.append(mm)
    res = run_bass_kernel_spmd(nc, maps, core_ids=list(range(len(batches))))
    return [r["y"] for r in res.results]


ALL = ("mix0", "ffn0", "mix1", "ffn1")


def kernel(**inputs):
    ys = run(inputs, [2 * i for i in range(NCORES)], 2, ALL)
    return np.concatenate(ys, axis=0).astype(np.float32)
```

```python
import math
import numpy as np
import concourse.bass as bass
import concourse.mybir as mybir
from concourse.bass_utils import run_bass_kernel_spmd

F32 = mybir.dt.float32
BF16 = mybir.dt.bfloat16
I32 = mybir.dt.int32
AF = mybir.ActivationFunctionType
ALU = mybir.AluOpType
AX = mybir.AxisListType

NCORES = 8
SEQ = 2048
D = 1024
NT = SEQ // 128
NB = SEQ // 512
EPS = 1e-6
GEN = 30000


class Prog:
    ENG = ("pe", "act", "dve", "pool", "sp")

    def __init__(self, nc):
        self.nc = nc
        self.q = {e: [] for e in self.ENG}
        self.cnt = {e: 0 for e in self.ENG}
        self.res = {}
        self.seen = {e: {} for e in self.ENG}
        self.dma_cnt = {}
        self.semkeys = set()
        self.off = 16512
        self.ntile = 0
        self.cap = nc.SBUF_PARTITION_SIZE_BYTES

    def tile(self, name, shape, dt):
        nbytes = int(np.prod(shape[1:])) * (2 if dt == BF16 else 4)
        self.off = (self.off + 63) // 64 * 64
        assert self.off + nbytes <= self.cap, (name, self.off, nbytes)
        self.ntile += 1
        t = self.nc.alloc_sbuf_tensor_at(f"{name}_{self.ntile}", list(shape), dt, offset=self.off)
        self.off += nbytes
        return t

    def mark(self):
        return self.off

    def release(self, m):
        self.barrier()
        self.off = m

    def _key(self, r):
        return r if isinstance(r, (str, tuple)) else r.name

    def _st(self, r):
        r = self._key(r)
        s = self.res.get(r)
        if s is None:
            s = self.res[r] = {"w": None, "r": []}
        return s

    def _deps(self, eng, reads, writes):
        deps = {}

        def add(d):
            if d is None:
                return
            k, v = d
            if eng == "pe" and isinstance(k, tuple) and k[0] == "pe":
                return
            if deps.get(k, 0) < v:
                deps[k] = v
        for r in reads:
            add(self._st(r)["w"])
        for r in writes:
            s = self._st(r)
            add(s["w"])
            for d in s["r"]:
                add(d)
        out = []
        seen = self.seen[eng]
        for k, v in deps.items():
            if seen.get(k, 0) < v:
                seen[k] = v
                out.append((k, v))
        return out

    def _commit(self, token, reads, writes):
        for r in reads:
            lst = self._st(r)["r"]
            lst[:] = [d for d in lst if d[0] != token[0]]
            lst.append(token)
        for r in writes:
            s = self._st(r)
            s["w"] = token
            s["r"] = []

    limit = None

    def op(self, eng, fn, reads=(), writes=()):
        if self.limit is not None:
            if self.limit <= 0:
                return None
            self.limit -= 1
        waits = self._deps(eng, reads, writes)
        if self.limit == 0:
            print("LAST OP", eng, "waits", waits, "cnt", self.cnt, "reads", [self._key(r) for r in reads], "writes", [self._key(r) for r in writes])
        self.cnt[eng] += 1
        n = self.cnt[eng]
        key = (eng, (n - 1) // GEN)
        self.semkeys.add(key)
        token = (key, (n - 1) % GEN + 1)
        self.q[eng].append((waits, fn, (key, 1)))
        self._commit(token, reads, writes)
        return token

    def dma(self, queue, fn, reads=(), writes=(), key=None):
        if key is None:
            r0 = writes[0] if writes else reads[0]
            key = ("dma", self._key(r0))
        self.semkeys.add(key)
        waits = self._deps(queue, reads, writes)
        v = self.dma_cnt.get(key, 0) + 16
        self.dma_cnt[key] = v
        token = (key, v)
        self.q[queue].append((waits, fn, (key, 16)))
        self._commit(token, reads, writes)
        return token

    def barrier(self):
        toks = []
        for e in self.ENG:
            n = self.cnt[e]
            if n:
                toks.append(((e, (n - 1) // GEN), (n - 1) % GEN + 1))
        toks += list(self.dma_cnt.items())
        for e in self.ENG:
            self.wait(e, toks)

    def wait(self, eng, tokens):
        waits = []
        for k, v in tokens:
            if eng == "pe" and isinstance(k, tuple) and k[0] == "pe":
                continue
            if self.seen[eng].get(k, 0) < v:
                self.seen[eng][k] = v
                waits.append((k, v))
        if waits:
            self.q[eng].append((waits, None, None))

    def emit(self):
        import contextlib
        nc = self.nc
        with contextlib.ExitStack() as es:
            sems = {}
            for i, k in enumerate(sorted(self.semkeys, key=str)):
                sems[k] = es.enter_context(nc.semaphore(f"s{i}"))
            block = es.enter_context(nc.Block())
            q = self.q

            def run(engname):
                def body(e):
                    for waits, fn, inc in q[engname]:
                        for k, v in waits:
                            e.wait_ge(sems[k], v)
                        if fn is not None:
                            fn(e).then_inc(sems[inc[0]], inc[1])
                return body
            block.tensor(run("pe"))
            block.scalar(run("act"))
            block.vector(run("dve"))
            block.gpsimd(run("pool"))
            block.sync(run("sp"))


class Builder:
    def __init__(self, nseq, stages):
        self.nseq = nseq
        self.stages = stages
        nc = self.nc = bass.Bass("TRN2", target_bir_lowering=False)
        self.p = Prog(nc)
        self.dram = {}
        self.rr = 0

    def din(self, name, shape, dt=F32):
        a = self.nc.dram_tensor(name, list(shape), dt, kind="ExternalInput").ap()
        self.dram[name] = a
        return a

    def setup(self):
        p, nc, ns = self.p, self.nc, self.nseq
        d = self.din
        d("x", [ns, SEQ, D]); d("cT", [ns, 128, 8]); d("pos", [ns, 128, SEQ], I32)
        d("ada", [2, 1025, 6 * D])
        d("gmix", [2, 128, 8]); d("gffn", [2, 128, 8]); d("gfin", [128, D])
        d("ffn_gu", [2, 11, 128, 8 * 256]); d("ffn_dn", [2, 1408, D])
        d("moe_gu", [8, 11, 128, 8 * 256]); d("moe_dn", [8, 1408, D]); d("moe_r", [128, 8, 8])
        d("mla_in", [128, 8, 896]); d("mla_qn", [128, 4]); d("mla_kvn", [128, 2])
        d("mla_qb", [128, 4, 2048]); d("mla_kb", [128, 2, 1024]); d("mla_vb", [128, 2, 1024])
        d("mla_out", [D, D]); d("ropec", [128, 2])
        d("gdn_in", [8, 128, 8, 512]); d("gdn_ba", [128, 8, 16]); d("gdn_conv", [128, 8, 3, 4])
        d("gdn_hp", [128, 24]); d("gdn_on", [128, 128]); d("gdn_out", [D, D])
        self.y = nc.dram_tensor("y", [ns, SEQ, D], F32, kind="ExternalOutput").ap()

        self.X = p.tile("X", [128, NT, D], F32)
        self.HT = p.tile("HT", [128, 8, SEQ], BF16)
        self.identb = p.tile("identb", [128, 128], BF16)
        self.identf = p.tile("identf", [128, 128], F32)
        self.onesb = p.tile("onesb", [128, 128], BF16)
        self.onesf = p.tile("onesf", [128, 128], F32)
        self.epsc = p.tile("epsc", [128, 1], F32)
        self.MODC = p.tile("MODC", [128, 2, 48, ns], F32)
        self.G = p.tile("G", [128, D], F32)
        self.Acol = p.tile("Acol", [128, 8], F32)
        self.gmix = p.tile("gmix", [128, 2, 8], F32)
        self.gffn = p.tile("gffn", [128, 2, 8], F32)
        self.GF = p.tile("GF", [128, D], F32)
        self.ssq = p.tile("ssq", [128, NT], F32)
        self.rstd = p.tile("rstd", [128, NT], F32)
        self.COMB = p.tile("COMB", [128, NT, 8], F32)
        self.PS = [nc.alloc_psum_tensor(f"ps{i}", [128, 512], F32) for i in range(8)]

        p.op("pool", lambda e: e.memset(self.identf[:], 1.0), writes=[self.identf])
        p.op("pool", lambda e: e.affine_select(out=self.identf[:], in_=self.identf[:], pattern=[[-1, 128]],
                                               compare_op=ALU.is_equal, fill=0.0, base=0, channel_multiplier=1),
             reads=[self.identf], writes=[self.identf])
        p.op("dve", lambda e: e.tensor_copy(self.identb[:], self.identf[:]), reads=[self.identf], writes=[self.identb])
        p.op("pool", lambda e: e.memset(self.onesb[:], 1.0), writes=[self.onesb])
        p.op("pool", lambda e: e.memset(self.onesf[:], 1.0), writes=[self.onesf])
        p.op("pool", lambda e: e.memset(self.epsc[:], EPS), writes=[self.epsc])
        p.dma("sp", lambda e: e.dma_start(out=self.gmix[:], in_=self.dram["gmix"].rearrange("l p k -> p l k")), writes=[self.gmix])
        p.dma("sp", lambda e: e.dma_start(out=self.gffn[:], in_=self.dram["gffn"].rearrange("l p k -> p l k")), writes=[self.gffn])
        p.dma("sp", lambda e: e.dma_start(out=self.GF[:], in_=self.dram["gfin"]), writes=[self.GF])

    def prologue_mod(self):
        p, ns = self.p, self.nseq
        m = p.mark()
        cT = p.tile("cTs", [128, ns, 8], F32)
        condb = p.tile("condb", [128, 8, ns], BF16)
        for s in range(ns):
            p.dma("sp", lambda e, s=s: e.dma_start(out=cT[:, s, :], in_=self.dram["cT"][s]), writes=[cT], key=("dma", "cT", s))
        for s in range(ns):
            p.op("act", lambda e, s=s: e.activation(out=condb[:, :, s], in_=cT[:, s, :], func=AF.Silu), reads=[cT], writes=[condb])
        W = [p.tile(f"adaw{i}", [128, 8, 512], BF16) for i in range(2)]
        Bv = [p.tile(f"adab{i}", [1, 512], BF16) for i in range(2)]
        ada = self.dram["ada"]
        it = 0
        for l in range(2):
            ps = self.PS[l]
            psv = ps[:, 0:48 * ns].rearrange("p (b s) -> p b s", s=ns)
            for ch in range(12):
                w, bv = W[it % 2], Bv[it % 2]
                it += 1
                cols = slice(ch * 512, (ch + 1) * 512)
                p.dma("pool", lambda e, w=w, l=l, cols=cols: e.dma_start(
                    out=w[:], in_=ada[l, 0:1024, cols].rearrange("(k p) n -> p k n", p=128)), writes=[w])
                p.dma("pool", lambda e, bv=bv, l=l, cols=cols: e.dma_start(out=bv[:], in_=ada[l, 1024:1025, cols]), writes=[bv])
                for qd in range(4):
                    blk = ch * 4 + qd
                    for k in range(8):
                        p.op("pe", lambda e, w=w, k=k, qd=qd, blk=blk, psv=psv: e.matmul(
                            psv[:, blk, :], lhsT=w[:, k, qd * 128:(qd + 1) * 128], rhs=condb[:, k, :], start=(k == 0), stop=False),
                            reads=[w, condb], writes=[ps])
                    p.op("pe", lambda e, bv=bv, qd=qd, blk=blk, psv=psv: e.matmul(
                        psv[:, blk, :], lhsT=bv[0:1, qd * 128:(qd + 1) * 128], rhs=self.onesb[0:1, 0:ns], start=False, stop=True),
                        reads=[bv, self.onesb], writes=[ps])
            p.op("act", lambda e, l=l, psv=psv: e.copy(self.MODC[:, l, :, :], psv), reads=[ps], writes=[self.MODC])
        p.release(m)

    def modc(self, l, s, j, k=None):
        if k is None:
            return self.MODC[:, l, j * 8:(j + 1) * 8, s]
        return self.MODC[:, l, j * 8 + k, s:s + 1]

    def make_gate(self, l, s, j):
        p = self.p
        m = p.mark()
        dg = [p.tile(f"dg{i}", [128, 128], F32) for i in range(2)]
        for k in range(8):
            t = dg[k % 2]
            ps = self.PS[k // 4]
            p.op("dve", lambda e, t=t, k=k: e.tensor_scalar(out=t[:], in0=self.identf[:], scalar1=self.modc(l, s, j, k), scalar2=None, op0=ALU.mult),
                 reads=[self.identf, self.MODC], writes=[t])
            p.op("pe", lambda e, t=t, k=k, ps=ps: e.matmul(ps[:, (k % 4) * 128:(k % 4 + 1) * 128], lhsT=self.onesf[:], rhs=t[:], start=True, stop=True),
                 reads=[t, self.onesf], writes=[ps])
        for h in range(2):
            p.op("act", lambda e, h=h: e.copy(self.G[:, h * 512:(h + 1) * 512], self.PS[h][:]), reads=[self.PS[h]], writes=[self.G])
        p.release(m)

    def load_x(self, s):
        p = self.p
        for t in range(NT):
            q = "sp" if t % 2 == 0 else "act"
            p.dma(q, lambda e, t=t: e.dma_start(out=self.X[:, t, :], in_=self.dram["x"][s, t * 128:(t + 1) * 128, :]),
                  writes=[("X", t)])

    def final_out(self, s):
        p = self.p
        m = p.mark()
        ob = [p.tile(f"ob{i}", [128, D], F32) for i in range(2)]
        junk = p.tile("junkf", [128, D], BF16)
        toks = []
        for t in range(NT):
            if t == 0 and getattr(self, "skip0", False):
                continue
            self.rms_stats(t, junk)
            o = ob[t % 2]
            p.op("dve", lambda e, t=t, o=o: e.scalar_tensor_tensor(out=o[:], in0=self.X[:, t, :], scalar=self.rstd[:, t:t + 1], in1=self.GF[:],
                                                                  op0=ALU.mult, op1=ALU.mult),
                 reads=[("X", t), ("rstd", t), self.GF], writes=[o])
            toks.append(p.dma("sp", lambda e, t=t, o=o: e.dma_start(out=self.y[s, t * 128:(t + 1) * 128, :], in_=o[:]), reads=[o]))
        p.release(m)
        return toks

    def rms_stats(self, t, junk):
        p = self.p
        p.op("pool", lambda e: e.memset(self.ssq[:, t:t + 1], 0.0), writes=[("ssq", t)])
        p.op("act", lambda e: e.activation(out=junk[:], in_=self.X[:, t, :], func=AF.Square, accum_out=self.ssq[:, t:t + 1]),
             reads=[("X", t)], writes=[junk, ("ssq", t)])
        p.op("act", lambda e: e.activation(out=self.ssq[:, t:t + 1], in_=self.ssq[:, t:t + 1], func=AF.Sqrt, bias=self.epsc[:], scale=1.0 / D),
             reads=[("ssq", t), self.epsc], writes=[("ssq", t)])
        p.op("dve", lambda e: e.reciprocal(out=self.rstd[:, t:t + 1], in_=self.ssq[:, t:t + 1]), reads=[("ssq", t)], writes=[("rstd", t)])

    def norm_phase(self, l, s, which, router=False):
        p = self.p
        m = p.mark()
        jsh, jsc = (0, 1) if which == 0 else (3, 4)
        gcol = (self.gmix if which == 0 else self.gffn)[:, l, :]
        p.op("dve", lambda e: e.tensor_scalar(out=self.Acol[:], in0=self.modc(l, s, jsc), scalar1=1.0, scalar2=None, op0=ALU.add),
             reads=[self.MODC], writes=[self.Acol])
        p.op("dve", lambda e: e.tensor_tensor(out=self.Acol[:], in0=self.Acol[:], in1=gcol, op=ALU.mult),
             reads=[self.Acol, self.gmix, self.gffn], writes=[self.Acol])
        junk = p.tile("junk", [128, D], BF16)
        xn = [p.tile(f"xn{i}", [128, D], BF16) for i in range(2)]
        if router:
            xnf2 = [p.tile(f"xnf{i}", [128, D], F32) for i in range(2)]
            hTf2 = [p.tile(f"hTf{i}", [128, 8, 128], F32) for i in range(2)]
            wr = p.tile("wr", [128, 8, 8], F32)
            lg2 = [p.tile(f"lg{i}", [128, 8], F32) for i in range(2)]
            mx2 = [p.tile(f"mx{i}", [128, 8], F32) for i in range(2)]
            sc2 = [p.tile(f"rsc{i}", [128, 4], F32) for i in range(2)]
            c12 = [p.tile(f"c1{i}", [128, 8], F32) for i in range(2)]
            p.dma("sp", lambda e: e.dma_start(out=wr[:], in_=self.dram["moe_r"]), writes=[wr])
        psT = [self.PS[i].bitcast(BF16) for i in range(4)]
        for blk in range(NB):
            for tt in range(4):
                t = blk * 4 + tt
                self.rms_stats(t, junk)
                x_ = xn[t % 2]
                p.op("dve", lambda e, t=t, x_=x_: e.tensor_scalar(out=x_[:], in0=self.X[:, t, :], scalar1=self.rstd[:, t:t + 1], scalar2=None, op0=ALU.mult),
                     reads=[("X", t), ("rstd", t)], writes=[x_])
                for k in range(8):
                    p.op("pe", lambda e, k=k, tt=tt, x_=x_: e.transpose(psT[k // 2][:, (k % 2) * 512 + tt * 128:(k % 2) * 512 + (tt + 1) * 128],
                                                                       x_[:, k * 128:(k + 1) * 128], self.identb[:]),
                         reads=[x_, self.identb], writes=[self.PS[k // 2]])
                if router:
                    def router_ops(t, xnf, hTf, lg, mx, sc, c1, part):
                        if part == 0:
                            p.op("dve", lambda e, t=t, xnf=xnf: e.tensor_scalar(out=xnf[:], in0=self.X[:, t, :], scalar1=self.rstd[:, t:t + 1], scalar2=None, op0=ALU.mult),
                                 reads=[("X", t), ("rstd", t)], writes=[xnf])
                            for k in range(8):
                                ps = self.PS[4 + k // 4]
                                p.op("pe", lambda e, k=k, ps=ps: e.transpose(ps[:, (k % 4) * 128:(k % 4 + 1) * 128], xnf[:, k * 128:(k + 1) * 128], self.identf[:]),
                                     reads=[xnf, self.identf], writes=[ps])
                            for k in range(8):
                                ps = self.PS[4 + k // 4]
                                p.op("act", lambda e, k=k, ps=ps: e.activation(out=hTf[:, k, :], in_=ps[:, (k % 4) * 128:(k % 4 + 1) * 128], func=AF.Identity,
                                                                              bias=self.modc(l, s, jsh, k), scale=self.Acol[:, k:k + 1]),
                                     reads=[ps, self.Acol, self.MODC], writes=[hTf])
                            return
                        for k in range(8):
                            p.op("pe", lambda e, k=k: e.matmul(self.PS[6][:, 0:8], lhsT=hTf[:, k, :], rhs=wr[:, k, :], start=(k == 0), stop=(k == 7)),
                                 reads=[hTf, wr], writes=[self.PS[6]])
                        p.op("act", lambda e: e.copy(lg[:], self.PS[6][:, 0:8]), reads=[self.PS[6]], writes=[lg])
                        p.op("dve", lambda e: e.max(out=mx[:], in_=lg[:]), reads=[lg], writes=[mx])
                        p.op("dve", lambda e: e.tensor_tensor(out=sc[:, 0:1], in0=mx[:, 1:2], in1=mx[:, 0:1], op=ALU.subtract), reads=[mx], writes=[sc])
                        p.op("act", lambda e: e.activation(out=sc[:, 0:1], in_=sc[:, 0:1], func=AF.Exp), reads=[sc], writes=[sc])
                        p.op("dve", lambda e: e.tensor_scalar(out=sc[:, 1:2], in0=sc[:, 0:1], scalar1=1.0, scalar2=None, op0=ALU.add), reads=[sc], writes=[sc])
                        p.op("dve", lambda e: e.reciprocal(out=sc[:, 1:2], in_=sc[:, 1:2]), reads=[sc], writes=[sc])
                        p.op("dve", lambda e: e.tensor_tensor(out=sc[:, 2:3], in0=sc[:, 0:1], in1=sc[:, 1:2], op=ALU.mult), reads=[sc], writes=[sc])
                        p.op("dve", lambda e: e.tensor_scalar(out=c1[:], in0=lg[:], scalar1=mx[:, 0:1], scalar2=sc[:, 1:2], op0=ALU.is_equal, op1=ALU.mult),
                             reads=[lg, mx, sc], writes=[c1])
                        p.op("dve", lambda e, t=t: e.tensor_scalar(out=self.COMB[:, t, :], in0=lg[:], scalar1=mx[:, 1:2], scalar2=sc[:, 2:3], op0=ALU.is_equal, op1=ALU.mult),
                             reads=[lg, mx, sc], writes=[("COMB", t)])
                        p.op("dve", lambda e, t=t: e.tensor_tensor(out=self.COMB[:, t, :], in0=self.COMB[:, t, :], in1=c1[:], op=ALU.add),
                             reads=[c1, ("COMB", t)], writes=[("COMB", t)])
                    ra = lambda u, part: router_ops(u, xnf2[u % 2], hTf2[u % 2], lg2[u % 2], mx2[u % 2], sc2[u % 2], c12[u % 2], part)
                    if t >= 1:
                        ra(t - 1, 0)
                    if t >= 2:
                        ra(t - 2, 1)
                    if t == NT - 1:
                        ra(t, 0)
                        ra(t - 1, 1)
                        ra(t, 1)
            for k in range(8):
                src = psT[k // 2][:, (k % 2) * 512:(k % 2 + 1) * 512]
                dst = self.HT[:, k, blk * 512:(blk + 1) * 512]
                if k % 2 == 0:
                    p.op("act", lambda e, k=k, src=src, dst=dst: e.activation(out=dst, in_=src, func=AF.Identity, bias=self.modc(l, s, jsh, k),
                                                                            scale=self.Acol[:, k:k + 1]),
                         reads=[self.PS[k // 2], self.Acol, self.MODC], writes=[("HT", blk)])
                else:
                    p.op("dve", lambda e, k=k, src=src, dst=dst: e.tensor_scalar(out=dst, in0=src, scalar1=self.Acol[:, k:k + 1], scalar2=self.modc(l, s, jsh, k),
                                                                               op0=ALU.mult, op1=ALU.add),
                         reads=[self.PS[k // 2], self.Acol, self.MODC], writes=[("HT", blk)])
        p.release(m)

    def ffn_phase(self, experts):
        p = self.p
        m = p.mark()
        ACT_ = p.tile("actT", [128, 11, SEQ], BF16)
        WGU = [p.tile(f"wgu{i}", [128, 8, 256], BF16) for i in range(3)]
        WD = p.tile("wd", [128, 11, D], BF16)
        SG = [p.tile(f"sg{i}", [128, 512], BF16) for i in range(2)]
        HTr = [("HT", b) for b in range(NB)]
        it = 0
        for (gu, dn, ci) in experts:
            p.dma("pool", lambda e, dn=dn: e.dma_start(out=WD[:], in_=dn.rearrange("(c p) n -> p c n", p=128)), writes=[WD])
            for c in range(11):
                w = WGU[c % 3]
                p.dma("pool", lambda e, w=w, c=c, gu=gu: e.dma_start(out=w[:].rearrange("p k n -> p (k n)"), in_=gu[c]), writes=[w])
                for blk in range(NB):
                    psg, psu = self.PS[it % 2], self.PS[2 + it % 2]
                    sg = SG[it % 2]
                    it += 1
                    for k in range(8):
                        p.op("pe", lambda e, w=w, k=k, blk=blk, psg=psg: e.matmul(psg[:], lhsT=w[:, k, 0:128], rhs=self.HT[:, k, blk * 512:(blk + 1) * 512],
                                                                                  start=(k == 0), stop=(k == 7)), reads=[w, HTr[blk]], writes=[psg])
                    for k in range(8):
                        p.op("pe", lambda e, w=w, k=k, blk=blk, psu=psu: e.matmul(psu[:], lhsT=w[:, k, 128:256], rhs=self.HT[:, k, blk * 512:(blk + 1) * 512],
                                                                                  start=(k == 0), stop=(k == 7)), reads=[w, HTr[blk]], writes=[psu])
                    p.op("act", lambda e, sg=sg, psg=psg: e.activation(out=sg[:], in_=psg[:], func=AF.Silu), reads=[psg], writes=[sg])
                    p.op("dve", lambda e, sg=sg, psu=psu, c=c, blk=blk: e.tensor_tensor(out=ACT_[:, c, blk * 512:(blk + 1) * 512], in0=psu[:], in1=sg[:], op=ALU.mult),
                         reads=[sg, psu], writes=[("actT", c)])
                if c == 5:
                    for cc in range(11):
                        p.op("pool", lambda e, cc=cc: e.tensor_tensor(out=WD[:, cc, :], in0=WD[:, cc, :], in1=self.G[:], op=ALU.mult),
                             reads=[WD, self.G], writes=[WD])
            for t in range(NT):
                for hf in range(2):
                    ps = self.PS[4 + (2 * t + hf) % 4]
                    for c in range(11):
                        p.op("pe", lambda e, c=c, t=t, hf=hf, ps=ps: e.matmul(ps[:], lhsT=ACT_[:, c, t * 128:(t + 1) * 128], rhs=WD[:, c, hf * 512:(hf + 1) * 512],
                                                                              start=(c == 0), stop=(c == 10)), reads=[("actT", c), WD], writes=[ps])
                    xs = self.X[:, t, hf * 512:(hf + 1) * 512]
                    if ci is None:
                        p.op("dve", lambda e, xs=xs, ps=ps: e.tensor_tensor(out=xs, in0=ps[:], in1=xs, op=ALU.add), reads=[ps, ("X", t)], writes=[("X", t)])
                    else:
                        p.op("dve", lambda e, xs=xs, ps=ps, t=t, ci=ci: e.scalar_tensor_tensor(out=xs, in0=ps[:], scalar=self.COMB[:, t, ci:ci + 1], in1=xs,
                                                                                              op0=ALU.mult, op1=ALU.add),
                             reads=[ps, ("X", t), ("COMB", t)], writes=[("X", t)])
        p.release(m)


    def mla_phase(self, s):
        p = self.p
        PS = self.PS
        m0 = p.mark()
        QNT = p.tile("QNT", [128, 4, SEQ], BF16)
        KVNT = p.tile("KVNT", [128, 2, SEQ], BF16)
        ROPE = p.tile("ROPE", [128, SEQ], F32)
        KT = p.tile("KT", [128, SEQ], BF16)
        QT = p.tile("QT", [128, SEQ], BF16)
        VA = p.tile("VA", [128, NT, 128], BF16)
        PT = [p.tile(f"PT{i}", [128, 512], BF16) for i in range(4)]
        TRI = p.tile("TRI", [128, 128], BF16)
        tA = p.tile("tA", [128, 512], F32)
        tB = p.tile("tB", [128, 512], F32)
        rec = p.tile("rec", [128, 512], F32)
        qnc = p.tile("qnc", [128, 4], F32)
        kvnc = p.tile("kvnc", [128, 2], F32)
        ropec = p.tile("ropec", [128, 2], F32)
        st = p.tile("mst", [128, 4], F32)
        p.dma("sp", lambda e: e.dma_start(out=qnc[:], in_=self.dram["mla_qn"]), writes=[qnc])
        p.dma("sp", lambda e: e.dma_start(out=kvnc[:], in_=self.dram["mla_kvn"]), writes=[kvnc])
        p.dma("sp", lambda e: e.dma_start(out=ropec[:], in_=self.dram["ropec"]), writes=[ropec])
        p.op("pool", lambda e: e.memset(TRI[:], 1.0), writes=[TRI])
        p.op("pool", lambda e: e.affine_select(out=TRI[:], in_=TRI[:], pattern=[[1, 128]], compare_op=ALU.is_ge, fill=0.0, base=0, channel_multiplier=-1),
             reads=[TRI], writes=[TRI])
        p.op("pool", lambda e: e.memset(VA[:, :, 64:128], 1.0), writes=[("VA", 0), ("VA", 1)])
        m1 = p.mark()
        WIN = p.tile("WIN", [128, 8, 896], BF16)
        p.dma("pool", lambda e: e.dma_start(out=WIN[:], in_=self.dram["mla_in"]), writes=[WIN])
        posi = p.tile("posi", [128, 512], I32)
        u = p.tile("ropu", [128, 512], F32)
        ki = p.tile("ropk", [128, 512], I32)
        kf = p.tile("ropkf", [128, 512], F32)
        qtok = [p.tile(f"qtok{i}", [128, 512], BF16) for i in range(2)]
        kvtok = [p.tile(f"kvtok{i}", [128, 256], BF16) for i in range(2)]
        junk = p.tile("mjunk", [128, 512], BF16)
        for blk in range(NB):
            cs = slice(blk * 512, (blk + 1) * 512)
            p.dma("sp", lambda e, cs=cs: e.dma_start(out=posi[:], in_=self.dram["pos"][s, :, cs]), writes=[posi])
            p.op("dve", lambda e: e.tensor_copy(u[:], posi[:]), reads=[posi], writes=[u])
            p.op("dve", lambda e: e.tensor_scalar(out=u[:], in0=u[:], scalar1=ropec[:, 0:1], scalar2=ropec[:, 1:2], op0=ALU.mult, op1=ALU.add),
                 reads=[u, ropec], writes=[u])
            p.op("dve", lambda e: e.tensor_copy(ki[:], u[:]), reads=[u], writes=[ki])
            p.op("dve", lambda e: e.tensor_copy(kf[:], ki[:]), reads=[ki], writes=[kf])
            p.op("dve", lambda e: e.tensor_tensor(out=u[:], in0=u[:], in1=kf[:], op=ALU.subtract), reads=[u, kf], writes=[u])
            p.op("act", lambda e, cs=cs: e.activation(out=ROPE[:, cs], in_=u[:], func=AF.Sin, scale=2.0 * math.pi * (1.0 - 1e-6)),
                 reads=[u], writes=[("ROPE", blk)])
        psTb = PS[4].bitcast(BF16)
        def mla_proj(t):
            ts_ = slice(t * 128, (t + 1) * 128)
            blk = t // 4
            pa, pb = PS[(t % 2) * 2], PS[(t % 2) * 2 + 1]
            for k in range(8):
                p.op("pe", lambda e, k=k, ts_=ts_, pa=pa: e.matmul(pa[:], lhsT=self.HT[:, k, ts_], rhs=WIN[:, k, 0:512], start=(k == 0), stop=(k == 7)),
                     reads=[("HT", blk), WIN], writes=[pa])
            for k in range(8):
                p.op("pe", lambda e, k=k, ts_=ts_, pb=pb: e.matmul(pb[:, 0:256], lhsT=self.HT[:, k, ts_], rhs=WIN[:, k, 512:768], start=(k == 0), stop=(k == 7)),
                     reads=[("HT", blk), WIN], writes=[pb])

        def mla_rest(t):
            ts_ = slice(t * 128, (t + 1) * 128)
            blk = t // 4
            pa, pb = PS[(t % 2) * 2], PS[(t % 2) * 2 + 1]
            p.op("pool", lambda e: e.memset(st[:, 0:2], 0.0), writes=[st])
            p.op("act", lambda e, pa=pa: e.activation(out=junk[:], in_=pa[:], func=AF.Square, accum_out=st[:, 0:1]), reads=[pa], writes=[junk, st])
            p.op("act", lambda e, pb=pb: e.activation(out=junk[:, 0:256], in_=pb[:, 0:256], func=AF.Square, accum_out=st[:, 1:2]), reads=[pb], writes=[junk, st])
            p.op("act", lambda e: e.activation(out=st[:, 0:1], in_=st[:, 0:1], func=AF.Sqrt, bias=self.epsc[:], scale=1.0 / 512), reads=[st, self.epsc], writes=[st])
            p.op("act", lambda e: e.activation(out=st[:, 1:2], in_=st[:, 1:2], func=AF.Sqrt, bias=self.epsc[:], scale=1.0 / 256), reads=[st, self.epsc], writes=[st])
            p.op("dve", lambda e: e.reciprocal(out=st[:, 2:4], in_=st[:, 0:2]), reads=[st], writes=[st])
            qt_, kvt_ = qtok[t % 2], kvtok[t % 2]
            p.op("dve", lambda e, pa=pa, qt_=qt_: e.tensor_scalar(out=qt_[:], in0=pa[:], scalar1=st[:, 2:3], scalar2=None, op0=ALU.mult), reads=[pa, st], writes=[qt_])
            p.op("act", lambda e, pb=pb, kvt_=kvt_: e.activation(out=kvt_[:], in_=pb[:, 0:256], func=AF.Copy, scale=st[:, 3:4]), reads=[pb, st], writes=[kvt_])
            for c in range(4):
                p.op("pe", lambda e, c=c, qt_=qt_: e.transpose(psTb[:, c * 128:(c + 1) * 128], qt_[:, c * 128:(c + 1) * 128], self.identb[:]),
                     reads=[qt_, self.identb], writes=[PS[4]])
            for c in range(2):
                p.op("pe", lambda e, c=c, kvt_=kvt_: e.transpose(psTb[:, 512 + c * 128:512 + (c + 1) * 128], kvt_[:, c * 128:(c + 1) * 128], self.identb[:]),
                     reads=[kvt_, self.identb], writes=[PS[4]])
            for c in range(4):
                if c % 2 == 0:
                    p.op("act", lambda e, c=c, ts_=ts_: e.activation(out=QNT[:, c, ts_], in_=psTb[:, c * 128:(c + 1) * 128], func=AF.Copy, scale=qnc[:, c:c + 1]),
                         reads=[PS[4], qnc], writes=[("QNT", blk)])
                else:
                    p.op("dve", lambda e, c=c, ts_=ts_: e.tensor_scalar(out=QNT[:, c, ts_], in0=psTb[:, c * 128:(c + 1) * 128], scalar1=qnc[:, c:c + 1], scalar2=None, op0=ALU.mult),
                         reads=[PS[4], qnc], writes=[("QNT", blk)])
            for c in range(2):
                if c % 2 == 0:
                    p.op("act", lambda e, c=c, ts_=ts_: e.activation(out=KVNT[:, c, ts_], in_=psTb[:, 512 + c * 128:512 + (c + 1) * 128], func=AF.Copy, scale=kvnc[:, c:c + 1]),
                         reads=[PS[4], kvnc], writes=[("KVNT", blk)])
                else:
                    p.op("dve", lambda e, c=c, ts_=ts_: e.tensor_scalar(out=KVNT[:, c, ts_], in0=psTb[:, 512 + c * 128:512 + (c + 1) * 128], scalar1=kvnc[:, c:c + 1], scalar2=None, op0=ALU.mult),
                         reads=[PS[4], kvnc], writes=[("KVNT", blk)])
        mla_proj(0)
        for t in range(NT):
            if t + 1 < NT:
                mla_proj(t + 1)
            mla_rest(t)
        for blk in range(NB):
            cs = slice(blk * 512, (blk + 1) * 512)
            ps = PS[5 + blk % 2]
            for k in range(8):
                p.op("pe", lambda e, k=k, cs=cs, ps=ps: e.matmul(ps[:], lhsT=WIN[:, k, 768:896], rhs=self.HT[:, k, cs], start=(k == 0), stop=(k == 7)),
                     reads=[("HT", blk), WIN], writes=[ps])
            p.op("dve", lambda e, cs=cs, ps=ps: e.tensor_tensor(out=tA[64:96, :], in0=ps[64:96, :], in1=ROPE[64:96, cs], op=ALU.mult), reads=[ps, ("ROPE", blk)], writes=[tA])
            p.op("dve", lambda e, cs=cs, ps=ps: e.tensor_tensor(out=tB[64:96, :], in0=ps[96:128, :], in1=ROPE[96:128, cs], op=ALU.mult), reads=[ps, ("ROPE", blk)], writes=[tB])
            p.op("pool", lambda e, cs=cs: e.tensor_tensor(out=KT[64:96, cs], in0=tA[64:96, :], in1=tB[64:96, :], op=ALU.add), reads=[tA, tB], writes=[("KTr", blk)])
        p.release(m1)
        m2 = p.mark()
        WQB = p.tile("WQB", [128, 4, 2048], BF16)
        WKB = p.tile("WKB", [128, 2, 1024], BF16)
        WVB = p.tile("WVB", [128, 2, 1024], BF16)
        p.dma("pool", lambda e: e.dma_start(out=WQB[:], in_=self.dram["mla_qb"]), writes=[WQB])
        p.dma("pool", lambda e: e.dma_start(out=WKB[:], in_=self.dram["mla_kb"]), writes=[WKB])
        p.dma("pool", lambda e: e.dma_start(out=WVB[:], in_=self.dram["mla_vb"]), writes=[WVB])
        OT = self.HT
        scale = 96.0 ** -0.5
        si = 0
        oi = 0
        pi_ = 0
        for h in range(16):
            for blk in range(NB):
                cs = slice(blk * 512, (blk + 1) * 512)
                ps = PS[blk % 2]
                for c in range(4):
                    p.op("pe", lambda e, c=c, cs=cs, ps=ps, h=h: e.matmul(ps[:], lhsT=WQB[:, c, h * 128:(h + 1) * 128], rhs=QNT[:, c, cs], start=(c == 0), stop=(c == 3)),
                         reads=[WQB, ("QNT", blk)], writes=[ps])
                p.op("act", lambda e, cs=cs, ps=ps: e.copy(QT[0:64, cs], ps[0:64, :]), reads=[ps], writes=[("QT", blk)])
                p.op("dve", lambda e, cs=cs, ps=ps: e.tensor_tensor(out=tA[64:96, :], in0=ps[64:96, :], in1=ROPE[64:96, cs], op=ALU.mult), reads=[ps, ("ROPE", blk)], writes=[tA])
                p.op("dve", lambda e, cs=cs, ps=ps: e.tensor_tensor(out=tB[64:96, :], in0=ps[96:128, :], in1=ROPE[96:128, cs], op=ALU.mult), reads=[ps, ("ROPE", blk)], writes=[tB])
                p.op("pool", lambda e, cs=cs: e.tensor_tensor(out=QT[64:96, cs], in0=tA[64:96, :], in1=tB[64:96, :], op=ALU.add), reads=[tA, tB], writes=[("QT", blk)])
            for blk in range(NB):
                cs = slice(blk * 512, (blk + 1) * 512)
                ps = PS[blk % 2]
                for c in range(2):
                    p.op("pe", lambda e, c=c, cs=cs, ps=ps, h=h: e.matmul(ps[0:64, :], lhsT=WKB[:, c, h * 64:(h + 1) * 64], rhs=KVNT[:, c, cs], start=(c == 0), stop=(c == 1)),
                         reads=[WKB, ("KVNT", blk)], writes=[ps])
                p.op("act", lambda e, cs=cs, ps=ps: e.copy(KT[0:64, cs], ps[0:64, :]), reads=[ps], writes=[("KTn", blk)])
            for half in range(2):
                ps = PS[half]
                psv = ps[:].rearrange("p (a b) -> p a b", b=64)
                for kk in range(8):
                    kt = half * 8 + kk
                    for c in range(2):
                        p.op("pe", lambda e, c=c, kt=kt, kk=kk, psv=psv, h=h: e.matmul(psv[:, kk, :], lhsT=KVNT[:, c, kt * 128:(kt + 1) * 128], rhs=WVB[:, c, h * 64:(h + 1) * 64],
                                                                                   start=(c == 0), stop=(c == 1)), reads=[WVB, ("KVNT", kt // 4)], writes=[ps])
                p.op("dve", lambda e, half=half, psv=psv: e.tensor_copy(VA[:, half * 8:(half + 1) * 8, 0:64], psv), reads=[ps], writes=[("VA", half)])
            steps = []
            for qb in range(NB):
                pso = PS[5 + oi % 2]
                oi += 1
                last = 4 * qb + 3
                for kt in range(last + 1):
                    steps.append((qb, kt, max(0, kt - 4 * qb) * 128, pso, last))
            LA = 2
            slot = {}

            def emit_s(i):
                qb, kt, col0, pso, last = steps[i]
                pss = PS[2 + (si + i) % 3]
                pt = PT[(pi_ + i) % 4]
                slot[i] = (pss, pt)
                p.op("pe", lambda e: e.matmul(pss[:, col0:512], lhsT=KT[0:96, kt * 128:(kt + 1) * 128],
                                              rhs=QT[0:96, qb * 512 + col0:(qb + 1) * 512], start=True, stop=True),
                     reads=[("KTn", kt // 4), ("KTr", kt // 4), ("QT", qb)], writes=[pss])
                p.op("act", lambda e: e.activation(out=pt[:, col0:512], in_=pss[:, col0:512], func=AF.Exp, scale=scale), reads=[pss], writes=[pt])
                if kt >= 4 * qb:
                    dc = (kt - 4 * qb) * 128
                    p.op("pool", lambda e: e.tensor_tensor(out=pt[:, dc:dc + 128], in0=pt[:, dc:dc + 128], in1=TRI[:], op=ALU.mult), reads=[pt, TRI], writes=[pt])

            def emit_pv(i):
                qb, kt, col0, pso, last = steps[i]
                pss, pt = slot.pop(i)
                p.op("pe", lambda e: e.matmul(pso[:, col0:512], lhsT=VA[:, kt, :], rhs=pt[:, col0:512], start=(kt == 0), stop=(kt == last)),
                     reads=[("VA", kt // 8), pt], writes=[pso])
                if kt == last:
                    p.op("dve", lambda e: e.reciprocal(out=rec[64:128, :], in_=pso[64:128, :]), reads=[pso], writes=[rec])
                    r0 = (h % 2) * 64
                    hh = h // 2
                    p.op("dve", lambda e: e.tensor_tensor(out=OT[r0:r0 + 64, hh, qb * 512:(qb + 1) * 512], in0=pso[0:64, :], in1=rec[64:128, :], op=ALU.mult),
                         reads=[pso, rec], writes=[("OT", qb)])
            for i in range(len(steps) + LA):
                if i < len(steps):
                    emit_s(i)
                if i - LA >= 0:
                    emit_pv(i - LA)
            si += len(steps)
            pi_ += len(steps)
        p.release(m2)
        WOUT = p.tile("WOUT", [128, 8, D], BF16)
        p.dma("pool", lambda e: e.dma_start(out=WOUT[:], in_=self.dram["mla_out"].rearrange("(c p) n -> p c n", p=128)), writes=[WOUT])
        for c in range(8):
            p.op("pool", lambda e, c=c: e.tensor_tensor(out=WOUT[:, c, :], in0=WOUT[:, c, :], in1=self.G[:], op=ALU.mult), reads=[WOUT, self.G], writes=[WOUT])
        self.out_proj(OT, WOUT)
        p.release(m0)

    def out_proj(self, OT, WOUT):
        p = self.p
        for t in range(NT):
            for hf in range(2):
                ps = self.PS[(2 * t + hf) % 4]
                for c in range(8):
                    p.op("pe", lambda e, c=c, t=t, hf=hf, ps=ps: e.matmul(ps[:], lhsT=OT[:, c, t * 128:(t + 1) * 128], rhs=WOUT[:, c, hf * 512:(hf + 1) * 512],
                                                                          start=(c == 0), stop=(c == 7)), reads=[("OT", t // 4), WOUT], writes=[ps])
                xs = self.X[:, t, hf * 512:(hf + 1) * 512]
                p.op("dve", lambda e, xs=xs, ps=ps: e.tensor_tensor(out=xs, in0=ps[:], in1=xs, op=ALU.add), reads=[ps, ("X", t)], writes=[("X", t)])


    def mm(self, out, lhsT, rhs, start, stop, reads, writes):
        self.p.op("pe", lambda e: e.matmul(out, lhsT=lhsT, rhs=rhs, start=start, stop=stop), reads=reads, writes=writes)

    def tr(self, out, in_, ident, reads, writes):
        self.p.op("pe", lambda e: e.transpose(out, in_, ident), reads=list(reads) + [ident], writes=writes)

    def gdn_phase(self, s):
        p = self.p
        PS = self.PS
        PSb = [b.bitcast(BF16) for b in PS]
        HTr = [("HT", b) for b in range(NB)]
        m0 = p.mark()
        NEG = -1.0e30
        LTRI = p.tile("LTRI", [128, 128], F32)
        BDm = p.tile("BDm", [128, 128], F32)
        SEL = [p.tile(f"SEL{i}", [128, 128], F32) for i in range(2)]
        MNi = p.tile("MNi", [128, 128], F32)
        MNs = p.tile("MNs", [128, 128], F32)
        hp = p.tile("hp", [128, 24], F32)
        nA = p.tile("nA", [128, 8], F32)
        GON = p.tile("GON", [128, 128], F32)
        WBA = p.tile("WBA", [128, 8, 16], BF16)
        CW = p.tile("CW", [128, 8, 3, 4], F32)
        pl = lambda fn, r=(), w=(): p.op("pool", fn, reads=r, writes=w)
        pl(lambda e: e.memset(LTRI[:], 1.0), w=[LTRI])
        pl(lambda e: e.affine_select(out=LTRI[:], in_=LTRI[:], pattern=[[1, 128]], compare_op=ALU.is_ge, fill=0.0, base=0, channel_multiplier=-1), r=[LTRI], w=[LTRI])
        pl(lambda e: e.memset(LTRI[0:64, 64:128], 0.0), r=[LTRI], w=[LTRI])
        pl(lambda e: e.memset(BDm[:], 0.0), w=[BDm])
        pl(lambda e: e.memset(BDm[0:64, 0:64], 1.0), r=[BDm], w=[BDm])
        pl(lambda e: e.memset(BDm[64:128, 64:128], 1.0), r=[BDm], w=[BDm])
        for i in range(2):
            pl(lambda e, i=i: e.memset(SEL[i][:], 0.0), w=[SEL[i]])
            pl(lambda e, i=i: e.memset(SEL[i][64 * i:64 * i + 1, :], 1.0), r=[SEL[i]], w=[SEL[i]])
        pl(lambda e: e.memset(MNi[:], 0.0), w=[MNi])
        pl(lambda e: e.affine_select(out=MNi[:], in_=MNi[:], pattern=[[1, 128]], compare_op=ALU.is_ge, fill=NEG, base=0, channel_multiplier=-1), r=[MNi], w=[MNi])
        pl(lambda e: e.memset(MNi[0:64, 64:128], NEG), r=[MNi], w=[MNi])
        pl(lambda e: e.memset(MNs[:], 0.0), w=[MNs])
        pl(lambda e: e.affine_select(out=MNs[:], in_=MNs[:], pattern=[[1, 128]], compare_op=ALU.is_gt, fill=NEG, base=0, channel_multiplier=-1), r=[MNs], w=[MNs])
        pl(lambda e: e.memset(MNs[0:64, 64:128], NEG), r=[MNs], w=[MNs])
        p.dma("sp", lambda e: e.dma_start(out=hp[:], in_=self.dram["gdn_hp"]), writes=[hp])
        p.dma("sp", lambda e: e.dma_start(out=GON[:], in_=self.dram["gdn_on"]), writes=[GON])
        p.dma("sp", lambda e: e.dma_start(out=CW[:], in_=self.dram["gdn_conv"]), writes=[CW])
        p.dma("pool", lambda e: e.dma_start(out=WBA[:], in_=self.dram["gdn_ba"]), writes=[WBA])
        p.op("act", lambda e: e.activation(out=nA[:], in_=hp[:, 0:8], func=AF.Exp), reads=[hp], writes=[nA])
        p.op("dve", lambda e: e.tensor_scalar(out=nA[:], in0=nA[:], scalar1=-1.0, scalar2=None, op0=ALU.mult), reads=[nA], writes=[nA])
        BETA = p.tile("BETA", [128, NT, 8], F32)
        GAMS = p.tile("GAMS", [128, NT, 8], F32)
        EGL = p.tile("EGL", [128, NT, 8], F32)
        BG = p.tile("BG", [128, NT, 8], F32)
        DECB = p.tile("DECB", [128, NT, 2, 8], F32)
        GC3 = p.tile("GC3", [128, NT, 3, 8], F32)
        m1 = p.mark()
        ba = p.tile("ba", [128, 16], F32)
        xs = p.tile("gxs", [128, 8], F32)
        ax = p.tile("gax", [128, 8], F32)
        gg = p.tile("ggg", [128, 8], F32)
        lnb = p.tile("lnb", [128, 8], F32)
        gcl = p.tile("gcl", [128, 2, 8], F32)
        dk_s = 128.0 ** -0.5
        import os
        pre = int(os.environ.get("GDN_PRE", "99"))
        if "GDN_OPS" in os.environ:
            p.limit = int(os.environ["GDN_OPS"])
        for t in range(NT if pre >= 2 else pre):
            ts_ = slice(t * 128, (t + 1) * 128)
            ps = PS[t % 2]
            for k in range(8):
                self.mm(ps[:, 0:16], self.HT[:, k, ts_], WBA[:, k, :], k == 0, k == 7, [HTr[t // 4], WBA], [ps])
            p.op("act", lambda e, ps=ps: e.copy(ba[:], ps[:, 0:16]), reads=[ps], writes=[ba])
            p.op("act", lambda e, t=t: e.activation(out=BETA[:, t, :], in_=ba[:, 0:8], func=AF.Sigmoid), reads=[ba], writes=[BETA])
            p.op("act", lambda e, t=t: e.activation(out=lnb[:], in_=BETA[:, t, :], func=AF.Ln), reads=[BETA], writes=[lnb])
            p.op("dve", lambda e: e.tensor_tensor(out=xs[:], in0=ba[:, 8:16], in1=hp[:, 8:16], op=ALU.add), reads=[ba, hp], writes=[xs])
            p.op("act", lambda e: e.activation(out=ax[:], in_=xs[:], func=AF.Abs), reads=[xs], writes=[ax])
            p.op("act", lambda e: e.activation(out=ax[:], in_=ax[:], func=AF.Exp, scale=-1.0), reads=[ax], writes=[ax])
            p.op("act", lambda e: e.activation(out=ax[:], in_=ax[:], func=AF.Ln, bias=self.onesf[:, 0:1], scale=1.0), reads=[ax, self.onesf], writes=[ax])
            p.op("dve", lambda e: e.scalar_tensor_tensor(out=gg[:], in0=xs[:], scalar=0.0, in1=ax[:], op0=ALU.max, op1=ALU.add), reads=[xs, ax], writes=[gg])
            p.op("dve", lambda e: e.tensor_tensor(out=gg[:], in0=gg[:], in1=nA[:], op=ALU.mult), reads=[gg, nA], writes=[gg])
            self.mm(ps[:, 16:24], LTRI[:], gg[:], True, True, [LTRI, gg], [ps])
            self.mm(ps[:, 24:32], BDm[:], gg[:], True, True, [BDm, gg], [ps])
            p.op("act", lambda e, ps=ps: e.copy(gcl[:].rearrange("p a b -> p (a b)"), ps[:, 16:32]), reads=[ps], writes=[gcl])
            p.op("act", lambda e, t=t: e.activation(out=BG[:, t, :], in_=gcl[:, 0, :], func=AF.Exp), reads=[gcl], writes=[BG])
            p.op("dve", lambda e, t=t: e.tensor_scalar(out=GAMS[:, t, :], in0=BG[:, t, :], scalar1=dk_s, scalar2=None, op0=ALU.mult), reads=[BG], writes=[GAMS])
            p.op("dve", lambda e, t=t: e.tensor_tensor(out=BG[:, t, :], in0=BG[:, t, :], in1=BETA[:, t, :], op=ALU.mult), reads=[BG, BETA], writes=[BG])
            p.op("dve", lambda e, t=t: e.tensor_tensor(out=EGL[:, t, :], in0=gcl[:, 1, :], in1=gcl[:, 0, :], op=ALU.subtract), reads=[gcl], writes=[EGL])
            p.op("act", lambda e, t=t: e.activation(out=EGL[:, t, :], in_=EGL[:, t, :], func=AF.Exp), reads=[EGL], writes=[EGL])
            p.op("dve", lambda e, t=t: e.tensor_copy(GC3[:, t, 0, :], gcl[:, 0, :]), reads=[gcl], writes=[GC3])
            p.op("dve", lambda e, t=t: e.tensor_scalar(out=GC3[:, t, 1, :], in0=gcl[:, 0, :], scalar1=-1.0, scalar2=None, op0=ALU.mult), reads=[gcl, GC3], writes=[GC3])
            p.op("dve", lambda e, t=t: e.tensor_tensor(out=GC3[:, t, 2, :], in0=gcl[:, 0, :], in1=lnb[:], op=ALU.add), reads=[gcl, lnb, GC3], writes=[GC3])
            for hf in range(2):
                self.mm(ps[:, 32 + hf * 8:40 + hf * 8], SEL[hf][:], gcl[:, 1, :], True, True, [SEL[hf], gcl], [ps])
            p.op("act", lambda e, t=t, ps=ps: e.activation(out=DECB[:, t, :, :].rearrange("p a b -> p (a b)"), in_=ps[:, 32:48], func=AF.Exp), reads=[ps], writes=[DECB])
        p.limit = None
        p.release(m1)
        NPAR = int(os.environ.get("GDN_NPAR", "2"))
        HB = []
        for sl in range(NPAR):
            b = {}
            b["WH"] = p.tile(f"WH{sl}", [128, 8, 512], BF16)
            b["WO"] = p.tile(f"WO{sl}", [128, D], BF16)
            b["DG"] = p.tile(f"DG{sl}", [128, 12, 128], BF16)
            b["S"] = p.tile(f"S{sl}", [128, 128], F32)
            b["Sb"] = p.tile(f"Sb{sl}", [128, 128], BF16)
            b["XC"] = p.tile(f"XC{sl}", [128, 3, 515], BF16)
            for nm, shp, dt in (("ZS", [128, 128], F32), ("GZ", [128, 128], F32), ("QKV", [128, 3, 128], F32), ("SQ", [128, 2, 128], F32),
                                ("st", [128, 8], F32), ("TOK", [128, 6, 128], BF16), ("DGC", [128, 2, 128], F32), ("DD", [128, 2, 128], F32), ("KQT", [128, 3, 128], BF16), ("DT2", [128, 2, 128], F32),
                                ("M2", [128, 2, 128], F32), ("ATb", [128, 128], BF16), ("YA0", [128, 2, 128], F32), ("YA1", [128, 2, 128], F32),
                                ("P0", [128, 128], F32), ("P1", [128, 128], F32), ("TTb", [128, 128], BF16), ("U", [128, 128], F32),
                                ("WTb", [128, 128], BF16), ("VNb", [128, 128], BF16), ("OT", [128, 128], F32), ("OG", [128, 128], BF16),
                                ("OGT", [128, 128], BF16), ("junk", [128, 128], BF16)):
                b[nm] = p.tile(f"{nm}{sl}", shp, dt)
            b["bank"] = [PS[4 * sl + i] for i in range(4)]
            b["bankb"] = [PSb[4 * sl + i] for i in range(4)]
            HB.append(b)

        def head_gen(h, b):
            sl = HB.index(b)
            WH, WO, DG, S, Sb, XC = b["WH"], b["WO"], b["DG"], b["S"], b["Sb"], b["XC"]
            bk, bkb = b["bank"], b["bankb"]
            R = lambda i, nm: (f"ps{sl}_{i}")
            r_zc = [R(0, "zc")]
            r_tr, r_d, r_og = [R(1, "tr")], [R(1, "d")], [R(1, "og")]
            r_kk, r_a, r_p = [R(2, "kk")], [R(2, "a")], [R(2, "p")]
            r_ya, r_u, r_wt = [R(3, "ya")], [R(3, "u")], [R(3, "wt")]
            allb = [r_zc, r_tr, r_kk, r_ya]
            p.dma("pool", lambda e: e.dma_start(out=WH[:].rearrange("p k n -> p (k n)"), in_=self.dram["gdn_in"][h].rearrange("p k n -> p (k n)")), writes=[WH])
            p.dma("pool", lambda e: e.dma_start(out=WO[:], in_=self.dram["gdn_out"][h * 128:(h + 1) * 128, :]), writes=[WO])
            p.op("pool", lambda e: e.tensor_tensor(out=WO[:], in0=WO[:], in1=self.G[:], op=ALU.mult), reads=[WO, self.G], writes=[WO])
            for part in range(3):
                for j in range(4):
                    p.op("dve", lambda e, part=part, j=j: e.tensor_scalar(out=DG[:, part * 4 + j, :], in0=self.identb[:], scalar1=CW[:, h, part, j:j + 1], scalar2=None, op0=ALU.mult),
                         reads=[self.identb, CW], writes=[DG])
            p.op("pool", lambda e: e.memset(S[:], 0.0), writes=[S])
            p.op("pool", lambda e: e.memset(Sb[:], 0.0), writes=[Sb])
            p.op("pool", lambda e: e.memset(XC[:, :, 0:3], 0.0), writes=[XC])
            yield
            for blk in range(NB):
                cs = slice(blk * 512, (blk + 1) * 512)
                for part in range(3):
                    ps = bk[part]
                    for k in range(8):
                        self.mm(ps[:], WH[:, k, part * 128:(part + 1) * 128], self.HT[:, k, cs], k == 0, k == 7, [WH, HTr[blk]], allb[part])
                    eng = ("act", "dve", "act")[part]
                    if eng == "act":
                        p.op("act", lambda e, part=part, ps=ps: e.copy(XC[:, part, 3:515], ps[:]), reads=allb[part], writes=[XC])
                    else:
                        p.op("dve", lambda e, part=part, ps=ps: e.tensor_copy(XC[:, part, 3:515], ps[:]), reads=allb[part], writes=[XC])
                yield
                for tt in range(4):
                    t = blk * 4 + tt
                    ts_ = slice(t * 128, (t + 1) * 128)
                    ZS, GZ, QKV, SQ, st, TOK, KQT, DT2, M2, ATb = (b[n] for n in ("ZS", "GZ", "QKV", "SQ", "st", "TOK", "KQT", "DT2", "M2", "ATb"))
                    YA, Pb, TTb, U, WTb, VNb, OTk, OG, OGT = [b["YA0"], b["YA1"]], [b["P0"], b["P1"]], b["TTb"], b["U"], b["WTb"], b["VNb"], b["OT"], b["OG"], b["OGT"]
                    for k in range(8):
                        self.mm(bk[0][:, 0:128], self.HT[:, k, ts_], WH[:, k, 384:512], k == 0, k == 7, [WH, HTr[blk]], r_zc)
                    for part in range(3):
                        for j in range(4):
                            self.mm(bk[0][:, 128 + part * 128:256 + part * 128], XC[:, part, tt * 128 + j:tt * 128 + j + 128], DG[:, part * 4 + j, :], j == 0, j == 3, [XC, DG], r_zc)
                    p.op("act", lambda e: e.activation(out=ZS[:], in_=bk[0][:, 0:128], func=AF.Silu), reads=r_zc, writes=[ZS])
                    p.op("act", lambda e: e.activation(out=QKV[:].rearrange("p a b -> p (a b)"), in_=bk[0][:, 128:512], func=AF.Silu), reads=r_zc, writes=[QKV])
                    p.op("pool", lambda e: e.tensor_tensor(out=GZ[:], in0=ZS[:], in1=GON[:], op=ALU.mult), reads=[ZS, GON], writes=[GZ])
                    yield
                    if "GDN_OPS2" in os.environ and h == 0 and t == 0:
                        p.limit = int(os.environ["GDN_OPS2"])
                    p.op("dve", lambda e: e.tensor_tensor(out=SQ[:], in0=QKV[:, 0:2, :], in1=QKV[:, 0:2, :], op=ALU.mult), reads=[QKV], writes=[SQ])
                    p.op("dve", lambda e: e.tensor_reduce(out=st[:, 0:2], in_=SQ[:], axis=AX.X, op=ALU.add), reads=[SQ], writes=[st])
                    p.op("act", lambda e: e.activation(out=st[:, 0:2], in_=st[:, 0:2], func=AF.Sqrt, bias=self.epsc[:], scale=1.0), reads=[st, self.epsc], writes=[st])
                    p.op("dve", lambda e: e.reciprocal(out=st[:, 2:4], in_=st[:, 0:2]), reads=[st], writes=[st])
                    p.op("act", lambda e: e.activation(out=TOK[:, 0, :], in_=QKV[:, 1, :], func=AF.Copy, scale=st[:, 3:4]), reads=[QKV, st], writes=[(TOK.name, 0)])
                    p.op("dve", lambda e: e.tensor_scalar(out=TOK[:, 1, :], in0=QKV[:, 0, :], scalar1=st[:, 2:3], scalar2=dk_s, op0=ALU.mult, op1=ALU.mult), reads=[QKV, st], writes=[(TOK.name, 0)])
                    p.op("dve", lambda e, t=t: e.tensor_scalar(out=TOK[:, 2, :], in0=QKV[:, 0, :], scalar1=st[:, 2:3], scalar2=GAMS[:, t, h:h + 1], op0=ALU.mult, op1=ALU.mult), reads=[QKV, st, GAMS], writes=[(TOK.name, 0)])
                    p.op("dve", lambda e, t=t: e.tensor_scalar(out=TOK[:, 3, :], in0=QKV[:, 1, :], scalar1=st[:, 3:4], scalar2=EGL[:, t, h:h + 1], op0=ALU.mult, op1=ALU.mult), reads=[QKV, st, EGL], writes=[(TOK.name, 1)])
                    p.op("dve", lambda e, t=t: e.tensor_scalar(out=TOK[:, 4, :], in0=QKV[:, 1, :], scalar1=st[:, 3:4], scalar2=BG[:, t, h:h + 1], op0=ALU.mult, op1=ALU.mult), reads=[QKV, st, BG], writes=[(TOK.name, 1)])
                    p.op("act", lambda e, t=t: e.activation(out=TOK[:, 5, :], in_=QKV[:, 2, :], func=AF.Copy, scale=BETA[:, t, h:h + 1]), reads=[QKV, BETA], writes=[(TOK.name, 1)])
                    for i in range(3):
                        self.tr(bkb[1][:, i * 128:(i + 1) * 128], TOK[:, i, :], self.identb[:], [(TOK.name, 0)], r_tr)
                    p.op("act", lambda e: e.copy(KQT[:].rearrange("p a b -> p (a b)"), bkb[1][:, 0:384]), reads=r_tr, writes=[KQT])
                    yield
                    d0 = 256
                    DGC = b["DGC"]
                    p.op("act", lambda e, t=t: e.activation(out=DGC[:, 0, :], in_=self.identf[:], func=AF.Copy, scale=GC3[:, t, 0, h:h + 1]), reads=[self.identf, GC3], writes=[DGC])
                    p.op("act", lambda e, t=t: e.activation(out=DGC[:, 1, :], in_=self.identf[:], func=AF.Copy, scale=GC3[:, t, 2, h:h + 1]), reads=[self.identf, GC3], writes=[DGC])
                    self.mm(bk[0][:, 0:128], self.onesf[:], DGC[:, 0, :], True, False, [self.onesf, DGC], r_zc)
                    self.mm(bk[0][:, 0:128], self.identf[:], MNi[:], False, True, [self.identf, MNi], r_zc)
                    self.mm(bk[0][:, 128:256], self.onesf[:], DGC[:, 1, :], True, False, [self.onesf, DGC], r_zc)
                    self.mm(bk[0][:, 128:256], self.identf[:], MNs[:], False, True, [self.identf, MNs], r_zc)
                    p.op("act", lambda e, t=t: e.activation(out=DT2[:].rearrange("p a b -> p (a b)"), in_=bk[0][:, 0:256], func=AF.Exp, bias=GC3[:, t, 1, h:h + 1], scale=1.0),
                         reads=r_zc + [GC3], writes=[DT2])
                    yield
                    self.mm(bk[2][:, 0:128], KQT[:, 0, :], KQT[:, 1, :], True, True, [KQT], r_kk)
                    self.mm(bk[2][:, 128:256], KQT[:, 0, :], KQT[:, 0, :], True, True, [KQT], r_kk)
                    p.op("dve", lambda e: e.tensor_tensor(out=M2[:].rearrange("p a b -> p (a b)"), in0=bk[2][:, 0:256], in1=DT2[:].rearrange("p a b -> p (a b)"), op=ALU.mult),
                         reads=r_kk + [DT2], writes=[M2])
                    p.op("act", lambda e: e.copy(ATb[:], M2[:, 0, :]), reads=[M2], writes=[ATb])
                    yield
                    Y = M2[:, 1, :]
                    self.tr(bk[2][:, 256:384], Y, self.identf[:], [M2], r_a)
                    p.op("act", lambda e: e.copy(YA[0][:, 1, :], bk[2][:, 256:384]), reads=r_a, writes=[(YA[0].name, 1)])
                    p.op("pool", lambda e: e.tensor_copy(YA[0][:, 0, :], Y), reads=[M2], writes=[(YA[0].name, 0)])
                    p.op("pool", lambda e: e.tensor_tensor(out=Pb[0][:], in0=self.identf[:], in1=Y, op=ALU.subtract), reads=[M2, self.identf], writes=[Pb[0]])
                    yield
                    for it in range(5):
                        cur, nxt = YA[it % 2], YA[(it + 1) % 2]
                        pc, pn = Pb[it % 2], Pb[(it + 1) % 2]
                        rc = [(cur.name, 0), (cur.name, 1)]
                        if it < 4:
                            self.mm(bk[3][:, 0:128], cur[:, 1, :], cur[:, 0, :], True, True, rc, r_ya)
                        self.mm(bk[3][:, 128:256], cur[:, 0, :], cur[:, 1, :], True, True, rc, r_ya)
                        if it < 4:
                            p.op("act", lambda e, nxt=nxt: e.copy(nxt[:].rearrange("p a b -> p (a b)"), bk[3][:, 0:256]), reads=r_ya, writes=[(nxt.name, 0), (nxt.name, 1)])
                        else:
                            p.op("act", lambda e, nxt=nxt: e.copy(nxt[:, 1, :], bk[3][:, 128:256]), reads=r_ya, writes=[(nxt.name, 1)])
                        yield
                        self.mm(bk[2][:, 384:512], self.identf[:], pc[:], True, False, [self.identf, pc], r_p)
                        self.mm(bk[2][:, 384:512], nxt[:, 1, :], pc[:], False, True, [(nxt.name, 1), pc], r_p)
                        if it < 4:
                            p.op("dve", lambda e, pn=pn: e.tensor_copy(pn[:], bk[2][:, 384:512]), reads=r_p, writes=[pn])
                        else:
                            p.op("dve", lambda e: e.tensor_copy(TTb[:], bk[2][:, 384:512]), reads=r_p, writes=[TTb])
                        yield
                    if "GDN_OPS3" in os.environ and h == 1 and t == 0:
                        p.limit = int(os.environ["GDN_OPS3"])
                    self.mm(bk[3][:, 256:384], TTb[:], TOK[:, 5, :], True, True, [TTb, (TOK.name, 1)], r_u)
                    self.mm(bk[0][:, 0:128], TOK[:, 4, :], TTb[:], True, True, [TTb, (TOK.name, 1)], r_zc)
                    p.op("act", lambda e: e.copy(U[:], bk[3][:, 256:384]), reads=r_u, writes=[U])
                    p.op("dve", lambda e: e.tensor_copy(WTb[:], bk[0][:, 0:128]), reads=r_zc, writes=[WTb])
                    yield
                    SD = b["DD"][:, 0, :]
                    for hf in range(2):
                        r0 = hf * 64
                        rr = slice(r0, r0 + 64)
                        self.mm(bk[2][0:64, 0:128], WTb[:, rr], Sb[:], True, True, [WTb, Sb], r_kk)
                        p.op("act", lambda e, t=t, hf=hf: e.activation(out=SD, in_=S[:], func=AF.Copy, scale=DECB[:, t, hf, h:h + 1]), reads=[S, DECB], writes=[b["DD"]])
                        p.op("dve", lambda e, rr=rr: e.scalar_tensor_tensor(out=VNb[rr, :], in0=bk[2][0:64, 0:128], scalar=-1.0, in1=U[rr, :], op0=ALU.mult, op1=ALU.add), reads=[U] + r_kk, writes=[VNb])
                        yield
                        self.mm(bk[3][:, 0:128], TOK[rr, 3, :], VNb[rr, :], True, True, [(TOK.name, 1), VNb], r_ya)
                        self.mm(bk[1][0:64, 0:128], KQT[:, 2, rr], Sb[:], True, False, [KQT, Sb], r_tr)
                        self.mm(bk[1][0:64, 0:128], ATb[rr, rr], VNb[rr, :], False, True, [ATb, VNb], r_tr)
                        p.op("dve", lambda e: e.tensor_tensor(out=Sb[:], in0=bk[3][:, 0:128], in1=SD, op=ALU.add), reads=r_ya + [b["DD"]], writes=[Sb])
                        p.op("dve", lambda e: e.tensor_tensor(out=S[:], in0=bk[3][:, 0:128], in1=SD, op=ALU.add), reads=r_ya + [b["DD"]], writes=[S])
                        p.op("act", lambda e, rr=rr: e.copy(OTk[rr, :], bk[1][0:64, 0:128]), reads=r_tr, writes=[OTk])
                        yield
                    p.op("pool", lambda e: e.memset(st[:, 4:5], 0.0), reads=[st], writes=[st])
                    p.op("act", lambda e: e.activation(out=b["junk"][:], in_=OTk[:], func=AF.Square, accum_out=st[:, 4:5]), reads=[OTk, st], writes=[b["junk"], st])
                    p.op("act", lambda e: e.activation(out=st[:, 4:5], in_=st[:, 4:5], func=AF.Sqrt, bias=self.epsc[:], scale=1.0 / 128), reads=[st, self.epsc], writes=[st])
                    p.op("dve", lambda e: e.reciprocal(out=st[:, 5:6], in_=st[:, 4:5]), reads=[st], writes=[st])
                    p.op("dve", lambda e: e.scalar_tensor_tensor(out=OG[:], in0=OTk[:], scalar=st[:, 5:6], in1=GZ[:], op0=ALU.mult, op1=ALU.mult), reads=[OTk, st, GZ], writes=[OG])
                    yield
                    self.tr(bkb[1][:, 384:512], OG[:], self.identb[:], [OG], r_og)
                    p.op("act", lambda e: e.copy(OGT[:], bkb[1][:, 384:512]), reads=r_og, writes=[OGT])
                    yield
                    obanks = [(bk[0], r_zc), (bk[3], r_ya)]
                    for hf in range(2):
                        self.mm(obanks[hf][0][:], OGT[:], WO[:, hf * 512:(hf + 1) * 512], True, True, [OGT, WO], obanks[hf][1])
                    for hf in range(2):
                        xs_ = self.X[:, t, hf * 512:(hf + 1) * 512]
                        p.op("dve", lambda e, xs_=xs_, ob_=obanks[hf][0]: e.tensor_tensor(out=xs_, in0=ob_[:], in1=xs_, op=ALU.add), reads=obanks[hf][1] + [("X", t)], writes=[("X", t)])
                    yield
                p.op("dve", lambda e: e.tensor_copy(XC[:, :, 0:3], XC[:, :, 512:515]), reads=[XC], writes=[XC])
                yield

        import os
        steps = int(os.environ.get("GDN_STEPS", "1000000000"))
        for h0 in range(0, 8, NPAR):
            gens = [head_gen(h0 + i, HB[i]) for i in range(NPAR)]
            alive = list(gens)
            while alive and steps > 0:
                for g_ in list(alive):
                    steps -= 1
                    if steps <= 0:
                        break
                    try:
                        next(g_)
                    except StopIteration:
                        alive.remove(g_)
        if "GDN_DUMP" in os.environ:
            nm = os.environ["GDN_DUMP"]
            src = HB[0][nm]
            self.dump = p.tile("dump", [128, 512], F32)
            n = int(np.prod(src.shape[1:]))
            v = src[:] if len(src.shape) == 2 else src[:].rearrange("p a b -> p (a b)")
            p.op("pool", lambda e: e.memset(self.dump[:], 0.0), writes=[self.dump])
            p.op("dve", lambda e: e.tensor_copy(self.dump[:, 0:min(n, 512)], v[:, 0:min(n, 512)]), reads=[src, self.dump] + [(src.name, i) for i in range(2)], writes=[self.dump])
            p.dma("sp", lambda e: e.dma_start(out=self.y[s, 0:128, 0:512], in_=self.dump[:]), reads=[self.dump], key="dump")
            p.barrier()
            self.skip0 = True
        p.release(m0)

    def build(self):
        p = self.p
        st = self.stages
        self.setup()
        self.prologue_mod()
        final = []
        for s in range(self.nseq):
            self.load_x(s)
            for l in range(2):
                if f"mix{l}" in st:
                    self.norm_phase(l, s, 0)
                    self.make_gate(l, s, 2)
                    (self.mla_phase if l == 0 else self.gdn_phase)(s)
                if f"ffn{l}" in st:
                    self.norm_phase(l, s, 1, router=(l == 1))
                    self.make_gate(l, s, 5)
                    if l == 0:
                        ex = [(self.dram["ffn_gu"][j], self.dram["ffn_dn"][j], None) for j in range(2)]
                    else:
                        ex = [(self.dram["moe_gu"][j], self.dram["moe_dn"][j], j) for j in range(8)]
                    self.ffn_phase(ex)
            final += self.final_out(s)
        p.wait("sp", final)
        p.emit()
        return self.nc


def _cols(v, nk):
    return np.ascontiguousarray(np.asarray(v).reshape(nk, 128).T)


def _gu_layout(w, ff):
    nch = ff // 128
    g = w[:, :ff].reshape(8, 128, nch, 128)
    u = w[:, ff:].reshape(8, 128, nch, 128)
    gu = np.concatenate([g, u], axis=-1)
    return np.ascontiguousarray(gu.transpose(2, 1, 0, 3)).reshape(nch, 128, 8 * 256)


def _rope_consts():
    c = np.zeros((128, 2), np.float64)
    inv = 10000.0 ** (-np.arange(16, dtype=np.float64) / 16.0)
    for j in range(32):
        c[64 + j, 0] = inv[j % 16] / (2 * math.pi); c[64 + j, 1] = 0.25
        c[96 + j, 0] = inv[j % 16] / (2 * math.pi); c[96 + j, 1] = 0.5 if j < 16 else 0.0
    return c.astype(np.float32)


ROPEC = _rope_consts()


def prep_shared(inp):
    f = lambda k: np.asarray(inp[k], dtype=np.float32)
    sh = {}
    sh["ada"] = np.ascontiguousarray(np.concatenate([f("ada_w"), f("ada_b")[:, None, :]], axis=1))
    sh["gmix"] = np.stack([_cols(f("norm_mix")[l], 8) for l in range(2)])
    sh["gffn"] = np.stack([_cols(f("norm_ffn")[l], 8) for l in range(2)])
    sh["gfin"] = np.ascontiguousarray(np.broadcast_to(f("final_norm")[None, :], (128, D)))
    wgu = f("ffn_w_gate_up")[0]
    halves = []
    for j in range(2):
        sub = np.concatenate([wgu[:, j * 1408:(j + 1) * 1408], wgu[:, 2816 + j * 1408:2816 + (j + 1) * 1408]], axis=1)
        halves.append(_gu_layout(sub, 1408))
    sh["ffn_gu"] = np.stack(halves)
    sh["ffn_dn"] = np.ascontiguousarray(f("ffn_w_down")[0].reshape(2, 1408, D))
    sh["moe_gu"] = np.stack([_gu_layout(f("moe_w_gate_up")[0, e], 1408) for e in range(8)])
    sh["moe_dn"] = np.ascontiguousarray(f("moe_w_down")[0])
    sh["moe_r"] = np.ascontiguousarray(f("moe_w_router")[0].reshape(8, 128, 8).transpose(1, 0, 2))

    perm = np.concatenate([np.arange(16, 32), np.arange(0, 16)])
    win = f("mla_w_in")[0]
    kr = win[:, 768:800]
    win2 = np.concatenate([win[:, :768], win[:, 0:64], kr, kr[:, perm]], axis=1)
    sh["mla_in"] = np.ascontiguousarray(win2.reshape(8, 128, 896).transpose(1, 0, 2))
    sh["mla_qn"] = _cols(f("mla_q_norm")[0], 4)
    sh["mla_kvn"] = _cols(f("mla_kv_norm")[0], 2)
    wqb = f("mla_w_qb")[0].reshape(512, 16, 96)
    wqb2 = np.concatenate([wqb[:, :, :64], wqb[:, :, 64:96], wqb[:, :, 64:96][:, :, perm]], axis=2).reshape(512, 2048)
    sh["mla_qb"] = np.ascontiguousarray(wqb2.reshape(4, 128, 2048).transpose(1, 0, 2))
    wkv = f("mla_w_kvb")[0].reshape(256, 16, 128)
    sh["mla_kb"] = np.ascontiguousarray(wkv[:, :, :64].reshape(2, 128, 1024).transpose(1, 0, 2))
    sh["mla_vb"] = np.ascontiguousarray(wkv[:, :, 64:].reshape(2, 128, 1024).transpose(1, 0, 2))
    sh["mla_out"] = np.ascontiguousarray(f("mla_w_out")[0])
    sh["ropec"] = ROPEC
    gw = f("gdn_w_in")[0]
    heads = []
    for h in range(8):
        cols = np.concatenate([np.arange(h * 128, (h + 1) * 128) + off for off in (0, 1024, 2048, 3072)])
        heads.append(gw[:, cols].reshape(8, 128, 512).transpose(1, 0, 2))
    sh["gdn_in"] = np.ascontiguousarray(np.stack(heads))
    sh["gdn_ba"] = np.ascontiguousarray(gw[:, 4096:4112].reshape(8, 128, 16).transpose(1, 0, 2))
    cw = f("gdn_conv_w")[0]
    sh["gdn_conv"] = np.ascontiguousarray(cw.reshape(4, 3, 8, 128).transpose(3, 2, 1, 0))
    hpv = np.zeros((128, 24), np.float32)
    hpv[:, 0:8] = f("gdn_a_log")[0][None, :]
    hpv[:, 8:16] = f("gdn_dt_bias")[0][None, :]
    sh["gdn_hp"] = hpv
    sh["gdn_on"] = np.ascontiguousarray(np.broadcast_to(f("gdn_out_norm")[0][None, :], (128, 128)))
    sh["gdn_out"] = np.ascontiguousarray(f("gdn_w_out")[0])
    return sh


def prep_core(inp, b0, ns):
    x = np.ascontiguousarray(np.asarray(inp["x"], np.float32)[b0:b0 + ns])
    c = np.asarray(inp["c"], np.float32)[b0:b0 + ns]
    cT = np.stack([_cols(c[i], 8) for i in range(ns)])
    pos = np.asarray(inp["positions"]).astype(np.int32)[b0:b0 + ns]
    posr = np.ascontiguousarray(np.broadcast_to(pos[:, None, :], (ns, 128, SEQ)))
    return {"x": x, "cT": cT, "pos": posr}


_CACHE = {}


def run(inp, batches, ns, stages):
    key = (ns, tuple(sorted(stages)))
    if key not in _CACHE:
        _CACHE[key] = Builder(ns, stages).build()
    nc = _CACHE[key]
    sh = prep_shared(inp)
    maps = []
    for b0 in batches:
        mm = dict(sh)
        mm.update(prep_core(inp, b0, ns))
        maps.append(mm)
    res = run_bass_kernel_spmd(nc, maps, core_ids=list(range(len(batches))))
    return [r["y"] for r in res.results]


ALL = ("mix0", "ffn0", "mix1", "ffn1")


def kernel(**inputs):
    ys = run(inputs, [2 * i for i in range(NCORES)], 2, ALL)
    return np.concatenate(ys, axis=0).astype(np.float32)
```

```python
import math
import numpy as np
import concourse.bass as bass
import concourse.mybir as mybir
from concourse.bass_utils import run_bass_kernel_spmd

F32 = mybir.dt.float32
BF16 = mybir.dt.bfloat16
I32 = mybir.dt.int32
AF = mybir.ActivationFunctionType
ALU = mybir.AluOpType
AX = mybir.AxisListType

NCORES = 8
SEQ = 2048
D = 1024
NT = SEQ // 128
NB = SEQ // 512
EPS = 1e-6
GEN = 30000


class Prog:
    ENG = ("pe", "act", "dve", "pool", "sp")

    def __init__(self, nc):
        self.nc = nc
        self.q = {e: [] for e in self.ENG}
        self.cnt = {e: 0 for e in self.ENG}
        self.res = {}
        self.seen = {e: {} for e in self.ENG}
        self.dma_cnt = {}
        self.semkeys = set()
        self.off = 16512
        self.ntile = 0
        self.cap = nc.SBUF_PARTITION_SIZE_BYTES

    def tile(self, name, shape, dt):
        nbytes = int(np.prod(shape[1:])) * (2 if dt == BF16 else 4)
        self.off = (self.off + 63) // 64 * 64
        assert self.off + nbytes <= self.cap, (name, self.off, nbytes)
        self.ntile += 1
        t = self.nc.alloc_sbuf_tensor_at(f"{name}_{self.ntile}", list(shape), dt, offset=self.off)
        self.off += nbytes
        return t

    def mark(self):
        return self.off

    def release(self, m):
        self.barrier()
        self.off = m

    def _key(self, r):
        return r if isinstance(r, (str, tuple)) else r.name

    def _st(self, r):
        r = self._key(r)
        s = self.res.get(r)
        if s is None:
            s = self.res[r] = {"w": None, "r": []}
        return s

    def _deps(self, eng, reads, writes):
        deps = {}

        def add(d):
            if d is None:
                return
            k, v = d
            if eng == "pe" and isinstance(k, tuple) and k[0] == "pe":
                return
            if deps.get(k, 0) < v:
                deps[k] = v
        for r in reads:
            add(self._st(r)["w"])
        for r in writes:
            s = self._st(r)
            add(s["w"])
            for d in s["r"]:
                add(d)
        out = []
        seen = self.seen[eng]
        for k, v in deps.items():
            if seen.get(k, 0) < v:
                seen[k] = v
                out.append((k, v))
        return out

    def _commit(self, token, reads, writes):
        for r in reads:
            lst = self._st(r)["r"]
            lst[:] = [d for d in lst if d[0] != token[0]]
            lst.append(token)
        for r in writes:
            s = self._st(r)
            s["w"] = token
            s["r"] = []

    limit = None

    def op(self, eng, fn, reads=(), writes=()):
        if self.limit is not None:
            if self.limit <= 0:
                return None
            self.limit -= 1
        waits = self._deps(eng, reads, writes)
        if self.limit == 0:
            print("LAST OP", eng, "waits", waits, "cnt", self.cnt, "reads", [self._key(r) for r in reads], "writes", [self._key(r) for r in writes])
        self.cnt[eng] += 1
        n = self.cnt[eng]
        key = (eng, (n - 1) // GEN)
        self.semkeys.add(key)
        token = (key, (n - 1) % GEN + 1)
        self.q[eng].append((waits, fn, (key, 1)))
        self._commit(token, reads, writes)
        return token

    def dma(self, queue, fn, reads=(), writes=(), key=None):
        if key is None:
            r0 = writes[0] if writes else reads[0]
            key = ("dma", self._key(r0))
        self.semkeys.add(key)
        waits = self._deps(queue, reads, writes)
        v = self.dma_cnt.get(key, 0) + 16
        self.dma_cnt[key] = v
        token = (key, v)
        self.q[queue].append((waits, fn, (key, 16)))
        self._commit(token, reads, writes)
        return token

    def barrier(self):
        toks = []
        for e in self.ENG:
            n = self.cnt[e]
            if n:
                toks.append(((e, (n - 1) // GEN), (n - 1) % GEN + 1))
        toks += list(self.dma_cnt.items())
        for e in self.ENG:
            self.wait(e, toks)

    def wait(self, eng, tokens):
        waits = []
        for k, v in tokens:
            if eng == "pe" and isinstance(k, tuple) and k[0] == "pe":
                continue
            if self.seen[eng].get(k, 0) < v:
                self.seen[eng][k] = v
                waits.append((k, v))
        if waits:
            self.q[eng].append((waits, None, None))

    def emit(self):
        import contextlib
        nc = self.nc
        with contextlib.ExitStack() as es:
            sems = {}
            for i, k in enumerate(sorted(self.semkeys, key=str)):
                sems[k] = es.enter_context(nc.semaphore(f"s{i}"))
            block = es.enter_context(nc.Block())
            q = self.q

            def run(engname):
                def body(e):
                    for waits, fn, inc in q[engname]:
                        for k, v in waits:
                            e.wait_ge(sems[k], v)
                        if fn is not None:
                            fn(e).then_inc(sems[inc[0]], inc[1])
                return body
            block.tensor(run("pe"))
            block.scalar(run("act"))
            block.vector(run("dve"))
            block.gpsimd(run("pool"))
            block.sync(run("sp"))


class Builder:
    def __init__(self, nseq, stages):
        self.nseq = nseq
        self.stages = stages
        nc = self.nc = bass.Bass("TRN2", target_bir_lowering=False)
        self.p = Prog(nc)
        self.dram = {}
        self.rr = 0

    def din(self, name, shape, dt=F32):
        a = self.nc.dram_tensor(name, list(shape), dt, kind="ExternalInput").ap()
        self.dram[name] = a
        return a

    def setup(self):
        p, nc, ns = self.p, self.nc, self.nseq
        d = self.din
        d("x", [ns, SEQ, D]); d("cT", [ns, 128, 8]); d("pos", [ns, 128, SEQ], I32)
        d("ada", [2, 1025, 6 * D])
        d("gmix", [2, 128, 8]); d("gffn", [2, 128, 8]); d("gfin", [128, D])
        d("ffn_gu", [2, 11, 128, 8 * 256]); d("ffn_dn", [2, 1408, D])
        d("moe_gu", [8, 11, 128, 8 * 256]); d("moe_dn", [8, 1408, D]); d("moe_r", [128, 8, 8])
        d("mla_in", [128, 8, 896]); d("mla_qn", [128, 4]); d("mla_kvn", [128, 2])
        d("mla_qb", [128, 4, 2048]); d("mla_kb", [128, 2, 1024]); d("mla_vb", [128, 2, 1024])
        d("mla_out", [D, D]); d("ropec", [128, 2])
        d("gdn_in", [8, 128, 8, 512]); d("gdn_ba", [128, 8, 16]); d("gdn_conv", [128, 8, 3, 4])
        d("gdn_hp", [128, 24]); d("gdn_on", [128, 128]); d("gdn_out", [D, D])
        self.y = nc.dram_tensor("y", [ns, SEQ, D], F32, kind="ExternalOutput").ap()

        self.X = p.tile("X", [128, NT, D], F32)
        self.HT = p.tile("HT", [128, 8, SEQ], BF16)
        self.identb = p.tile("identb", [128, 128], BF16)
        self.identf = p.tile("identf", [128, 128], F32)
        self.onesb = p.tile("onesb", [128, 128], BF16)
        self.onesf = p.tile("onesf", [128, 128], F32)
        self.epsc = p.tile("epsc", [128, 1], F32)
        self.MODC = p.tile("MODC", [128, 2, 48, ns], F32)
        self.G = p.tile("G", [128, D], F32)
        self.Acol = p.tile("Acol", [128, 8], F32)
        self.gmix = p.tile("gmix", [128, 2, 8], F32)
        self.gffn = p.tile("gffn", [128, 2, 8], F32)
        self.GF = p.tile("GF", [128, D], F32)
        self.ssq = p.tile("ssq", [128, NT], F32)
        self.rstd = p.tile("rstd", [128, NT], F32)
        self.COMB = p.tile("COMB", [128, NT, 8], F32)
        self.PS = [nc.alloc_psum_tensor(f"ps{i}", [128, 512], F32) for i in range(8)]

        p.op("pool", lambda e: e.memset(self.identf[:], 1.0), writes=[self.identf])
        p.op("pool", lambda e: e.affine_select(out=self.identf[:], in_=self.identf[:], pattern=[[-1, 128]],
                                               compare_op=ALU.is_equal, fill=0.0, base=0, channel_multiplier=1),
             reads=[self.identf], writes=[self.identf])
        p.op("dve", lambda e: e.tensor_copy(self.identb[:], self.identf[:]), reads=[self.identf], writes=[self.identb])
        p.op("pool", lambda e: e.memset(self.onesb[:], 1.0), writes=[self.onesb])
        p.op("pool", lambda e: e.memset(self.onesf[:], 1.0), writes=[self.onesf])
        p.op("pool", lambda e: e.memset(self.epsc[:], EPS), writes=[self.epsc])
        p.dma("sp", lambda e: e.dma_start(out=self.gmix[:], in_=self.dram["gmix"].rearrange("l p k -> p l k")), writes=[self.gmix])
        p.dma("sp", lambda e: e.dma_start(out=self.gffn[:], in_=self.dram["gffn"].rearrange("l p k -> p l k")), writes=[self.gffn])
        p.dma("sp", lambda e: e.dma_start(out=self.GF[:], in_=self.dram["gfin"]), writes=[self.GF])

    def prologue_mod(self):
        p, ns = self.p, self.nseq
        m = p.mark()
        cT = p.tile("cTs", [128, ns, 8], F32)
        condb = p.tile("condb", [128, 8, ns], BF16)
        for s in range(ns):
            p.dma("sp", lambda e, s=s: e.dma_start(out=cT[:, s, :], in_=self.dram["cT"][s]), writes=[cT], key=("dma", "cT", s))
        for s in range(ns):
            p.op("act", lambda e, s=s: e.activation(out=condb[:, :, s], in_=cT[:, s, :], func=AF.Silu), reads=[cT], writes=[condb])
        W = [p.tile(f"adaw{i}", [128, 8, 512], BF16) for i in range(2)]
        Bv = [p.tile(f"adab{i}", [1, 512], BF16) for i in range(2)]
        ada = self.dram["ada"]
        it = 0
        for l in range(2):
            ps = self.PS[l]
            psv = ps[:, 0:48 * ns].rearrange("p (b s) -> p b s", s=ns)
            for ch in range(12):
                w, bv = W[it % 2], Bv[it % 2]
                it += 1
                cols = slice(ch * 512, (ch + 1) * 512)
                p.dma("pool", lambda e, w=w, l=l, cols=cols: e.dma_start(
                    out=w[:], in_=ada[l, 0:1024, cols].rearrange("(k p) n -> p k n", p=128)), writes=[w])
                p.dma("pool", lambda e, bv=bv, l=l, cols=cols: e.dma_start(out=bv[:], in_=ada[l, 1024:1025, cols]), writes=[bv])
                for qd in range(4):
                    blk = ch * 4 + qd
                    for k in range(8):
                        p.op("pe", lambda e, w=w, k=k, qd=qd, blk=blk, psv=psv: e.matmul(
                            psv[:, blk, :], lhsT=w[:, k, qd * 128:(qd + 1) * 128], rhs=condb[:, k, :], start=(k == 0), stop=False),
                            reads=[w, condb], writes=[ps])
                    p.op("pe", lambda e, bv=bv, qd=qd, blk=blk, psv=psv: e.matmul(
                        psv[:, blk, :], lhsT=bv[0:1, qd * 128:(qd + 1) * 128], rhs=self.onesb[0:1, 0:ns], start=False, stop=True),
                        reads=[bv, self.onesb], writes=[ps])
            p.op("act", lambda e, l=l, psv=psv: e.copy(self.MODC[:, l, :, :], psv), reads=[ps], writes=[self.MODC])
        p.release(m)

    def modc(self, l, s, j, k=None):
        if k is None:
            return self.MODC[:, l, j * 8:(j + 1) * 8, s]
        return self.MODC[:, l, j * 8 + k, s:s + 1]

    def make_gate(self, l, s, j):
        p = self.p
        m = p.mark()
        dg = [p.tile(f"dg{i}", [128, 128], F32) for i in range(2)]
        for k in range(8):
            t = dg[k % 2]
            ps = self.PS[k // 4]
            p.op("dve", lambda e, t=t, k=k: e.tensor_scalar(out=t[:], in0=self.identf[:], scalar1=self.modc(l, s, j, k), scalar2=None, op0=ALU.mult),
                 reads=[self.identf, self.MODC], writes=[t])
            p.op("pe", lambda e, t=t, k=k, ps=ps: e.matmul(ps[:, (k % 4) * 128:(k % 4 + 1) * 128], lhsT=self.onesf[:], rhs=t[:], start=True, stop=True),
                 reads=[t, self.onesf], writes=[ps])
        for h in range(2):
            p.op("act", lambda e, h=h: e.copy(self.G[:, h * 512:(h + 1) * 512], self.PS[h][:]), reads=[self.PS[h]], writes=[self.G])
        p.release(m)

    def load_x(self, s):
        p = self.p
        for t in range(NT):
            q = "sp" if t % 2 == 0 else "act"
            p.dma(q, lambda e, t=t: e.dma_start(out=self.X[:, t, :], in_=self.dram["x"][s, t * 128:(t + 1) * 128, :]),
                  writes=[("X", t)])

    def final_out(self, s):
        p = self.p
        m = p.mark()
        ob = [p.tile(f"ob{i}", [128, D], F32) for i in range(2)]
        junk = p.tile("junkf", [128, D], BF16)
        toks = []
        for t in range(NT):
            if t == 0 and getattr(self, "skip0", False):
                continue
            self.rms_stats(t, junk)
            o = ob[t % 2]
            p.op("dve", lambda e, t=t, o=o: e.scalar_tensor_tensor(out=o[:], in0=self.X[:, t, :], scalar=self.rstd[:, t:t + 1], in1=self.GF[:],
                                                                  op0=ALU.mult, op1=ALU.mult),
                 reads=[("X", t), ("rstd", t), self.GF], writes=[o])
            toks.append(p.dma("sp", lambda e, t=t, o=o: e.dma_start(out=self.y[s, t * 128:(t + 1) * 128, :], in_=o[:]), reads=[o]))
        p.release(m)
        return toks

    def rms_stats(self, t, junk):
        p = self.p
        p.op("pool", lambda e: e.memset(self.ssq[:, t:t + 1], 0.0), writes=[("ssq", t)])
        p.op("act", lambda e: e.activation(out=junk[:], in_=self.X[:, t, :], func=AF.Square, accum_out=self.ssq[:, t:t + 1]),
             reads=[("X", t)], writes=[junk, ("ssq", t)])
        p.op("act", lambda e: e.activation(out=self.ssq[:, t:t + 1], in_=self.ssq[:, t:t + 1], func=AF.Sqrt, bias=self.epsc[:], scale=1.0 / D),
             reads=[("ssq", t), self.epsc], writes=[("ssq", t)])
        p.op("dve", lambda e: e.reciprocal(out=self.rstd[:, t:t + 1], in_=self.ssq[:, t:t + 1]), reads=[("ssq", t)], writes=[("rstd", t)])

    def norm_phase(self, l, s, which, router=False):
        p = self.p
        m = p.mark()
        jsh, jsc = (0, 1) if which == 0 else (3, 4)
        gcol = (self.gmix if which == 0 else self.gffn)[:, l, :]
        p.op("dve", lambda e: e.tensor_scalar(out=self.Acol[:], in0=self.modc(l, s, jsc), scalar1=1.0, scalar2=None, op0=ALU.add),
             reads=[self.MODC], writes=[self.Acol])
        p.op("dve", lambda e: e.tensor_tensor(out=self.Acol[:], in0=self.Acol[:], in1=gcol, op=ALU.mult),
             reads=[self.Acol, self.gmix, self.gffn], writes=[self.Acol])
        junk = p.tile("junk", [128, D], BF16)
        xn = [p.tile(f"xn{i}", [128, D], BF16) for i in range(2)]
        if router:
            xnf2 = [p.tile(f"xnf{i}", [128, D], F32) for i in range(2)]
            hTf2 = [p.tile(f"hTf{i}", [128, 8, 128], F32) for i in range(2)]
            wr = p.tile("wr", [128, 8, 8], F32)
            lg2 = [p.tile(f"lg{i}", [128, 8], F32) for i in range(2)]
            mx2 = [p.tile(f"mx{i}", [128, 8], F32) for i in range(2)]
            sc2 = [p.tile(f"rsc{i}", [128, 4], F32) for i in range(2)]
            c12 = [p.tile(f"c1{i}", [128, 8], F32) for i in range(2)]
            p.dma("sp", lambda e: e.dma_start(out=wr[:], in_=self.dram["moe_r"]), writes=[wr])
        psT = [self.PS[i].bitcast(BF16) for i in range(4)]
        for blk in range(NB):
            for tt in range(4):
                t = blk * 4 + tt
                self.rms_stats(t, junk)
                x_ = xn[t % 2]
                p.op("dve", lambda e, t=t, x_=x_: e.tensor_scalar(out=x_[:], in0=self.X[:, t, :], scalar1=self.rstd[:, t:t + 1], scalar2=None, op0=ALU.mult),
                     reads=[("X", t), ("rstd", t)], writes=[x_])
                for k in range(8):
                    p.op("pe", lambda e, k=k, tt=tt, x_=x_: e.transpose(psT[k // 2][:, (k % 2) * 512 + tt * 128:(k % 2) * 512 + (tt + 1) * 128],
                                                                       x_[:, k * 128:(k + 1) * 128], self.identb[:]),
                         reads=[x_, self.identb], writes=[self.PS[k // 2]])
                if router:
                    def router_ops(t, xnf, hTf, lg, mx, sc, c1, part):
                        if part == 0:
                            p.op("dve", lambda e, t=t, xnf=xnf: e.tensor_scalar(out=xnf[:], in0=self.X[:, t, :], scalar1=self.rstd[:, t:t + 1], scalar2=None, op0=ALU.mult),
                                 reads=[("X", t), ("rstd", t)], writes=[xnf])
                            for k in range(8):
                                ps = self.PS[4 + k // 4]
                                p.op("pe", lambda e, k=k, ps=ps: e.transpose(ps[:, (k % 4) * 128:(k % 4 + 1) * 128], xnf[:, k * 128:(k + 1) * 128], self.identf[:]),
                                     reads=[xnf, self.identf], writes=[ps])
                            for k in range(8):
                                ps = self.PS[4 + k // 4]
                                p.op("act", lambda e, k=k, ps=ps: e.activation(out=hTf[:, k, :], in_=ps[:, (k % 4) * 128:(k % 4 + 1) * 128], func=AF.Identity,
                                                                              bias=self.modc(l, s, jsh, k), scale=self.Acol[:, k:k + 1]),
                                     reads=[ps, self.Acol, self.MODC], writes=[hTf])
                            return
                        for k in range(8):
                            p.op("pe", lambda e, k=k: e.matmul(self.PS[6][:, 0:8], lhsT=hTf[:, k, :], rhs=wr[:, k, :], start=(k == 0), stop=(k == 7)),
                                 reads=[hTf, wr], writes=[self.PS[6]])
                        p.op("act", lambda e: e.copy(lg[:], self.PS[6][:, 0:8]), reads=[self.PS[6]], writes=[lg])
                        p.op("dve", lambda e: e.max(out=mx[:], in_=lg[:]), reads=[lg], writes=[mx])
                        p.op("dve", lambda e: e.tensor_tensor(out=sc[:, 0:1], in0=mx[:, 1:2], in1=mx[:, 0:1], op=ALU.subtract), reads=[mx], writes=[sc])
                        p.op("act", lambda e: e.activation(out=sc[:, 0:1], in_=sc[:, 0:1], func=AF.Exp), reads=[sc], writes=[sc])
                        p.op("dve", lambda e: e.tensor_scalar(out=sc[:, 1:2], in0=sc[:, 0:1], scalar1=1.0, scalar2=None, op0=ALU.add), reads=[sc], writes=[sc])
                        p.op("dve", lambda e: e.reciprocal(out=sc[:, 1:2], in_=sc[:, 1:2]), reads=[sc], writes=[sc])
                        p.op("dve", lambda e: e.tensor_tensor(out=sc[:, 2:3], in0=sc[:, 0:1], in1=sc[:, 1:2], op=ALU.mult), reads=[sc], writes=[sc])
                        p.op("dve", lambda e: e.tensor_scalar(out=c1[:], in0=lg[:], scalar1=mx[:, 0:1], scalar2=sc[:, 1:2], op0=ALU.is_equal, op1=ALU.mult),
                             reads=[lg, mx, sc], writes=[c1])
                        p.op("dve", lambda e, t=t: e.tensor_scalar(out=self.COMB[:, t, :], in0=lg[:], scalar1=mx[:, 1:2], scalar2=sc[:, 2:3], op0=ALU.is_equal, op1=ALU.mult),
                             reads=[lg, mx, sc], writes=[("COMB", t)])
                        p.op("dve", lambda e, t=t: e.tensor_tensor(out=self.COMB[:, t, :], in0=self.COMB[:, t, :], in1=c1[:], op=ALU.add),
                             reads=[c1, ("COMB", t)], writes=[("COMB", t)])
                    ra = lambda u, part: router_ops(u, xnf2[u % 2], hTf2[u % 2], lg2[u % 2], mx2[u % 2], sc2[u % 2], c12[u % 2], part)
                    if t >= 1:
                        ra(t - 1, 0)
                    if t >= 2:
                        ra(t - 2, 1)
                    if t == NT - 1:
                        ra(t, 0)
                        ra(t - 1, 1)
                        ra(t, 1)
            for k in range(8):
                src = psT[k // 2][:, (k % 2) * 512:(k % 2 + 1) * 512]
                dst = self.HT[:, k, blk * 512:(blk + 1) * 512]
                if k % 2 == 0:
                    p.op("act", lambda e, k=k, src=src, dst=dst: e.activation(out=dst, in_=src, func=AF.Identity, bias=self.modc(l, s, jsh, k),
                                                                            scale=self.Acol[:, k:k + 1]),
                         reads=[self.PS[k // 2], self.Acol, self.MODC], writes=[("HT", blk)])
                else:
                    p.op("dve", lambda e, k=k, src=src, dst=dst: e.tensor_scalar(out=dst, in0=src, scalar1=self.Acol[:, k:k + 1], scalar2=self.modc(l, s, jsh, k),
                                                                               op0=ALU.mult, op1=ALU.add),
                         reads=[self.PS[k // 2], self.Acol, self.MODC], writes=[("HT", blk)])
        p.release(m)

    def ffn_phase(self, experts):
        p = self.p
        m = p.mark()
        ACT_ = p.tile("actT", [128, 11, SEQ], BF16)
        WGU = [p.tile(f"wgu{i}", [128, 8, 256], BF16) for i in range(3)]
        WD = p.tile("wd", [128, 11, D], BF16)
        SG = [p.tile(f"sg{i}", [128, 512], BF16) for i in range(2)]
        HTr = [("HT", b) for b in range(NB)]
        it = 0
        for (gu, dn, ci) in experts:
            p.dma("pool", lambda e, dn=dn: e.dma_start(out=WD[:], in_=dn.rearrange("(c p) n -> p c n", p=128)), writes=[WD])
            for c in range(11):
                w = WGU[c % 3]
                p.dma("pool", lambda e, w=w, c=c, gu=gu: e.dma_start(out=w[:].rearrange("p k n -> p (k n)"), in_=gu[c]), writes=[w])
                for blk in range(NB):
                    psg, psu = self.PS[it % 2], self.PS[2 + it % 2]
                    sg = SG[it % 2]
                    it += 1
                    for k in range(8):
                        p.op("pe", lambda e, w=w, k=k, blk=blk, psg=psg: e.matmul(psg[:], lhsT=w[:, k, 0:128], rhs=self.HT[:, k, blk * 512:(blk + 1) * 512],
                                                                                  start=(k == 0), stop=(k == 7)), reads=[w, HTr[blk]], writes=[psg])
                    for k in range(8):
                        p.op("pe", lambda e, w=w, k=k, blk=blk, psu=psu: e.matmul(psu[:], lhsT=w[:, k, 128:256], rhs=self.HT[:, k, blk * 512:(blk + 1) * 512],
                                                                                  start=(k == 0), stop=(k == 7)), reads=[w, HTr[blk]], writes=[psu])
                    p.op("act", lambda e, sg=sg, psg=psg: e.activation(out=sg[:], in_=psg[:], func=AF.Silu), reads=[psg], writes=[sg])
                    p.op("dve", lambda e, sg=sg, psu=psu, c=c, blk=blk: e.tensor_tensor(out=ACT_[:, c, blk * 512:(blk + 1) * 512], in0=psu[:], in1=sg[:], op=ALU.mult),
                         reads=[sg, psu], writes=[("actT", c)])
                if c == 5:
                    for cc in range(11):
                        p.op("pool", lambda e, cc=cc: e.tensor_tensor(out=WD[:, cc, :], in0=WD[:, cc, :], in1=self.G[:], op=ALU.mult),
                             reads=[WD, self.G], writes=[WD])
            for t in range(NT):
                for hf in range(2):
                    ps = self.PS[4 + (2 * t + hf) % 4]
                    for c in range(11):
                        p.op("pe", lambda e, c=c, t=t, hf=hf, ps=ps: e.matmul(ps[:], lhsT=ACT_[:, c, t * 128:(t + 1) * 128], rhs=WD[:, c, hf * 512:(hf + 1) * 512],
                                                                              start=(c == 0), stop=(c == 10)), reads=[("actT", c), WD], writes=[ps])
                    xs = self.X[:, t, hf * 512:(hf + 1) * 512]
                    if ci is None:
                        p.op("dve", lambda e, xs=xs, ps=ps: e.tensor_tensor(out=xs, in0=ps[:], in1=xs, op=ALU.add), reads=[ps, ("X", t)], writes=[("X", t)])
                    else:
                        p.op("dve", lambda e, xs=xs, ps=ps, t=t, ci=ci: e.scalar_tensor_tensor(out=xs, in0=ps[:], scalar=self.COMB[:, t, ci:ci + 1], in1=xs,
                                                                                              op0=ALU.mult, op1=ALU.add),
                             reads=[ps, ("X", t), ("COMB", t)], writes=[("X", t)])
        p.release(m)


    def mla_phase(self, s):
        p = self.p
        PS = self.PS
        m0 = p.mark()
        QNT = p.tile("QNT", [128, 4, SEQ], BF16)
        KVNT = p.tile("KVNT", [128, 2, SEQ], BF16)
        ROPE = p.tile("ROPE", [128, SEQ], F32)
        KT = p.tile("KT", [128, SEQ], BF16)
        QT = p.tile("QT", [128, SEQ], BF16)
        VA = p.tile("VA", [128, NT, 128], BF16)
        PT = [p.tile(f"PT{i}", [128, 512], BF16) for i in range(4)]
        TRI = p.tile("TRI", [128, 128], BF16)
        tA = p.tile("tA", [128, 512], F32)
        tB = p.tile("tB", [128, 512], F32)
        rec = p.tile("rec", [128, 512], F32)
        qnc = p.tile("qnc", [128, 4], F32)
        kvnc = p.tile("kvnc", [128, 2], F32)
        ropec = p.tile("ropec", [128, 2], F32)
        st = p.tile("mst", [128, 4], F32)
        p.dma("sp", lambda e: e.dma_start(out=qnc[:], in_=self.dram["mla_qn"]), writes=[qnc])
        p.dma("sp", lambda e: e.dma_start(out=kvnc[:], in_=self.dram["mla_kvn"]), writes=[kvnc])
        p.dma("sp", lambda e: e.dma_start(out=ropec[:], in_=self.dram["ropec"]), writes=[ropec])
        p.op("pool", lambda e: e.memset(TRI[:], 1.0), writes=[TRI])
        p.op("pool", lambda e: e.affine_select(out=TRI[:], in_=TRI[:], pattern=[[1, 128]], compare_op=ALU.is_ge, fill=0.0, base=0, channel_multiplier=-1),
             reads=[TRI], writes=[TRI])
        p.op("pool", lambda e: e.memset(VA[:, :, 64:128], 1.0), writes=[("VA", 0), ("VA", 1)])
        m1 = p.mark()
        WIN = p.tile("WIN", [128, 8, 896], BF16)
        p.dma("pool", lambda e: e.dma_start(out=WIN[:], in_=self.dram["mla_in"]), writes=[WIN])
        posi = p.tile("posi", [128, 512], I32)
        u = p.tile("ropu", [128, 512], F32)
        ki = p.tile("ropk", [128, 512], I32)
        kf = p.tile("ropkf", [128, 512], F32)
        qtok = [p.tile(f"qtok{i}", [128, 512], BF16) for i in range(2)]
        kvtok = [p.tile(f"kvtok{i}", [128, 256], BF16) for i in range(2)]
        junk = p.tile("mjunk", [128, 512], BF16)
        for blk in range(NB):
            cs = slice(blk * 512, (blk + 1) * 512)
            p.dma("sp", lambda e, cs=cs: e.dma_start(out=posi[:], in_=self.dram["pos"][s, :, cs]), writes=[posi])
            p.op("dve", lambda e: e.tensor_copy(u[:], posi[:]), reads=[posi], writes=[u])
            p.op("dve", lambda e: e.tensor_scalar(out=u[:], in0=u[:], scalar1=ropec[:, 0:1], scalar2=ropec[:, 1:2], op0=ALU.mult, op1=ALU.add),
                 reads=[u, ropec], writes=[u])
            p.op("dve", lambda e: e.tensor_copy(ki[:], u[:]), reads=[u], writes=[ki])
            p.op("dve", lambda e: e.tensor_copy(kf[:], ki[:]), reads=[ki], writes=[kf])
            p.op("dve", lambda e: e.tensor_tensor(out=u[:], in0=u[:], in1=kf[:], op=ALU.subtract), reads=[u, kf], writes=[u])
            p.op("act", lambda e, cs=cs: e.activation(out=ROPE[:, cs], in_=u[:], func=AF.Sin, scale=2.0 * math.pi * (1.0 - 1e-6)),
                 reads=[u], writes=[("ROPE", blk)])
        psTb = PS[4].bitcast(BF16)
        def mla_proj(t):
            ts_ = slice(t * 128, (t + 1) * 128)
            blk = t // 4
            pa, pb = PS[(t % 2) * 2], PS[(t % 2) * 2 + 1]
            for k in range(8):
                p.op("pe", lambda e, k=k, ts_=ts_, pa=pa: e.matmul(pa[:], lhsT=self.HT[:, k, ts_], rhs=WIN[:, k, 0:512], start=(k == 0), stop=(k == 7)),
                     reads=[("HT", blk), WIN], writes=[pa])
            for k in range(8):
                p.op("pe", lambda e, k=k, ts_=ts_, pb=pb: e.matmul(pb[:, 0:256], lhsT=self.HT[:, k, ts_], rhs=WIN[:, k, 512:768], start=(k == 0), stop=(k == 7)),
                     reads=[("HT", blk), WIN], writes=[pb])

        def mla_rest(t):
            ts_ = slice(t * 128, (t + 1) * 128)
            blk = t // 4
            pa, pb = PS[(t % 2) * 2], PS[(t % 2) * 2 + 1]
            p.op("pool", lambda e: e.memset(st[:, 0:2], 0.0), writes=[st])
            p.op("act", lambda e, pa=pa: e.activation(out=junk[:], in_=pa[:], func=AF.Square, accum_out=st[:, 0:1]), reads=[pa], writes=[junk, st])
            p.op("act", lambda e, pb=pb: e.activation(out=junk[:, 0:256], in_=pb[:, 0:256], func=AF.Square, accum_out=st[:, 1:2]), reads=[pb], writes=[junk, st])
            p.op("act", lambda e: e.activation(out=st[:, 0:1], in_=st[:, 0:1], func=AF.Sqrt, bias=self.epsc[:], scale=1.0 / 512), reads=[st, self.epsc], writes=[st])
            p.op("act", lambda e: e.activation(out=st[:, 1:2], in_=st[:, 1:2], func=AF.Sqrt, bias=self.epsc[:], scale=1.0 / 256), reads=[st, self.epsc], writes=[st])
            p.op("dve", lambda e: e.reciprocal(out=st[:, 2:4], in_=st[:, 0:2]), reads=[st], writes=[st])
            qt_, kvt_ = qtok[t % 2], kvtok[t % 2]
            p.op("dve", lambda e, pa=pa, qt_=qt_: e.tensor_scalar(out=qt_[:], in0=pa[:], scalar1=st[:, 2:3], scalar2=None, op0=ALU.mult), reads=[pa, st], writes=[qt_])
            p.op("act", lambda e, pb=pb, kvt_=kvt_: e.activation(out=kvt_[:], in_=pb[:, 0:256], func=AF.Copy, scale=st[:, 3:4]), reads=[pb, st], writes=[kvt_])
            for c in range(4):
                p.op("pe", lambda e, c=c, qt_=qt_: e.transpose(psTb[:, c * 128:(c + 1) * 128], qt_[:, c * 128:(c + 1) * 128], self.identb[:]),
                     reads=[qt_, self.identb], writes=[PS[4]])
            for c in range(2):
                p.op("pe", lambda e, c=c, kvt_=kvt_: e.transpose(psTb[:, 512 + c * 128:512 + (c + 1) * 128], kvt_[:, c * 128:(c + 1) * 128], self.identb[:]),
                     reads=[kvt_, self.identb], writes=[PS[4]])
            for c in range(4):
                if c % 2 == 0:
                    p.op("act", lambda e, c=c, ts_=ts_: e.activation(out=QNT[:, c, ts_], in_=psTb[:, c * 128:(c + 1) * 128], func=AF.Copy, scale=qnc[:, c:c + 1]),
                         reads=[PS[4], qnc], writes=[("QNT", blk)])
                else:
                    p.op("dve", lambda e, c=c, ts_=ts_: e.tensor_scalar(out=QNT[:, c, ts_], in0=psTb[:, c * 128:(c + 1) * 128], scalar1=qnc[:, c:c + 1], scalar2=None, op0=ALU.mult),
                         reads=[PS[4], qnc], writes=[("QNT", blk)])
            for c in range(2):
                if c % 2 == 0:
                    p.op("act", lambda e, c=c, ts_=ts_: e.activation(out=KVNT[:, c, ts_], in_=psTb[:, 512 + c * 128:512 + (c + 1) * 128], func=AF.Copy, scale=kvnc[:, c:c + 1]),
                         reads=[PS[4], kvnc], writes=[("KVNT", blk)])
                else:
                    p.op("dve", lambda e, c=c, ts_=ts_: e.tensor_scalar(out=KVNT[:, c, ts_], in0=psTb[:, 512 + c * 128:512 + (c + 1) * 128], scalar1=kvnc[:, c:c + 1], scalar2=None, op0=ALU.mult),
                         reads=[PS[4], kvnc], writes=[("KVNT", blk)])
        mla_proj(0)
        for t in range(NT):
            if t + 1 < NT:
                mla_proj(t + 1)
            mla_rest(t)
        for blk in range(NB):
            cs = slice(blk * 512, (blk + 1) * 512)
            ps = PS[5 + blk % 2]
            for k in range(8):
                p.op("pe", lambda e, k=k, cs=cs, ps=ps: e.matmul(ps[:], lhsT=WIN[:, k, 768:896], rhs=self.HT[:, k, cs], start=(k == 0), stop=(k == 7)),
                     reads=[("HT", blk), WIN], writes=[ps])
            p.op("dve", lambda e, cs=cs, ps=ps: e.tensor_tensor(out=tA[64:96, :], in0=ps[64:96, :], in1=ROPE[64:96, cs], op=ALU.mult), reads=[ps, ("ROPE", blk)], writes=[tA])
            p.op("dve", lambda e, cs=cs, ps=ps: e.tensor_tensor(out=tB[64:96, :], in0=ps[96:128, :], in1=ROPE[96:128, cs], op=ALU.mult), reads=[ps, ("ROPE", blk)], writes=[tB])
            p.op("pool", lambda e, cs=cs: e.tensor_tensor(out=KT[64:96, cs], in0=tA[64:96, :], in1=tB[64:96, :], op=ALU.add), reads=[tA, tB], writes=[("KTr", blk)])
        p.release(m1)
        m2 = p.mark()
        WQB = p.tile("WQB", [128, 4, 2048], BF16)
        WKB = p.tile("WKB", [128, 2, 1024], BF16)
        WVB = p.tile("WVB", [128, 2, 1024], BF16)
        p.dma("pool", lambda e: e.dma_start(out=WQB[:], in_=self.dram["mla_qb"]), writes=[WQB])
        p.dma("pool", lambda e: e.dma_start(out=WKB[:], in_=self.dram["mla_kb"]), writes=[WKB])
        p.dma("pool", lambda e: e.dma_start(out=WVB[:], in_=self.dram["mla_vb"]), writes=[WVB])
        OT = self.HT
        scale = 96.0 ** -0.5
        si = 0
        oi = 0
        pi_ = 0
        for h in range(16):
            for blk in range(NB):
                cs = slice(blk * 512, (blk + 1) * 512)
                ps = PS[blk % 2]
                for c in range(4):
                    p.op("pe", lambda e, c=c, cs=cs, ps=ps, h=h: e.matmul(ps[:], lhsT=WQB[:, c, h * 128:(h + 1) * 128], rhs=QNT[:, c, cs], start=(c == 0), stop=(c == 3)),
                         reads=[WQB, ("QNT", blk)], writes=[ps])
                p.op("act", lambda e, cs=cs, ps=ps: e.copy(QT[0:64, cs], ps[0:64, :]), reads=[ps], writes=[("QT", blk)])
                p.op("dve", lambda e, cs=cs, ps=ps: e.tensor_tensor(out=tA[64:96, :], in0=ps[64:96, :], in1=ROPE[64:96, cs], op=ALU.mult), reads=[ps, ("ROPE", blk)], writes=[tA])
                p.op("dve", lambda e, cs=cs, ps=ps: e.tensor_tensor(out=tB[64:96, :], in0=ps[96:128, :], in1=ROPE[96:128, cs], op=ALU.mult), reads=[ps, ("ROPE", blk)], writes=[tB])
                p.op("pool", lambda e, cs=cs: e.tensor_tensor(out=QT[64:96, cs], in0=tA[64:96, :], in1=tB[64:96, :], op=ALU.add), reads=[tA, tB], writes=[("QT", blk)])
            for blk in range(NB):
                cs = slice(blk * 512, (blk + 1) * 512)
                ps = PS[blk % 2]
                for c in range(2):
                    p.op("pe", lambda e, c=c, cs=cs, ps=ps, h=h: e.matmul(ps[0:64, :], lhsT=WKB[:, c, h * 64:(h + 1) * 64], rhs=KVNT[:, c, cs], start=(c == 0), stop=(c == 1)),
                         reads=[WKB, ("KVNT", blk)], writes=[ps])
                p.op("act", lambda e, cs=cs, ps=ps: e.copy(KT[0:64, cs], ps[0:64, :]), reads=[ps], writes=[("KTn", blk)])
            for half in range(2):
                ps = PS[half]
                psv = ps[:].rearrange("p (a b) -> p a b", b=64)
                for kk in range(8):
                    kt = half * 8 + kk
                    for c in range(2):
                        p.op("pe", lambda e, c=c, kt=kt, kk=kk, psv=psv, h=h: e.matmul(psv[:, kk, :], lhsT=KVNT[:, c, kt * 128:(kt + 1) * 128], rhs=WVB[:, c, h * 64:(h + 1) * 64],
                                                                                   start=(c == 0), stop=(c == 1)), reads=[WVB, ("KVNT", kt // 4)], writes=[ps])
                p.op("dve", lambda e, half=half, psv=psv: e.tensor_copy(VA[:, half * 8:(half + 1) * 8, 0:64], psv), reads=[ps], writes=[("VA", half)])
            steps = []
            for qb in range(NB):
                pso = PS[5 + oi % 2]
                oi += 1
                last = 4 * qb + 3
                for kt in range(last + 1):
                    steps.append((qb, kt, max(0, kt - 4 * qb) * 128, pso, last))
            LA = 2
            slot = {}

            def emit_s(i):
                qb, kt, col0, pso, last = steps[i]
                pss = PS[2 + (si + i) % 3]
                pt = PT[(pi_ + i) % 4]
                slot[i] = (pss, pt)
                p.op("pe", lambda e: e.matmul(pss[:, col0:512], lhsT=KT[0:96, kt * 128:(kt + 1) * 128],
                                              rhs=QT[0:96, qb * 512 + col0:(qb + 1) * 512], start=True, stop=True),
                     reads=[("KTn", kt // 4), ("KTr", kt // 4), ("QT", qb)], writes=[pss])
                p.op("act", lambda e: e.activation(out=pt[:, col0:512], in_=pss[:, col0:512], func=AF.Exp, scale=scale), reads=[pss], writes=[pt])
                if kt >= 4 * qb:
                    dc = (kt - 4 * qb) * 128
                    p.op("pool", lambda e: e.tensor_tensor(out=pt[:, dc:dc + 128], in0=pt[:, dc:dc + 128], in1=TRI[:], op=ALU.mult), reads=[pt, TRI], writes=[pt])

            def emit_pv(i):
                qb, kt, col0, pso, last = steps[i]
                pss, pt = slot.pop(i)
                p.op("pe", lambda e: e.matmul(pso[:, col0:512], lhsT=VA[:, kt, :], rhs=pt[:, col0:512], start=(kt == 0), stop=(kt == last)),
                     reads=[("VA", kt // 8), pt], writes=[pso])
                if kt == last:
                    p.op("dve", lambda e: e.reciprocal(out=rec[64:128, :], in_=pso[64:128, :]), reads=[pso], writes=[rec])
                    r0 = (h % 2) * 64
                    hh = h // 2
                    p.op("dve", lambda e: e.tensor_tensor(out=OT[r0:r0 + 64, hh, qb * 512:(qb + 1) * 512], in0=pso[0:64, :], in1=rec[64:128, :], op=ALU.mult),
                         reads=[pso, rec], writes=[("OT", qb)])
            for i in range(len(steps) + LA):
                if i < len(steps):
                    emit_s(i)
                if i - LA >= 0:
                    emit_pv(i - LA)
            si += len(steps)
            pi_ += len(steps)
        p.release(m2)
        WOUT = p.tile("WOUT", [128, 8, D], BF16)
        p.dma("pool", lambda e: e.dma_start(out=WOUT[:], in_=self.dram["mla_out"].rearrange("(c p) n -> p c n", p=128)), writes=[WOUT])
        for c in range(8):
            p.op("pool", lambda e, c=c: e.tensor_tensor(out=WOUT[:, c, :], in0=WOUT[:, c, :], in1=self.G[:], op=ALU.mult), reads=[WOUT, self.G], writes=[WOUT])
        self.out_proj(OT, WOUT)
        p.release(m0)

    def out_proj(self, OT, WOUT):
        p = self.p
        for t in range(NT):
            for hf in range(2):
                ps = self.PS[(2 * t + hf) % 4]
                for c in range(8):
                    p.op("pe", lambda e, c=c, t=t, hf=hf, ps=ps: e.matmul(ps[:], lhsT=OT[:, c, t * 128:(t + 1) * 128], rhs=WOUT[:, c, hf * 512:(hf + 1) * 512],
                                                                          start=(c == 0), stop=(c == 7)), reads=[("OT", t // 4), WOUT], writes=[ps])
                xs = self.X[:, t, hf * 512:(hf + 1) * 512]
                p.op("dve", lambda e, xs=xs, ps=ps: e.tensor_tensor(out=xs, in0=ps[:], in1=xs, op=ALU.add), reads=[ps, ("X", t)], writes=[("X", t)])


    def mm(self, out, lhsT, rhs, start, stop, reads, writes):
        self.p.op("pe", lambda e: e.matmul(out, lhsT=lhsT, rhs=rhs, start=start, stop=stop), reads=reads, writes=writes)

    def tr(self, out, in_, ident, reads, writes):
        self.p.op("pe", lambda e: e.transpose(out, in_, ident), reads=list(reads) + [ident], writes=writes)

    def gdn_phase(self, s):
        p = self.p
        PS = self.PS
        PSb = [b.bitcast(BF16) for b in PS]
        HTr = [("HT", b) for b in range(NB)]
        m0 = p.mark()
        NEG = -1.0e30
        LTRI = p.tile("LTRI", [128, 128], F32)
        BDm = p.tile("BDm", [128, 128], F32)
        SEL = [p.tile(f"SEL{i}", [128, 128], F32) for i in range(2)]
        MNi = p.tile("MNi", [128, 128], F32)
        MNs = p.tile("MNs", [128, 128], F32)
        hp = p.tile("hp", [128, 24], F32)
        nA = p.tile("nA", [128, 8], F32)
        GON = p.tile("GON", [128, 128], F32)
        WBA = p.tile("WBA", [128, 8, 16], BF16)
        CW = p.tile("CW", [128, 8, 3, 4], F32)
        pl = lambda fn, r=(), w=(): p.op("pool", fn, reads=r, writes=w)
        pl(lambda e: e.memset(LTRI[:], 1.0), w=[LTRI])
        pl(lambda e: e.affine_select(out=LTRI[:], in_=LTRI[:], pattern=[[1, 128]], compare_op=ALU.is_ge, fill=0.0, base=0, channel_multiplier=-1), r=[LTRI], w=[LTRI])
        pl(lambda e: e.memset(LTRI[0:64, 64:128], 0.0), r=[LTRI], w=[LTRI])
        pl(lambda e: e.memset(BDm[:], 0.0), w=[BDm])
        pl(lambda e: e.memset(BDm[0:64, 0:64], 1.0), r=[BDm], w=[BDm])
        pl(lambda e: e.memset(BDm[64:128, 64:128], 1.0), r=[BDm], w=[BDm])
        for i in range(2):
            pl(lambda e, i=i: e.memset(SEL[i][:], 0.0), w=[SEL[i]])
            pl(lambda e, i=i: e.memset(SEL[i][64 * i:64 * i + 1, :], 1.0), r=[SEL[i]], w=[SEL[i]])
        pl(lambda e: e.memset(MNi[:], 0.0), w=[MNi])
        pl(lambda e: e.affine_select(out=MNi[:], in_=MNi[:], pattern=[[1, 128]], compare_op=ALU.is_ge, fill=NEG, base=0, channel_multiplier=-1), r=[MNi], w=[MNi])
        pl(lambda e: e.memset(MNi[0:64, 64:128], NEG), r=[MNi], w=[MNi])
        pl(lambda e: e.memset(MNs[:], 0.0), w=[MNs])
        pl(lambda e: e.affine_select(out=MNs[:], in_=MNs[:], pattern=[[1, 128]], compare_op=ALU.is_gt, fill=NEG, base=0, channel_multiplier=-1), r=[MNs], w=[MNs])
        pl(lambda e: e.memset(MNs[0:64, 64:128], NEG), r=[MNs], w=[MNs])
        p.dma("sp", lambda e: e.dma_start(out=hp[:], in_=self.dram["gdn_hp"]), writes=[hp])
        p.dma("sp", lambda e: e.dma_start(out=GON[:], in_=self.dram["gdn_on"]), writes=[GON])
        p.dma("sp", lambda e: e.dma_start(out=CW[:], in_=self.dram["gdn_conv"]), writes=[CW])
        p.dma("pool", lambda e: e.dma_start(out=WBA[:], in_=self.dram["gdn_ba"]), writes=[WBA])
        p.op("act", lambda e: e.activation(out=nA[:], in_=hp[:, 0:8], func=AF.Exp), reads=[hp], writes=[nA])
        p.op("dve", lambda e: e.tensor_scalar(out=nA[:], in0=nA[:], scalar1=-1.0, scalar2=None, op0=ALU.mult), reads=[nA], writes=[nA])
        BETA = p.tile("BETA", [128, NT, 8], F32)
        GAMS = p.tile("GAMS", [128, NT, 8], F32)
        EGL = p.tile("EGL", [128, NT, 8], F32)
        BG = p.tile("BG", [128, NT, 8], F32)
        DECB = p.tile("DECB", [128, NT, 2, 8], F32)
        GC3 = p.tile("GC3", [128, NT, 3, 8], F32)
        m1 = p.mark()
        ba = p.tile("ba", [128, 16], F32)
        xs = p.tile("gxs", [128, 8], F32)
        ax = p.tile("gax", [128, 8], F32)
        gg = p.tile("ggg", [128, 8], F32)
        lnb = p.tile("lnb", [128, 8], F32)
        gcl = p.tile("gcl", [128, 2, 8], F32)
        dk_s = 128.0 ** -0.5
        import os
        pre = int(os.environ.get("GDN_PRE", "99"))
        if "GDN_OPS" in os.environ:
            p.limit = int(os.environ["GDN_OPS"])
        for t in range(NT if pre >= 2 else pre):
            ts_ = slice(t * 128, (t + 1) * 128)
            ps = PS[t % 2]
            for k in range(8):
                self.mm(ps[:, 0:16], self.HT[:, k, ts_], WBA[:, k, :], k == 0, k == 7, [HTr[t // 4], WBA], [ps])
            p.op("act", lambda e, ps=ps: e.copy(ba[:], ps[:, 0:16]), reads=[ps], writes=[ba])
            p.op("act", lambda e, t=t: e.activation(out=BETA[:, t, :], in_=ba[:, 0:8], func=AF.Sigmoid), reads=[ba], writes=[BETA])
            p.op("act", lambda e, t=t: e.activation(out=lnb[:], in_=BETA[:, t, :], func=AF.Ln), reads=[BETA], writes=[lnb])
            p.op("dve", lambda e: e.tensor_tensor(out=xs[:], in0=ba[:, 8:16], in1=hp[:, 8:16], op=ALU.add), reads=[ba, hp], writes=[xs])
            p.op("act", lambda e: e.activation(out=ax[:], in_=xs[:], func=AF.Abs), reads=[xs], writes=[ax])
            p.op("act", lambda e: e.activation(out=ax[:], in_=ax[:], func=AF.Exp, scale=-1.0), reads=[ax], writes=[ax])
            p.op("act", lambda e: e.activation(out=ax[:], in_=ax[:], func=AF.Ln, bias=self.onesf[:, 0:1], scale=1.0), reads=[ax, self.onesf], writes=[ax])
            p.op("dve", lambda e: e.scalar_tensor_tensor(out=gg[:], in0=xs[:], scalar=0.0, in1=ax[:], op0=ALU.max, op1=ALU.add), reads=[xs, ax], writes=[gg])
            p.op("dve", lambda e: e.tensor_tensor(out=gg[:], in0=gg[:], in1=nA[:], op=ALU.mult), reads=[gg, nA], writes=[gg])
            self.mm(ps[:, 16:24], LTRI[:], gg[:], True, True, [LTRI, gg], [ps])
            self.mm(ps[:, 24:32], BDm[:], gg[:], True, True, [BDm, gg], [ps])
            p.op("act", lambda e, ps=ps: e.copy(gcl[:].rearrange("p a b -> p (a b)"), ps[:, 16:32]), reads=[ps], writes=[gcl])
            p.op("act", lambda e, t=t: e.activation(out=BG[:, t, :], in_=gcl[:, 0, :], func=AF.Exp), reads=[gcl], writes=[BG])
            p.op("dve", lambda e, t=t: e.tensor_scalar(out=GAMS[:, t, :], in0=BG[:, t, :], scalar1=dk_s, scalar2=None, op0=ALU.mult), reads=[BG], writes=[GAMS])
            p.op("dve", lambda e, t=t: e.tensor_tensor(out=BG[:, t, :], in0=BG[:, t, :], in1=BETA[:, t, :], op=ALU.mult), reads=[BG, BETA], writes=[BG])
            p.op("dve", lambda e, t=t: e.tensor_tensor(out=EGL[:, t, :], in0=gcl[:, 1, :], in1=gcl[:, 0, :], op=ALU.subtract), reads=[gcl], writes=[EGL])
            p.op("act", lambda e, t=t: e.activation(out=EGL[:, t, :], in_=EGL[:, t, :], func=AF.Exp), reads=[EGL], writes=[EGL])
            p.op("dve", lambda e, t=t: e.tensor_copy(GC3[:, t, 0, :], gcl[:, 0, :]), reads=[gcl], writes=[GC3])
            p.op("dve", lambda e, t=t: e.tensor_scalar(out=GC3[:, t, 1, :], in0=gcl[:, 0, :], scalar1=-1.0, scalar2=None, op0=ALU.mult), reads=[gcl, GC3], writes=[GC3])
            p.op("dve", lambda e, t=t: e.tensor_tensor(out=GC3[:, t, 2, :], in0=gcl[:, 0, :], in1=lnb[:], op=ALU.add), reads=[gcl, lnb, GC3], writes=[GC3])
            for hf in range(2):
                self.mm(ps[:, 32 + hf * 8:40 + hf * 8], SEL[hf][:], gcl[:, 1, :], True, True, [SEL[hf], gcl], [ps])
            p.op("act", lambda e, t=t, ps=ps: e.activation(out=DECB[:, t, :, :].rearrange("p a b -> p (a b)"), in_=ps[:, 32:48], func=AF.Exp), reads=[ps], writes=[DECB])
        p.limit = None
        p.release(m1)
        NPAR = int(os.environ.get("GDN_NPAR", "2"))
        HB = []
        for sl in range(NPAR):
            b = {}
            b["WH"] = p.tile(f"WH{sl}", [128, 8, 512], BF16)
            b["WO"] = p.tile(f"WO{sl}", [128, D], BF16)
            b["DG"] = p.tile(f"DG{sl}", [128, 12, 128], BF16)
            b["S"] = p.tile(f"S{sl}", [128, 128], F32)
            b["Sb"] = p.tile(f"Sb{sl}", [128, 128], BF16)
            b["XC"] = p.tile(f"XC{sl}", [128, 3, 515], BF16)
            for nm, shp, dt in (("ZS", [128, 128], F32), ("GZ", [128, 128], F32), ("QKV", [128, 3, 128], F32), ("SQ", [128, 2, 128], F32),
                                ("st", [128, 8], F32), ("TOK", [128, 6, 128], BF16), ("DGC", [128, 2, 128], F32), ("DD", [128, 2, 128], F32), ("KQT", [128, 3, 128], BF16), ("DT2", [128, 2, 128], F32),
                                ("M2", [128, 2, 128], F32), ("ATb", [128, 128], BF16), ("YA0", [128, 2, 128], F32), ("YA1", [128, 2, 128], F32),
                                ("P0", [128, 128], F32), ("P1", [128, 128], F32), ("TTb", [128, 128], BF16), ("U", [128, 128], F32),
                                ("WTb", [128, 128], BF16), ("VNb", [128, 128], BF16), ("OT", [128, 128], F32), ("OG", [128, 128], BF16),
                                ("OGT", [128, 128], BF16), ("junk", [128, 128], BF16)):
                b[nm] = p.tile(f"{nm}{sl}", shp, dt)
            b["bank"] = [PS[4 * sl + i] for i in range(4)]
            b["bankb"] = [PSb[4 * sl + i] for i in range(4)]
            HB.append(b)

        def head_gen(h, b):
            sl = HB.index(b)
            WH, WO, DG, S, Sb, XC = b["WH"], b["WO"], b["DG"], b["S"], b["Sb"], b["XC"]
            bk, bkb = b["bank"], b["bankb"]
            R = lambda i, nm: (f"ps{sl}_{i}")
            r_zc = [R(0, "zc")]
            r_tr, r_d, r_og = [R(1, "tr")], [R(1, "d")], [R(1, "og")]
            r_kk, r_a, r_p = [R(2, "kk")], [R(2, "a")], [R(2, "p")]
            r_ya, r_u, r_wt = [R(3, "ya")], [R(3, "u")], [R(3, "wt")]
            allb = [r_zc, r_tr, r_kk, r_ya]
            p.dma("pool", lambda e: e.dma_start(out=WH[:].rearrange("p k n -> p (k n)"), in_=self.dram["gdn_in"][h].rearrange("p k n -> p (k n)")), writes=[WH])
            p.dma("pool", lambda e: e.dma_start(out=WO[:], in_=self.dram["gdn_out"][h * 128:(h + 1) * 128, :]), writes=[WO])
            p.op("pool", lambda e: e.tensor_tensor(out=WO[:], in0=WO[:], in1=self.G[:], op=ALU.mult), reads=[WO, self.G], writes=[WO])
            for part in range(3):
                for j in range(4):
                    p.op("dve", lambda e, part=part, j=j: e.tensor_scalar(out=DG[:, part * 4 + j, :], in0=self.identb[:], scalar1=CW[:, h, part, j:j + 1], scalar2=None, op0=ALU.mult),
                         reads=[self.identb, CW], writes=[DG])
            p.op("pool", lambda e: e.memset(S[:], 0.0), writes=[S])
            p.op("pool", lambda e: e.memset(Sb[:], 0.0), writes=[Sb])
            p.op("pool", lambda e: e.memset(XC[:, :, 0:3], 0.0), writes=[XC])
            yield
            for blk in range(NB):
                cs = slice(blk * 512, (blk + 1) * 512)
                for part in range(3):
                    ps = bk[part]
                    for k in range(8):
                        self.mm(ps[:], WH[:, k, part * 128:(part + 1) * 128], self.HT[:, k, cs], k == 0, k == 7, [WH, HTr[blk]], allb[part])
                    eng = ("act", "dve", "act")[part]
                    if eng == "act":
                        p.op("act", lambda e, part=part, ps=ps: e.copy(XC[:, part, 3:515], ps[:]), reads=allb[part], writes=[XC])
                    else:
                        p.op("dve", lambda e, part=part, ps=ps: e.tensor_copy(XC[:, part, 3:515], ps[:]), reads=allb[part], writes=[XC])
                yield
                for tt in range(4):
                    t = blk * 4 + tt
                    ts_ = slice(t * 128, (t + 1) * 128)
                    ZS, GZ, QKV, SQ, st, TOK, KQT, DT2, M2, ATb = (b[n] for n in ("ZS", "GZ", "QKV", "SQ", "st", "TOK", "KQT", "DT2", "M2", "ATb"))
                    YA, Pb, TTb, U, WTb, VNb, OTk, OG, OGT = [b["YA0"], b["YA1"]], [b["P0"], b["P1"]], b["TTb"], b["U"], b["WTb"], b["VNb"], b["OT"], b["OG"], b["OGT"]
                    d0 = 256
                    DGC = b["DGC"]
                    p.op("act", lambda e, t=t: e.activation(out=DGC[:, 0, :], in_=self.identf[:], func=AF.Copy, scale=GC3[:, t, 0, h:h + 1]), reads=[self.identf, GC3], writes=[DGC])
                    p.op("act", lambda e, t=t: e.activation(out=DGC[:, 1, :], in_=self.identf[:], func=AF.Copy, scale=GC3[:, t, 2, h:h + 1]), reads=[self.identf, GC3], writes=[DGC])
                    self.mm(bk[1][:, 256:384], self.onesf[:], DGC[:, 0, :], True, False, [self.onesf, DGC], r_d)
                    self.mm(bk[1][:, 256:384], self.identf[:], MNi[:], False, True, [self.identf, MNi], r_d)
                    self.mm(bk[1][:, 384:512], self.onesf[:], DGC[:, 1, :], True, False, [self.onesf, DGC], r_d)
                    self.mm(bk[1][:, 384:512], self.identf[:], MNs[:], False, True, [self.identf, MNs], r_d)
                    p.op("act", lambda e, t=t: e.activation(out=DT2[:].rearrange("p a b -> p (a b)"), in_=bk[1][:, 256:512], func=AF.Exp, bias=GC3[:, t, 1, h:h + 1], scale=1.0),
                         reads=r_d + [GC3], writes=[DT2])
                    for k in range(8):
                        self.mm(bk[0][:, 0:128], self.HT[:, k, ts_], WH[:, k, 384:512], k == 0, k == 7, [WH, HTr[blk]], r_zc)
                    for part in range(3):
                        for j in range(4):
                            self.mm(bk[0][:, 128 + part * 128:256 + part * 128], XC[:, part, tt * 128 + j:tt * 128 + j + 128], DG[:, part * 4 + j, :], j == 0, j == 3, [XC, DG], r_zc)
                    p.op("act", lambda e: e.activation(out=ZS[:], in_=bk[0][:, 0:128], func=AF.Silu), reads=r_zc, writes=[ZS])
                    p.op("act", lambda e: e.activation(out=QKV[:].rearrange("p a b -> p (a b)"), in_=bk[0][:, 128:512], func=AF.Silu), reads=r_zc, writes=[QKV])
                    p.op("pool", lambda e: e.tensor_tensor(out=GZ[:], in0=ZS[:], in1=GON[:], op=ALU.mult), reads=[ZS, GON], writes=[GZ])
                    yield
                    if "GDN_OPS2" in os.environ and h == 0 and t == 0:
                        p.limit = int(os.environ["GDN_OPS2"])
                    p.op("dve", lambda e: e.tensor_tensor(out=SQ[:], in0=QKV[:, 0:2, :], in1=QKV[:, 0:2, :], op=ALU.mult), reads=[QKV], writes=[SQ])
                    p.op("dve", lambda e: e.tensor_reduce(out=st[:, 0:2], in_=SQ[:], axis=AX.X, op=ALU.add), reads=[SQ], writes=[st])
                    p.op("act", lambda e: e.activation(out=st[:, 0:2], in_=st[:, 0:2], func=AF.Sqrt, bias=self.epsc[:], scale=1.0), reads=[st, self.epsc], writes=[st])
                    p.op("dve", lambda e: e.reciprocal(out=st[:, 2:4], in_=st[:, 0:2]), reads=[st], writes=[st])
                    p.op("act", lambda e: e.activation(out=TOK[:, 0, :], in_=QKV[:, 1, :], func=AF.Copy, scale=st[:, 3:4]), reads=[QKV, st], writes=[(TOK.name, 0)])
                    p.op("dve", lambda e: e.tensor_scalar(out=TOK[:, 1, :], in0=QKV[:, 0, :], scalar1=st[:, 2:3], scalar2=dk_s, op0=ALU.mult, op1=ALU.mult), reads=[QKV, st], writes=[(TOK.name, 0)])
                    p.op("dve", lambda e, t=t: e.tensor_scalar(out=TOK[:, 2, :], in0=QKV[:, 0, :], scalar1=st[:, 2:3], scalar2=GAMS[:, t, h:h + 1], op0=ALU.mult, op1=ALU.mult), reads=[QKV, st, GAMS], writes=[(TOK.name, 0)])
                    p.op("dve", lambda e, t=t: e.tensor_scalar(out=TOK[:, 3, :], in0=QKV[:, 1, :], scalar1=st[:, 3:4], scalar2=EGL[:, t, h:h + 1], op0=ALU.mult, op1=ALU.mult), reads=[QKV, st, EGL], writes=[(TOK.name, 1)])
                    p.op("dve", lambda e, t=t: e.tensor_scalar(out=TOK[:, 4, :], in0=QKV[:, 1, :], scalar1=st[:, 3:4], scalar2=BG[:, t, h:h + 1], op0=ALU.mult, op1=ALU.mult), reads=[QKV, st, BG], writes=[(TOK.name, 1)])
                    p.op("act", lambda e, t=t: e.activation(out=TOK[:, 5, :], in_=QKV[:, 2, :], func=AF.Copy, scale=BETA[:, t, h:h + 1]), reads=[QKV, BETA], writes=[(TOK.name, 1)])
                    for i in range(3):
                        self.tr(bkb[1][:, i * 128:(i + 1) * 128], TOK[:, i, :], self.identb[:], [(TOK.name, 0)], r_tr)
                    p.op("act", lambda e: e.copy(KQT[:].rearrange("p a b -> p (a b)"), bkb[1][:, 0:384]), reads=r_tr, writes=[KQT])
                    yield
                    yield
                    self.mm(bk[2][:, 0:128], KQT[:, 0, :], KQT[:, 1, :], True, True, [KQT], r_kk)
                    self.mm(bk[2][:, 128:256], KQT[:, 0, :], KQT[:, 0, :], True, True, [KQT], r_kk)
                    p.op("dve", lambda e: e.tensor_tensor(out=M2[:].rearrange("p a b -> p (a b)"), in0=bk[2][:, 0:256], in1=DT2[:].rearrange("p a b -> p (a b)"), op=ALU.mult),
                         reads=r_kk + [DT2], writes=[M2])
                    p.op("act", lambda e: e.copy(ATb[:], M2[:, 0, :]), reads=[M2], writes=[ATb])
                    yield
                    Y = M2[:, 1, :]
                    self.tr(bk[2][:, 256:384], Y, self.identf[:], [M2], r_a)
                    p.op("act", lambda e: e.copy(YA[0][:, 1, :], bk[2][:, 256:384]), reads=r_a, writes=[(YA[0].name, 1)])
                    p.op("pool", lambda e: e.tensor_copy(YA[0][:, 0, :], Y), reads=[M2], writes=[(YA[0].name, 0)])
                    p.op("pool", lambda e: e.tensor_tensor(out=Pb[0][:], in0=self.identf[:], in1=Y, op=ALU.subtract), reads=[M2, self.identf], writes=[Pb[0]])
                    yield
                    for it in range(5):
                        cur, nxt = YA[it % 2], YA[(it + 1) % 2]
                        pc, pn = Pb[it % 2], Pb[(it + 1) % 2]
                        rc = [(cur.name, 0), (cur.name, 1)]
                        if it < 4:
                            self.mm(bk[3][:, 0:128], cur[:, 1, :], cur[:, 0, :], True, True, rc, r_ya)
                        self.mm(bk[3][:, 128:256], cur[:, 0, :], cur[:, 1, :], True, True, rc, r_ya)
                        if it < 4:
                            p.op("act", lambda e, nxt=nxt: e.copy(nxt[:].rearrange("p a b -> p (a b)"), bk[3][:, 0:256]), reads=r_ya, writes=[(nxt.name, 0), (nxt.name, 1)])
                        else:
                            p.op("act", lambda e, nxt=nxt: e.copy(nxt[:, 1, :], bk[3][:, 128:256]), reads=r_ya, writes=[(nxt.name, 1)])
                        yield
                        self.mm(bk[2][:, 384:512], self.identf[:], pc[:], True, False, [self.identf, pc], r_p)
                        self.mm(bk[2][:, 384:512], nxt[:, 1, :], pc[:], False, True, [(nxt.name, 1), pc], r_p)
                        if it < 4:
                            p.op("dve", lambda e, pn=pn: e.tensor_copy(pn[:], bk[2][:, 384:512]), reads=r_p, writes=[pn])
                        else:
                            p.op("dve", lambda e: e.tensor_copy(TTb[:], bk[2][:, 384:512]), reads=r_p, writes=[TTb])
                        yield
                    if "GDN_OPS3" in os.environ and h == 1 and t == 0:
                        p.limit = int(os.environ["GDN_OPS3"])
                    self.mm(bk[3][:, 256:384], TTb[:], TOK[:, 5, :], True, True, [TTb, (TOK.name, 1)], r_u)
                    self.mm(bk[0][:, 0:128], TOK[:, 4, :], TTb[:], True, True, [TTb, (TOK.name, 1)], r_zc)
                    p.op("act", lambda e: e.copy(U[:], bk[3][:, 256:384]), reads=r_u, writes=[U])
                    p.op("dve", lambda e: e.tensor_copy(WTb[:], bk[0][:, 0:128]), reads=r_zc, writes=[WTb])
                    yield
                    SD = b["DD"][:, 0, :]
                    for hf in range(2):
                        r0 = hf * 64
                        rr = slice(r0, r0 + 64)
                        self.mm(bk[2][0:64, 0:128], WTb[:, rr], Sb[:], True, True, [WTb, Sb], r_kk)
                        p.op("act", lambda e, t=t, hf=hf: e.activation(out=SD, in_=S[:], func=AF.Copy, scale=DECB[:, t, hf, h:h + 1]), reads=[S, DECB], writes=[b["DD"]])
                        p.op("dve", lambda e, rr=rr: e.scalar_tensor_tensor(out=VNb[rr, :], in0=bk[2][0:64, 0:128], scalar=-1.0, in1=U[rr, :], op0=ALU.mult, op1=ALU.add), reads=[U] + r_kk, writes=[VNb])
                        yield
                        self.mm(bk[3][:, 0:128], TOK[rr, 3, :], VNb[rr, :], True, True, [(TOK.name, 1), VNb], r_ya)
                        self.mm(bk[1][0:64, 0:128], KQT[:, 2, rr], Sb[:], True, False, [KQT, Sb], r_tr)
                        self.mm(bk[1][0:64, 0:128], ATb[rr, rr], VNb[rr, :], False, True, [ATb, VNb], r_tr)
                        p.op("dve", lambda e: e.tensor_tensor(out=Sb[:], in0=bk[3][:, 0:128], in1=SD, op=ALU.add), reads=r_ya + [b["DD"]], writes=[Sb])
                        p.op("dve", lambda e: e.tensor_tensor(out=S[:], in0=bk[3][:, 0:128], in1=SD, op=ALU.add), reads=r_ya + [b["DD"]], writes=[S])
                        p.op("act", lambda e, rr=rr: e.copy(OTk[rr, :], bk[1][0:64, 0:128]), reads=r_tr, writes=[OTk])
                        yield
                    p.op("pool", lambda e: e.memset(st[:, 4:5], 0.0), reads=[st], writes=[st])
                    p.op("act", lambda e: e.activation(out=b["junk"][:], in_=OTk[:], func=AF.Square, accum_out=st[:, 4:5]), reads=[OTk, st], writes=[b["junk"], st])
                    p.op("act", lambda e: e.activation(out=st[:, 4:5], in_=st[:, 4:5], func=AF.Sqrt, bias=self.epsc[:], scale=1.0 / 128), reads=[st, self.epsc], writes=[st])
                    p.op("dve", lambda e: e.reciprocal(out=st[:, 5:6], in_=st[:, 4:5]), reads=[st], writes=[st])
                    p.op("dve", lambda e: e.scalar_tensor_tensor(out=OG[:], in0=OTk[:], scalar=st[:, 5:6], in1=GZ[:], op0=ALU.mult, op1=ALU.mult), reads=[OTk, st, GZ], writes=[OG])
                    yield
                    self.tr(bkb[1][:, 384:512], OG[:], self.identb[:], [OG], r_og)
                    p.op("act", lambda e: e.copy(OGT[:], bkb[1][:, 384:512]), reads=r_og, writes=[OGT])
                    yield
                    obanks = [(bk[0], r_zc), (bk[3], r_ya)]
                    for hf in range(2):
                        self.mm(obanks[hf][0][:], OGT[:], WO[:, hf * 512:(hf + 1) * 512], True, True, [OGT, WO], obanks[hf][1])
                    for hf in range(2):
                        xs_ = self.X[:, t, hf * 512:(hf + 1) * 512]
                        p.op("dve", lambda e, xs_=xs_, ob_=obanks[hf][0]: e.tensor_tensor(out=xs_, in0=ob_[:], in1=xs_, op=ALU.add), reads=obanks[hf][1] + [("X", t)], writes=[("X", t)])
                    yield
                p.op("dve", lambda e: e.tensor_copy(XC[:, :, 0:3], XC[:, :, 512:515]), reads=[XC], writes=[XC])
                yield

        import os
        steps = int(os.environ.get("GDN_STEPS", "1000000000"))
        for h0 in range(0, 8, NPAR):
            gens = [head_gen(h0 + i, HB[i]) for i in range(NPAR)]
            alive = list(gens)
            while alive and steps > 0:
                for g_ in list(alive):
                    steps -= 1
                    if steps <= 0:
                        break
                    try:
                        next(g_)
                    except StopIteration:
                        alive.remove(g_)
        if "GDN_DUMP" in os.environ:
            nm = os.environ["GDN_DUMP"]
            src = HB[0][nm]
            self.dump = p.tile("dump", [128, 512], F32)
            n = int(np.prod(src.shape[1:]))
            v = src[:] if len(src.shape) == 2 else src[:].rearrange("p a b -> p (a b)")
            p.op("pool", lambda e: e.memset(self.dump[:], 0.0), writes=[self.dump])
            p.op("dve", lambda e: e.tensor_copy(self.dump[:, 0:min(n, 512)], v[:, 0:min(n, 512)]), reads=[src, self.dump] + [(src.name, i) for i in range(2)], writes=[self.dump])
            p.dma("sp", lambda e: e.dma_start(out=self.y[s, 0:128, 0:512], in_=self.dump[:]), reads=[self.dump], key="dump")
            p.barrier()
            self.skip0 = True
        p.release(m0)

    def build(self):
        p = self.p
        st = self.stages
        self.setup()
        self.prologue_mod()
        final = []
        for s in range(self.nseq):
            self.load_x(s)
            for l in range(2):
                if f"mix{l}" in st:
                    self.norm_phase(l, s, 0)
                    self.make_gate(l, s, 2)
                    (self.mla_phase if l == 0 else self.gdn_phase)(s)
                if f"ffn{l}" in st:
                    self.norm_phase(l, s, 1, router=(l == 1))
                    self.make_gate(l, s, 5)
                    if l == 0:
                        ex = [(self.dram["ffn_gu"][j], self.dram["ffn_dn"][j], None) for j in range(2)]
                    else:
                        ex = [(self.dram["moe_gu"][j], self.dram["moe_dn"][j], j) for j in range(8)]
                    self.ffn_phase(ex)
            final += self.final_out(s)
        p.wait("sp", final)
        p.emit()
        return self.nc


def _cols(v, nk):
    return np.ascontiguousarray(np.asarray(v).reshape(nk, 128).T)


def _gu_layout(w, ff):
    nch = ff // 128
    g = w[:, :ff].reshape(8, 128, nch, 128)
    u = w[:, ff:].reshape(8, 128, nch, 128)
    gu = np.concatenate([g, u], axis=-1)
    return np.ascontiguousarray(gu.transpose(2, 1, 0, 3)).reshape(nch, 128, 8 * 256)


def _rope_consts():
    c = np.zeros((128, 2), np.float64)
    inv = 10000.0 ** (-np.arange(16, dtype=np.float64) / 16.0)
    for j in range(32):
        c[64 + j, 0] = inv[j % 16] / (2 * math.pi); c[64 + j, 1] = 0.25
        c[96 + j, 0] = inv[j % 16] / (2 * math.pi); c[96 + j, 1] = 0.5 if j < 16 else 0.0
    return c.astype(np.float32)


ROPEC = _rope_consts()


def prep_shared(inp):
    f = lambda k: np.asarray(inp[k], dtype=np.float32)
    sh = {}
    sh["ada"] = np.ascontiguousarray(np.concatenate([f("ada_w"), f("ada_b")[:, None, :]], axis=1))
    sh["gmix"] = np.stack([_cols(f("norm_mix")[l], 8) for l in range(2)])
    sh["gffn"] = np.stack([_cols(f("norm_ffn")[l], 8) for l in range(2)])
    sh["gfin"] = np.ascontiguousarray(np.broadcast_to(f("final_norm")[None, :], (128, D)))
    wgu = f("ffn_w_gate_up")[0]
    halves = []
    for j in range(2):
        sub = np.concatenate([wgu[:, j * 1408:(j + 1) * 1408], wgu[:, 2816 + j * 1408:2816 + (j + 1) * 1408]], axis=1)
        halves.append(_gu_layout(sub, 1408))
    sh["ffn_gu"] = np.stack(halves)
    sh["ffn_dn"] = np.ascontiguousarray(f("ffn_w_down")[0].reshape(2, 1408, D))
    sh["moe_gu"] = np.stack([_gu_layout(f("moe_w_gate_up")[0, e], 1408) for e in range(8)])
    sh["moe_dn"] = np.ascontiguousarray(f("moe_w_down")[0])
    sh["moe_r"] = np.ascontiguousarray(f("moe_w_router")[0].reshape(8, 128, 8).transpose(1, 0, 2))

    perm = np.concatenate([np.arange(16, 32), np.arange(0, 16)])
    win = f("mla_w_in")[0]
    kr = win[:, 768:800]
    win2 = np.concatenate([win[:, :768], win[:, 0:64], kr, kr[:, perm]], axis=1)
    sh["mla_in"] = np.ascontiguousarray(win2.reshape(8, 128, 896).transpose(1, 0, 2))
    sh["mla_qn"] = _cols(f("mla_q_norm")[0], 4)
    sh["mla_kvn"] = _cols(f("mla_kv_norm")[0], 2)
    wqb = f("mla_w_qb")[0].reshape(512, 16, 96)
    wqb2 = np.concatenate([wqb[:, :, :64], wqb[:, :, 64:96], wqb[:, :, 64:96][:, :, perm]], axis=2).reshape(512, 2048)
    sh["mla_qb"] = np.ascontiguousarray(wqb2.reshape(4, 128, 2048).transpose(1, 0, 2))
    wkv = f("mla_w_kvb")[0].reshape(256, 16, 128)
    sh["mla_kb"] = np.ascontiguousarray(wkv[:, :, :64].reshape(2, 128, 1024).transpose(1, 0, 2))
    sh["mla_vb"] = np.ascontiguousarray(wkv[:, :, 64:].reshape(2, 128, 1024).transpose(1, 0, 2))
    sh["mla_out"] = np.ascontiguousarray(f("mla_w_out")[0])
    sh["ropec"] = ROPEC
    gw = f("gdn_w_in")[0]
    heads = []
    for h in range(8):
        cols = np.concatenate([np.arange(h * 128, (h + 1) * 128) + off for off in (0, 1024, 2048, 3072)])
        heads.append(gw[:, cols].reshape(8, 128, 512).transpose(1, 0, 2))
    sh["gdn_in"] = np.ascontiguousarray(np.stack(heads))
    sh["gdn_ba"] = np.ascontiguousarray(gw[:, 4096:4112].reshape(8, 128, 16).transpose(1, 0, 2))
    cw = f("gdn_conv_w")[0]
    sh["gdn_conv"] = np.ascontiguousarray(cw.reshape(4, 3, 8, 128).transpose(3, 2, 1, 0))
    hpv = np.zeros((128, 24), np.float32)
    hpv[:, 0:8] = f("gdn_a_log")[0][None, :]
    hpv[:, 8:16] = f("gdn_dt_bias")[0][None, :]
    sh["gdn_hp"] = hpv
    sh["gdn_on"] = np.ascontiguousarray(np.broadcast_to(f("gdn_out_norm")[0][None, :], (128, 128)))
    sh["gdn_out"] = np.ascontiguousarray(f("gdn_w_out")[0])
    return sh


def prep_core(inp, b0, ns):
    x = np.ascontiguousarray(np.asarray(inp["x"], np.float32)[b0:b0 + ns])
    c = np.asarray(inp["c"], np.float32)[b0:b0 + ns]
    cT = np.stack([_cols(c[i], 8) for i in range(ns)])
    pos = np.asarray(inp["positions"]).astype(np.int32)[b0:b0 + ns]
    posr = np.ascontiguousarray(np.broadcast_to(pos[:, None, :], (ns, 128, SEQ)))
    return {"x": x, "cT": cT, "pos": posr}


_CACHE = {}


def run(inp, batches, ns, stages):
    key = (ns, tuple(sorted(stages)))
    if key not in _CACHE:
        _CACHE[key] = Builder(ns, stages).build()
    nc = _CACHE[key]
    sh = prep_shared(inp)
    maps = []
    for b0 in batches:
        mm = dict(sh)
        mm.update(prep_core(inp, b0, ns))
        maps.append(mm)
    res = run_bass_kernel_spmd(nc, maps, core_ids=list(range(len(batches))))
    return [r["y"] for r in res.results]


ALL = ("mix0", "ffn0", "mix1", "ffn1")


def kernel(**inputs):
    ys = run(inputs, [2 * i for i in range(NCORES)], 2, ALL)
    return np.concatenate(ys, axis=0).astype(np.float32)
```
